# Optimizing a Trainium2 kernel written in Bass

```python
import math
import jax, jax.numpy as jnp
from jax import lax
import numpy as np

D_MODEL = 1024
BATCH = 8
SEQ = 2048
DEPTH = 1

CTX_LEN = 256
GRID_W = 64
H_A = 4
DK_A = 128
DV_A = 128
C_A = H_A * DV_A
SHORT_CONV = 5
CHUNK = 64
H_B = 8
N_B = 64
C_B = H_B * N_B
LORA_W = 64
LORA_A = 64
LORA_G = 128
A_COLS = 4 * C_A + 4 * H_A
B_COLS = 3 * C_B + 2 * LORA_W + 2 * LORA_A + LORA_G
IN_COLS = A_COLS + B_COLS + 2 * D_MODEL
N_GROUPS = 4
EXPERTS_PER_GROUP = 8
N_EXPERTS = N_GROUPS * EXPERTS_PER_GROUP
TOP_K = 2
D_EXPERT = 256
NORM_EPS = 1e-6
LNX_EPS = 1e-5 * N_B

kernel_name = 'hybrid_gdn_rwkv7_hmoe_dit_block'


def rmsnorm(x, g):
    xf = x.astype(jnp.float32)
    y = xf * lax.rsqrt(jnp.mean(xf * xf, axis=-1, keepdims=True) + NORM_EPS)
    return y.astype(x.dtype) * g


def l2norm(x):
    xf = x.astype(jnp.float32)
    return (xf * lax.rsqrt(jnp.sum(xf * xf, axis=-1, keepdims=True) + NORM_EPS)).astype(x.dtype)


def modulate(x, shift, scale):
    return x * (1 + scale) + shift


def both_dirs(t):
    return jnp.stack([t, jnp.flip(t, 1)], 0)


def per_dir(t):
    return jnp.stack([t[:, :, 0], jnp.flip(t[:, :, 1], 1)], 0)


def merge_dirs(y):
    return y[0] + jnp.flip(y[1], 1)


def to_col_major(x, rows):
    b, l, c = x.shape
    return x.reshape(b, rows, GRID_W, c).transpose(0, 2, 1, 3).reshape(b, l, c)


def to_row_major(x, rows):
    b, l, c = x.shape
    return x.reshape(b, GRID_W, rows, c).transpose(0, 2, 1, 3).reshape(b, l, c)


def depthwise_conv(x, w):
    pad = SHORT_CONV // 2
    return lax.conv_general_dilated(x, w[:, None, :], window_strides=(1,), padding=[(pad, pad)],
                                    dimension_numbers=('NWC', 'WIO', 'NWC'),
                                    feature_group_count=x.shape[-1])


def bidir_shift(x):
    xp = jnp.pad(x, ((0, 0), (1, 1), (0, 0)))
    return 0.5 * (xp[:, :-2] + xp[:, 2:])


def gated_delta_chunked(q, k, v, g, beta, s0):
    dtype = v.dtype
    q, k, v, g, beta = (t.astype(jnp.float32) for t in (q, k, v, g, beta))
    n = q.shape[-2] // CHUNK
    dv = v.shape[-1]

    def blk(t):
        return t.reshape(t.shape[:-2] + (n, CHUNK, t.shape[-1]))

    q, k, v = blk(q), blk(k), blk(v)
    g = g.reshape(g.shape[:-1] + (n, CHUNK))
    beta = beta.reshape(beta.shape[:-1] + (n, CHUNK))
    gc = jnp.cumsum(g, axis=-1)
    incl = jnp.tril(jnp.ones((CHUNK, CHUNK), bool))
    strict = jnp.tril(jnp.ones((CHUNK, CHUNK), bool), -1)
    decay = jnp.where(incl, jnp.exp(jnp.where(incl, gc[..., :, None] - gc[..., None, :], 0.0)), 0.0)
    k_beta = k * beta[..., None]
    lower = jnp.where(strict, jnp.einsum('...id,...jd->...ij', k_beta, k) * decay, 0.0)
    a_mat = jnp.eye(CHUNK, dtype=jnp.float32) + lower
    rhs = jnp.concatenate([v * beta[..., None], k_beta * jnp.exp(gc)[..., None]], axis=-1)
    sol = lax.linalg.triangular_solve(a_mat, rhs, left_side=True, lower=True, unit_diagonal=True)
    u, w = sol[..., :dv], sol[..., dv:]
    qk = jnp.where(incl, jnp.einsum('...id,...jd->...ij', q, k) * decay, 0.0)
    q_dec = q * jnp.exp(gc)[..., None]
    k_dec = k * jnp.exp(gc[..., -1:] - gc)[..., None]
    g_end = jnp.exp(gc[..., -1])
    xs = (jnp.moveaxis(u, -3, 0), jnp.moveaxis(w, -3, 0), jnp.moveaxis(qk, -3, 0),
          jnp.moveaxis(q_dec, -3, 0), jnp.moveaxis(k_dec, -3, 0), jnp.moveaxis(g_end, -1, 0))

    def step(s, inp):
        u_c, w_c, qk_c, qd_c, kd_c, ge_c = inp
        v_new = u_c - jnp.einsum('...ck,...kv->...cv', w_c, s)
        o_c = jnp.einsum('...ck,...kv->...cv', qd_c, s) + jnp.einsum('...ij,...jv->...iv', qk_c, v_new)
        s = s * ge_c[..., None, None] + jnp.einsum('...ck,...cv->...kv', kd_c, v_new)
        return s, o_c

    s_fin, o = lax.scan(step, s0.astype(jnp.float32), xs)
    o = jnp.moveaxis(o, 0, -3)
    return o.reshape(o.shape[:-3] + (n * CHUNK, dv)).astype(dtype), s_fin


def gdn_branch(cols, s0, conv_w, a_log, dt_bias, onorm_g):
    bsz, length, _ = cols.shape
    qkv, z, a, bt = jnp.split(cols, [3 * C_A, 4 * C_A, 4 * C_A + 2 * H_A], axis=-1)
    qkv = jax.nn.silu(depthwise_conv(qkv, conv_w))
    q, k, v = (t.reshape(bsz, length, H_A, DK_A) for t in jnp.split(qkv, 3, axis=-1))
    q = l2norm(q) * DK_A ** -0.5
    k = l2norm(k)
    g = -jnp.exp(a_log) * jax.nn.softplus(a.reshape(bsz, length, 2, H_A) + dt_bias)
    beta = jax.nn.sigmoid(bt.reshape(bsz, length, 2, H_A))
    sw = lambda t: jnp.swapaxes(t, 2, 3)
    o, s_fin = gated_delta_chunked(sw(both_dirs(q)), sw(both_dirs(k)), sw(both_dirs(v)),
                                   sw(per_dir(g)), sw(per_dir(beta)), s0)
    o = merge_dirs(sw(o))
    o = rmsnorm(o, onorm_g) * jax.nn.silu(z.reshape(bsz, length, H_A, DV_A))
    return o.reshape(bsz, length, C_A), s_fin


def rwkv7_scan(r, w, k, v, a, b, s0):
    dtype = r.dtype
    xs = tuple(jnp.moveaxis(t.astype(jnp.float32), 2, 0) for t in (r, w, k, v, a, b))

    def step(s, inp):
        r_t, w_t, k_t, v_t, a_t, b_t = inp
        sa = jnp.einsum('...vk,...k->...v', s, a_t)
        s = s * w_t[..., None, :] + sa[..., :, None] * b_t[..., None, :] + v_t[..., :, None] * k_t[..., None, :]
        return s, jnp.einsum('...vk,...k->...v', s, r_t)

    s_fin, y = lax.scan(step, s0.astype(jnp.float32), xs)
    return jnp.moveaxis(y, 0, 2).astype(dtype), s_fin


def rwkv_branch(cols, s0, mu, w0, w2, a0, a2, g2, k_k, k_a, r_k, lnx_g, lnx_b):
    bsz, length, _ = cols.shape
    cols = cols + (bidir_shift(cols) - cols) * mu
    r, k, v, w_lo, a_lo, g_lo = jnp.split(
        cols, [C_B, 2 * C_B, 3 * C_B, 3 * C_B + 2 * LORA_W, 3 * C_B + 2 * LORA_W + 2 * LORA_A], axis=-1)
    w_lo = w_lo.reshape(bsz, length, 2, LORA_W)
    a_lo = a_lo.reshape(bsz, length, 2, LORA_A)
    w_log = -jax.nn.softplus(-(w0 + jnp.einsum('bldr,drc->bldc', jnp.tanh(w_lo), w2))) - 0.5
    decay = jnp.exp(-jnp.exp(w_log.astype(jnp.float32))).astype(cols.dtype)
    iclr = jax.nn.sigmoid(a0 + jnp.einsum('bldr,drc->bldc', a_lo, a2))
    gate = jax.nn.sigmoid(g_lo) @ g2
    heads = lambda t: t.reshape(t.shape[:-1] + (H_B, N_B))
    kk = l2norm(heads(k * k_k))
    k_dir = heads(k[:, :, None, :] * (1 + (iclr - 1) * k_a))
    b_dir = kk[:, :, None] * heads(iclr)
    r_h, v_h = heads(r), heads(v)
    y, s_fin = rwkv7_scan(both_dirs(r_h), per_dir(heads(decay)), per_dir(k_dir), both_dirs(v_h),
                          both_dirs(-kk), per_dir(b_dir), s0)
    yf = merge_dirs(y).astype(jnp.float32)
    mean = jnp.mean(yf, axis=-1, keepdims=True)
    var = jnp.mean(jnp.square(yf - mean), axis=-1, keepdims=True)
    y = ((yf - mean) * lax.rsqrt(var + LNX_EPS)).astype(cols.dtype).reshape(bsz, length, C_B) * lnx_g + lnx_b
    bonus = jnp.einsum('blhn,bldhn,hn->blh', r_h, k_dir, r_k)[..., None] * v_h
    y = (y + bonus.reshape(bsz, length, C_B)) * gate
    return y, s_fin


def merge_branches(gates, o_a, o_b, w_o_a, w_o_b, w_out):
    g_a, g_b = jnp.split(gates, 2, axis=-1)
    y = jax.nn.sigmoid(g_a) * (o_a @ w_o_a) + jax.nn.sigmoid(g_b) * (o_b @ w_o_b)
    return y @ w_out


def mixing_sublayer(u_lat, u_ctx, rows, with_ctx_out, w_in, gdn_conv, gdn_a_log, gdn_dt_bias, gdn_onorm_g,
                    rwkv_mu, rwkv_w0, rwkv_w2, rwkv_a0, rwkv_a2, rwkv_g2, rwkv_k_k, rwkv_k_a, rwkv_r_k,
                    rwkv_lnx_g, rwkv_lnx_b, w_o_a, w_o_b, w_out):
    bsz = u_lat.shape[0]
    split_at = [A_COLS, A_COLS + B_COLS]
    a_lat, b_lat, gates_lat = jnp.split(u_lat @ w_in, split_at, axis=-1)
    a_ctx, b_ctx, gates_ctx = jnp.split(u_ctx @ w_in, split_at, axis=-1)

    def run_a(cols, s0):
        return gdn_branch(cols, s0, gdn_conv, gdn_a_log, gdn_dt_bias, gdn_onorm_g)

    def run_b(cols, s0):
        return rwkv_branch(cols, s0, rwkv_mu, rwkv_w0, rwkv_w2, rwkv_a0, rwkv_a2, rwkv_g2,
                           rwkv_k_k, rwkv_k_a, rwkv_r_k, rwkv_lnx_g, rwkv_lnx_b)

    zeros_a = jnp.zeros((2, bsz, H_A, DK_A, DV_A), jnp.float32)
    zeros_b = jnp.zeros((2, bsz, H_B, N_B, N_B), jnp.float32)
    oa_ctx, state_a = run_a(a_ctx, zeros_a)
    ob_ctx, state_b = run_b(b_ctx, zeros_b)
    oa_lat, _ = run_a(a_lat, state_a)
    ob_lat, _ = run_b(to_col_major(b_lat, rows), state_b)
    ob_lat = to_row_major(ob_lat, rows)
    y_lat = merge_branches(gates_lat, oa_lat, ob_lat, w_o_a, w_o_b, w_out)
    if not with_ctx_out:
        return y_lat, None
    return y_lat, merge_branches(gates_ctx, oa_ctx, ob_ctx, w_o_a, w_o_b, w_out)


def hier_moe(h, router_grp, router_grp_b, router_exp, router_exp_b, w_gate, w_up, w_down):
    shape = h.shape
    t = h.reshape(-1, shape[-1])
    grp_p = jax.nn.softmax((t @ router_grp + router_grp_b).astype(jnp.float32), axis=-1)
    p_grp, g_sel = lax.top_k(grp_p, 1)
    exp_logits = (t @ router_exp + router_exp_b).astype(jnp.float32).reshape(-1, N_GROUPS, EXPERTS_PER_GROUP)
    in_grp = jnp.take_along_axis(exp_logits, g_sel[:, :, None], axis=1)[:, 0]
    top_logit, top_idx = lax.top_k(in_grp, TOP_K)
    w_top = jax.nn.softmax(top_logit, axis=-1) * p_grp
    w_exp = jnp.einsum('tk,tke->te', w_top, jax.nn.one_hot(top_idx, EXPERTS_PER_GROUP, dtype=jnp.float32))
    combine = (jax.nn.one_hot(g_sel[:, 0], N_GROUPS, dtype=jnp.float32)[:, :, None]
               * w_exp[:, None, :]).astype(h.dtype)
    out = jnp.zeros_like(t)
    for grp in range(N_GROUPS):
        hid = jax.nn.silu(jnp.einsum('td,edf->tef', t, w_gate[grp])) * jnp.einsum('td,edf->tef', t, w_up[grp])
        out = out + jnp.einsum('tef,efd->td', hid * combine[:, grp, :, None], w_down[grp])
    return out.reshape(shape)


def setup_inputs(seed: int = 0) -> dict:
    key = jax.random.key(seed)
    ks = jax.random.split(key, 36)
    d = D_MODEL
    nl = DEPTH

    def nrm(k, shape, scale):
        return jax.random.normal(k, shape, jnp.float32) * scale

    dt = jnp.exp(jax.random.uniform(ks[11], (nl, 2, H_A), minval=math.log(1e-3), maxval=math.log(1e-1)))
    return {
        'x': nrm(ks[0], (BATCH, SEQ, d), 1.0),
        'c': nrm(ks[1], (BATCH, d), 1.0),
        'ctx': nrm(ks[2], (BATCH, CTX_LEN, d), 1.0),
        'c_ctx': nrm(ks[3], (d,), 1.0),
        'ada_w': nrm(ks[4], (nl, d, 6 * d), 0.5 * d ** -0.5),
        'ada_b': nrm(ks[5], (nl, 6 * d), 0.01),
        'norm_mix_g': 1.0 + nrm(ks[6], (nl, d), 0.05),
        'norm_ffn_g': 1.0 + nrm(ks[7], (nl, d), 0.05),
        'w_in': nrm(ks[8], (nl, d, IN_COLS), d ** -0.5),
        'gdn_conv': nrm(ks[9], (nl, SHORT_CONV, 3 * C_A), SHORT_CONV ** -0.5),
        'gdn_a_log': jnp.log(jax.random.uniform(ks[10], (nl, 2, H_A), minval=1.0, maxval=16.0)),
        'gdn_dt_bias': dt + jnp.log(-jnp.expm1(-dt)),
        'gdn_onorm_g': 1.0 + nrm(ks[12], (nl, DV_A), 0.05),
        'rwkv_mu': jax.random.uniform(ks[13], (nl, B_COLS)),
        'rwkv_w0': jax.random.uniform(ks[14], (nl, 2, C_B), minval=-6.0, maxval=-1.0),
        'rwkv_w2': nrm(ks[15], (nl, 2, LORA_W, C_B), 0.1),
        'rwkv_a0': nrm(ks[16], (nl, 2, C_B), 0.1),
        'rwkv_a2': nrm(ks[17], (nl, 2, LORA_A, C_B), 0.1),
        'rwkv_g2': nrm(ks[18], (nl, LORA_G, C_B), LORA_G ** -0.5),
        'rwkv_k_k': 0.85 + nrm(ks[19], (nl, C_B), 0.05),
        'rwkv_k_a': 1.0 + nrm(ks[20], (nl, C_B), 0.05),
        'rwkv_r_k': nrm(ks[21], (nl, H_B, N_B), 0.1),
        'rwkv_lnx_g': 1.0 + nrm(ks[22], (nl, C_B), 0.05),
        'rwkv_lnx_b': nrm(ks[23], (nl, C_B), 0.01),
        'w_o_a': nrm(ks[24], (nl, C_A, d), C_A ** -0.5),
        'w_o_b': nrm(ks[25], (nl, C_B, d), C_B ** -0.5),
        'w_out': nrm(ks[26], (nl, d, d), d ** -0.5),
        'router_grp': nrm(ks[27], (nl, d, N_GROUPS), d ** -0.5),
        'router_grp_b': nrm(ks[28], (nl, N_GROUPS), 0.01),
        'router_exp': nrm(ks[29], (nl, d, N_EXPERTS), d ** -0.5),
        'router_exp_b': nrm(ks[30], (nl, N_EXPERTS), 0.01),
        'moe_w_gate': nrm(ks[31], (nl, N_GROUPS, EXPERTS_PER_GROUP, d, D_EXPERT), d ** -0.5),
        'moe_w_up': nrm(ks[32], (nl, N_GROUPS, EXPERTS_PER_GROUP, d, D_EXPERT), d ** -0.5),
        'moe_w_down': nrm(ks[33], (nl, N_GROUPS, EXPERTS_PER_GROUP, D_EXPERT, d), D_EXPERT ** -0.5),
        'final_norm_g': 1.0 + nrm(ks[34], (d,), 0.05),
    }


def reference(x, c, ctx, c_ctx, ada_w, ada_b, norm_mix_g, norm_ffn_g, w_in, gdn_conv, gdn_a_log, gdn_dt_bias,
              gdn_onorm_g, rwkv_mu, rwkv_w0, rwkv_w2, rwkv_a0, rwkv_a2, rwkv_g2, rwkv_k_k, rwkv_k_a, rwkv_r_k,
              rwkv_lnx_g, rwkv_lnx_b, w_o_a, w_o_b, w_out, router_grp, router_grp_b, router_exp, router_exp_b,
              moe_w_gate, moe_w_up, moe_w_down, final_norm_g):
    rows = x.shape[1] // GRID_W
    s_lat = jax.nn.silu(c)[:, None, :]
    s_ctx = jax.nn.silu(c_ctx)
    h_lat, h_ctx = x, ctx
    for l in range(DEPTH):
        last = l == DEPTH - 1
        m_lat = jnp.split(s_lat @ ada_w[l] + ada_b[l], 6, axis=-1)
        m_ctx = jnp.split(s_ctx @ ada_w[l] + ada_b[l], 6, axis=-1)
        u_lat = modulate(rmsnorm(h_lat, norm_mix_g[l]), m_lat[0], m_lat[1])
        u_ctx = modulate(rmsnorm(h_ctx, norm_mix_g[l]), m_ctx[0], m_ctx[1])
        y_lat, y_ctx = mixing_sublayer(
            u_lat, u_ctx, rows, not last, w_in[l], gdn_conv[l], gdn_a_log[l], gdn_dt_bias[l], gdn_onorm_g[l],
            rwkv_mu[l], rwkv_w0[l], rwkv_w2[l], rwkv_a0[l], rwkv_a2[l], rwkv_g2[l], rwkv_k_k[l], rwkv_k_a[l],
            rwkv_r_k[l], rwkv_lnx_g[l], rwkv_lnx_b[l], w_o_a[l], w_o_b[l], w_out[l])
        h_lat = h_lat + m_lat[2] * y_lat
        h_lat = h_lat + m_lat[5] * hier_moe(
            modulate(rmsnorm(h_lat, norm_ffn_g[l]), m_lat[3], m_lat[4]), router_grp[l], router_grp_b[l],
            router_exp[l], router_exp_b[l], moe_w_gate[l], moe_w_up[l], moe_w_down[l])
        if not last:
            h_ctx = h_ctx + m_ctx[2] * y_ctx
            h_ctx = h_ctx + m_ctx[5] * hier_moe(
                modulate(rmsnorm(h_ctx, norm_ffn_g[l]), m_ctx[3], m_ctx[4]), router_grp[l], router_grp_b[l],
                router_exp[l], router_exp_b[l], moe_w_gate[l], moe_w_up[l], moe_w_down[l])
    return rmsnorm(h_lat, final_norm_g)
```

```python
import os
import numpy as np
import concourse.bass as bass
import concourse.mybir as mybir
from concourse.bass_utils import run_bass_kernel_spmd
from concourse.alu_op_type import AluOpType as ALU
from contextlib import ExitStack

F32 = mybir.dt.float32
F32R = mybir.dt.float32r
BF16 = mybir.dt.bfloat16
AF = mybir.ActivationFunctionType
AX = mybir.AxisListType

NCORES = 8
D = 1024
SEQ = 2048
CTX = 256
NTOK = SEQ + CTX
IN_COLS = 6032
A_COLS = 2064
B_COLS = 1920
EPS = 1e-6
LNX_EPS = 1e-5 * 64


class Buf:
    def __init__(self, t, name, psum=False):
        self.t = t
        self.name = name
        self.lw = None
        self.rd = {}
        self.psum = psum
        self.bankrd = None

    def __getitem__(self, idx):
        return self.t[idx]


class Ctx:
    ENG = ['pe', 'dve', 'act', 'pool', 'sp']
    NDMA = 8

    def __init__(self, nc, es):
        self.nc = nc
        self.e = {'pe': nc.tensor, 'dve': nc.vector, 'act': nc.scalar, 'pool': nc.gpsimd, 'sp': nc.sync}
        self.sem = {}
        self.cnt = {}
        for n in self.ENG:
            self.sem[n] = es.enter_context(nc.semaphore('s_' + n))
            self.cnt[n] = 0
        for i in range(self.NDMA):
            n = 'd%d' % i
            self.sem[n] = es.enter_context(nc.semaphore('s_' + n))
            self.cnt[n] = 0
        self.dma_rr = 0
        self.waited = {n: {} for n in self.ENG}
        self.nbuf = 0
        self.ninstr = 0

    def sb(self, es, shape, dt=F32, name=None):
        self.nbuf += 1
        name = (name or 'b') + '_%d' % self.nbuf
        t = es.enter_context(self.nc.sbuf_tensor(name, list(shape), dt))
        return Buf(t, name)

    def ps(self, es, shape, dt=F32, name=None):
        self.nbuf += 1
        name = (name or 'p') + '_%d' % self.nbuf
        t = es.enter_context(self.nc.psum_tensor(name, list(shape), dt))
        return Buf(t, name, psum=True)

    def view(self, buf, name='v'):
        self.nbuf += 1
        return Buf(buf.t, name + '_%d' % self.nbuf)

    def _deps(self, reads, writes):
        deps = {}

        def add(k, v):
            if v > deps.get(k, 0):
                deps[k] = v
        for b in reads:
            if b.lw:
                add(*b.lw)
            if b.psum:
                for k, v in b.rd.items():
                    add(k, v)
                if b.bankrd is not None:
                    for k, v in b.bankrd.items():
                        add(k, v)
        for b in writes:
            if b.lw:
                add(*b.lw)
            for k, v in b.rd.items():
                add(k, v)
        return deps

    def _wait(self, E, deps):
        eng = self.e[E]
        w = self.waited[E]
        nw = 0
        for k, v in deps.items():
            if k == E and E == 'pe' and v > self.cnt['pe']:
                continue
            if w.get(k, 0) >= v:
                continue
            eng.wait_ge(self.sem[k], v)
            nw += 1
            w[k] = v

    def op(self, E, fn, reads=(), writes=(), inc=True):
        deps = self._deps(reads, writes)
        self._wait(E, deps)
        ins = fn(self.e[E])
        self.ninstr += 1
        if inc:
            self.cnt[E] += 1
            ins.then_inc(self.sem[E], 1)
            cval = self.cnt[E]
        else:
            cval = self.cnt[E] + 1
        for b in writes:
            b.lw = (E, cval)
            b.rd = {}
        for b in reads:
            if b not in writes:
                b.rd[E] = max(b.rd.get(E, 0), cval)
            if b.bankrd is not None and E != 'pe':
                b.bankrd[E] = max(b.bankrd.get(E, 0), cval)
        return ins

    def dma(self, out, in_, reads=(), writes=(), q='sp', **kw):
        slot = 'd%d' % self.dma_rr
        self.dma_rr = (self.dma_rr + 1) % self.NDMA
        deps = self._deps(reads, writes)
        if self.cnt[slot] > 0:
            deps[slot] = max(deps.get(slot, 0), self.cnt[slot])
        self._wait(q, deps)
        ins = self.e[q].dma_start(out=out, in_=in_, **kw)
        self.ninstr += 1
        self.cnt[slot] += 16
        ins.then_inc(self.sem[slot], 16)
        cval = self.cnt[slot]
        for b in writes:
            b.lw = (slot, cval)
            b.rd = {}
        for b in reads:
            b.rd[slot] = max(b.rd.get(slot, 0), cval)
        return ins

    def barrier(self):
        for E in self.ENG:
            deps = {k: v for k, v in self.cnt.items() if v > 0 and k != E}
            self._wait(E, deps)

    def finish(self):
        for k in self.sem:
            if k.startswith('d') and self.cnt[k] > 0:
                self.e['sp'].wait_ge(self.sem[k], self.cnt[k])

    def mm(self, out, lhsT, rhs, start, stop, reads, writes, inc=None):
        if inc is None:
            inc = stop
        return self.op('pe', lambda e: e.matmul(out, lhsT=lhsT, rhs=rhs, start=start, stop=stop),
                       reads=reads, writes=writes, inc=inc)

    def tr(self, out, in_, ident, reads, writes, inc=True):
        return self.op('pe', lambda e: e.transpose(out=out, in_=in_, identity=ident), reads=reads, writes=writes, inc=inc)

    def act(self, out, in_, func, reads, writes, E='act', **kw):
        return self.op('act', lambda e: e.activation(out=out, in_=in_, func=func, **kw), reads=reads, writes=writes)

    def ts(self, E, out, in0, s1, s2, op0, op1, reads, writes):
        if op1 is None:
            return self.op(E, lambda e: e.tensor_scalar(out=out, in0=in0, scalar1=s1, scalar2=None, op0=op0), reads=reads, writes=writes)
        return self.op(E, lambda e: e.tensor_scalar(out=out, in0=in0, scalar1=s1, scalar2=s2, op0=op0, op1=op1), reads=reads, writes=writes)

    def tt(self, E, out, in0, in1, op, reads, writes):
        return self.op(E, lambda e: e.tensor_tensor(out=out, in0=in0, in1=in1, op=op), reads=reads, writes=writes)

    def stt(self, out, in0, scalar, in1, op0, op1, reads, writes):
        return self.op('dve', lambda e: e.scalar_tensor_tensor(out=out, in0=in0, scalar=scalar, in1=in1, op0=op0, op1=op1),
                       reads=reads, writes=writes)

    def cp(self, E, out, in_, reads, writes):
        if E == 'act':
            return self.op('act', lambda e: e.copy(out=out, in_=in_), reads=reads, writes=writes)
        return self.op(E, lambda e: e.tensor_copy(out=out, in_=in_), reads=reads, writes=writes)


def R(ap):
    return ap.bitcast(F32R)


def build(dbg=(), stage=99):
    nc = bass.Bass("TRN2", target_bir_lowering=False)
    T = {}

    def din(name, shape, dt=F32):
        T[name] = nc.dram_tensor(name, list(shape), dt, kind="ExternalInput").ap()
        return T[name]

    def dout(name, shape, dt=F32):
        T[name] = nc.dram_tensor(name, list(shape), dt, kind="ExternalOutput").ap()
        return T[name]

    x = din("x", [SEQ, D])
    ctx = din("ctx", [CTX, D])
    cT = din("cT", [128, 8, 2])
    ada_w = din("ada_w", [D, 6 * D])
    ada_bT = din("ada_bT", [128, 48])
    gmixT = din("gmixT", [128, 8])
    gffnT = din("gffnT", [128, 8])
    gfin_bc = din("gfin_bc", [128, D])
    w_in = din("w_in", [D, IN_COLS])
    convT = din("convT", [128, 12, 5])
    alog_bc = din("alog_bc", [128, 18, 8])
    dtb_bc = din("dtb_bc", [128, 18, 8])
    onormT = din("onormT", [128, 1])
    muT = din("muT", [128, 15])
    w0T = din("w0T", [128, 2, 4])
    a0T = din("a0T", [128, 2, 4])
    w2m = din("w2m", [128, 512])
    a2m = din("a2m", [128, 512])
    g2m = din("g2m", [128, 512])
    rwv = din("rwv", [128, 5, 4])
    scr_ob = nc.dram_tensor("scr_ob", [SEQ, 512], BF16, kind="Internal").ap()
    scr_h = nc.dram_tensor("scr_h", [SEQ, D], F32, kind="Internal").ap()
    scr_u = nc.dram_tensor("scr_u", [128, 8, SEQ], BF16, kind="Internal").ap()
    w_o_a = din("w_o_a", [512, D])
    w_o_b = din("w_o_b", [512, D])
    w_out = din("w_out", [D, D])
    wrt = din("wrt", [128, 8, 36])
    rb_bc = din("rb_bc", [128, 36])
    moe_wg = din("moe_wg", [32, D, 256])
    moe_wu = din("moe_wu", [32, D, 256])
    moe_wd = din("moe_wd", [32, 256, D])
    out = dout("out", [SEQ, D])
    for name, shape, dt in dbg:
        dout(name, shape, dt)

    with ExitStack() as es:
        c = Ctx(nc, es)
        PS = [c.ps(es, [128, 512], F32, 'ps%d' % i) for i in range(8)]

        def psb(i):
            return PS[i][:].bitcast(BF16)

        bank_reads = [dict() for _ in range(8)]

        class Reg:
            def __init__(self, bank, c0, n, name):
                self.bank, self.c0, self.n = bank, c0, n
                self.buf = PS[bank]

            def ap(self, lo=0, hi=None, rows=slice(None)):
                hi = self.n if hi is None else hi
                return PS[self.bank].t[rows, self.c0 + lo:self.c0 + hi]

        def run_threads(gens):
            gens = list(gens)
            while gens:
                for g_ in list(gens):
                    try:
                        next(g_)
                    except StopIteration:
                        gens.remove(g_)

        def par(*gens):
            gens = list(gens)
            while gens:
                for g_ in list(gens):
                    try:
                        next(g_)
                    except StopIteration:
                        gens.remove(g_)
                        continue
                    yield

        ident = c.sb(es, [128, 128], F32, 'ident')
        identb = c.sb(es, [128, 128], BF16, 'identb')
        onesf = c.sb(es, [128, 128], F32, 'onesf')
        c.op('pool', lambda e: e.memset(ident[:], 0.0), writes=[ident])
        c.op('pool', lambda e: e.affine_select(out=ident[:], in_=ident[:], pattern=[[-1, 128]], compare_op=ALU.not_equal,
                                                fill=1.0, base=0, channel_multiplier=1), reads=[ident], writes=[ident])
        c.cp('dve', identb[:], ident[:], [ident], [identb])
        c.op('pool', lambda e: e.memset(onesf[:], 1.0), writes=[onesf])
        onesb = c.sb(es, [128, 128], BF16, 'onesb')
        c.cp('dve', onesb[:], onesf[:], [onesf], [onesb])
        ones_r = c.sb(es, [128, 128], F32, 'ones_r')
        nones_r = c.sb(es, [128, 128], F32, 'nones_r')
        ident_r = c.sb(es, [128, 128], F32, 'ident_r')
        c.cp('dve', R(ones_r[:]), onesf[:], [onesf], [ones_r])
        c.ts('dve', R(nones_r[:]), onesf[:], -1.0, None, ALU.mult, None, [onesf], [nones_r])
        c.cp('dve', R(ident_r[:]), ident[:], [ident], [ident_r])
        blk = c.sb(es, [128, 128], F32, 'blk')
        c.op('pool', lambda e: e.memset(blk[:], 0.0), writes=[blk])
        c.op('pool', lambda e: e.memset(blk[0:64, 0:64], 1.0), reads=[blk], writes=[blk])
        c.op('pool', lambda e: e.memset(blk[64:128, 64:128], 1.0), reads=[blk], writes=[blk])
        incl = [c.sb(es, [128, 128], F32, 'incl%d' % d) for d in range(2)]
        strict = [c.sb(es, [128, 128], F32, 'strict%d' % d) for d in range(2)]
        incl_r = [c.sb(es, [128, 128], F32, 'inclr%d' % d) for d in range(2)]
        negm_r = [c.sb(es, [128, 128], F32, 'negm%d' % d) for d in range(2)]
        blk_r = c.sb(es, [128, 128], F32, 'blk_r')
        sel_r = [c.sb(es, [128, 128], F32, 'sel%d' % k_) for k_ in range(2)]
        notI = c.sb(es, [128, 128], F32, 'notI')
        for d in range(2):
            pat = [[1, 128]] if d == 0 else [[-1, 128]]
            cm = -1 if d == 0 else 1
            c.op('pool', lambda e, d=d, pat=pat, cm=cm: e.affine_select(out=incl[d][:], in_=blk[:], pattern=pat, compare_op=ALU.is_ge,
                                                                        fill=0.0, base=0, channel_multiplier=cm), reads=[blk], writes=[incl[d]])
            c.op('pool', lambda e, d=d, pat=pat, cm=cm: e.affine_select(out=strict[d][:], in_=blk[:], pattern=pat, compare_op=ALU.is_gt,
                                                                        fill=0.0, base=0, channel_multiplier=cm), reads=[blk], writes=[strict[d]])
            c.cp('dve', R(incl_r[d][:]), incl[d][:], [incl[d]], [incl_r[d]])
            c.ts('dve', R(negm_r[d][:]), incl[d][:], 1.0e5, -1.0e5, ALU.mult, ALU.add, [incl[d]], [negm_r[d]])
        c.cp('dve', R(blk_r[:]), blk[:], [blk], [blk_r])
        c.ts('dve', notI[:], ident[:], -1.0, 1.0, ALU.mult, ALU.add, [ident], [notI])
        zf = c.sb(es, [128, 128], F32, 'zf')
        c.op('pool', lambda e: e.memset(zf[:], 0.0), writes=[zf])
        for k_ in range(2):
            c.cp('dve', R(sel_r[k_][:]), zf[:], [zf], [sel_r[k_]])
            c.cp('dve', R(sel_r[k_][k_ * 64:(k_ + 1) * 64, :]), onesf[k_ * 64:(k_ + 1) * 64, :], [onesf, sel_r[k_]], [sel_r[k_]])

        mT = c.sb(es, [128, 48, 2], F32, 'mT')
        A1g = c.sb(es, [128, 8, 2], F32, 'A1g')
        A2g = c.sb(es, [128, 8, 2], F32, 'A2g')
        oaT = c.sb(es, [128, 4, SEQ], BF16, 'oaT')

        with ExitStack() as es1:
            sT = c.sb(es1, [128, 8, 2], F32, 'sT')
            abT = c.sb(es1, [128, 48], F32, 'abT')
            gm = c.sb(es1, [128, 8], F32, 'gm')
            gf = c.sb(es1, [128, 8], F32, 'gf')
            c.dma(sT[:], cT, writes=[sT])
            c.dma(abT[:], ada_bT, writes=[abT])
            c.dma(gm[:], gmixT, writes=[gm])
            c.dma(gf[:], gffnT, writes=[gf])
            c.act(sT[:], sT[:], AF.Silu, [sT], [sT])
            Wb = [c.sb(es1, [128, 8, 512], F32, 'adaw%d' % i) for i in range(4)]
            ada_v = ada_w.rearrange("(kc p) n -> p kc n", p=128)
            for blk in range(12):
                wb = Wb[blk % 4]
                c.dma(wb[:], ada_v[:, :, blk * 512:(blk + 1) * 512], writes=[wb], q=('sp' if blk % 2 == 0 else 'act'))
                for mc in range(4):
                    col = (blk * 4 + mc) * 2
                    for kc in range(8):
                        c.mm(PS[0][:, col:col + 2], wb[:, kc, mc * 128:(mc + 1) * 128], sT[:, kc, :],
                             kc == 0, kc == 7, [wb, sT], [PS[0]])
            pv = PS[0][:, 0:96].rearrange("p (m s) -> p m s", s=2)
            for s in range(2):
                c.tt('dve', mT[:, :, s], pv[:, :, s], abT[:], ALU.add, [PS[0], abT], [mT])
            for s in range(2):
                c.stt(A1g[:, :, s], mT[:, 8:16, s], 1.0, gm[:], ALU.add, ALU.mult, [mT, gm], [A1g])
                c.stt(A2g[:, :, s], mT[:, 32:40, s], 1.0, gf[:], ALU.add, ALU.mult, [mT, gf], [A2g])
            c.barrier()

        def make_Bg(es_, Bg, base):
            dg = [c.sb(es_, [128, 128], F32, 'dg%d' % i) for i in range(2)]
            for fc in range(8):
                d_ = dg[fc % 2]
                c.ts('dve', d_[:], ident[:], mT[:, base + fc, 0:1], None, ALU.mult, None, [ident, mT], [d_])
                bank = PS[1 + fc // 4]
                c.mm(bank[:, (fc % 4) * 128:(fc % 4 + 1) * 128], onesf[:], d_[:], True, True, [onesf, d_], [bank])
            c.cp('act', Bg[:, 0:512], PS[1][:], [PS[1]], [Bg])
            c.cp('act', Bg[:, 512:1024], PS[2][:], [PS[2]], [Bg])

        scru_buf = Buf(None, 'scr_u_dram')

        def make_uT_g(srcs, s, dst, tok0, Ag, shift_base, bufs, k):
            X = bufs['X'][k % 4]
            xnb = bufs['xnb'][k % 2]
            ss = bufs['ss'][k % 2]
            sq_ = bufs['sq'][k % 2]
            for (p0, p1, ap) in srcs:
                c.dma(X[p0:p1, :], ap, writes=[X], q=('sp' if k % 2 == 0 else 'pool'))
            c.act(sq_[:], X[:], AF.Square, [X], [sq_, ss], accum_out=ss[:, 0:1])
            yield
            c.ts('dve', ss[:, 1:2], ss[:, 0:1], 1.0 / D, EPS, ALU.mult, ALU.add, [ss], [ss])
            yield
            c.act(ss[:, 2:3], ss[:, 1:2], AF.Sqrt, [ss], [ss])
            yield
            c.op('dve', lambda e: e.reciprocal(out=ss[:, 3:4], in_=ss[:, 2:3]), reads=[ss], writes=[ss])
            c.ts('dve', xnb[:], X[:], ss[:, 3:4], None, ALU.mult, None, [X, ss], [xnb])
            yield
            bank = PS[3 + k % 2]
            pb = psb(3 + k % 2)
            for fc in range(8):
                c.tr(pb[:, fc * 128:(fc + 1) * 128], xnb[:, fc * 128:(fc + 1) * 128], identb[:], [xnb, identb], [bank],
                     inc=(fc == 7))
            yield
            for fc in range(8):
                o_ = dst[:, fc, tok0:tok0 + 128]
                i_ = pb[:, fc * 128:(fc + 1) * 128]
                if fc % 2 == 0:
                    c.ts('dve', o_, i_, Ag[:, fc, s:s + 1], mT[:, shift_base + fc, s:s + 1], ALU.mult, ALU.add,
                         [bank, Ag, mT], [dst])
                else:
                    c.act(o_, i_, AF.Identity, [bank, Ag, mT], [dst], scale=Ag[:, fc, s:s + 1],
                          bias=mT[:, shift_base + fc, s:s + 1])
                if fc % 4 == 3:
                    yield

        def run_uT(jobs, bufs):
            def chain(par_):
                for k in range(par_, len(jobs), 2):
                    srcs, s_, dst, tok0 = jobs[k]
                    yield from make_uT_g(srcs, s_, dst, tok0, A1g, 0, bufs, k)
            run_threads([chain(0), chain(1)])

        def uT_bufs(es_):
            return {'X': [c.sb(es_, [128, D], F32, 'X%d' % i) for i in range(4)],
                    'xnb': [c.sb(es_, [128, D], BF16, 'xnb%d' % i) for i in range(2)],
                    'ss': [c.sb(es_, [128, 4], F32, 'ss%d' % i) for i in range(2)],
                    'sq': [c.sb(es_, [128, D], BF16, 'sq%d' % i) for i in range(2)]}

        esG = ExitStack()
        with esG:
            uT_r = c.sb(esG, [128, 8, NTOK], BF16, 'uT_r')
            with ExitStack() as es2:
                bufs = uT_bufs(es2)
                jobs = [([(0, 128, ctx[t * 128:(t + 1) * 128, :])], 1, uT_r, t * 128) for t in range(2)]
                jobs += [([(0, 128, x[t * 128:(t + 1) * 128, :])], 0, uT_r, CTX + t * 128) for t in range(16)]
                run_uT(jobs, bufs)
                for kc in range(8):
                    c.dma(scr_u[:, kc, :], uT_r[:, kc, CTX:NTOK], reads=[uT_r], writes=[scru_buf])
                c.barrier()


            w_in_v = w_in.rearrange("(kc p) n -> p kc n", p=128)
            TBLK = [(0, 256), (256, 768), (768, 1280), (1280, 1792), (1792, 2304)]
            with ExitStack() as es3:
                g_tok = c.sb(es3, [128, 18, 8], F32, 'g_tok')
                b_tok = c.sb(es3, [128, 18, 8], F32, 'b_tok')
                with ExitStack() as es3a:
                    wab = c.sb(es3a, [128, 8, 16], BF16, 'wab')
                    ab = c.sb(es3a, [128, 18, 16], F32, 'ab')
                    alog = c.sb(es3a, [128, 18, 8], F32, 'alog')
                    dtb = c.sb(es3a, [128, 18, 8], F32, 'dtb')
                    t1 = c.sb(es3a, [128, 18, 8], F32, 't1')
                    t2 = c.sb(es3a, [128, 18, 8], F32, 't2')
                    c.dma(wab[:], w_in_v[:, :, 2048:2064], writes=[wab], q='pool')
                    c.dma(alog[:], alog_bc, writes=[alog])
                    c.dma(dtb[:], dtb_bc, writes=[dtb])
                    for t in range(18):
                        bank = PS[t % 2]
                        for kc in range(8):
                            c.mm(bank[:, 0:16], uT_r[:, kc, t * 128:(t + 1) * 128], wab[:, kc, :], kc == 0, kc == 7, [uT_r, wab], [bank])
                        c.cp('act', ab[:, t, :], bank[:, 0:16], [bank], [ab])
                    c.tt('dve', t1[:], ab[:, :, 0:8], dtb[:], ALU.add, [ab, dtb], [t1])
                    c.stt(t2[:], t1[:], -1.0, t1[:], ALU.mult, ALU.max, [t1], [t2])
                    c.act(t2[:], t2[:], AF.Exp, [t2], [t2], scale=-1.0)
                    c.ts('dve', t2[:], t2[:], 1.0, None, ALU.add, None, [t2], [t2])
                    c.act(t2[:], t2[:], AF.Ln, [t2], [t2])
                    c.stt(t1[:], t1[:], 0.0, t2[:], ALU.max, ALU.add, [t1, t2], [t1])
                    c.act(alog[:], alog[:], AF.Exp, [alog], [alog])
                    c.stt(R(g_tok[:]), t1[:], -1.0, alog[:], ALU.mult, ALU.mult, [t1, alog], [g_tok])
                    c.act(b_tok[:], ab[:, :, 8:16], AF.Sigmoid, [ab], [b_tok])
                    c.barrier()

                cv = c.sb(es3, [128, 12, 5], F32, 'cv')
                onm = c.sb(es3, [128, 1], F32, 'onm')
                c.dma(cv[:], convT, writes=[cv])
                c.dma(onm[:], onormT, writes=[onm])
                raws = [c.sb(es3, [128, NTOK], F32, 'raw%d' % i) for i in range(2)]
                accs = [c.sb(es3, [128, NTOK], F32, 'acc%d' % i) for i in range(2)]
                sqs = [c.sb(es3, [128, NTOK], F32, 'sqg%d' % i) for i in range(2)]
                qT = c.sb(es3, [128, NTOK], F32, 'qT')
                kT = c.sb(es3, [128, NTOK], F32, 'kT')
                vT = c.sb(es3, [128, NTOK], F32, 'vT')
                zs = c.sb(es3, [128, SEQ], F32, 'zs')
                o_acc = c.sb(es3, [128, 16, 128], F32, 'o_acc')
                wc = [c.sb(es3, [128, 8, 128], BF16, 'wc%d' % i) for i in range(2)]
                rns = [[c.sb(es3, [128, 512], F32, 'rn%d_%d' % (j, i)) for i in range(2)] for j in range(2)]
                S = [c.sb(es3, [128, 128], F32, 'S%d' % d) for d in range(2)]
                gcs = [c.sb(es3, [128, 8], F32, 'gcs%d' % i) for i in range(2)]
                egc = [c.sb(es3, [128, 8], F32, 'egc%d' % i) for i in range(2)]
                negc = [c.sb(es3, [128, 8], F32, 'negc%d' % i) for i in range(2)]
                ekd = [c.sb(es3, [128, 8], F32, 'ekd%d' % i) for i in range(2)]
                gend = [c.sb(es3, [128, 2, 8], F32, 'gend%d' % i) for i in range(2)]
                k_tok = [c.sb(es3, [128, 128], F32, 'k_tok%d' % i) for i in range(2)]
                v_tok = [c.sb(es3, [128, 128], F32, 'v_tok%d' % i) for i in range(2)]
                NS = 2
                Gt = [c.sb(es3, [128, 128], F32, 'Gt%d' % i) for i in range(NS)]
                Ei = [c.sb(es3, [128, 128], F32, 'Ei%d' % i) for i in range(NS)]
                Es = [c.sb(es3, [128, 128], F32, 'Es%d' % i) for i in range(NS)]
                QKm = [c.sb(es3, [128, 128], F32, 'QKm%d' % i) for i in range(NS)]
                Pb = [[c.sb(es3, [128, 128], F32, 'P%d_%d' % (i, j)) for j in range(2)] for i in range(NS)]
                Qb = [[c.sb(es3, [128, 128], F32, 'Q%d_%d' % (i, j)) for j in range(2)] for i in range(NS)]
                Wb_ = [[c.sb(es3, [128, 128], F32, 'W%d_%d' % (i, j)) for j in range(2)] for i in range(NS)]
                kdec = [c.sb(es3, [128, 128], F32, 'kdec%d' % i) for i in range(NS)]
                Zb = [c.sb(es3, [128, 128], F32, 'Z%d' % i) for i in range(NS)]
                vnew = [c.sb(es3, [128, 128], F32, 'vnew%d' % i) for i in range(NS)]
                otmp = [c.sb(es3, [128, 128], F32, 'otmp%d' % i) for i in range(NS)]
                otmp2 = [c.sb(es3, [128, 128], F32, 'otmp2%d' % i) for i in range(NS)]
                fin = [c.sb(es3, [128, 132], F32, 'fin%d' % i) for i in range(2)]
                wcnt = 0
                unit = 0
                import os
                H_all = c.sb(es3, [128, 18, 32], F32, 'H_all')
                egc_all = c.sb(es3, [128, 18, 8], F32, 'egc_all')
                negc_all = c.sb(es3, [128, 18, 8], F32, 'negc_all')
                ekd_all = c.sb(es3, [128, 18, 8], F32, 'ekd_all')
                gend_all = c.sb(es3, [128, 18, 16], F32, 'gend_all')
                for t in range(18):
                    bankH = PS[t % 2]
                    c.mm(bankH[:, 0:4], R(incl_r[0][:]), R(g_tok[:, t, 0:4]), True, True, [incl_r[0], g_tok], [bankH])
                    c.mm(bankH[:, 4:8], R(incl_r[1][:]), R(g_tok[:, t, 4:8]), True, True, [incl_r[1], g_tok], [bankH])
                    c.mm(bankH[:, 8:16], R(blk_r[:]), R(g_tok[:, t, :]), True, True, [blk_r, g_tok], [bankH])
                    c.mm(bankH[:, 16:24], R(sel_r[0][:]), R(g_tok[:, t, :]), True, True, [sel_r[0], g_tok], [bankH])
                    c.mm(bankH[:, 24:32], R(sel_r[1][:]), R(g_tok[:, t, :]), True, True, [sel_r[1], g_tok], [bankH])
                    c.cp('dve', H_all[:, t, :], bankH[:, 0:32], [bankH], [H_all])
                c.act(egc_all[:], H_all[:, :, 0:8], AF.Exp, [H_all], [egc_all])
                c.ts('dve', negc_all[:], egc_all[:], -1.0, None, ALU.mult, None, [egc_all], [negc_all])
                c.tt('dve', ekd_all[:], H_all[:, :, 8:16], H_all[:, :, 0:8], ALU.subtract, [H_all], [ekd_all])
                c.act(ekd_all[:], ekd_all[:], AF.Exp, [ekd_all], [ekd_all])
                c.act(gend_all[:], H_all[:, :, 16:32], AF.Exp, [H_all], [gend_all])
                NH = int(os.environ.get('K_NH', '4'))
                NSTEP = int(os.environ.get('K_NSTEP', '18'))
                KLAT = int(os.environ.get('K_LAT', '9'))
                for h in range(NH):
                    def proj_g(ci, slot, h=h):
                        col0, dst = [(h * 128, qT), (512 + h * 128, kT), (1024 + h * 128, vT), (1536 + h * 128, zs)][ci]
                        raw, acc, sq = raws[slot], accs[slot], sqs[slot]
                        w_ = wc[slot]
                        pb0 = 4 * slot
                        if h == 0 and ci < 2:
                            c.dma(w_[:], w_in_v[:, :, col0:col0 + 128], writes=[w_], q='pool')
                        for bi, (t0, t1_) in enumerate(TBLK):
                            if ci == 3 and bi == 0:
                                continue
                            bank = PS[pb0 + bi % 2]
                            n = t1_ - t0
                            for kc in range(8):
                                c.mm(bank[:, 0:n], w_[:, kc, :], uT_r[:, kc, t0:t1_], kc == 0, kc == 7, [w_, uT_r], [bank])
                            if ci == 3:
                                c.act(zs[:, t0 - CTX:t1_ - CTX], bank[:, 0:n], AF.Silu, [bank], [zs])
                            else:
                                c.cp('act', raw[:, t0:t1_], bank[:, 0:n], [bank], [raw])
                            yield
                        nh_, nci = (h, ci + 2) if ci < 2 else (h + 1, ci - 2)
                        if nh_ < NH:
                            ncol = [nh_ * 128, 512 + nh_ * 128, 1024 + nh_ * 128, 1536 + nh_ * 128][nci]
                            c.dma(w_[:], w_in_v[:, :, ncol:ncol + 128], writes=[w_], q='pool')
                        if ci == 3:
                            return
                        cch = ci * 4 + h
                        c.ts('dve', acc[:], raw[:], cv[:, cch, 2:3], None, ALU.mult, None, [raw, cv], [acc])
                        yield
                        for kk_ in (0, 1, 3, 4):
                            sft = kk_ - 2
                            for (a_, b_) in ((0, CTX), (CTX, NTOK)):
                                lo = max(a_, a_ - sft)
                                hi = min(b_, b_ - sft)
                                c.stt(acc[:, lo:hi], raw[:, lo + sft:hi + sft], cv[:, cch, kk_:kk_ + 1], acc[:, lo:hi], ALU.mult, ALU.add,
                                      [raw, cv, acc], [acc])
                            yield
                        if ci == 2:
                            c.act(R(vT[:]), acc[:], AF.Silu, [acc], [vT])
                            return
                        c.act(acc[:], acc[:], AF.Silu, [acc], [acc])
                        c.act(R(sq[:]), acc[:], AF.Square, [acc], [sq])
                        yield
                        sc = 128.0 if ci == 0 else 1.0
                        for bi, (t0, t1_) in enumerate(TBLK):
                            bank = PS[pb0 + 2 + bi % 2]
                            n = t1_ - t0
                            r_ = rns[slot][bi % 2]
                            c.mm(bank[:, 0:n], R(ones_r[:]), R(sq[:, t0:t1_]), True, True, [ones_r, sq], [bank])
                            c.ts('dve', r_[:, 0:n], bank[:, 0:n], sc, EPS * sc, ALU.mult, ALU.add, [bank], [r_])
                            c.act(r_[:, 0:n], r_[:, 0:n], AF.Sqrt, [r_], [r_])
                            yield
                            c.op('dve', lambda e, r_=r_, n=n: e.reciprocal(out=r_[:, 0:n], in_=r_[:, 0:n]), reads=[r_], writes=[r_])
                            c.tt('dve', R(dst[:, t0:t1_]), acc[:, t0:t1_], r_[:, 0:n], ALU.mult, [acc, r_], [dst])
                            yield

                    run_threads([proj_g(0, 0), proj_g(1, 1)])
                    run_threads([proj_g(2, 0), proj_g(3, 1)])
                    if 'd_qkv' in T and h == 0:
                        c.dma(T['d_qkv'][0], qT[:], reads=[qT])
                        c.dma(T['d_qkv'][1], kT[:], reads=[kT])
                        c.dma(T['d_qkv'][2], vT[:], reads=[vT])
                    for d in range(2):
                        c.ts('dve', R(S[d][:]), zf[:], 0.0, None, ALU.mult, None, [zf], [S[d]])
                    order_f = list(range(18))
                    order_b = [1, 0] + list(range(17, 1, -1))
                    def gdn_unit_g(d, step):
                        tile = order_f[step] if d == 0 else order_b[step]
                        is_lat = tile >= 2
                        ts0 = tile * 128
                        col = d * 4 + h
                        pi = d
                        u = d
                        X0, X1, X2, X3 = (PS[4 * d + i_] for i_ in range(4))
                        bankT = X3
                        c.tr(bankT[:, 0:128], kT[:, ts0:ts0 + 128], ident[:], [kT, ident], [bankT])
                        c.tr(bankT[:, 128:256], vT[:, ts0:ts0 + 128], ident[:], [vT, ident], [bankT])
                        bankA = X0
                        c.mm(bankA[:, 0:128], R(kT[:, ts0:ts0 + 128]), R(kT[:, ts0:ts0 + 128]), True, True, [kT], [bankA])
                        c.mm(bankA[:, 128:256], R(kT[:, ts0:ts0 + 128]), R(qT[:, ts0:ts0 + 128]), True, True, [kT, qT], [bankA])
                        c.ts('pool', R(Gt[u][:]), incl[d][:], g_tok[:, tile, col:col + 1], None, ALU.mult, None, [incl[d], g_tok], [Gt[u]])
                        yield
                        bankB = X1
                        c.mm(bankB[:, 0:128], R(ones_r[:]), R(Gt[u][:]), True, False, [ones_r, Gt[u]], [bankB])
                        c.mm(bankB[:, 0:128], R(Gt[u][:]), R(nones_r[:]), False, False, [nones_r, Gt[u]], [bankB])
                        c.mm(bankB[:, 0:128], R(ident_r[:]), R(negm_r[d][:]), False, True, [ident_r, negm_r[d]], [bankB])
                        yield
                        c.cp('act', k_tok[pi][:], bankT[:, 0:128], [bankT], [k_tok[pi]])
                        c.cp('act', R(v_tok[pi][:]), bankT[:, 128:256], [bankT], [v_tok[pi]])
                        c.act(Ei[u][:], bankB[:, 0:128], AF.Exp, [bankB], [Ei[u]])
                        yield
                        c.tt('pool', Es[u][:], Ei[u][:], notI[:], ALU.mult, [Ei[u], notI], [Es[u]])
                        P, Q, W = Pb[u], Qb[u], Wb_[u]
                        yield
                        c.stt(R(P[0][:]), bankA[:, 0:128], b_tok[:, tile, col:col + 1], Es[u][:], ALU.mult, ALU.mult,
                              [bankA, b_tok, Es[u]], [P[0]])
                        c.tt('dve', R(QKm[u][:]), bankA[:, 128:256], Ei[u][:], ALU.mult, [bankA, Ei[u]], [QKm[u]])
                        c.ts('dve', R(kdec[u][:]), k_tok[pi][:], ekd_all[:, tile, col:col + 1], None, ALU.mult, None, [k_tok[pi], ekd_all], [kdec[u]])
                        yield
                        bankC = X2
                        bankD = X3
                        c.tr(bankC[:, 0:128], P[0][:], ident[:], [P[0], ident], [bankC])
                        c.tt('pool', R(W[0][:]), ident[:], P[0][:], ALU.subtract, [ident, P[0]], [W[0]])
                        yield
                        c.cp('act', R(Q[0][:]), bankC[:, 0:128], [bankC], [Q[0]])
                        yield
                        for k_ in range(5):
                            a_, b_ = k_ % 2, (k_ + 1) % 2
                            c.mm(bankC[:, 128:256], R(P[a_][:]), R(Q[a_][:]), True, True, [P[a_], Q[a_]], [bankC])
                            if k_ < 4:
                                c.mm(bankD[:, 0:128], R(Q[a_][:]), R(P[a_][:]), True, True, [P[a_], Q[a_]], [bankD])
                            yield
                            c.cp('act', R(Q[b_][:]), bankC[:, 128:256], [bankC], [Q[b_]])
                            if k_ < 4:
                                c.cp('dve', R(P[b_][:]), bankD[:, 0:128], [bankD], [P[b_]])
                            yield
                            c.mm(bankD[:, 128:256], R(Q[b_][:]), R(W[a_][:]), True, True, [Q[b_], W[a_]], [bankD])
                            yield
                            c.tt('dve', R(W[b_][:]), bankD[:, 128:256], W[a_][:], ALU.add, [bankD, W[a_]], [W[b_]])
                            yield
                        Wf = W[1]
                        for cs in ((0, 64) if d == 0 else (64, 0)):
                            sl_ = slice(cs, cs + 64)
                            chunk = cs // 64
                            c.mm(X0[:, 0:128], R(kT[:, ts0:ts0 + 128]), R(S[d][:]), True, True, [kT, S[d]], [X0])
                            if is_lat:
                                c.mm(X0[:, 128:256], R(qT[:, ts0:ts0 + 128]), R(S[d][:]), True, True, [qT, S[d]], [X0])
                            yield
                            c.stt(R(Zb[u][sl_, :]), X0[sl_, 0:128], negc_all[sl_, tile, col:col + 1], v_tok[pi][sl_, :], ALU.mult, ALU.add,
                                  [X0, negc_all, v_tok[pi]], [Zb[u]])
                            if is_lat:
                                c.ts('dve', otmp[u][sl_, :], X0[sl_, 128:256], egc_all[sl_, tile, col:col + 1], None, ALU.mult, None, [X0, egc_all], [otmp[u]])
                            yield
                            c.mm(X1[:, 0:128], R(Wf[sl_, :]), R(Zb[u][sl_, :]), True, True, [Wf, Zb[u]], [X1])
                            yield
                            c.ts('dve', R(vnew[u][sl_, :]), X1[sl_, 0:128], b_tok[sl_, tile, col:col + 1], None, ALU.mult, None,
                                 [X1, b_tok], [vnew[u]])
                            yield
                            c.mm(X3[:, 256:384], R(kdec[u][sl_, :]), R(vnew[u][sl_, :]), True, True, [kdec[u], vnew[u]], [X3])
                            if is_lat:
                                c.mm(X2[:, 0:128], R(QKm[u][sl_, :]), R(vnew[u][sl_, :]), True, True, [QKm[u], vnew[u]], [X2])
                            yield
                            c.stt(R(S[d][:]), S[d][:], gend_all[:, tile, chunk * 8 + col:chunk * 8 + col + 1], X3[:, 256:384], ALU.mult, ALU.add,
                                  [S[d], gend_all, X3], [S[d]])
                            if is_lat:
                                lt = tile - 2
                                if (d == 0) == (lt <= 7):
                                    c.tt('dve', o_acc[sl_, lt, :], X2[sl_, 0:128], otmp[u][sl_, :], ALU.add, [X2, otmp[u]], [o_acc])
                                else:
                                    c.tt('dve', otmp2[u][sl_, :], X2[sl_, 0:128], otmp[u][sl_, :], ALU.add, [X2, otmp[u]], [otmp2[u]])
                                    c.tt('pool', o_acc[sl_, lt, :], o_acc[sl_, lt, :], otmp2[u][sl_, :], ALU.add, [o_acc, otmp2[u]], [o_acc])
                            yield

                    def gdn_dir_g(d):
                        for step in range(NSTEP):
                            yield from gdn_unit_g(d, step)

                    run_threads([gdn_dir_g(0), gdn_dir_g(1)])
                    def gdn_out_g(par_, h=h):
                        f_ = fin[par_]
                        bank = PS[par_]
                        for lt in range(par_, 16, 2):
                            c.act(f_[:, 0:128], o_acc[:, lt, :], AF.Square, [o_acc], [f_], accum_out=f_[:, 128:129])
                            yield
                            c.ts('dve', f_[:, 129:130], f_[:, 128:129], 1.0 / 128, EPS, ALU.mult, ALU.add, [f_], [f_])
                            yield
                            c.act(f_[:, 130:131], f_[:, 129:130], AF.Sqrt, [f_], [f_])
                            yield
                            c.op('dve', lambda e, f_=f_: e.reciprocal(out=f_[:, 131:132], in_=f_[:, 130:131]), reads=[f_], writes=[f_])
                            c.ts('dve', f_[:, 0:128], o_acc[:, lt, :], f_[:, 131:132], None, ALU.mult, None, [o_acc, f_], [f_])
                            yield
                            c.tr(bank[:, 0:128], f_[:, 0:128], ident[:], [f_, ident], [bank])
                            yield
                            c.stt(oaT[:, h, lt * 128:(lt + 1) * 128], bank[:, 0:128], onm[:, 0:1], zs[:, lt * 128:(lt + 1) * 128], ALU.mult, ALU.mult,
                                  [bank, onm, zs], [oaT])
                            yield

                    run_threads([gdn_out_g(0), gdn_out_g(1)])
                c.barrier()
        if 'd_oaT' in T:
            c.dma(T['d_oaT'], oaT[:], reads=[oaT])


        def inv_chain(P, Q, W, bankC, bankD):
            c.tr(bankC[:, 0:128], P[0][:], ident[:], [P[0], ident], [bankC])
            c.cp('act', R(Q[0][:]), bankC[:, 0:128], [bankC], [Q[0]])
            c.tt('pool', R(W[0][:]), ident[:], P[0][:], ALU.subtract, [ident, P[0]], [W[0]])
            for k_ in range(5):
                a_, b_ = k_ % 2, (k_ + 1) % 2
                c.mm(bankC[:, 128:256], R(P[a_][:]), R(Q[a_][:]), True, True, [P[a_], Q[a_]], [bankC])
                if k_ < 4:
                    c.mm(bankD[:, 0:128], R(Q[a_][:]), R(P[a_][:]), True, True, [P[a_], Q[a_]], [bankD])
                c.cp('act', R(Q[b_][:]), bankC[:, 128:256], [bankC], [Q[b_]])
                if k_ < 4:
                    c.cp('dve', R(P[b_][:]), bankD[:, 0:128], [bankD], [P[b_]])
                c.mm(bankD[:, 128:256], R(Q[b_][:]), R(W[a_][:]), True, True, [Q[b_], W[a_]], [bankD])
                c.tt('dve', R(W[b_][:]), bankD[:, 128:256], W[a_][:], ALU.add, [bankD, W[a_]], [W[b_]])
            return W[1]

        BW = 256
        NBLK = NTOK // BW
        with ExitStack() as esR:
            uT_c = c.sb(esR, [128, 8, NTOK], BF16, 'uT_c')
            with ExitStack() as es2:
                bufs = uT_bufs(es2)
                x_cm = x.rearrange("(r w) d -> w r d", w=64)
                jobs = [([(0, 128, ctx[t * 128:(t + 1) * 128, :])], 1, uT_c, t * 128) for t in range(2)]
                jobs += [([(wl * 32, wl * 32 + 32, x_cm[4 * j + wl]) for wl in range(4)], 0, uT_c, CTX + j * 128) for j in range(16)]
                run_uT(jobs, bufs)
                c.barrier()
            mu = c.sb(esR, [128, 15], F32, 'mu')
            hmu = c.sb(esR, [128, 15], F32, 'hmu')
            omu = c.sb(esR, [128, 15], F32, 'omu')
            w0 = c.sb(esR, [128, 2, 4], F32, 'w0')
            a0 = c.sb(esR, [128, 2, 4], F32, 'a0')
            rw = c.sb(esR, [128, 5, 4], F32, 'rw')
            oka = c.sb(esR, [128, 4], F32, 'oka')
            oka2 = c.sb(esR, [128, 4], F32, 'oka2')
            w2b = c.sb(esR, [128, 512], BF16, 'w2b')
            a2b = c.sb(esR, [128, 512], BF16, 'a2b')
            g2b = c.sb(esR, [128, 512], BF16, 'g2b')
            c.dma(mu[:], muT, writes=[mu])
            c.dma(w0[:], w0T, writes=[w0])
            c.dma(a0[:], a0T, writes=[a0])
            c.dma(rw[:], rwv, writes=[rw])
            c.dma(w2b[:], w2m, writes=[w2b], q='pool')
            c.dma(a2b[:], a2m, writes=[a2b], q='pool')
            c.dma(g2b[:], g2m, writes=[g2b], q='pool')
            c.ts('dve', hmu[:], mu[:], 0.5, None, ALU.mult, None, [mu], [hmu])
            c.ts('dve', omu[:], mu[:], -1.0, 1.0, ALU.mult, ALU.add, [mu], [omu])
            c.ts('dve', oka[:], rw[:, 1, :], -1.0, 1.0, ALU.mult, ALU.add, [rw], [oka])
            c.ts('dve', oka2[:], rw[:, 1, :], -2.0, 2.0, ALU.mult, ALU.add, [rw], [oka2])
            cmask = c.sb(esR, [128, BW], F32, 'cmask')
            c.op('pool', lambda e: e.memset(cmask[:], 1.0), writes=[cmask])
            c.op('pool', lambda e: e.memset(cmask[:].rearrange("p (a b) -> p a b", b=64)[:, :, 0:1], 0.0), reads=[cmask], writes=[cmask])
            mskA = [c.sb(esR, [128, 256], F32, 'mskA%d' % d) for d in range(2)]
            mskB = [c.sb(esR, [128, 256], F32, 'mskB%d' % d) for d in range(2)]
            for d in range(2):
                c.ts('dve', mskA[d][:, 0:128], strict[d][:], -1.0, None, ALU.mult, None, [strict[d]], [mskA[d]])
                c.cp('dve', mskA[d][:, 128:256], incl[d][:], [incl[d]], [mskA[d]])
                c.cp('dve', mskB[d][:, 0:128], strict[d][:], [strict[d]], [mskB[d]])
                c.cp('dve', mskB[d][:, 128:256], incl[d][:], [incl[d]], [mskB[d]])

            raw = c.sb(esR, [128, NTOK], F32, 'rraw')
            t1 = c.sb(esR, [128, NTOK], F32, 'rt1')
            twl = c.sb(esR, [128, NTOK], BF16, 'twl')
            alo = c.sb(esR, [128, NTOK], BF16, 'alo')
            sgl = c.sb(esR, [128, NTOK], BF16, 'sgl')
            rb = c.sb(esR, [128, NTOK], BF16, 'rb')
            kb = c.sb(esR, [128, NTOK], BF16, 'kb')
            vb = c.sb(esR, [128, NTOK], BF16, 'vb')
            G1 = c.sb(esR, [128, SEQ], BF16, 'G1')
            G2 = c.sb(esR, [128, SEQ], BF16, 'G2')
            y_acc = c.sb(esR, [128, 16, 128], F32, 'y_acc')
            wcr = [c.sb(esR, [128, 8, 128], BF16, 'wcr%d' % i) for i in range(2)]
            wcn = [0]

            pm_sched = [1536, 1664, 1792]
            for hc_ in range(4):
                pm_sched += [hc_ * 128, 512 + hc_ * 128, 1024 + hc_ * 128]

            def proj_mix_g(bcol, dst, func, mi):
                n_ = wcn[0]
                wcn[0] += 1
                assert pm_sched[n_] == bcol
                w_ = wcr[n_ % 2]
                if n_ == 0:
                    c.dma(w_[:], w_in_v[:, :, A_COLS + bcol:A_COLS + bcol + 128], writes=[w_], q='pool')
                for bi, (t0, t1_) in enumerate(TBLK):
                    bank = PS[4 + bi % 2]
                    n = t1_ - t0
                    for kc in range(8):
                        c.mm(bank[:, 0:n], w_[:, kc, :], uT_c[:, kc, t0:t1_], kc == 0, kc == 7, [w_, uT_c], [bank])
                    c.cp('act', raw[:, t0:t1_], bank[:, 0:n], [bank], [raw])
                    if bi == 0 and n_ + 1 < len(pm_sched):
                        nb_ = A_COLS + pm_sched[n_ + 1]
                        c.dma(wcr[(n_ + 1) % 2][:], w_in_v[:, :, nb_:nb_ + 128], writes=[wcr[(n_ + 1) % 2]], q='pool')
                    yield
                for (a_, b_) in ((0, CTX), (CTX, NTOK)):
                    c.tt('pool', t1[:, a_ + 1:b_ - 1], raw[:, a_:b_ - 2], raw[:, a_ + 2:b_], ALU.add, [raw], [t1])
                    c.cp('pool', t1[:, a_:a_ + 1], raw[:, a_ + 1:a_ + 2], [raw], [t1])
                    c.cp('pool', t1[:, b_ - 1:b_], raw[:, b_ - 2:b_ - 1], [raw], [t1])
                yield
                c.ts('dve', t1[:], t1[:], hmu[:, mi:mi + 1], None, ALU.mult, None, [t1, hmu], [t1])
                yield
                if func is None:
                    c.stt(dst[:], raw[:], omu[:, mi:mi + 1], t1[:], ALU.mult, ALU.add, [raw, omu, t1], [dst])
                else:
                    c.stt(t1[:], raw[:], omu[:, mi:mi + 1], t1[:], ALU.mult, ALU.add, [raw, omu, t1], [t1])
                    yield
                    c.act(dst[:], t1[:], func, [t1], [dst])
                yield

            def proj_mix(bcol, dst, func, mi):
                for _ in proj_mix_g(bcol, dst, func, mi):
                    pass

            def proj3_g(hc_):
                yield from proj_mix_g(hc_ * 128, rb, None, hc_)
                yield from proj_mix_g(512 + hc_ * 128, kb, None, 4 + hc_)
                yield from proj_mix_g(1024 + hc_ * 128, vb, None, 8 + hc_)

            proj_mix(1536, twl, AF.Tanh, 12)
            proj_mix(1664, alo, None, 13)
            proj_mix(1792, sgl, AF.Sigmoid, 14)

            def blkbuf(name, dt=F32, w=BW):
                return c.sb(esR, [128, w], dt, name)
            DB = []
            for d in range(2):
                g = {}
                arena = (raw, t1)[d]
                for i_, nm in enumerate(('lw', 'icl', 'pre', 'cumd', 'kkn', 'kdir', 'bvec', 'tmpa', 'tmpb', 'opr', 'btT', 'ktT', 'bhT', 'khT')):
                    if i_ < 9:
                        g[nm] = Buf(arena.t[:, i_ * BW:(i_ + 1) * BW], '%s%d' % (nm, d))
                    else:
                        g[nm] = blkbuf('%s%d' % (nm, d))
                g['AR'] = c.sb(esR, [128, 2, BW], F32, 'AR%d' % d)
                g['gC'] = c.sb(esR, [128, BW // 64], F32, 'gC%d' % d)
                for nm in ('Bh_tok', 'Kh_tok', 'Vt'):
                    g[nm] = c.sb(esR, [128, 128], F32, '%s%d' % (nm, d))
                g['BL0'] = Reg(4 * d, 0, 256, 'BL0_%d' % d)
                g['BL1'] = Reg(4 * d + 2, 0, 256, 'BL1_%d' % d)
                g['TL0'] = Reg(4 * d + 1, 0, 128, 'TL0_%d' % d)
                g['TLb'] = Reg(4 * d + 1, 128, 128, 'TLb_%d' % d)
                g['TL1'] = Reg(4 * d + 3, 0, 128, 'TL1_%d' % d)
                g['U'] = []
                for hh in range(2):
                    u = {'bankA': 4 * d + 2 * hh, 'bankB': 4 * d + 2 * hh + 1}
                    sfx = '%d%d' % (d, hh)
                    u['AB1'] = c.sb(esR, [128, 256], F32, 'AB1_' + sfx)
                    u['AB2'] = c.sb(esR, [128, 256], F32, 'AB2_' + sfx)
                    u['XY2'] = c.sb(esR, [128, 128], F32, 'XY2_' + sfx)
                    for nm in ('P', 'Q', 'W'):
                        u[nm] = [c.sb(esR, [128, 128], F32, '%sr%d_%s' % (nm, j, sfx)) for j in range(2)]
                    for nm in ('Xs', 'Us', 'yt', 'yt2', 'Tst'):
                        u[nm] = c.sb(esR, [128, 64], F32, nm + sfx)
                    g['U'].append(u)
                DB.append(g)
            gst2 = [c.sb(esR, [128, 24], F32, 'gst%d' % i) for i in range(2)]
            yn2 = [c.sb(esR, [128, 128], F32, 'yn%d' % i) for i in range(2)]
            obf2 = [c.sb(esR, [128, 128], BF16, 'obf%d' % i) for i in range(2)]
            otf2 = [c.sb(esR, [128, 128], F32, 'otf%d' % i) for i in range(2)]
            NHC = int(os.environ.get('K_NHC', '4'))
            NBS = int(os.environ.get('K_NBS', '9'))
            LG = -0.6065306597126334
            KTG = int(os.environ.get('K_TG', '9'))
            KSTAG = int(os.environ.get('K_STAG', '80'))

            def block_g(d, g, b, hc):
                rowsd = slice(d * 64, d * 64 + 64)
                cs_ = slice(hc * 128, (hc + 1) * 128)
                t0 = b * BW
                tsl = slice(t0, t0 + BW)
                lw, icl, pre, cumd, kkn, kdir, bvec, tmpa, tmpb, opr = (g[n_] for n_ in ('lw', 'icl', 'pre', 'cumd', 'kkn', 'kdir', 'bvec', 'tmpa', 'tmpb', 'opr'))
                AR, btT, ktT, bhT, khT, gC = (g[n_] for n_ in ('AR', 'btT', 'ktT', 'bhT', 'khT', 'gC'))
                BL0, BL1 = g['BL0'], g['BL1']
                c.mm(BL0.ap(), w2b[rowsd, cs_], twl[rowsd, tsl], True, True, [w2b, twl], [BL0.buf])
                c.mm(BL1.ap(), a2b[rowsd, cs_], alo[rowsd, tsl], True, True, [a2b, alo], [BL1.buf])
                c.ts('dve', kkn[:], kb[:, tsl], rw[:, 0, hc:hc + 1], None, ALU.mult, None, [kb, rw], [kkn])
                yield
                c.act(lw[:], BL0.ap(), AF.Sigmoid, [BL0.buf, w0], [lw], bias=w0[:, d, hc:hc + 1])
                c.act(icl[:], BL1.ap(), AF.Sigmoid, [BL1.buf, a0], [icl], bias=a0[:, d, hc:hc + 1])
                c.act(R(opr[:]), kkn[:], AF.Square, [kkn], [opr])
                yield
                c.ts('dve', lw[:], lw[:], LG, None, ALU.mult, None, [lw], [lw])
                c.mm(BL0.ap(), R(blk_r[:]), R(opr[:]), True, True, [blk_r, opr], [BL0.buf])
                c.op('dve', lambda e: e.tensor_tensor_scan(out=pre[:], data0=cmask[:], data1=lw[:], initial=0.0,
                                                           op0=ALU.mult, op1=ALU.add), reads=[cmask, lw], writes=[pre])
                yield
                pre3 = pre[:].rearrange("p (a b) -> p a b", b=64)
                tot_bc = pre3[:, :, 63:64].to_broadcast([128, BW // 64, 64])
                if d == 0:
                    c.cp('pool', cumd[:], pre[:], [pre], [cumd])
                else:
                    c.tt('dve', cumd[:], lw[:], pre[:], ALU.subtract, [lw, pre], [cumd])
                    c.tt('dve', cumd[:].rearrange("p (a b) -> p a b", b=64), cumd[:].rearrange("p (a b) -> p a b", b=64), tot_bc,
                         ALU.add, [cumd, pre], [cumd])
                c.ts('dve', tmpb[:], BL0.ap(), EPS, None, ALU.add, None, [BL0.buf], [tmpb])
                yield
                c.act(gC[:], pre3[:, :, 63], AF.Exp, [pre], [gC])
                c.act(tmpb[:], tmpb[:], AF.Sqrt, [tmpb], [tmpb])
                c.ts('dve', kdir[:], icl[:], rw[:, 1, hc:hc + 1], oka[:, hc:hc + 1], ALU.mult, ALU.add, [icl, rw, oka], [kdir])
                c.tt('dve', kdir[:], kdir[:], kb[:, tsl], ALU.mult, [kdir, kb], [kdir])
                yield
                c.op('dve', lambda e: e.reciprocal(out=tmpb[:], in_=tmpb[:]), reads=[tmpb], writes=[tmpb])
                c.tt('dve', kkn[:], kkn[:], tmpb[:], ALU.mult, [kkn, tmpb], [kkn])
                c.tt('dve', tmpa[:], cumd[:], lw[:], ALU.subtract, [cumd, lw], [tmpa])
                yield
                c.tt('pool', bvec[:], kkn[:], icl[:], ALU.mult, [kkn, icl], [bvec])
                c.act(tmpa[:], tmpa[:], AF.Exp, [tmpa], [tmpa])
                yield
                c.stt(R(AR[:, 0, :]), kkn[:], -1.0, tmpa[:], ALU.mult, ALU.mult, [kkn, tmpa], [AR])
                yield
                c.act(tmpa[:], cumd[:], AF.Exp, [cumd], [tmpa])
                c.tt('dve', tmpb[:].rearrange("p (a b) -> p a b", b=64), cumd[:].rearrange("p (a b) -> p a b", b=64), tot_bc,
                     ALU.subtract, [cumd, pre], [tmpb])
                yield
                c.tt('dve', R(AR[:, 1, :]), rb[:, tsl], tmpa[:], ALU.mult, [rb, tmpa], [AR])
                yield
                c.act(tmpa[:], cumd[:], AF.Exp, [cumd], [tmpa], scale=-1.0)
                c.act(tmpb[:], tmpb[:], AF.Exp, [tmpb], [tmpb], scale=-1.0)
                yield
                c.tt('dve', R(btT[:]), bvec[:], tmpa[:], ALU.mult, [bvec, tmpa], [btT])
                c.tt('pool', R(ktT[:]), kdir[:], tmpa[:], ALU.mult, [kdir, tmpa], [ktT])
                yield
                c.tt('dve', R(bhT[:]), bvec[:], tmpb[:], ALU.mult, [bvec, tmpb], [bhT])
                c.tt('pool', R(khT[:]), kdir[:], tmpb[:], ALU.mult, [kdir, tmpb], [khT])
                yield

            def tile_g(d, g, b, tl):
                lsl_ = slice(tl * 128, tl * 128 + 128)
                gts = (2 * b + tl) * 128
                TL0, TL1, TLb = g['TL0'], g['TL1'], g['TLb']
                bhT, khT, Bh_tok, Kh_tok, Vt = g['bhT'], g['khT'], g['Bh_tok'], g['Kh_tok'], g['Vt']
                c.mm(TL0.ap(), R(bhT[:, lsl_]), R(ident_r[:]), True, True, [bhT, ident_r], [TL0.buf])
                c.mm(TLb.ap(), vb[:, gts:gts + 128], identb[:], True, True, [vb, identb], [TLb.buf])
                c.mm(TL1.ap(), R(khT[:, lsl_]), R(ident_r[:]), True, True, [khT, ident_r], [TL1.buf])
                yield
                c.cp('act', R(Bh_tok[:]), TL0.ap(), [TL0.buf], [Bh_tok])
                c.cp('dve', R(Kh_tok[:]), TL1.ap(), [TL1.buf], [Kh_tok])
                c.cp('act', R(Vt[:]), TLb.ap(), [TLb.buf], [Vt])
                yield

            def unit_g(d, g, u, hh, b, tl):
                rows = slice(hh * 64, hh * 64 + 64)
                tile = 2 * b + tl
                is_lat = tile >= 2
                lt = tile - 2
                lsl_ = slice(tl * 128, tl * 128 + 128)
                AR, btT, ktT, Vt, Bh_tok, Kh_tok, gC = (g[n_] for n_ in ('AR', 'btT', 'ktT', 'Vt', 'Bh_tok', 'Kh_tok', 'gC'))
                AB1, AB2, XY2, Xs, Us, yt, yt2, Tst = (u[n_] for n_ in ('AB1', 'AB2', 'XY2', 'Xs', 'Us', 'yt', 'yt2', 'Tst'))
                P, Q, W = u['P'], u['Q'], u['W']
                A, B = PS[u['bankA']], PS[u['bankB']]
                At, Bt = A.t, B.t
                ARt = AR[rows, :, lsl_]
                c.mm(At[:, 0:256].rearrange("p (a b) -> p a b", a=2), R(btT[rows, lsl_]), R(ARt), True, True, [btT, AR], [A])
                c.mm(Bt[:, 0:256].rearrange("p (a b) -> p a b", a=2), R(ktT[rows, lsl_]), R(ARt), True, True, [ktT, AR], [B])
                yield
                c.tt('dve', R(AB1[:]), At[:, 0:256], mskA[d][:], ALU.mult, [A, mskA[d]], [AB1])
                c.tt('dve', R(AB2[:]), Bt[:, 0:256], mskB[d][:], ALU.mult, [B, mskB[d]], [AB2])
                yield
                if KTG < 3:
                    return
                c.cp('pool', R(P[0][:]), AB1[:, 0:128], [AB1], [P[0]])
                c.mm(At[:, 0:64], R(AB2[:, 0:128]), R(Vt[:, rows]), True, True, [AB2, Vt], [A])
                c.mm(At[:, 64:128], R(AB2[:, 128:256]), R(Vt[:, rows]), True, True, [AB2, Vt], [A])
                yield
                c.cp('act', XY2[:], At[:, 0:128], [A], [XY2])
                c.mm(Bt[:, 0:128], R(P[0][:]), R(ident_r[:]), True, True, [P[0], ident_r], [B])
                c.tt('pool', R(W[0][:]), ident[:], P[0][:], ALU.subtract, [ident, P[0]], [W[0]])
                yield
                c.cp('act', R(Q[0][:]), Bt[:, 0:128], [B], [Q[0]])
                yield
                for k_ in range(5):
                    a_, b_ = k_ % 2, (k_ + 1) % 2
                    c.mm(At[:, 128:256], R(P[a_][:]), R(Q[a_][:]), True, True, [P[a_], Q[a_]], [A])
                    if k_ < 4:
                        c.mm(Bt[:, 0:128], R(Q[a_][:]), R(P[a_][:]), True, True, [P[a_], Q[a_]], [B])
                    yield
                    c.cp('act', R(Q[b_][:]), At[:, 128:256], [A], [Q[b_]])
                    if k_ < 4:
                        c.cp('act', R(P[b_][:]), Bt[:, 0:128], [B], [P[b_]])
                    yield
                    c.mm(Bt[:, 128:256], R(Q[b_][:]), R(W[a_][:]), True, True, [Q[b_], W[a_]], [B])
                    yield
                    c.tt('dve', R(W[b_][:]), Bt[:, 128:256], W[a_][:], ALU.add, [B, W[a_]], [W[b_]])
                    yield
                Wt = W[1]
                if KTG < 4:
                    return
                for cs in ((0, 64) if d == 0 else (64, 0)):
                    sl_ = slice(cs, cs + 64)
                    chunk = tl * 2 + cs // 64
                    c.mm(At[:, 0:64], R(AR[rows, 0, lsl_]), R(Tst[rows, :]), True, True, [AR, Tst], [A])
                    if is_lat:
                        c.mm(At[:, 64:128], R(AR[rows, 1, lsl_]), R(Tst[rows, :]), True, True, [AR, Tst], [A])
                    yield
                    c.tt('dve', R(Xs[sl_, :]), At[sl_, 0:64], XY2[sl_, 0:64], ALU.add, [A, XY2], [Xs])
                    yield
                    c.mm(Bt[:, 0:64], R(Wt[sl_, :]), R(Xs[sl_, :]), True, True, [Wt, Xs], [B])
                    yield
                    c.cp('act', R(Us[sl_, :]), Bt[sl_, 0:64], [B], [Us])
                    yield
                    c.mm(Bt[:, 64:128], R(Bh_tok[sl_, :]), R(Us[sl_, :]), True, False, [Bh_tok, Us], [B])
                    c.mm(Bt[:, 64:128], R(Kh_tok[sl_, :]), R(Vt[sl_, rows]), False, True, [Kh_tok, Vt], [B])
                    if is_lat:
                        c.mm(At[:, 128:192], R(AB1[sl_, 128:256]), R(Us[sl_, :]), True, True, [AB1, Us], [A])
                    yield
                    c.stt(R(Tst[rows, :]), Tst[rows, :], gC[rows, chunk:chunk + 1], Bt[rows, 64:128], ALU.mult, ALU.add,
                          [Tst, gC, B], [Tst])
                    if is_lat:
                        c.tt('dve', yt[sl_, :], At[sl_, 128:192], XY2[sl_, 64:128], ALU.add, [A, XY2], [yt])
                        ykey = (lt, hh, cs)
                        if ykey not in yacc_written:
                            yacc_written.add(ykey)
                            c.tt('dve', y_acc[sl_, lt, rows], At[sl_, 64:128], yt[sl_, :], ALU.add, [A, yt], [y_acc])
                        else:
                            c.tt('dve', yt2[sl_, :], At[sl_, 64:128], yt[sl_, :], ALU.add, [A, yt], [yt2])
                            c.tt('pool', y_acc[sl_, lt, rows], y_acc[sl_, lt, rows], yt2[sl_, :], ALU.add, [y_acc, yt2], [y_acc])
                    yield

            yacc_written = set()

            def dir_g(d, hc):
                g = DB[d]
                if d == 1:
                    for _ in range(KSTAG):
                        yield
                for hh in range(2):
                    c.ts('dve', R(g['U'][hh]['Tst'][:]), zf[:, 0:64], 0.0, None, ALU.mult, None, [zf], [g['U'][hh]['Tst']])
                border = list(range(NBLK)) if d == 0 else [0] + list(range(NBLK - 1, 0, -1))
                for b in border[:NBS]:
                    yield from block_g(d, g, b, hc)
                    for tl in ((0, 1) if d == 0 else (1, 0)):
                        if KTG < 1:
                            continue
                        yield from tile_g(d, g, b, tl)
                        if KTG < 2:
                            continue
                        yield from par(unit_g(d, g, g['U'][0], 0, b, tl), unit_g(d, g, g['U'][1], 1, b, tl))

            ob_hc = c.sb(esR, [128, 16, 128], BF16, 'ob_hc')
            scr_ob_v = scr_ob.rearrange("(lt p) cc -> p lt cc", p=128)
            scrw = Buf(None, 'scrw')
            for _ in proj3_g(0):
                pass
            for hc in range(NHC):
                c.barrier()
                cs_ = slice(hc * 128, (hc + 1) * 128)
                def prepass_g(par_, hc=hc, cs_=cs_):
                    g_ = DB[par_]
                    icl, icl1, tmpa, opr = g_['icl'], g_['pre'], g_['tmpa'], g_['opr']
                    P0, P3, P1, P2 = (PS[4 * par_ + j] for j in range(4))
                    for b in range(1 + par_, NBLK, 2):
                        t0 = b * BW
                        tsl = slice(t0, t0 + BW)
                        lsl = slice(t0 - CTX, t0 - CTX + BW)
                        c.mm(P0[:, 0:BW], a2b[0:64, cs_], alo[0:64, tsl], True, True, [a2b, alo], [P0])
                        c.mm(P3[:, 0:BW], a2b[64:128, cs_], alo[64:128, tsl], True, True, [a2b, alo], [P3])
                        c.mm(P2[:, 0:BW], g2b[:, cs_], sgl[:, tsl], True, True, [g2b, sgl], [P2])
                        yield
                        c.act(icl[:], P0[:, 0:BW], AF.Sigmoid, [P0, a0], [icl], bias=a0[:, 0, hc:hc + 1])
                        c.act(icl1[:], P3[:, 0:BW], AF.Sigmoid, [P3, a0], [icl1], bias=a0[:, 1, hc:hc + 1])
                        c.cp('act', G1[:, lsl], P2[:, 0:BW], [P2], [G1])
                        yield
                        c.tt('dve', tmpa[:], icl[:], icl1[:], ALU.add, [icl, icl1], [tmpa])
                        c.ts('dve', tmpa[:], tmpa[:], rw[:, 1, hc:hc + 1], oka2[:, hc:hc + 1], ALU.mult, ALU.add, [tmpa, rw, oka2], [tmpa])
                        yield
                        c.tt('dve', tmpa[:], tmpa[:], kb[:, tsl], ALU.mult, [tmpa, kb], [tmpa])
                        c.tt('dve', tmpa[:], tmpa[:], rb[:, tsl], ALU.mult, [tmpa, rb], [tmpa])
                        yield
                        c.ts('dve', R(opr[:]), tmpa[:], rw[:, 2, hc:hc + 1], None, ALU.mult, None, [tmpa, rw], [opr])
                        yield
                        c.mm(P1[:, 0:BW], R(blk_r[:]), R(opr[:]), True, True, [blk_r, opr], [P1])
                        yield
                        c.tt('dve', tmpa[:], P1[:, 0:BW], vb[:, tsl], ALU.mult, [P1, vb], [tmpa])
                        yield
                        c.stt(G2[:, lsl], tmpa[:], rw[:, 4, hc:hc + 1], P2[:, 0:BW], ALU.add, ALU.mult, [tmpa, rw, P2], [G2])
                        yield

                run_threads([prepass_g(0), prepass_g(1)])
                c.barrier()
                yacc_written.clear()
                run_threads([dir_g(0, hc), dir_g(1, hc)])
                c.barrier()
                def rwkv_out_g(par_, hc=hc, cs_=cs_):
                    gst, yn, obf, otf = gst2[par_], yn2[par_], obf2[par_], otf2[par_]
                    bkA, bkB = PS[par_], PS[2 + par_]
                    pbk = psb(2 + par_)
                    for lt in range(par_, 0 if os.environ.get('K_NOOUT') else 16, 2):
                        for hh in range(2):
                            rows = slice(hh * 64, hh * 64 + 64)
                            c.op('dve', lambda e, hh=hh, rows=rows: e.bn_stats(out=gst[:, hh * 6:(hh + 1) * 6], in_=y_acc[:, lt, rows]), reads=[y_acc], writes=[gst])
                            c.op('dve', lambda e, hh=hh: e.bn_aggr(out=gst[:, 12 + hh * 2:14 + hh * 2], in_=gst[:, hh * 6:(hh + 1) * 6]), reads=[gst], writes=[gst])
                        yield
                        c.ts('dve', gst[:, 16:18], gst[:, 13:16:2], LNX_EPS, None, ALU.add, None, [gst], [gst])
                        yield
                        c.act(gst[:, 16:18], gst[:, 16:18], AF.Sqrt, [gst], [gst])
                        yield
                        c.op('dve', lambda e: e.reciprocal(out=gst[:, 18:20], in_=gst[:, 16:18]), reads=[gst], writes=[gst])
                        for hh in range(2):
                            rows = slice(hh * 64, hh * 64 + 64)
                            c.ts('dve', yn[:, rows], y_acc[:, lt, rows], gst[:, 12 + hh * 2:13 + hh * 2], gst[:, 18 + hh:19 + hh], ALU.subtract, ALU.mult,
                                 [y_acc, gst], [yn])
                        yield
                        c.tr(bkA[:, 0:128], yn[:], ident[:], [yn, ident], [bkA])
                        yield
                        lsl = slice(lt * 128, (lt + 1) * 128)
                        c.stt(otf[:], bkA[:, 0:128], rw[:, 3, hc:hc + 1], G1[:, lsl], ALU.mult, ALU.mult, [bkA, rw, G1], [otf])
                        c.tt('dve', obf[:], otf[:], G2[:, lsl], ALU.add, [otf, G2], [obf])
                        yield
                        c.tr(pbk[:, 0:128], obf[:], identb[:], [obf, identb], [bkB])
                        yield
                        c.cp('act', ob_hc[:, lt, :], pbk[:, 0:128], [bkB], [ob_hc])
                        yield

                run_threads([rwkv_out_g(0), rwkv_out_g(1)] + ([proj3_g(hc + 1)] if hc + 1 < 4 else []))
                for q4 in range(4):
                    c.dma(scr_ob_v[:, q4 * 4:(q4 + 1) * 4, cs_], ob_hc[:, q4 * 4:(q4 + 1) * 4, :], reads=[ob_hc], writes=[scrw])
            c.barrier()

        scrob_buf = scrw
        scrh_buf = Buf(None, 'scr_h_dram')
        esF = ExitStack()
        with esF:
            tT = c.sb(esF, [128, 8, SEQ], BF16, 'tT')
            cwT = c.sb(esF, [32, SEQ], F32, 'cwT')
            with ExitStack() as esY:
                yT = c.sb(esY, [128, 8, SEQ], BF16, 'yT')
                with ExitStack() as esM1:
                    woa = c.sb(esM1, [128, 4, D], BF16, 'woa')
                    wob = c.sb(esM1, [128, 4, D], BF16, 'wob')
                    c.dma(woa[:], w_o_a.rearrange("(cc p) n -> p cc n", p=128), writes=[woa], q='pool')
                    c.dma(wob[:], w_o_b.rearrange("(cc p) n -> p cc n", p=128), writes=[wob], q='pool')
                    uTb = c.sb(esM1, [128, 8, 512], BF16, 'uTb')
                    obTb = c.sb(esM1, [128, 4, 512], BF16, 'obTb')
                    obt = [c.sb(esM1, [128, 512], BF16, 'obt%d' % i) for i in range(2)]
                    wg_ = [c.sb(esM1, [128, 8, 128], BF16, 'wgm%d' % i) for i in range(4)]
                    sga2 = [c.sb(esM1, [128, 512], F32, 'sga%d' % i) for i in range(2)]
                    sgb2 = [c.sb(esM1, [128, 512], F32, 'sgb%d' % i) for i in range(2)]
                    ya2 = [c.sb(esM1, [128, 512], F32, 'ya%d' % i) for i in range(2)]
                    yb2 = [c.sb(esM1, [128, 512], F32, 'yb%d' % i) for i in range(2)]
                    ob_v = scr_ob.rearrange("(w r) cc -> r w cc", r=32)
                    k = 0
                    wn = 0
                    for tb in range(4):
                        c.dma(uTb[:], scr_u[:, :, tb * 512:(tb + 1) * 512], reads=[scru_buf], writes=[uTb])
                        for j in range(4):
                            i = tb * 4 + j
                            o_ = obt[i % 2]
                            c.dma(o_[0:64, :], ob_v[2 * i], reads=[scrob_buf], writes=[o_])
                            c.dma(o_[64:128, :], ob_v[2 * i + 1], reads=[scrob_buf], writes=[o_])
                            pb = psb(5 + i % 2)
                            for hc in range(4):
                                c.tr(pb[:, hc * 128:(hc + 1) * 128], o_[:, hc * 128:(hc + 1) * 128], identb[:], [o_, identb], [PS[5 + i % 2]],
                                     inc=(hc == 3))
                            c.cp('act', obTb[:, :, j * 128:(j + 1) * 128], pb[:, 0:512].rearrange("p (a b) -> p a b", a=4), [PS[5 + i % 2]], [obTb])
                        tsl = slice(tb * 512, (tb + 1) * 512)
                        for fc in range(8):
                            wa_ = wg_[wn % 4]
                            wb_ = wg_[(wn + 1) % 4]
                            wn += 2
                            ga0 = A_COLS + B_COLS + fc * 128
                            c.dma(wa_[:], w_in_v[:, :, ga0:ga0 + 128], writes=[wa_], q='pool')
                            c.dma(wb_[:], w_in_v[:, :, ga0 + D:ga0 + D + 128], writes=[wb_], q='pool')
                            fsl = slice(fc * 128, (fc + 1) * 128)
                            fp_ = fc % 2
                            Q0, Q1, Q2, Q3 = (PS[4 * fp_ + j_] for j_ in range(4))
                            sga, sgb, ya, yb = sga2[fp_], sgb2[fp_], ya2[fp_], yb2[fp_]
                            for kc in range(8):
                                c.mm(Q2[:, :], wa_[:, kc, :], uTb[:, kc, :], kc == 0, kc == 7, [wa_, uTb], [Q2])
                            for kc in range(8):
                                c.mm(Q3[:, :], wb_[:, kc, :], uTb[:, kc, :], kc == 0, kc == 7, [wb_, uTb], [Q3])
                            for cc in range(4):
                                c.mm(Q0[:, :], woa[:, cc, fsl], oaT[:, cc, tsl], cc == 0, cc == 3, [woa, oaT], [Q0])
                            for cc in range(4):
                                c.mm(Q1[:, :], wob[:, cc, fsl], obTb[:, cc, :], cc == 0, cc == 3, [wob, obTb], [Q1])
                            c.act(sga[:], Q2[:, :], AF.Sigmoid, [Q2], [sga])
                            c.act(sgb[:], Q3[:, :], AF.Sigmoid, [Q3], [sgb])
                            c.tt('dve', ya[:], Q0[:, :], sga[:], ALU.mult, [Q0, sga], [ya])
                            c.tt('dve', yb[:], Q1[:, :], sgb[:], ALU.mult, [Q1, sgb], [yb])
                            c.tt('pool', yT[:, fc, tsl], ya[:], yb[:], ALU.add, [ya, yb], [yT])
                    c.barrier()
                if 'd_yT' in T:
                    c.dma(T['d_yT'], yT[:], reads=[yT])
                with ExitStack() as esM2:
                    wout = c.sb(esM2, [128, 8, D], BF16, 'wout')
                    c.dma(wout[:, 0:4, :], w_out.rearrange("(cc p) n -> p cc n", p=128)[:, 0:4, :], writes=[wout], q='pool')
                    c.dma(wout[:, 4:8, :], w_out.rearrange("(cc p) n -> p cc n", p=128)[:, 4:8, :], writes=[wout], q='pool')
                    Bg1 = c.sb(esM2, [128, D], F32, 'Bg1')
                    make_Bg(esM2, Bg1, 16)
                    wr32 = c.sb(esM2, [128, 8, 36], F32, 'wr32')
                    rbb = c.sb(esM2, [128, 36], F32, 'rbb')
                    c.dma(wr32[:], wrt, writes=[wr32])
                    c.dma(rbb[:], rb_bc, writes=[rbb])
                    Xh = [c.sb(esM2, [128, D], F32, 'Xh%d' % i) for i in range(2)]
                    Hh = [c.sb(esM2, [128, D], F32, 'Hh%d' % i) for i in range(2)]
                    hn2 = [c.sb(esM2, [128, D], F32, 'hn%d' % i) for i in range(2)]
                    sqh2 = [c.sb(esM2, [128, D], BF16, 'sqh%d' % i) for i in range(2)]
                    t322 = [c.sb(esM2, [128, 8, 128], F32, 't32%d' % i) for i in range(2)]
                    st2 = [c.sb(esM2, [128, 64], F32, 'st%d' % i) for i in range(2)]
                    lg2 = [c.sb(esM2, [128, 36], F32, 'lg%d' % i) for i in range(2)]
                    tm32 = [c.sb(esM2, [128, 32], F32, 'tm3%d' % i) for i in range(2)]
                    cw2 = [c.sb(esM2, [128, 32], F32, 'cw%d' % i) for i in range(2)]

                    def tile_chain(t):
                        hn, sqh, t32, st, lg, tm3, cw = hn2[t], sqh2[t], t322[t], st2[t], lg2[t], tm32[t], cw2[t]
                        B = [PS[4 * t + j] for j in range(4)]
                        X_, H_ = Xh[t], Hh[t]
                        for i in range(t, 16, 2):
                            c.dma(X_[:], x[i * 128:(i + 1) * 128, :], writes=[X_])
                            for nh in range(2):
                                bank = B[nh]
                                for fc in range(8):
                                    c.mm(bank[:, :], yT[:, fc, i * 128:(i + 1) * 128], wout[:, fc, nh * 512:(nh + 1) * 512], fc == 0, fc == 7, [yT, wout], [bank])
                                hs = slice(nh * 512, (nh + 1) * 512)
                                c.tt('dve', H_[:, hs], bank[:, :], Bg1[:, hs], ALU.mult, [bank, Bg1], [H_])
                                yield
                            c.tt('pool', H_[:], H_[:], X_[:], ALU.add, [H_, X_], [H_])
                            yield
                            c.dma(scr_h[i * 128:(i + 1) * 128, :], H_[:], reads=[H_], writes=[scrh_buf])
                            c.act(sqh[:], H_[:], AF.Square, [H_], [sqh, st], accum_out=st[:, 0:1])
                            yield
                            c.ts('dve', st[:, 1:2], st[:, 0:1], 1.0 / D, EPS, ALU.mult, ALU.add, [st], [st])
                            c.act(st[:, 2:3], st[:, 1:2], AF.Sqrt, [st], [st])
                            yield
                            c.op('dve', lambda e: e.reciprocal(out=st[:, 3:4], in_=st[:, 2:3]), reads=[st], writes=[st])
                            c.ts('dve', hn[:], H_[:], st[:, 3:4], None, ALU.mult, None, [H_, st], [hn])
                            yield
                            for fc in range(8):
                                bank = B[2 + fc // 4]
                                c.tr(bank[:, (fc % 4) * 128:(fc % 4 + 1) * 128], hn[:, fc * 128:(fc + 1) * 128], ident[:], [hn, ident], [bank],
                                     inc=(fc % 4 == 3))
                            yield
                            for fc in range(8):
                                bank = B[2 + fc // 4]
                                i_ = bank[:, (fc % 4) * 128:(fc % 4 + 1) * 128]
                                c.ts('dve', t32[:, fc, :], i_, A2g[:, fc, 0:1], mT[:, 24 + fc, 0:1], ALU.mult, ALU.add, [bank, A2g, mT], [t32])
                                c.cp('act', tT[:, fc, i * 128:(i + 1) * 128], t32[:, fc, :], [t32], [tT])
                                if fc % 4 == 3:
                                    yield
                            for kc in range(8):
                                c.mm(B[0][:, 0:36], t32[:, kc, :], wr32[:, kc, :], kc == 0, kc == 7, [t32, wr32], [B[0]])
                            yield
                            c.tt('dve', lg[:], B[0][:, 0:36], rbb[:], ALU.add, [B[0], rbb], [lg])
                            c.op('dve', lambda e: e.tensor_reduce(out=st[:, 8:9], in_=lg[:, 0:4], axis=AX.X, op=ALU.max), reads=[lg], writes=[st])
                            c.ts('dve', st[:, 9:10], st[:, 8:9], -1.0, None, ALU.mult, None, [st], [st])
                            yield
                            c.act(st[:, 16:20], lg[:, 0:4], AF.Exp, [lg, st], [st], bias=st[:, 9:10], accum_out=st[:, 10:11])
                            yield
                            c.op('dve', lambda e: e.reciprocal(out=st[:, 11:12], in_=st[:, 10:11]), reads=[st], writes=[st])
                            c.ts('dve', st[:, 20:24], lg[:, 0:4], st[:, 8:9], None, ALU.is_ge, None, [lg, st], [st])
                            c.tt('dve', tm3[:].rearrange("p (g e) -> p g e", g=4), lg[:, 4:36].rearrange("p (g e) -> p g e", g=4),
                                 st[:, 20:24].unsqueeze(2).to_broadcast([128, 4, 8]), ALU.mult, [lg, st], [tm3])
                            yield
                            c.op('dve', lambda e: e.tensor_reduce(out=st[:, 24:32], in_=tm3[:].rearrange("p (g e) -> p e g", g=4), axis=AX.X, op=ALU.add),
                                 reads=[tm3], writes=[st])
                            c.op('dve', lambda e: e.max(out=st[:, 32:40], in_=st[:, 24:32]), reads=[st], writes=[st])
                            c.tt('dve', st[:, 40:41], st[:, 33:34], st[:, 32:33], ALU.subtract, [st], [st])
                            yield
                            c.act(st[:, 41:42], st[:, 40:41], AF.Exp, [st], [st])
                            yield
                            c.ts('dve', st[:, 42:43], st[:, 41:42], 1.0, None, ALU.add, None, [st], [st])
                            c.op('dve', lambda e: e.reciprocal(out=st[:, 43:44], in_=st[:, 42:43]), reads=[st], writes=[st])
                            c.tt('dve', st[:, 44:45], st[:, 43:44], st[:, 11:12], ALU.mult, [st], [st])
                            yield
                            c.tt('dve', st[:, 45:46], st[:, 44:45], st[:, 41:42], ALU.mult, [st], [st])
                            c.tt('dve', st[:, 46:47], st[:, 44:45], st[:, 45:46], ALU.subtract, [st], [st])
                            c.ts('dve', st[:, 48:56], st[:, 24:32], st[:, 32:33], st[:, 46:47], ALU.is_ge, ALU.mult, [st], [st])
                            yield
                            c.ts('dve', st[:, 56:64], st[:, 24:32], st[:, 33:34], st[:, 45:46], ALU.is_ge, ALU.mult, [st], [st])
                            c.tt('dve', st[:, 48:56], st[:, 48:56], st[:, 56:64], ALU.add, [st], [st])
                            c.tt('dve', cw[:].rearrange("p (g e) -> p g e", g=4), st[:, 20:24].unsqueeze(2).to_broadcast([128, 4, 8]),
                                 st[:, 48:56].unsqueeze(1).to_broadcast([128, 4, 8]), ALU.mult, [st], [cw])
                            yield
                            c.tr(B[1][0:32, 0:128], cw[:], ident[:], [cw, ident], [B[1]])
                            yield
                            c.cp('act', R(cwT[:, i * 128:(i + 1) * 128]), B[1][0:32, 0:128], [B[1]], [cwT])
                            yield

                    run_threads([tile_chain(0), tile_chain(1)])
                    c.barrier()
            if 'd_tT' in T:
                c.dma(T['d_tT'], tT[:], reads=[tT])
            if 'd_cwT' in T:
                c.dma(T['d_cwT'], cwT[:], reads=[cwT])

            with ExitStack() as esE:
                moe_acc = c.sb(esE, [128, 16, D], F32, 'moe_acc')
                wgb = [c.sb(esE, [128, 8, 256], BF16, 'wgb%d' % i) for i in range(2)]
                wub = [c.sb(esE, [128, 8, 256], BF16, 'wub%d' % i) for i in range(2)]
                wdb = [c.sb(esE, [128, 2, D], BF16, 'wdb%d' % i) for i in range(2)]
                selt = [c.sb(esE, [32, 128], F32, 'selt%d' % i) for i in range(2)]
                sg_ = [c.sb(esE, [128, 512], F32, 'sg%d' % i) for i in range(2)]
                hu_ = [c.sb(esE, [128, 512], F32, 'hu%d' % i) for i in range(2)]
                hid = [c.sb(esE, [128, 2, 512], BF16, 'hid%d' % i) for i in range(2)]
                NEXP = int(os.environ.get('K_NEXP', '32'))
                def moe_gu(e_, tg, k):
                    wg, wu, wd = wgb[e_ % 2], wub[e_ % 2], wdb[e_ % 2]
                    se = selt[e_ % 2]
                    if tg == 0:
                        c.dma(wg[:], moe_wg[e_].rearrange("(kc p) f -> p kc f", p=128), writes=[wg], q='pool')
                        c.dma(wu[:], moe_wu[e_].rearrange("(kc p) f -> p kc f", p=128), writes=[wu], q='pool')
                        c.dma(wd[:], moe_wd[e_].rearrange("(fc p) n -> p fc n", p=128), writes=[wd], q='pool')
                        c.ts('dve', R(se[:]), onesf[0:32, :], ident[0:32, e_:e_ + 1], None, ALU.mult, None, [onesf, ident], [se])
                    tsl = slice(tg * 512, (tg + 1) * 512)
                    hd = hid[k % 2]
                    c.mm(PS[4][:, :], R(se[:]), R(cwT[:, tsl]), True, True, [se, cwT], [PS[4]])
                    for f2 in range(2):
                        fs = slice(f2 * 128, (f2 + 1) * 128)
                        for kc in range(8):
                            c.mm(PS[f2][:, :], wg[:, kc, fs], tT[:, kc, tsl], kc == 0, kc == 7, [wg, tT], [PS[f2]])
                        for kc in range(8):
                            c.mm(PS[2 + f2][:, :], wu[:, kc, fs], tT[:, kc, tsl], kc == 0, kc == 7, [wu, tT], [PS[2 + f2]])
                        c.act(sg_[f2][:], PS[f2][:, :], AF.Silu, [PS[f2]], [sg_[f2]])
                        c.tt('dve', hu_[f2][:], PS[2 + f2][:, :], sg_[f2][:], ALU.mult, [PS[2 + f2], sg_[f2]], [hu_[f2]])
                        c.tt('dve', hd[:, f2, :], PS[4][:, :], hu_[f2][:], ALU.mult, [PS[4], hu_[f2]], [hd])

                def moe_dn(e_, tg, k):
                    wd = wdb[e_ % 2]
                    hd = hid[k % 2]
                    for tt_ in range(4):
                        tile = tg * 4 + tt_
                        for nh in range(2):
                            bank = PS[5 + (tt_ * 2 + nh) % 3]
                            for f2 in range(2):
                                c.mm(bank[:, :], hd[:, f2, tt_ * 128:(tt_ + 1) * 128], wd[:, f2, nh * 512:(nh + 1) * 512], f2 == 0, f2 == 1,
                                     [hd, wd], [bank])
                            hs = slice(nh * 512, (nh + 1) * 512)
                            if e_ == 0:
                                c.cp('act', moe_acc[:, tile, hs], bank[:, :], [bank], [moe_acc])
                            else:
                                c.tt('dve', moe_acc[:, tile, hs], bank[:, :], moe_acc[:, tile, hs], ALU.add, [bank, moe_acc], [moe_acc])

                its = [(e_, tg) for e_ in range(NEXP) for tg in range(4)]
                for k, (e_, tg) in enumerate(its):
                    moe_gu(e_, tg, k)
                    if k > 0:
                        moe_dn(its[k - 1][0], its[k - 1][1], k - 1)
                moe_dn(its[-1][0], its[-1][1], len(its) - 1)
                Bg2 = c.sb(esE, [128, D], F32, 'Bg2')
                make_Bg(esE, Bg2, 40)
                gfin = c.sb(esE, [128, D], F32, 'gfin')
                c.dma(gfin[:], gfin_bc, writes=[gfin])
                Hf = [c.sb(esE, [128, D], F32, 'Hf%d' % i) for i in range(2)]
                sf = [c.sb(esE, [128, 4], F32, 'sf%d' % i) for i in range(2)]
                c.barrier()
                Hm = [Buf(wgb[i].t.bitcast(F32).rearrange("p a b -> p (a b)"), 'Hm%d' % i) for i in range(2)]
                sqf2 = [Buf(wub[i].t.rearrange("p a b -> p (a b)"), 'sqf%d' % i) for i in range(2)]

                def fin_g(par_):
                    H_, s_, Hm_, sq_ = Hf[par_], sf[par_], Hm[par_], sqf2[par_]
                    for i in range(par_, 16, 2):
                        c.dma(H_[:], scr_h[i * 128:(i + 1) * 128, :], reads=[scrh_buf], writes=[H_])
                        c.tt('dve', Hm_[:], moe_acc[:, i, :], Bg2[:], ALU.mult, [moe_acc, Bg2], [Hm_])
                        yield
                        c.tt('pool', H_[:], H_[:], Hm_[:], ALU.add, [H_, Hm_], [H_])
                        yield
                        c.act(sq_[:, 0:D], H_[:], AF.Square, [H_], [sq_, s_], accum_out=s_[:, 0:1])
                        yield
                        c.ts('dve', s_[:, 1:2], s_[:, 0:1], 1.0 / D, EPS, ALU.mult, ALU.add, [s_], [s_])
                        yield
                        c.act(s_[:, 2:3], s_[:, 1:2], AF.Sqrt, [s_], [s_])
                        yield
                        c.op('dve', lambda e, s_=s_: e.reciprocal(out=s_[:, 3:4], in_=s_[:, 2:3]), reads=[s_], writes=[s_])
                        c.stt(H_[:], H_[:], s_[:, 3:4], gfin[:], ALU.mult, ALU.mult, [H_, s_, gfin], [H_])
                        yield
                        c.dma(out[i * 128:(i + 1) * 128, :], H_[:], reads=[H_])
                        yield

                run_threads([fin_g(0), fin_g(1)])
                c.barrier()

        c.finish()
        print("ninstr", c.ninstr, {k_: v for k_, v in c.cnt.items()})
    return nc


def prep_inputs(inp):
    f = lambda a: np.ascontiguousarray(a, dtype=np.float32)
    fm = lambda v: f(np.asarray(v).reshape(-1, 128).T)
    shared = {
        "ada_w": f(inp["ada_w"][0]),
        "ada_bT": fm(inp["ada_b"][0]),
        "gmixT": fm(inp["norm_mix_g"][0]),
        "gffnT": fm(inp["norm_ffn_g"][0]),
        "gfin_bc": f(np.broadcast_to(inp["final_norm_g"][None, :], (128, D))),
        "w_in": f(inp["w_in"][0]),
        "convT": f(inp["gdn_conv"][0].T.reshape(12, 128, 5).transpose(1, 0, 2)),
        "alog_bc": f(np.broadcast_to(inp["gdn_a_log"][0].reshape(1, 1, 8), (128, 18, 8))),
        "dtb_bc": f(np.broadcast_to(inp["gdn_dt_bias"][0].reshape(1, 1, 8), (128, 18, 8))),
        "onormT": f(inp["gdn_onorm_g"][0].reshape(128, 1)),
        "muT": fm(inp["rwkv_mu"][0]),
        "w0T": f(inp["rwkv_w0"][0].reshape(2, 4, 128).transpose(2, 0, 1)),
        "a0T": f(inp["rwkv_a0"][0].reshape(2, 4, 128).transpose(2, 0, 1)),
        "w2m": f(inp["rwkv_w2"][0].reshape(128, 512)),
        "a2m": f(inp["rwkv_a2"][0].reshape(128, 512)),
        "g2m": f(inp["rwkv_g2"][0]),
        "w_o_a": f(inp["w_o_a"][0]),
        "w_o_b": f(inp["w_o_b"][0]),
        "w_out": f(inp["w_out"][0]),
        "wrt": f(np.concatenate([inp["router_grp"][0], inp["router_exp"][0]], axis=1).reshape(8, 128, 36).transpose(1, 0, 2)),
        "rb_bc": f(np.broadcast_to(np.concatenate([inp["router_grp_b"][0], inp["router_exp_b"][0]])[None, :], (128, 36))),
        "moe_wg": f(inp["moe_w_gate"][0].reshape(32, D, 256)),
        "moe_wu": f(inp["moe_w_up"][0].reshape(32, D, 256)),
        "moe_wd": f(inp["moe_w_down"][0].reshape(32, 256, D)),
        "rwv": f(np.stack([fm(inp["rwkv_k_k"][0]), fm(inp["rwkv_k_a"][0]), fm(inp["rwkv_r_k"][0].reshape(-1)),
                           fm(inp["rwkv_lnx_g"][0]), fm(inp["rwkv_lnx_b"][0])], axis=1)),
    }
    maps = []
    for b in range(NCORES):
        m = dict(shared)
        m["x"] = f(inp["x"][b])
        m["ctx"] = f(inp["ctx"][b])
        m["cT"] = f(np.stack([fm(inp["c"][b]), fm(inp["c_ctx"])], axis=-1))
        maps.append(m)
    return maps


def kernel(**inputs):
    maps = prep_inputs(inputs)
    nc = build()
    res = run_bass_kernel_spmd(nc, maps, core_ids=list(range(NCORES)))
    return np.stack([np.asarray(r["out"]) for r in res.results], axis=0).astype(np.float32)
```

```python
import os
import numpy as np
import concourse.bass as bass
import concourse.mybir as mybir
from concourse.bass_utils import run_bass_kernel_spmd
from concourse.alu_op_type import AluOpType as ALU
from contextlib import ExitStack

F32 = mybir.dt.float32
F32R = mybir.dt.float32r
BF16 = mybir.dt.bfloat16
AF = mybir.ActivationFunctionType
AX = mybir.AxisListType

NCORES = 8
D = 1024
SEQ = 2048
CTX = 256
NTOK = SEQ + CTX
IN_COLS = 6032
A_COLS = 2064
B_COLS = 1920
EPS = 1e-6
LNX_EPS = 1e-5 * 64


class Buf:
    def __init__(self, t, name, psum=False):
        self.t = t
        self.name = name
        self.lw = None
        self.rd = {}
        self.psum = psum
        self.bankrd = None

    def __getitem__(self, idx):
        return self.t[idx]


class Ctx:
    ENG = ['pe', 'dve', 'act', 'pool', 'sp']
    NDMA = 8

    def __init__(self, nc, es):
        self.nc = nc
        self.e = {'pe': nc.tensor, 'dve': nc.vector, 'act': nc.scalar, 'pool': nc.gpsimd, 'sp': nc.sync}
        self.sem = {}
        self.cnt = {}
        for n in self.ENG:
            self.sem[n] = es.enter_context(nc.semaphore('s_' + n))
            self.cnt[n] = 0
        for i in range(self.NDMA):
            n = 'd%d' % i
            self.sem[n] = es.enter_context(nc.semaphore('s_' + n))
            self.cnt[n] = 0
        self.dma_rr = 0
        self.waited = {n: {} for n in self.ENG}
        self.nbuf = 0
        self.ninstr = 0

    def sb(self, es, shape, dt=F32, name=None):
        self.nbuf += 1
        name = (name or 'b') + '_%d' % self.nbuf
        t = es.enter_context(self.nc.sbuf_tensor(name, list(shape), dt))
        return Buf(t, name)

    def ps(self, es, shape, dt=F32, name=None):
        self.nbuf += 1
        name = (name or 'p') + '_%d' % self.nbuf
        t = es.enter_context(self.nc.psum_tensor(name, list(shape), dt))
        return Buf(t, name, psum=True)

    def view(self, buf, name='v'):
        self.nbuf += 1
        return Buf(buf.t, name + '_%d' % self.nbuf)

    def _deps(self, reads, writes):
        deps = {}

        def add(k, v):
            if v > deps.get(k, 0):
                deps[k] = v
        for b in reads:
            if b.lw:
                add(*b.lw)
            if b.psum:
                for k, v in b.rd.items():
                    add(k, v)
                if b.bankrd is not None:
                    for k, v in b.bankrd.items():
                        add(k, v)
        for b in writes:
            if b.lw:
                add(*b.lw)
            for k, v in b.rd.items():
                add(k, v)
        return deps

    def _wait(self, E, deps):
        eng = self.e[E]
        w = self.waited[E]
        nw = 0
        for k, v in deps.items():
            if k == E and E == 'pe' and v > self.cnt['pe']:
                continue
            if w.get(k, 0) >= v:
                continue
            eng.wait_ge(self.sem[k], v)
            nw += 1
            w[k] = v

    def op(self, E, fn, reads=(), writes=(), inc=True):
        deps = self._deps(reads, writes)
        self._wait(E, deps)
        ins = fn(self.e[E])
        self.ninstr += 1
        if inc:
            self.cnt[E] += 1
            ins.then_inc(self.sem[E], 1)
            cval = self.cnt[E]
        else:
            cval = self.cnt[E] + 1
        for b in writes:
            b.lw = (E, cval)
            b.rd = {}
        for b in reads:
            if b not in writes:
                b.rd[E] = max(b.rd.get(E, 0), cval)
            if b.bankrd is not None and E != 'pe':
                b.bankrd[E] = max(b.bankrd.get(E, 0), cval)
        return ins

    def dma(self, out, in_, reads=(), writes=(), q='sp', **kw):
        slot = 'd%d' % self.dma_rr
        self.dma_rr = (self.dma_rr + 1) % self.NDMA
        deps = self._deps(reads, writes)
        if self.cnt[slot] > 0:
            deps[slot] = max(deps.get(slot, 0), self.cnt[slot])
        self._wait(q, deps)
        ins = self.e[q].dma_start(out=out, in_=in_, **kw)
        self.ninstr += 1
        self.cnt[slot] += 16
        ins.then_inc(self.sem[slot], 16)
        cval = self.cnt[slot]
        for b in writes:
            b.lw = (slot, cval)
            b.rd = {}
        for b in reads:
            b.rd[slot] = max(b.rd.get(slot, 0), cval)
        return ins

    def barrier(self):
        for E in self.ENG:
            deps = {k: v for k, v in self.cnt.items() if v > 0 and k != E}
            self._wait(E, deps)

    def finish(self):
        for k in self.sem:
            if k.startswith('d') and self.cnt[k] > 0:
                self.e['sp'].wait_ge(self.sem[k], self.cnt[k])

    def mm(self, out, lhsT, rhs, start, stop, reads, writes, inc=None):
        if inc is None:
            inc = stop
        return self.op('pe', lambda e: e.matmul(out, lhsT=lhsT, rhs=rhs, start=start, stop=stop),
                       reads=reads, writes=writes, inc=inc)

    def tr(self, out, in_, ident, reads, writes, inc=True):
        return self.op('pe', lambda e: e.transpose(out=out, in_=in_, identity=ident), reads=reads, writes=writes, inc=inc)

    def act(self, out, in_, func, reads, writes, E='act', **kw):
        return self.op('act', lambda e: e.activation(out=out, in_=in_, func=func, **kw), reads=reads, writes=writes)

    def ts(self, E, out, in0, s1, s2, op0, op1, reads, writes):
        if op1 is None:
            return self.op(E, lambda e: e.tensor_scalar(out=out, in0=in0, scalar1=s1, scalar2=None, op0=op0), reads=reads, writes=writes)
        return self.op(E, lambda e: e.tensor_scalar(out=out, in0=in0, scalar1=s1, scalar2=s2, op0=op0, op1=op1), reads=reads, writes=writes)

    def tt(self, E, out, in0, in1, op, reads, writes):
        return self.op(E, lambda e: e.tensor_tensor(out=out, in0=in0, in1=in1, op=op), reads=reads, writes=writes)

    def stt(self, out, in0, scalar, in1, op0, op1, reads, writes):
        return self.op('dve', lambda e: e.scalar_tensor_tensor(out=out, in0=in0, scalar=scalar, in1=in1, op0=op0, op1=op1),
                       reads=reads, writes=writes)

    def cp(self, E, out, in_, reads, writes):
        if E == 'act':
            return self.op('act', lambda e: e.copy(out=out, in_=in_), reads=reads, writes=writes)
        return self.op(E, lambda e: e.tensor_copy(out=out, in_=in_), reads=reads, writes=writes)


def R(ap):
    return ap.bitcast(F32R)


def build(dbg=(), stage=99):
    nc = bass.Bass("TRN2", target_bir_lowering=False)
    T = {}

    def din(name, shape, dt=F32):
        T[name] = nc.dram_tensor(name, list(shape), dt, kind="ExternalInput").ap()
        return T[name]

    def dout(name, shape, dt=F32):
        T[name] = nc.dram_tensor(name, list(shape), dt, kind="ExternalOutput").ap()
        return T[name]

    x = din("x", [SEQ, D])
    ctx = din("ctx", [CTX, D])
    cT = din("cT", [128, 8, 2])
    ada_w = din("ada_w", [D, 6 * D])
    ada_bT = din("ada_bT", [128, 48])
    gmixT = din("gmixT", [128, 8])
    gffnT = din("gffnT", [128, 8])
    gfin_bc = din("gfin_bc", [128, D])
    w_in = din("w_in", [D, IN_COLS])
    convT = din("convT", [128, 12, 5])
    alog_bc = din("alog_bc", [128, 18, 8])
    dtb_bc = din("dtb_bc", [128, 18, 8])
    onormT = din("onormT", [128, 1])
    muT = din("muT", [128, 15])
    w0T = din("w0T", [128, 2, 4])
    a0T = din("a0T", [128, 2, 4])
    w2m = din("w2m", [128, 512])
    a2m = din("a2m", [128, 512])
    g2m = din("g2m", [128, 512])
    rwv = din("rwv", [128, 5, 4])
    scr_ob = nc.dram_tensor("scr_ob", [SEQ, 512], BF16, kind="Internal").ap()
    scr_h = nc.dram_tensor("scr_h", [SEQ, D], F32, kind="Internal").ap()
    scr_u = nc.dram_tensor("scr_u", [128, 8, SEQ], BF16, kind="Internal").ap()
    w_o_a = din("w_o_a", [512, D])
    w_o_b = din("w_o_b", [512, D])
    w_out = din("w_out", [D, D])
    wrt = din("wrt", [128, 8, 36])
    rb_bc = din("rb_bc", [128, 36])
    moe_wg = din("moe_wg", [32, D, 256])
    moe_wu = din("moe_wu", [32, D, 256])
    moe_wd = din("moe_wd", [32, 256, D])
    out = dout("out", [SEQ, D])
    for name, shape, dt in dbg:
        dout(name, shape, dt)

    with ExitStack() as es:
        c = Ctx(nc, es)
        PS = [c.ps(es, [128, 512], F32, 'ps%d' % i) for i in range(8)]

        def psb(i):
            return PS[i][:].bitcast(BF16)

        bank_reads = [dict() for _ in range(8)]

        class Reg:
            def __init__(self, bank, c0, n, name):
                self.bank, self.c0, self.n = bank, c0, n
                self.buf = PS[bank]

            def ap(self, lo=0, hi=None, rows=slice(None)):
                hi = self.n if hi is None else hi
                return PS[self.bank].t[rows, self.c0 + lo:self.c0 + hi]

        def run_threads(gens):
            gens = list(gens)
            while gens:
                for g_ in list(gens):
                    try:
                        next(g_)
                    except StopIteration:
                        gens.remove(g_)

        def par(*gens):
            gens = list(gens)
            while gens:
                for g_ in list(gens):
                    try:
                        next(g_)
                    except StopIteration:
                        gens.remove(g_)
                        continue
                    yield

        ident = c.sb(es, [128, 128], F32, 'ident')
        identb = c.sb(es, [128, 128], BF16, 'identb')
        onesf = c.sb(es, [128, 128], F32, 'onesf')
        c.op('pool', lambda e: e.memset(ident[:], 0.0), writes=[ident])
        c.op('pool', lambda e: e.affine_select(out=ident[:], in_=ident[:], pattern=[[-1, 128]], compare_op=ALU.not_equal,
                                                fill=1.0, base=0, channel_multiplier=1), reads=[ident], writes=[ident])
        c.cp('dve', identb[:], ident[:], [ident], [identb])
        c.op('pool', lambda e: e.memset(onesf[:], 1.0), writes=[onesf])
        onesb = c.sb(es, [128, 128], BF16, 'onesb')
        c.cp('dve', onesb[:], onesf[:], [onesf], [onesb])
        ones_r = c.sb(es, [128, 128], F32, 'ones_r')
        nones_r = c.sb(es, [128, 128], F32, 'nones_r')
        ident_r = c.sb(es, [128, 128], F32, 'ident_r')
        c.cp('dve', R(ones_r[:]), onesf[:], [onesf], [ones_r])
        c.ts('dve', R(nones_r[:]), onesf[:], -1.0, None, ALU.mult, None, [onesf], [nones_r])
        c.cp('dve', R(ident_r[:]), ident[:], [ident], [ident_r])
        blk = c.sb(es, [128, 128], F32, 'blk')
        c.op('pool', lambda e: e.memset(blk[:], 0.0), writes=[blk])
        c.op('pool', lambda e: e.memset(blk[0:64, 0:64], 1.0), reads=[blk], writes=[blk])
        c.op('pool', lambda e: e.memset(blk[64:128, 64:128], 1.0), reads=[blk], writes=[blk])
        incl = [c.sb(es, [128, 128], F32, 'incl%d' % d) for d in range(2)]
        strict = [c.sb(es, [128, 128], F32, 'strict%d' % d) for d in range(2)]
        incl_r = [c.sb(es, [128, 128], F32, 'inclr%d' % d) for d in range(2)]
        negm_r = [c.sb(es, [128, 128], F32, 'negm%d' % d) for d in range(2)]
        blk_r = c.sb(es, [128, 128], F32, 'blk_r')
        sel_r = [c.sb(es, [128, 128], F32, 'sel%d' % k_) for k_ in range(2)]
        notI = c.sb(es, [128, 128], F32, 'notI')
        for d in range(2):
            pat = [[1, 128]] if d == 0 else [[-1, 128]]
            cm = -1 if d == 0 else 1
            c.op('pool', lambda e, d=d, pat=pat, cm=cm: e.affine_select(out=incl[d][:], in_=blk[:], pattern=pat, compare_op=ALU.is_ge,
                                                                        fill=0.0, base=0, channel_multiplier=cm), reads=[blk], writes=[incl[d]])
            c.op('pool', lambda e, d=d, pat=pat, cm=cm: e.affine_select(out=strict[d][:], in_=blk[:], pattern=pat, compare_op=ALU.is_gt,
                                                                        fill=0.0, base=0, channel_multiplier=cm), reads=[blk], writes=[strict[d]])
            c.cp('dve', R(incl_r[d][:]), incl[d][:], [incl[d]], [incl_r[d]])
            c.ts('dve', R(negm_r[d][:]), incl[d][:], 1.0e5, -1.0e5, ALU.mult, ALU.add, [incl[d]], [negm_r[d]])
        c.cp('dve', R(blk_r[:]), blk[:], [blk], [blk_r])
        c.ts('dve', notI[:], ident[:], -1.0, 1.0, ALU.mult, ALU.add, [ident], [notI])
        zf = c.sb(es, [128, 128], F32, 'zf')
        c.op('pool', lambda e: e.memset(zf[:], 0.0), writes=[zf])
        for k_ in range(2):
            c.cp('dve', R(sel_r[k_][:]), zf[:], [zf], [sel_r[k_]])
            c.cp('dve', R(sel_r[k_][k_ * 64:(k_ + 1) * 64, :]), onesf[k_ * 64:(k_ + 1) * 64, :], [onesf, sel_r[k_]], [sel_r[k_]])

        mT = c.sb(es, [128, 48, 2], F32, 'mT')
        A1g = c.sb(es, [128, 8, 2], F32, 'A1g')
        A2g = c.sb(es, [128, 8, 2], F32, 'A2g')
        oaT = c.sb(es, [128, 4, SEQ], BF16, 'oaT')

        with ExitStack() as es1:
            sT = c.sb(es1, [128, 8, 2], F32, 'sT')
            abT = c.sb(es1, [128, 48], F32, 'abT')
            gm = c.sb(es1, [128, 8], F32, 'gm')
            gf = c.sb(es1, [128, 8], F32, 'gf')
            c.dma(sT[:], cT, writes=[sT])
            c.dma(abT[:], ada_bT, writes=[abT])
            c.dma(gm[:], gmixT, writes=[gm])
            c.dma(gf[:], gffnT, writes=[gf])
            c.act(sT[:], sT[:], AF.Silu, [sT], [sT])
            Wb = [c.sb(es1, [128, 8, 512], F32, 'adaw%d' % i) for i in range(4)]
            ada_v = ada_w.rearrange("(kc p) n -> p kc n", p=128)
            for blk in range(12):
                wb = Wb[blk % 4]
                c.dma(wb[:], ada_v[:, :, blk * 512:(blk + 1) * 512], writes=[wb], q=('sp' if blk % 2 == 0 else 'act'))
                for mc in range(4):
                    col = (blk * 4 + mc) * 2
                    for kc in range(8):
                        c.mm(PS[0][:, col:col + 2], wb[:, kc, mc * 128:(mc + 1) * 128], sT[:, kc, :],
                             kc == 0, kc == 7, [wb, sT], [PS[0]])
            pv = PS[0][:, 0:96].rearrange("p (m s) -> p m s", s=2)
            for s in range(2):
                c.tt('dve', mT[:, :, s], pv[:, :, s], abT[:], ALU.add, [PS[0], abT], [mT])
            for s in range(2):
                c.stt(A1g[:, :, s], mT[:, 8:16, s], 1.0, gm[:], ALU.add, ALU.mult, [mT, gm], [A1g])
                c.stt(A2g[:, :, s], mT[:, 32:40, s], 1.0, gf[:], ALU.add, ALU.mult, [mT, gf], [A2g])
            c.barrier()

        def make_Bg(es_, Bg, base):
            dg = [c.sb(es_, [128, 128], F32, 'dg%d' % i) for i in range(2)]
            for fc in range(8):
                d_ = dg[fc % 2]
                c.ts('dve', d_[:], ident[:], mT[:, base + fc, 0:1], None, ALU.mult, None, [ident, mT], [d_])
                bank = PS[1 + fc // 4]
                c.mm(bank[:, (fc % 4) * 128:(fc % 4 + 1) * 128], onesf[:], d_[:], True, True, [onesf, d_], [bank])
            c.cp('act', Bg[:, 0:512], PS[1][:], [PS[1]], [Bg])
            c.cp('act', Bg[:, 512:1024], PS[2][:], [PS[2]], [Bg])

        scru_buf = Buf(None, 'scr_u_dram')

        def make_uT_g(srcs, s, dst, tok0, Ag, shift_base, bufs, k):
            X = bufs['X'][k % 4]
            xnb = bufs['xnb'][k % 2]
            ss = bufs['ss'][k % 2]
            sq_ = bufs['sq'][k % 2]
            for (p0, p1, ap) in srcs:
                c.dma(X[p0:p1, :], ap, writes=[X], q=('sp' if k % 2 == 0 else 'pool'))
            c.act(sq_[:], X[:], AF.Square, [X], [sq_, ss], accum_out=ss[:, 0:1])
            yield
            c.ts('dve', ss[:, 1:2], ss[:, 0:1], 1.0 / D, EPS, ALU.mult, ALU.add, [ss], [ss])
            yield
            c.act(ss[:, 2:3], ss[:, 1:2], AF.Sqrt, [ss], [ss])
            yield
            c.op('dve', lambda e: e.reciprocal(out=ss[:, 3:4], in_=ss[:, 2:3]), reads=[ss], writes=[ss])
            c.ts('dve', xnb[:], X[:], ss[:, 3:4], None, ALU.mult, None, [X, ss], [xnb])
            yield
            bank = PS[3 + k % 2]
            pb = psb(3 + k % 2)
            for fc in range(8):
                c.tr(pb[:, fc * 128:(fc + 1) * 128], xnb[:, fc * 128:(fc + 1) * 128], identb[:], [xnb, identb], [bank],
                     inc=(fc == 7))
            yield
            for fc in range(8):
                o_ = dst[:, fc, tok0:tok0 + 128]
                i_ = pb[:, fc * 128:(fc + 1) * 128]
                if fc % 2 == 0:
                    c.ts('dve', o_, i_, Ag[:, fc, s:s + 1], mT[:, shift_base + fc, s:s + 1], ALU.mult, ALU.add,
                         [bank, Ag, mT], [dst])
                else:
                    c.act(o_, i_, AF.Identity, [bank, Ag, mT], [dst], scale=Ag[:, fc, s:s + 1],
                          bias=mT[:, shift_base + fc, s:s + 1])
                if fc % 4 == 3:
                    yield

        def run_uT(jobs, bufs):
            def chain(par_):
                for k in range(par_, len(jobs), 2):
                    srcs, s_, dst, tok0 = jobs[k]
                    yield from make_uT_g(srcs, s_, dst, tok0, A1g, 0, bufs, k)
            run_threads([chain(0), chain(1)])

        def uT_bufs(es_):
            return {'X': [c.sb(es_, [128, D], F32, 'X%d' % i) for i in range(4)],
                    'xnb': [c.sb(es_, [128, D], BF16, 'xnb%d' % i) for i in range(2)],
                    'ss': [c.sb(es_, [128, 4], F32, 'ss%d' % i) for i in range(2)],
                    'sq': [c.sb(es_, [128, D], BF16, 'sq%d' % i) for i in range(2)]}

        esG = ExitStack()
        with esG:
            uT_r = c.sb(esG, [128, 8, NTOK], BF16, 'uT_r')
            with ExitStack() as es2:
                bufs = uT_bufs(es2)
                jobs = [([(0, 128, ctx[t * 128:(t + 1) * 128, :])], 1, uT_r, t * 128) for t in range(2)]
                jobs += [([(0, 128, x[t * 128:(t + 1) * 128, :])], 0, uT_r, CTX + t * 128) for t in range(16)]
                run_uT(jobs, bufs)
                for kc in range(8):
                    c.dma(scr_u[:, kc, :], uT_r[:, kc, CTX:NTOK], reads=[uT_r], writes=[scru_buf])
                c.barrier()


            w_in_v = w_in.rearrange("(kc p) n -> p kc n", p=128)
            TBLK = [(0, 256), (256, 768), (768, 1280), (1280, 1792), (1792, 2304)]
            with ExitStack() as es3:
                g_tok = c.sb(es3, [128, 18, 8], F32, 'g_tok')
                b_tok = c.sb(es3, [128, 18, 8], F32, 'b_tok')
                with ExitStack() as es3a:
                    wab = c.sb(es3a, [128, 8, 16], BF16, 'wab')
                    ab = c.sb(es3a, [128, 18, 16], F32, 'ab')
                    alog = c.sb(es3a, [128, 18, 8], F32, 'alog')
                    dtb = c.sb(es3a, [128, 18, 8], F32, 'dtb')
                    t1 = c.sb(es3a, [128, 18, 8], F32, 't1')
                    t2 = c.sb(es3a, [128, 18, 8], F32, 't2')
                    c.dma(wab[:], w_in_v[:, :, 2048:2064], writes=[wab], q='pool')
                    c.dma(alog[:], alog_bc, writes=[alog])
                    c.dma(dtb[:], dtb_bc, writes=[dtb])
                    for t in range(18):
                        bank = PS[t % 2]
                        for kc in range(8):
                            c.mm(bank[:, 0:16], uT_r[:, kc, t * 128:(t + 1) * 128], wab[:, kc, :], kc == 0, kc == 7, [uT_r, wab], [bank])
                        c.cp('act', ab[:, t, :], bank[:, 0:16], [bank], [ab])
                    c.tt('dve', t1[:], ab[:, :, 0:8], dtb[:], ALU.add, [ab, dtb], [t1])
                    c.stt(t2[:], t1[:], -1.0, t1[:], ALU.mult, ALU.max, [t1], [t2])
                    c.act(t2[:], t2[:], AF.Exp, [t2], [t2], scale=-1.0)
                    c.ts('dve', t2[:], t2[:], 1.0, None, ALU.add, None, [t2], [t2])
                    c.act(t2[:], t2[:], AF.Ln, [t2], [t2])
                    c.stt(t1[:], t1[:], 0.0, t2[:], ALU.max, ALU.add, [t1, t2], [t1])
                    c.act(alog[:], alog[:], AF.Exp, [alog], [alog])
                    c.stt(R(g_tok[:]), t1[:], -1.0, alog[:], ALU.mult, ALU.mult, [t1, alog], [g_tok])
                    c.act(b_tok[:], ab[:, :, 8:16], AF.Sigmoid, [ab], [b_tok])
                    c.barrier()

                cv = c.sb(es3, [128, 12, 5], F32, 'cv')
                onm = c.sb(es3, [128, 1], F32, 'onm')
                c.dma(cv[:], convT, writes=[cv])
                c.dma(onm[:], onormT, writes=[onm])
                raws = [c.sb(es3, [128, NTOK], F32, 'raw%d' % i) for i in range(2)]
                accs = [c.sb(es3, [128, NTOK], F32, 'acc%d' % i) for i in range(2)]
                sqs = [c.sb(es3, [128, NTOK], F32, 'sqg%d' % i) for i in range(2)]
                qT = c.sb(es3, [128, NTOK], F32, 'qT')
                kT = c.sb(es3, [128, NTOK], F32, 'kT')
                vT = c.sb(es3, [128, NTOK], F32, 'vT')
                zs = c.sb(es3, [128, SEQ], F32, 'zs')
                o_acc = c.sb(es3, [128, 16, 128], F32, 'o_acc')
                wc = [c.sb(es3, [128, 8, 128], BF16, 'wc%d' % i) for i in range(2)]
                rns = [[c.sb(es3, [128, 512], F32, 'rn%d_%d' % (j, i)) for i in range(2)] for j in range(2)]
                S = [c.sb(es3, [128, 128], F32, 'S%d' % d) for d in range(2)]
                gcs = [c.sb(es3, [128, 8], F32, 'gcs%d' % i) for i in range(2)]
                egc = [c.sb(es3, [128, 8], F32, 'egc%d' % i) for i in range(2)]
                negc = [c.sb(es3, [128, 8], F32, 'negc%d' % i) for i in range(2)]
                ekd = [c.sb(es3, [128, 8], F32, 'ekd%d' % i) for i in range(2)]
                gend = [c.sb(es3, [128, 2, 8], F32, 'gend%d' % i) for i in range(2)]
                k_tok = [c.sb(es3, [128, 128], F32, 'k_tok%d' % i) for i in range(2)]
                v_tok = [c.sb(es3, [128, 128], F32, 'v_tok%d' % i) for i in range(2)]
                NS = 2
                Gt = [c.sb(es3, [128, 128], F32, 'Gt%d' % i) for i in range(NS)]
                Ei = [c.sb(es3, [128, 128], F32, 'Ei%d' % i) for i in range(NS)]
                Es = [c.sb(es3, [128, 128], F32, 'Es%d' % i) for i in range(NS)]
                QKm = [c.sb(es3, [128, 128], F32, 'QKm%d' % i) for i in range(NS)]
                Pb = [[c.sb(es3, [128, 128], F32, 'P%d_%d' % (i, j)) for j in range(2)] for i in range(NS)]
                Qb = [[c.sb(es3, [128, 128], F32, 'Q%d_%d' % (i, j)) for j in range(2)] for i in range(NS)]
                Wb_ = [[c.sb(es3, [128, 128], F32, 'W%d_%d' % (i, j)) for j in range(2)] for i in range(NS)]
                kdec = [c.sb(es3, [128, 128], F32, 'kdec%d' % i) for i in range(NS)]
                Zb = [c.sb(es3, [128, 128], F32, 'Z%d' % i) for i in range(NS)]
                vnew = [c.sb(es3, [128, 128], F32, 'vnew%d' % i) for i in range(NS)]
                otmp = [c.sb(es3, [128, 128], F32, 'otmp%d' % i) for i in range(NS)]
                otmp2 = [c.sb(es3, [128, 128], F32, 'otmp2%d' % i) for i in range(NS)]
                fin = [c.sb(es3, [128, 132], F32, 'fin%d' % i) for i in range(2)]
                wcnt = 0
                unit = 0
                import os
                H_all = c.sb(es3, [128, 18, 32], F32, 'H_all')
                egc_all = c.sb(es3, [128, 18, 8], F32, 'egc_all')
                negc_all = c.sb(es3, [128, 18, 8], F32, 'negc_all')
                ekd_all = c.sb(es3, [128, 18, 8], F32, 'ekd_all')
                gend_all = c.sb(es3, [128, 18, 16], F32, 'gend_all')
                for t in range(18):
                    bankH = PS[t % 2]
                    c.mm(bankH[:, 0:4], R(incl_r[0][:]), R(g_tok[:, t, 0:4]), True, True, [incl_r[0], g_tok], [bankH])
                    c.mm(bankH[:, 4:8], R(incl_r[1][:]), R(g_tok[:, t, 4:8]), True, True, [incl_r[1], g_tok], [bankH])
                    c.mm(bankH[:, 8:16], R(blk_r[:]), R(g_tok[:, t, :]), True, True, [blk_r, g_tok], [bankH])
                    c.mm(bankH[:, 16:24], R(sel_r[0][:]), R(g_tok[:, t, :]), True, True, [sel_r[0], g_tok], [bankH])
                    c.mm(bankH[:, 24:32], R(sel_r[1][:]), R(g_tok[:, t, :]), True, True, [sel_r[1], g_tok], [bankH])
                    c.cp('dve', H_all[:, t, :], bankH[:, 0:32], [bankH], [H_all])
                c.act(egc_all[:], H_all[:, :, 0:8], AF.Exp, [H_all], [egc_all])
                c.ts('dve', negc_all[:], egc_all[:], -1.0, None, ALU.mult, None, [egc_all], [negc_all])
                c.tt('dve', ekd_all[:], H_all[:, :, 8:16], H_all[:, :, 0:8], ALU.subtract, [H_all], [ekd_all])
                c.act(ekd_all[:], ekd_all[:], AF.Exp, [ekd_all], [ekd_all])
                c.act(gend_all[:], H_all[:, :, 16:32], AF.Exp, [H_all], [gend_all])
                NH = int(os.environ.get('K_NH', '4'))
                NSTEP = int(os.environ.get('K_NSTEP', '18'))
                KLAT = int(os.environ.get('K_LAT', '9'))
                for h in range(NH):
                    def proj_g(ci, slot, h=h):
                        col0, dst = [(h * 128, qT), (512 + h * 128, kT), (1024 + h * 128, vT), (1536 + h * 128, zs)][ci]
                        raw, acc, sq = raws[slot], accs[slot], sqs[slot]
                        w_ = wc[slot]
                        pb0 = 4 * slot
                        if h == 0 and ci < 2:
                            c.dma(w_[:], w_in_v[:, :, col0:col0 + 128], writes=[w_], q='pool')
                        for bi, (t0, t1_) in enumerate(TBLK):
                            if ci == 3 and bi == 0:
                                continue
                            bank = PS[pb0 + bi % 2]
                            n = t1_ - t0
                            for kc in range(8):
                                c.mm(bank[:, 0:n], w_[:, kc, :], uT_r[:, kc, t0:t1_], kc == 0, kc == 7, [w_, uT_r], [bank])
                            if ci == 3:
                                c.act(zs[:, t0 - CTX:t1_ - CTX], bank[:, 0:n], AF.Silu, [bank], [zs])
                            else:
                                c.cp('act', raw[:, t0:t1_], bank[:, 0:n], [bank], [raw])
                            yield
                        nh_, nci = (h, ci + 2) if ci < 2 else (h + 1, ci - 2)
                        if nh_ < NH:
                            ncol = [nh_ * 128, 512 + nh_ * 128, 1024 + nh_ * 128, 1536 + nh_ * 128][nci]
                            c.dma(w_[:], w_in_v[:, :, ncol:ncol + 128], writes=[w_], q='pool')
                        if ci == 3:
                            return
                        cch = ci * 4 + h
                        c.ts('dve', acc[:], raw[:], cv[:, cch, 2:3], None, ALU.mult, None, [raw, cv], [acc])
                        yield
                        for kk_ in (0, 1, 3, 4):
                            sft = kk_ - 2
                            for (a_, b_) in ((0, CTX), (CTX, NTOK)):
                                lo = max(a_, a_ - sft)
                                hi = min(b_, b_ - sft)
                                c.stt(acc[:, lo:hi], raw[:, lo + sft:hi + sft], cv[:, cch, kk_:kk_ + 1], acc[:, lo:hi], ALU.mult, ALU.add,
                                      [raw, cv, acc], [acc])
                            yield
                        if ci == 2:
                            c.act(R(vT[:]), acc[:], AF.Silu, [acc], [vT])
                            return
                        c.act(acc[:], acc[:], AF.Silu, [acc], [acc])
                        c.act(R(sq[:]), acc[:], AF.Square, [acc], [sq])
                        yield
                        sc = 128.0 if ci == 0 else 1.0
                        for bi, (t0, t1_) in enumerate(TBLK):
                            bank = PS[pb0 + 2 + bi % 2]
                            n = t1_ - t0
                            r_ = rns[slot][bi % 2]
                            c.mm(bank[:, 0:n], R(ones_r[:]), R(sq[:, t0:t1_]), True, True, [ones_r, sq], [bank])
                            c.ts('dve', r_[:, 0:n], bank[:, 0:n], sc, EPS * sc, ALU.mult, ALU.add, [bank], [r_])
                            c.act(r_[:, 0:n], r_[:, 0:n], AF.Sqrt, [r_], [r_])
                            yield
                            c.op('dve', lambda e, r_=r_, n=n: e.reciprocal(out=r_[:, 0:n], in_=r_[:, 0:n]), reads=[r_], writes=[r_])
                            c.tt('dve', R(dst[:, t0:t1_]), acc[:, t0:t1_], r_[:, 0:n], ALU.mult, [acc, r_], [dst])
                            yield

                    run_threads([proj_g(0, 0), proj_g(1, 1)])
                    run_threads([proj_g(2, 0), proj_g(3, 1)])
                    if 'd_qkv' in T and h == 0:
                        c.dma(T['d_qkv'][0], qT[:], reads=[qT])
                        c.dma(T['d_qkv'][1], kT[:], reads=[kT])
                        c.dma(T['d_qkv'][2], vT[:], reads=[vT])
                    for d in range(2):
                        c.ts('dve', R(S[d][:]), zf[:], 0.0, None, ALU.mult, None, [zf], [S[d]])
                    order_f = list(range(18))
                    order_b = [1, 0] + list(range(17, 1, -1))
                    def gdn_unit_g(d, step):
                        tile = order_f[step] if d == 0 else order_b[step]
                        is_lat = tile >= 2
                        ts0 = tile * 128
                        col = d * 4 + h
                        pi = d
                        u = d
                        X0, X1, X2, X3 = (PS[4 * d + i_] for i_ in range(4))
                        bankT = X3
                        c.tr(bankT[:, 0:128], kT[:, ts0:ts0 + 128], ident[:], [kT, ident], [bankT])
                        c.tr(bankT[:, 128:256], vT[:, ts0:ts0 + 128], ident[:], [vT, ident], [bankT])
                        bankA = X0
                        c.mm(bankA[:, 0:128], R(kT[:, ts0:ts0 + 128]), R(kT[:, ts0:ts0 + 128]), True, True, [kT], [bankA])
                        c.mm(bankA[:, 128:256], R(kT[:, ts0:ts0 + 128]), R(qT[:, ts0:ts0 + 128]), True, True, [kT, qT], [bankA])
                        c.ts('pool', R(Gt[u][:]), incl[d][:], g_tok[:, tile, col:col + 1], None, ALU.mult, None, [incl[d], g_tok], [Gt[u]])
                        yield
                        bankB = X1
                        c.mm(bankB[:, 0:128], R(ones_r[:]), R(Gt[u][:]), True, False, [ones_r, Gt[u]], [bankB])
                        c.mm(bankB[:, 0:128], R(Gt[u][:]), R(nones_r[:]), False, False, [nones_r, Gt[u]], [bankB])
                        c.mm(bankB[:, 0:128], R(ident_r[:]), R(negm_r[d][:]), False, True, [ident_r, negm_r[d]], [bankB])
                        yield
                        c.cp('act', k_tok[pi][:], bankT[:, 0:128], [bankT], [k_tok[pi]])
                        c.cp('act', R(v_tok[pi][:]), bankT[:, 128:256], [bankT], [v_tok[pi]])
                        c.act(Ei[u][:], bankB[:, 0:128], AF.Exp, [bankB], [Ei[u]])
                        yield
                        c.tt('pool', Es[u][:], Ei[u][:], notI[:], ALU.mult, [Ei[u], notI], [Es[u]])
                        P, Q, W = Pb[u], Qb[u], Wb_[u]
                        yield
                        c.stt(R(P[0][:]), bankA[:, 0:128], b_tok[:, tile, col:col + 1], Es[u][:], ALU.mult, ALU.mult,
                              [bankA, b_tok, Es[u]], [P[0]])
                        c.tt('dve', R(QKm[u][:]), bankA[:, 128:256], Ei[u][:], ALU.mult, [bankA, Ei[u]], [QKm[u]])
                        c.ts('dve', R(kdec[u][:]), k_tok[pi][:], ekd_all[:, tile, col:col + 1], None, ALU.mult, None, [k_tok[pi], ekd_all], [kdec[u]])
                        yield
                        bankC = X2
                        bankD = X3
                        c.tr(bankC[:, 0:128], P[0][:], ident[:], [P[0], ident], [bankC])
                        c.tt('pool', R(W[0][:]), ident[:], P[0][:], ALU.subtract, [ident, P[0]], [W[0]])
                        yield
                        c.cp('act', R(Q[0][:]), bankC[:, 0:128], [bankC], [Q[0]])
                        yield
                        for k_ in range(5):
                            a_, b_ = k_ % 2, (k_ + 1) % 2
                            c.mm(bankC[:, 128:256], R(P[a_][:]), R(Q[a_][:]), True, True, [P[a_], Q[a_]], [bankC])
                            if k_ < 4:
                                c.mm(bankD[:, 0:128], R(Q[a_][:]), R(P[a_][:]), True, True, [P[a_], Q[a_]], [bankD])
                            yield
                            c.cp('act', R(Q[b_][:]), bankC[:, 128:256], [bankC], [Q[b_]])
                            if k_ < 4:
                                c.cp('dve', R(P[b_][:]), bankD[:, 0:128], [bankD], [P[b_]])
                            yield
                            c.mm(bankD[:, 128:256], R(Q[b_][:]), R(W[a_][:]), True, True, [Q[b_], W[a_]], [bankD])
                            yield
                            c.tt('dve', R(W[b_][:]), bankD[:, 128:256], W[a_][:], ALU.add, [bankD, W[a_]], [W[b_]])
                            yield
                        Wf = W[1]
                        for cs in ((0, 64) if d == 0 else (64, 0)):
                            sl_ = slice(cs, cs + 64)
                            chunk = cs // 64
                            c.mm(X0[:, 0:128], R(kT[:, ts0:ts0 + 128]), R(S[d][:]), True, True, [kT, S[d]], [X0])
                            if is_lat:
                                c.mm(X0[:, 128:256], R(qT[:, ts0:ts0 + 128]), R(S[d][:]), True, True, [qT, S[d]], [X0])
                            yield
                            c.stt(R(Zb[u][sl_, :]), X0[sl_, 0:128], negc_all[sl_, tile, col:col + 1], v_tok[pi][sl_, :], ALU.mult, ALU.add,
                                  [X0, negc_all, v_tok[pi]], [Zb[u]])
                            if is_lat:
                                c.ts('dve', otmp[u][sl_, :], X0[sl_, 128:256], egc_all[sl_, tile, col:col + 1], None, ALU.mult, None, [X0, egc_all], [otmp[u]])
                            yield
                            c.mm(X1[:, 0:128], R(Wf[sl_, :]), R(Zb[u][sl_, :]), True, True, [Wf, Zb[u]], [X1])
                            yield
                            c.ts('dve', R(vnew[u][sl_, :]), X1[sl_, 0:128], b_tok[sl_, tile, col:col + 1], None, ALU.mult, None,
                                 [X1, b_tok], [vnew[u]])
                            yield
                            c.mm(X3[:, 256:384], R(kdec[u][sl_, :]), R(vnew[u][sl_, :]), True, True, [kdec[u], vnew[u]], [X3])
                            if is_lat:
                                c.mm(X2[:, 0:128], R(QKm[u][sl_, :]), R(vnew[u][sl_, :]), True, True, [QKm[u], vnew[u]], [X2])
                            yield
                            c.stt(R(S[d][:]), S[d][:], gend_all[:, tile, chunk * 8 + col:chunk * 8 + col + 1], X3[:, 256:384], ALU.mult, ALU.add,
                                  [S[d], gend_all, X3], [S[d]])
                            if is_lat:
                                lt = tile - 2
                                if (d == 0) == (lt <= 7):
                                    c.tt('dve', o_acc[sl_, lt, :], X2[sl_, 0:128], otmp[u][sl_, :], ALU.add, [X2, otmp[u]], [o_acc])
                                else:
                                    c.tt('dve', otmp2[u][sl_, :], X2[sl_, 0:128], otmp[u][sl_, :], ALU.add, [X2, otmp[u]], [otmp2[u]])
                                    c.tt('pool', o_acc[sl_, lt, :], o_acc[sl_, lt, :], otmp2[u][sl_, :], ALU.add, [o_acc, otmp2[u]], [o_acc])
                            yield

                    def gdn_dir_g(d):
                        for step in range(NSTEP):
                            yield from gdn_unit_g(d, step)

                    run_threads([gdn_dir_g(0), gdn_dir_g(1)])
                    def gdn_out_g(par_, h=h):
                        f_ = fin[par_]
                        bank = PS[par_]
                        for lt in range(par_, 16, 2):
                            c.act(f_[:, 0:128], o_acc[:, lt, :], AF.Square, [o_acc], [f_], accum_out=f_[:, 128:129])
                            yield
                            c.ts('dve', f_[:, 129:130], f_[:, 128:129], 1.0 / 128, EPS, ALU.mult, ALU.add, [f_], [f_])
                            yield
                            c.act(f_[:, 130:131], f_[:, 129:130], AF.Sqrt, [f_], [f_])
                            yield
                            c.op('dve', lambda e, f_=f_: e.reciprocal(out=f_[:, 131:132], in_=f_[:, 130:131]), reads=[f_], writes=[f_])
                            c.ts('dve', f_[:, 0:128], o_acc[:, lt, :], f_[:, 131:132], None, ALU.mult, None, [o_acc, f_], [f_])
                            yield
                            c.tr(bank[:, 0:128], f_[:, 0:128], ident[:], [f_, ident], [bank])
                            yield
                            c.stt(oaT[:, h, lt * 128:(lt + 1) * 128], bank[:, 0:128], onm[:, 0:1], zs[:, lt * 128:(lt + 1) * 128], ALU.mult, ALU.mult,
                                  [bank, onm, zs], [oaT])
                            yield

                    run_threads([gdn_out_g(0), gdn_out_g(1)])
                c.barrier()
        if 'd_oaT' in T:
            c.dma(T['d_oaT'], oaT[:], reads=[oaT])


        def inv_chain(P, Q, W, bankC, bankD):
            c.tr(bankC[:, 0:128], P[0][:], ident[:], [P[0], ident], [bankC])
            c.cp('act', R(Q[0][:]), bankC[:, 0:128], [bankC], [Q[0]])
            c.tt('pool', R(W[0][:]), ident[:], P[0][:], ALU.subtract, [ident, P[0]], [W[0]])
            for k_ in range(5):
                a_, b_ = k_ % 2, (k_ + 1) % 2
                c.mm(bankC[:, 128:256], R(P[a_][:]), R(Q[a_][:]), True, True, [P[a_], Q[a_]], [bankC])
                if k_ < 4:
                    c.mm(bankD[:, 0:128], R(Q[a_][:]), R(P[a_][:]), True, True, [P[a_], Q[a_]], [bankD])
                c.cp('act', R(Q[b_][:]), bankC[:, 128:256], [bankC], [Q[b_]])
                if k_ < 4:
                    c.cp('dve', R(P[b_][:]), bankD[:, 0:128], [bankD], [P[b_]])
                c.mm(bankD[:, 128:256], R(Q[b_][:]), R(W[a_][:]), True, True, [Q[b_], W[a_]], [bankD])
                c.tt('dve', R(W[b_][:]), bankD[:, 128:256], W[a_][:], ALU.add, [bankD, W[a_]], [W[b_]])
            return W[1]

        BW = 256
        NBLK = NTOK // BW
        with ExitStack() as esR:
            uT_c = c.sb(esR, [128, 8, NTOK], BF16, 'uT_c')
            with ExitStack() as es2:
                bufs = uT_bufs(es2)
                x_cm = x.rearrange("(r w) d -> w r d", w=64)
                jobs = [([(0, 128, ctx[t * 128:(t + 1) * 128, :])], 1, uT_c, t * 128) for t in range(2)]
                jobs += [([(wl * 32, wl * 32 + 32, x_cm[4 * j + wl]) for wl in range(4)], 0, uT_c, CTX + j * 128) for j in range(16)]
                run_uT(jobs, bufs)
                c.barrier()
            mu = c.sb(esR, [128, 15], F32, 'mu')
            hmu = c.sb(esR, [128, 15], F32, 'hmu')
            omu = c.sb(esR, [128, 15], F32, 'omu')
            w0 = c.sb(esR, [128, 2, 4], F32, 'w0')
            a0 = c.sb(esR, [128, 2, 4], F32, 'a0')
            rw = c.sb(esR, [128, 5, 4], F32, 'rw')
            oka = c.sb(esR, [128, 4], F32, 'oka')
            oka2 = c.sb(esR, [128, 4], F32, 'oka2')
            w2b = c.sb(esR, [128, 512], BF16, 'w2b')
            a2b = c.sb(esR, [128, 512], BF16, 'a2b')
            g2b = c.sb(esR, [128, 512], BF16, 'g2b')
            c.dma(mu[:], muT, writes=[mu])
            c.dma(w0[:], w0T, writes=[w0])
            c.dma(a0[:], a0T, writes=[a0])
            c.dma(rw[:], rwv, writes=[rw])
            c.dma(w2b[:], w2m, writes=[w2b], q='pool')
            c.dma(a2b[:], a2m, writes=[a2b], q='pool')
            c.dma(g2b[:], g2m, writes=[g2b], q='pool')
            c.ts('dve', hmu[:], mu[:], 0.5, None, ALU.mult, None, [mu], [hmu])
            c.ts('dve', omu[:], mu[:], -1.0, 1.0, ALU.mult, ALU.add, [mu], [omu])
            c.ts('dve', oka[:], rw[:, 1, :], -1.0, 1.0, ALU.mult, ALU.add, [rw], [oka])
            c.ts('dve', oka2[:], rw[:, 1, :], -2.0, 2.0, ALU.mult, ALU.add, [rw], [oka2])
            cmask = c.sb(esR, [128, BW], F32, 'cmask')
            c.op('pool', lambda e: e.memset(cmask[:], 1.0), writes=[cmask])
            c.op('pool', lambda e: e.memset(cmask[:].rearrange("p (a b) -> p a b", b=64)[:, :, 0:1], 0.0), reads=[cmask], writes=[cmask])
            mskA = [c.sb(esR, [128, 256], F32, 'mskA%d' % d) for d in range(2)]
            mskB = [c.sb(esR, [128, 256], F32, 'mskB%d' % d) for d in range(2)]
            for d in range(2):
                c.ts('dve', mskA[d][:, 0:128], strict[d][:], -1.0, None, ALU.mult, None, [strict[d]], [mskA[d]])
                c.cp('dve', mskA[d][:, 128:256], incl[d][:], [incl[d]], [mskA[d]])
                c.cp('dve', mskB[d][:, 0:128], strict[d][:], [strict[d]], [mskB[d]])
                c.cp('dve', mskB[d][:, 128:256], incl[d][:], [incl[d]], [mskB[d]])

            raw = c.sb(esR, [128, NTOK], F32, 'rraw')
            t1 = c.sb(esR, [128, NTOK], F32, 'rt1')
            twl = c.sb(esR, [128, NTOK], BF16, 'twl')
            alo = c.sb(esR, [128, NTOK], BF16, 'alo')
            sgl = c.sb(esR, [128, NTOK], BF16, 'sgl')
            rb = c.sb(esR, [128, NTOK], BF16, 'rb')
            kb = c.sb(esR, [128, NTOK], BF16, 'kb')
            vb = c.sb(esR, [128, NTOK], BF16, 'vb')
            G1 = c.sb(esR, [128, SEQ], BF16, 'G1')
            G2 = c.sb(esR, [128, SEQ], BF16, 'G2')
            y_acc = c.sb(esR, [128, 16, 128], F32, 'y_acc')
            wcr = [c.sb(esR, [128, 8, 128], BF16, 'wcr%d' % i) for i in range(2)]
            wcn = [0]

            pm_sched = [1536, 1664, 1792]
            for hc_ in range(4):
                pm_sched += [hc_ * 128, 512 + hc_ * 128, 1024 + hc_ * 128]

            def proj_mix(bcol, dst, func, mi):
                n_ = wcn[0]
                wcn[0] += 1
                assert pm_sched[n_] == bcol
                w_ = wcr[n_ % 2]
                if n_ == 0:
                    c.dma(w_[:], w_in_v[:, :, A_COLS + bcol:A_COLS + bcol + 128], writes=[w_], q='pool')
                for bi, (t0, t1_) in enumerate(TBLK):
                    bank = PS[bi % 2]
                    n = t1_ - t0
                    for kc in range(8):
                        c.mm(bank[:, 0:n], w_[:, kc, :], uT_c[:, kc, t0:t1_], kc == 0, kc == 7, [w_, uT_c], [bank])
                    c.cp('act', raw[:, t0:t1_], bank[:, 0:n], [bank], [raw])
                    if bi == 0 and n_ + 1 < len(pm_sched):
                        nb_ = A_COLS + pm_sched[n_ + 1]
                        c.dma(wcr[(n_ + 1) % 2][:], w_in_v[:, :, nb_:nb_ + 128], writes=[wcr[(n_ + 1) % 2]], q='pool')
                for (a_, b_) in ((0, CTX), (CTX, NTOK)):
                    c.tt('pool', t1[:, a_ + 1:b_ - 1], raw[:, a_:b_ - 2], raw[:, a_ + 2:b_], ALU.add, [raw], [t1])
                    c.cp('pool', t1[:, a_:a_ + 1], raw[:, a_ + 1:a_ + 2], [raw], [t1])
                    c.cp('pool', t1[:, b_ - 1:b_], raw[:, b_ - 2:b_ - 1], [raw], [t1])
                c.ts('dve', t1[:], t1[:], hmu[:, mi:mi + 1], None, ALU.mult, None, [t1, hmu], [t1])
                if func is None:
                    c.stt(dst[:], raw[:], omu[:, mi:mi + 1], t1[:], ALU.mult, ALU.add, [raw, omu, t1], [dst])
                else:
                    c.stt(t1[:], raw[:], omu[:, mi:mi + 1], t1[:], ALU.mult, ALU.add, [raw, omu, t1], [t1])
                    c.act(dst[:], t1[:], func, [t1], [dst])

            proj_mix(1536, twl, AF.Tanh, 12)
            proj_mix(1664, alo, None, 13)
            proj_mix(1792, sgl, AF.Sigmoid, 14)

            def blkbuf(name, dt=F32, w=BW):
                return c.sb(esR, [128, w], dt, name)
            DB = []
            for d in range(2):
                g = {}
                arena = (raw, t1)[d]
                for i_, nm in enumerate(('lw', 'icl', 'pre', 'cumd', 'kkn', 'kdir', 'bvec', 'tmpa', 'tmpb', 'opr', 'btT', 'ktT', 'bhT', 'khT')):
                    if i_ < 9:
                        g[nm] = Buf(arena.t[:, i_ * BW:(i_ + 1) * BW], '%s%d' % (nm, d))
                    else:
                        g[nm] = blkbuf('%s%d' % (nm, d))
                g['AR'] = c.sb(esR, [128, 2, BW], F32, 'AR%d' % d)
                g['gC'] = c.sb(esR, [128, BW // 64], F32, 'gC%d' % d)
                for nm in ('Bh_tok', 'Kh_tok', 'Vt'):
                    g[nm] = c.sb(esR, [128, 128], F32, '%s%d' % (nm, d))
                g['BL0'] = Reg(4 * d, 0, 256, 'BL0_%d' % d)
                g['BL1'] = Reg(4 * d + 2, 0, 256, 'BL1_%d' % d)
                g['TL0'] = Reg(4 * d + 1, 0, 128, 'TL0_%d' % d)
                g['TLb'] = Reg(4 * d + 1, 128, 128, 'TLb_%d' % d)
                g['TL1'] = Reg(4 * d + 3, 0, 128, 'TL1_%d' % d)
                g['U'] = []
                for hh in range(2):
                    u = {'bankA': 4 * d + 2 * hh, 'bankB': 4 * d + 2 * hh + 1}
                    sfx = '%d%d' % (d, hh)
                    u['AB1'] = c.sb(esR, [128, 256], F32, 'AB1_' + sfx)
                    u['AB2'] = c.sb(esR, [128, 256], F32, 'AB2_' + sfx)
                    u['XY2'] = c.sb(esR, [128, 128], F32, 'XY2_' + sfx)
                    for nm in ('P', 'Q', 'W'):
                        u[nm] = [c.sb(esR, [128, 128], F32, '%sr%d_%s' % (nm, j, sfx)) for j in range(2)]
                    for nm in ('Xs', 'Us', 'yt', 'yt2', 'Tst'):
                        u[nm] = c.sb(esR, [128, 64], F32, nm + sfx)
                    g['U'].append(u)
                DB.append(g)
            gst2 = [c.sb(esR, [128, 24], F32, 'gst%d' % i) for i in range(2)]
            yn2 = [c.sb(esR, [128, 128], F32, 'yn%d' % i) for i in range(2)]
            obf2 = [c.sb(esR, [128, 128], BF16, 'obf%d' % i) for i in range(2)]
            otf2 = [c.sb(esR, [128, 128], F32, 'otf%d' % i) for i in range(2)]
            NHC = int(os.environ.get('K_NHC', '4'))
            NBS = int(os.environ.get('K_NBS', '9'))
            LG = -0.6065306597126334
            KTG = int(os.environ.get('K_TG', '9'))

            def block_g(d, g, b, hc):
                rowsd = slice(d * 64, d * 64 + 64)
                cs_ = slice(hc * 128, (hc + 1) * 128)
                t0 = b * BW
                tsl = slice(t0, t0 + BW)
                lw, icl, pre, cumd, kkn, kdir, bvec, tmpa, tmpb, opr = (g[n_] for n_ in ('lw', 'icl', 'pre', 'cumd', 'kkn', 'kdir', 'bvec', 'tmpa', 'tmpb', 'opr'))
                AR, btT, ktT, bhT, khT, gC = (g[n_] for n_ in ('AR', 'btT', 'ktT', 'bhT', 'khT', 'gC'))
                BL0, BL1 = g['BL0'], g['BL1']
                c.mm(BL0.ap(), w2b[rowsd, cs_], twl[rowsd, tsl], True, True, [w2b, twl], [BL0.buf])
                c.mm(BL1.ap(), a2b[rowsd, cs_], alo[rowsd, tsl], True, True, [a2b, alo], [BL1.buf])
                c.ts('dve', kkn[:], kb[:, tsl], rw[:, 0, hc:hc + 1], None, ALU.mult, None, [kb, rw], [kkn])
                yield
                c.act(lw[:], BL0.ap(), AF.Sigmoid, [BL0.buf, w0], [lw], bias=w0[:, d, hc:hc + 1])
                c.act(icl[:], BL1.ap(), AF.Sigmoid, [BL1.buf, a0], [icl], bias=a0[:, d, hc:hc + 1])
                c.act(R(opr[:]), kkn[:], AF.Square, [kkn], [opr])
                yield
                c.ts('dve', lw[:], lw[:], LG, None, ALU.mult, None, [lw], [lw])
                c.mm(BL0.ap(), R(blk_r[:]), R(opr[:]), True, True, [blk_r, opr], [BL0.buf])
                c.op('dve', lambda e: e.tensor_tensor_scan(out=pre[:], data0=cmask[:], data1=lw[:], initial=0.0,
                                                           op0=ALU.mult, op1=ALU.add), reads=[cmask, lw], writes=[pre])
                yield
                pre3 = pre[:].rearrange("p (a b) -> p a b", b=64)
                tot_bc = pre3[:, :, 63:64].to_broadcast([128, BW // 64, 64])
                if d == 0:
                    c.cp('pool', cumd[:], pre[:], [pre], [cumd])
                else:
                    c.tt('dve', cumd[:], lw[:], pre[:], ALU.subtract, [lw, pre], [cumd])
                    c.tt('dve', cumd[:].rearrange("p (a b) -> p a b", b=64), cumd[:].rearrange("p (a b) -> p a b", b=64), tot_bc,
                         ALU.add, [cumd, pre], [cumd])
                c.ts('dve', tmpb[:], BL0.ap(), EPS, None, ALU.add, None, [BL0.buf], [tmpb])
                yield
                c.act(gC[:], pre3[:, :, 63], AF.Exp, [pre], [gC])
                c.act(tmpb[:], tmpb[:], AF.Sqrt, [tmpb], [tmpb])
                c.ts('dve', kdir[:], icl[:], rw[:, 1, hc:hc + 1], oka[:, hc:hc + 1], ALU.mult, ALU.add, [icl, rw, oka], [kdir])
                c.tt('dve', kdir[:], kdir[:], kb[:, tsl], ALU.mult, [kdir, kb], [kdir])
                yield
                c.op('dve', lambda e: e.reciprocal(out=tmpb[:], in_=tmpb[:]), reads=[tmpb], writes=[tmpb])
                c.tt('dve', kkn[:], kkn[:], tmpb[:], ALU.mult, [kkn, tmpb], [kkn])
                c.tt('dve', tmpa[:], cumd[:], lw[:], ALU.subtract, [cumd, lw], [tmpa])
                yield
                c.tt('pool', bvec[:], kkn[:], icl[:], ALU.mult, [kkn, icl], [bvec])
                c.act(tmpa[:], tmpa[:], AF.Exp, [tmpa], [tmpa])
                yield
                c.stt(R(AR[:, 0, :]), kkn[:], -1.0, tmpa[:], ALU.mult, ALU.mult, [kkn, tmpa], [AR])
                yield
                c.act(tmpa[:], cumd[:], AF.Exp, [cumd], [tmpa])
                c.tt('dve', tmpb[:].rearrange("p (a b) -> p a b", b=64), cumd[:].rearrange("p (a b) -> p a b", b=64), tot_bc,
                     ALU.subtract, [cumd, pre], [tmpb])
                yield
                c.tt('dve', R(AR[:, 1, :]), rb[:, tsl], tmpa[:], ALU.mult, [rb, tmpa], [AR])
                yield
                c.act(tmpa[:], cumd[:], AF.Exp, [cumd], [tmpa], scale=-1.0)
                c.act(tmpb[:], tmpb[:], AF.Exp, [tmpb], [tmpb], scale=-1.0)
                yield
                c.tt('dve', R(btT[:]), bvec[:], tmpa[:], ALU.mult, [bvec, tmpa], [btT])
                c.tt('pool', R(ktT[:]), kdir[:], tmpa[:], ALU.mult, [kdir, tmpa], [ktT])
                yield
                c.tt('dve', R(bhT[:]), bvec[:], tmpb[:], ALU.mult, [bvec, tmpb], [bhT])
                c.tt('pool', R(khT[:]), kdir[:], tmpb[:], ALU.mult, [kdir, tmpb], [khT])
                yield

            def tile_g(d, g, b, tl):
                lsl_ = slice(tl * 128, tl * 128 + 128)
                gts = (2 * b + tl) * 128
                TL0, TL1, TLb = g['TL0'], g['TL1'], g['TLb']
                bhT, khT, Bh_tok, Kh_tok, Vt = g['bhT'], g['khT'], g['Bh_tok'], g['Kh_tok'], g['Vt']
                c.mm(TL0.ap(), R(bhT[:, lsl_]), R(ident_r[:]), True, True, [bhT, ident_r], [TL0.buf])
                c.mm(TLb.ap(), vb[:, gts:gts + 128], identb[:], True, True, [vb, identb], [TLb.buf])
                c.mm(TL1.ap(), R(khT[:, lsl_]), R(ident_r[:]), True, True, [khT, ident_r], [TL1.buf])
                yield
                c.cp('act', R(Bh_tok[:]), TL0.ap(), [TL0.buf], [Bh_tok])
                c.cp('dve', R(Kh_tok[:]), TL1.ap(), [TL1.buf], [Kh_tok])
                c.cp('act', R(Vt[:]), TLb.ap(), [TLb.buf], [Vt])
                yield

            def unit_g(d, g, u, hh, b, tl):
                rows = slice(hh * 64, hh * 64 + 64)
                tile = 2 * b + tl
                is_lat = tile >= 2
                lt = tile - 2
                lsl_ = slice(tl * 128, tl * 128 + 128)
                AR, btT, ktT, Vt, Bh_tok, Kh_tok, gC = (g[n_] for n_ in ('AR', 'btT', 'ktT', 'Vt', 'Bh_tok', 'Kh_tok', 'gC'))
                AB1, AB2, XY2, Xs, Us, yt, yt2, Tst = (u[n_] for n_ in ('AB1', 'AB2', 'XY2', 'Xs', 'Us', 'yt', 'yt2', 'Tst'))
                P, Q, W = u['P'], u['Q'], u['W']
                A, B = PS[u['bankA']], PS[u['bankB']]
                At, Bt = A.t, B.t
                ARt = AR[rows, :, lsl_]
                c.mm(At[:, 0:256].rearrange("p (a b) -> p a b", a=2), R(btT[rows, lsl_]), R(ARt), True, True, [btT, AR], [A])
                c.mm(Bt[:, 0:256].rearrange("p (a b) -> p a b", a=2), R(ktT[rows, lsl_]), R(ARt), True, True, [ktT, AR], [B])
                yield
                c.tt('dve', R(AB1[:]), At[:, 0:256], mskA[d][:], ALU.mult, [A, mskA[d]], [AB1])
                c.tt('dve', R(AB2[:]), Bt[:, 0:256], mskB[d][:], ALU.mult, [B, mskB[d]], [AB2])
                yield
                if KTG < 3:
                    return
                c.cp('pool', R(P[0][:]), AB1[:, 0:128], [AB1], [P[0]])
                c.mm(At[:, 0:64], R(AB2[:, 0:128]), R(Vt[:, rows]), True, True, [AB2, Vt], [A])
                c.mm(At[:, 64:128], R(AB2[:, 128:256]), R(Vt[:, rows]), True, True, [AB2, Vt], [A])
                yield
                c.cp('act', XY2[:], At[:, 0:128], [A], [XY2])
                c.mm(Bt[:, 0:128], R(P[0][:]), R(ident_r[:]), True, True, [P[0], ident_r], [B])
                c.tt('pool', R(W[0][:]), ident[:], P[0][:], ALU.subtract, [ident, P[0]], [W[0]])
                yield
                c.cp('act', R(Q[0][:]), Bt[:, 0:128], [B], [Q[0]])
                yield
                for k_ in range(5):
                    a_, b_ = k_ % 2, (k_ + 1) % 2
                    c.mm(At[:, 128:256], R(P[a_][:]), R(Q[a_][:]), True, True, [P[a_], Q[a_]], [A])
                    if k_ < 4:
                        c.mm(Bt[:, 0:128], R(Q[a_][:]), R(P[a_][:]), True, True, [P[a_], Q[a_]], [B])
                    yield
                    c.cp('act', R(Q[b_][:]), At[:, 128:256], [A], [Q[b_]])
                    if k_ < 4:
                        c.cp('act', R(P[b_][:]), Bt[:, 0:128], [B], [P[b_]])
                    yield
                    c.mm(Bt[:, 128:256], R(Q[b_][:]), R(W[a_][:]), True, True, [Q[b_], W[a_]], [B])
                    yield
                    c.tt('dve', R(W[b_][:]), Bt[:, 128:256], W[a_][:], ALU.add, [B, W[a_]], [W[b_]])
                    yield
                Wt = W[1]
                if KTG < 4:
                    return
                for cs in ((0, 64) if d == 0 else (64, 0)):
                    sl_ = slice(cs, cs + 64)
                    chunk = tl * 2 + cs // 64
                    c.mm(At[:, 0:64], R(AR[rows, 0, lsl_]), R(Tst[rows, :]), True, True, [AR, Tst], [A])
                    if is_lat:
                        c.mm(At[:, 64:128], R(AR[rows, 1, lsl_]), R(Tst[rows, :]), True, True, [AR, Tst], [A])
                    yield
                    c.tt('dve', R(Xs[sl_, :]), At[sl_, 0:64], XY2[sl_, 0:64], ALU.add, [A, XY2], [Xs])
                    yield
                    c.mm(Bt[:, 0:64], R(Wt[sl_, :]), R(Xs[sl_, :]), True, True, [Wt, Xs], [B])
                    yield
                    c.cp('act', R(Us[sl_, :]), Bt[sl_, 0:64], [B], [Us])
                    yield
                    c.mm(Bt[:, 64:128], R(Bh_tok[sl_, :]), R(Us[sl_, :]), True, False, [Bh_tok, Us], [B])
                    c.mm(Bt[:, 64:128], R(Kh_tok[sl_, :]), R(Vt[sl_, rows]), False, True, [Kh_tok, Vt], [B])
                    if is_lat:
                        c.mm(At[:, 128:192], R(AB1[sl_, 128:256]), R(Us[sl_, :]), True, True, [AB1, Us], [A])
                    yield
                    c.stt(R(Tst[rows, :]), Tst[rows, :], gC[rows, chunk:chunk + 1], Bt[rows, 64:128], ALU.mult, ALU.add,
                          [Tst, gC, B], [Tst])
                    if is_lat:
                        c.tt('dve', yt[sl_, :], At[sl_, 128:192], XY2[sl_, 64:128], ALU.add, [A, XY2], [yt])
                        if (d == 0) == (lt <= 7):
                            c.tt('dve', y_acc[sl_, lt, rows], At[sl_, 64:128], yt[sl_, :], ALU.add, [A, yt], [y_acc])
                        else:
                            c.tt('dve', yt2[sl_, :], At[sl_, 64:128], yt[sl_, :], ALU.add, [A, yt], [yt2])
                            c.tt('pool', y_acc[sl_, lt, rows], y_acc[sl_, lt, rows], yt2[sl_, :], ALU.add, [y_acc, yt2], [y_acc])
                    yield

            def dir_g(d, hc):
                g = DB[d]
                for hh in range(2):
                    c.ts('dve', R(g['U'][hh]['Tst'][:]), zf[:, 0:64], 0.0, None, ALU.mult, None, [zf], [g['U'][hh]['Tst']])
                border = list(range(NBLK)) if d == 0 else [0] + list(range(NBLK - 1, 0, -1))
                for b in border[:NBS]:
                    yield from block_g(d, g, b, hc)
                    for tl in ((0, 1) if d == 0 else (1, 0)):
                        if KTG < 1:
                            continue
                        yield from tile_g(d, g, b, tl)
                        if KTG < 2:
                            continue
                        yield from par(unit_g(d, g, g['U'][0], 0, b, tl), unit_g(d, g, g['U'][1], 1, b, tl))

            ob_hc = c.sb(esR, [128, 16, 128], BF16, 'ob_hc')
            scr_ob_v = scr_ob.rearrange("(lt p) cc -> p lt cc", p=128)
            scrw = Buf(None, 'scrw')
            for hc in range(NHC):
                proj_mix(hc * 128, rb, None, hc)
                proj_mix(512 + hc * 128, kb, None, 4 + hc)
                proj_mix(1024 + hc * 128, vb, None, 8 + hc)
                c.barrier()
                cs_ = slice(hc * 128, (hc + 1) * 128)
                def prepass_g(par_, hc=hc, cs_=cs_):
                    g_ = DB[par_]
                    icl, icl1, tmpa, opr = g_['icl'], g_['pre'], g_['tmpa'], g_['opr']
                    P0, P3, P1, P2 = (PS[4 * par_ + j] for j in range(4))
                    for b in range(1 + par_, NBLK, 2):
                        t0 = b * BW
                        tsl = slice(t0, t0 + BW)
                        lsl = slice(t0 - CTX, t0 - CTX + BW)
                        c.mm(P0[:, 0:BW], a2b[0:64, cs_], alo[0:64, tsl], True, True, [a2b, alo], [P0])
                        c.mm(P3[:, 0:BW], a2b[64:128, cs_], alo[64:128, tsl], True, True, [a2b, alo], [P3])
                        c.mm(P2[:, 0:BW], g2b[:, cs_], sgl[:, tsl], True, True, [g2b, sgl], [P2])
                        yield
                        c.act(icl[:], P0[:, 0:BW], AF.Sigmoid, [P0, a0], [icl], bias=a0[:, 0, hc:hc + 1])
                        c.act(icl1[:], P3[:, 0:BW], AF.Sigmoid, [P3, a0], [icl1], bias=a0[:, 1, hc:hc + 1])
                        c.cp('act', G1[:, lsl], P2[:, 0:BW], [P2], [G1])
                        yield
                        c.tt('dve', tmpa[:], icl[:], icl1[:], ALU.add, [icl, icl1], [tmpa])
                        c.ts('dve', tmpa[:], tmpa[:], rw[:, 1, hc:hc + 1], oka2[:, hc:hc + 1], ALU.mult, ALU.add, [tmpa, rw, oka2], [tmpa])
                        yield
                        c.tt('dve', tmpa[:], tmpa[:], kb[:, tsl], ALU.mult, [tmpa, kb], [tmpa])
                        c.tt('dve', tmpa[:], tmpa[:], rb[:, tsl], ALU.mult, [tmpa, rb], [tmpa])
                        yield
                        c.ts('dve', R(opr[:]), tmpa[:], rw[:, 2, hc:hc + 1], None, ALU.mult, None, [tmpa, rw], [opr])
                        yield
                        c.mm(P1[:, 0:BW], R(blk_r[:]), R(opr[:]), True, True, [blk_r, opr], [P1])
                        yield
                        c.tt('dve', tmpa[:], P1[:, 0:BW], vb[:, tsl], ALU.mult, [P1, vb], [tmpa])
                        yield
                        c.stt(G2[:, lsl], tmpa[:], rw[:, 4, hc:hc + 1], P2[:, 0:BW], ALU.add, ALU.mult, [tmpa, rw, P2], [G2])
                        yield

                run_threads([prepass_g(0), prepass_g(1)])
                c.barrier()
                run_threads([dir_g(0, hc), dir_g(1, hc)])
                c.barrier()
                def rwkv_out_g(par_, hc=hc, cs_=cs_):
                    gst, yn, obf, otf = gst2[par_], yn2[par_], obf2[par_], otf2[par_]
                    bkA, bkB = PS[par_], PS[2 + par_]
                    pbk = psb(2 + par_)
                    for lt in range(par_, 0 if os.environ.get('K_NOOUT') else 16, 2):
                        for hh in range(2):
                            rows = slice(hh * 64, hh * 64 + 64)
                            c.op('dve', lambda e, hh=hh, rows=rows: e.bn_stats(out=gst[:, hh * 6:(hh + 1) * 6], in_=y_acc[:, lt, rows]), reads=[y_acc], writes=[gst])
                            c.op('dve', lambda e, hh=hh: e.bn_aggr(out=gst[:, 12 + hh * 2:14 + hh * 2], in_=gst[:, hh * 6:(hh + 1) * 6]), reads=[gst], writes=[gst])
                        yield
                        c.ts('dve', gst[:, 16:18], gst[:, 13:16:2], LNX_EPS, None, ALU.add, None, [gst], [gst])
                        yield
                        c.act(gst[:, 16:18], gst[:, 16:18], AF.Sqrt, [gst], [gst])
                        yield
                        c.op('dve', lambda e: e.reciprocal(out=gst[:, 18:20], in_=gst[:, 16:18]), reads=[gst], writes=[gst])
                        for hh in range(2):
                            rows = slice(hh * 64, hh * 64 + 64)
                            c.ts('dve', yn[:, rows], y_acc[:, lt, rows], gst[:, 12 + hh * 2:13 + hh * 2], gst[:, 18 + hh:19 + hh], ALU.subtract, ALU.mult,
                                 [y_acc, gst], [yn])
                        yield
                        c.tr(bkA[:, 0:128], yn[:], ident[:], [yn, ident], [bkA])
                        yield
                        lsl = slice(lt * 128, (lt + 1) * 128)
                        c.stt(otf[:], bkA[:, 0:128], rw[:, 3, hc:hc + 1], G1[:, lsl], ALU.mult, ALU.mult, [bkA, rw, G1], [otf])
                        c.tt('dve', obf[:], otf[:], G2[:, lsl], ALU.add, [otf, G2], [obf])
                        yield
                        c.tr(pbk[:, 0:128], obf[:], identb[:], [obf, identb], [bkB])
                        yield
                        c.cp('act', ob_hc[:, lt, :], pbk[:, 0:128], [bkB], [ob_hc])
                        yield

                run_threads([rwkv_out_g(0), rwkv_out_g(1)])
                for q4 in range(4):
                    c.dma(scr_ob_v[:, q4 * 4:(q4 + 1) * 4, cs_], ob_hc[:, q4 * 4:(q4 + 1) * 4, :], reads=[ob_hc], writes=[scrw])
            c.barrier()

        scrob_buf = scrw
        scrh_buf = Buf(None, 'scr_h_dram')
        esF = ExitStack()
        with esF:
            tT = c.sb(esF, [128, 8, SEQ], BF16, 'tT')
            cwT = c.sb(esF, [32, SEQ], F32, 'cwT')
            with ExitStack() as esY:
                yT = c.sb(esY, [128, 8, SEQ], BF16, 'yT')
                with ExitStack() as esM1:
                    woa = c.sb(esM1, [128, 4, D], BF16, 'woa')
                    wob = c.sb(esM1, [128, 4, D], BF16, 'wob')
                    c.dma(woa[:], w_o_a.rearrange("(cc p) n -> p cc n", p=128), writes=[woa], q='pool')
                    c.dma(wob[:], w_o_b.rearrange("(cc p) n -> p cc n", p=128), writes=[wob], q='pool')
                    uTb = c.sb(esM1, [128, 8, 512], BF16, 'uTb')
                    obTb = c.sb(esM1, [128, 4, 512], BF16, 'obTb')
                    obt = [c.sb(esM1, [128, 512], BF16, 'obt%d' % i) for i in range(2)]
                    wg_ = [c.sb(esM1, [128, 8, 128], BF16, 'wgm%d' % i) for i in range(4)]
                    sga2 = [c.sb(esM1, [128, 512], F32, 'sga%d' % i) for i in range(2)]
                    sgb2 = [c.sb(esM1, [128, 512], F32, 'sgb%d' % i) for i in range(2)]
                    ya2 = [c.sb(esM1, [128, 512], F32, 'ya%d' % i) for i in range(2)]
                    yb2 = [c.sb(esM1, [128, 512], F32, 'yb%d' % i) for i in range(2)]
                    ob_v = scr_ob.rearrange("(w r) cc -> r w cc", r=32)
                    k = 0
                    wn = 0
                    for tb in range(4):
                        c.dma(uTb[:], scr_u[:, :, tb * 512:(tb + 1) * 512], reads=[scru_buf], writes=[uTb])
                        for j in range(4):
                            i = tb * 4 + j
                            o_ = obt[i % 2]
                            c.dma(o_[0:64, :], ob_v[2 * i], reads=[scrob_buf], writes=[o_])
                            c.dma(o_[64:128, :], ob_v[2 * i + 1], reads=[scrob_buf], writes=[o_])
                            pb = psb(5 + i % 2)
                            for hc in range(4):
                                c.tr(pb[:, hc * 128:(hc + 1) * 128], o_[:, hc * 128:(hc + 1) * 128], identb[:], [o_, identb], [PS[5 + i % 2]],
                                     inc=(hc == 3))
                            c.cp('act', obTb[:, :, j * 128:(j + 1) * 128], pb[:, 0:512].rearrange("p (a b) -> p a b", a=4), [PS[5 + i % 2]], [obTb])
                        tsl = slice(tb * 512, (tb + 1) * 512)
                        for fc in range(8):
                            wa_ = wg_[wn % 4]
                            wb_ = wg_[(wn + 1) % 4]
                            wn += 2
                            ga0 = A_COLS + B_COLS + fc * 128
                            c.dma(wa_[:], w_in_v[:, :, ga0:ga0 + 128], writes=[wa_], q='pool')
                            c.dma(wb_[:], w_in_v[:, :, ga0 + D:ga0 + D + 128], writes=[wb_], q='pool')
                            fsl = slice(fc * 128, (fc + 1) * 128)
                            fp_ = fc % 2
                            Q0, Q1, Q2, Q3 = (PS[4 * fp_ + j_] for j_ in range(4))
                            sga, sgb, ya, yb = sga2[fp_], sgb2[fp_], ya2[fp_], yb2[fp_]
                            for kc in range(8):
                                c.mm(Q2[:, :], wa_[:, kc, :], uTb[:, kc, :], kc == 0, kc == 7, [wa_, uTb], [Q2])
                            for kc in range(8):
                                c.mm(Q3[:, :], wb_[:, kc, :], uTb[:, kc, :], kc == 0, kc == 7, [wb_, uTb], [Q3])
                            for cc in range(4):
                                c.mm(Q0[:, :], woa[:, cc, fsl], oaT[:, cc, tsl], cc == 0, cc == 3, [woa, oaT], [Q0])
                            for cc in range(4):
                                c.mm(Q1[:, :], wob[:, cc, fsl], obTb[:, cc, :], cc == 0, cc == 3, [wob, obTb], [Q1])
                            c.act(sga[:], Q2[:, :], AF.Sigmoid, [Q2], [sga])
                            c.act(sgb[:], Q3[:, :], AF.Sigmoid, [Q3], [sgb])
                            c.tt('dve', ya[:], Q0[:, :], sga[:], ALU.mult, [Q0, sga], [ya])
                            c.tt('dve', yb[:], Q1[:, :], sgb[:], ALU.mult, [Q1, sgb], [yb])
                            c.tt('pool', yT[:, fc, tsl], ya[:], yb[:], ALU.add, [ya, yb], [yT])
                    c.barrier()
                if 'd_yT' in T:
                    c.dma(T['d_yT'], yT[:], reads=[yT])
                with ExitStack() as esM2:
                    wout = c.sb(esM2, [128, 8, D], BF16, 'wout')
                    c.dma(wout[:, 0:4, :], w_out.rearrange("(cc p) n -> p cc n", p=128)[:, 0:4, :], writes=[wout], q='pool')
                    c.dma(wout[:, 4:8, :], w_out.rearrange("(cc p) n -> p cc n", p=128)[:, 4:8, :], writes=[wout], q='pool')
                    Bg1 = c.sb(esM2, [128, D], F32, 'Bg1')
                    make_Bg(esM2, Bg1, 16)
                    wr32 = c.sb(esM2, [128, 8, 36], F32, 'wr32')
                    rbb = c.sb(esM2, [128, 36], F32, 'rbb')
                    c.dma(wr32[:], wrt, writes=[wr32])
                    c.dma(rbb[:], rb_bc, writes=[rbb])
                    Xh = [c.sb(esM2, [128, D], F32, 'Xh%d' % i) for i in range(2)]
                    Hh = [c.sb(esM2, [128, D], F32, 'Hh%d' % i) for i in range(2)]
                    hn2 = [c.sb(esM2, [128, D], F32, 'hn%d' % i) for i in range(2)]
                    sqh2 = [c.sb(esM2, [128, D], BF16, 'sqh%d' % i) for i in range(2)]
                    t322 = [c.sb(esM2, [128, 8, 128], F32, 't32%d' % i) for i in range(2)]
                    st2 = [c.sb(esM2, [128, 64], F32, 'st%d' % i) for i in range(2)]
                    lg2 = [c.sb(esM2, [128, 36], F32, 'lg%d' % i) for i in range(2)]
                    tm32 = [c.sb(esM2, [128, 32], F32, 'tm3%d' % i) for i in range(2)]
                    cw2 = [c.sb(esM2, [128, 32], F32, 'cw%d' % i) for i in range(2)]

                    def tile_chain(t):
                        hn, sqh, t32, st, lg, tm3, cw = hn2[t], sqh2[t], t322[t], st2[t], lg2[t], tm32[t], cw2[t]
                        B = [PS[4 * t + j] for j in range(4)]
                        X_, H_ = Xh[t], Hh[t]
                        for i in range(t, 16, 2):
                            c.dma(X_[:], x[i * 128:(i + 1) * 128, :], writes=[X_])
                            for nh in range(2):
                                bank = B[nh]
                                for fc in range(8):
                                    c.mm(bank[:, :], yT[:, fc, i * 128:(i + 1) * 128], wout[:, fc, nh * 512:(nh + 1) * 512], fc == 0, fc == 7, [yT, wout], [bank])
                                hs = slice(nh * 512, (nh + 1) * 512)
                                c.tt('dve', H_[:, hs], bank[:, :], Bg1[:, hs], ALU.mult, [bank, Bg1], [H_])
                                yield
                            c.tt('pool', H_[:], H_[:], X_[:], ALU.add, [H_, X_], [H_])
                            yield
                            c.dma(scr_h[i * 128:(i + 1) * 128, :], H_[:], reads=[H_], writes=[scrh_buf])
                            c.act(sqh[:], H_[:], AF.Square, [H_], [sqh, st], accum_out=st[:, 0:1])
                            yield
                            c.ts('dve', st[:, 1:2], st[:, 0:1], 1.0 / D, EPS, ALU.mult, ALU.add, [st], [st])
                            c.act(st[:, 2:3], st[:, 1:2], AF.Sqrt, [st], [st])
                            yield
                            c.op('dve', lambda e: e.reciprocal(out=st[:, 3:4], in_=st[:, 2:3]), reads=[st], writes=[st])
                            c.ts('dve', hn[:], H_[:], st[:, 3:4], None, ALU.mult, None, [H_, st], [hn])
                            yield
                            for fc in range(8):
                                bank = B[2 + fc // 4]
                                c.tr(bank[:, (fc % 4) * 128:(fc % 4 + 1) * 128], hn[:, fc * 128:(fc + 1) * 128], ident[:], [hn, ident], [bank],
                                     inc=(fc % 4 == 3))
                            yield
                            for fc in range(8):
                                bank = B[2 + fc // 4]
                                i_ = bank[:, (fc % 4) * 128:(fc % 4 + 1) * 128]
                                c.ts('dve', t32[:, fc, :], i_, A2g[:, fc, 0:1], mT[:, 24 + fc, 0:1], ALU.mult, ALU.add, [bank, A2g, mT], [t32])
                                c.cp('act', tT[:, fc, i * 128:(i + 1) * 128], t32[:, fc, :], [t32], [tT])
                                if fc % 4 == 3:
                                    yield
                            for kc in range(8):
                                c.mm(B[0][:, 0:36], t32[:, kc, :], wr32[:, kc, :], kc == 0, kc == 7, [t32, wr32], [B[0]])
                            yield
                            c.tt('dve', lg[:], B[0][:, 0:36], rbb[:], ALU.add, [B[0], rbb], [lg])
                            c.op('dve', lambda e: e.tensor_reduce(out=st[:, 8:9], in_=lg[:, 0:4], axis=AX.X, op=ALU.max), reads=[lg], writes=[st])
                            c.ts('dve', st[:, 9:10], st[:, 8:9], -1.0, None, ALU.mult, None, [st], [st])
                            yield
                            c.act(st[:, 16:20], lg[:, 0:4], AF.Exp, [lg, st], [st], bias=st[:, 9:10], accum_out=st[:, 10:11])
                            yield
                            c.op('dve', lambda e: e.reciprocal(out=st[:, 11:12], in_=st[:, 10:11]), reads=[st], writes=[st])
                            c.ts('dve', st[:, 20:24], lg[:, 0:4], st[:, 8:9], None, ALU.is_ge, None, [lg, st], [st])
                            c.tt('dve', tm3[:].rearrange("p (g e) -> p g e", g=4), lg[:, 4:36].rearrange("p (g e) -> p g e", g=4),
                                 st[:, 20:24].unsqueeze(2).to_broadcast([128, 4, 8]), ALU.mult, [lg, st], [tm3])
                            yield
                            c.op('dve', lambda e: e.tensor_reduce(out=st[:, 24:32], in_=tm3[:].rearrange("p (g e) -> p e g", g=4), axis=AX.X, op=ALU.add),
                                 reads=[tm3], writes=[st])
                            c.op('dve', lambda e: e.max(out=st[:, 32:40], in_=st[:, 24:32]), reads=[st], writes=[st])
                            c.tt('dve', st[:, 40:41], st[:, 33:34], st[:, 32:33], ALU.subtract, [st], [st])
                            yield
                            c.act(st[:, 41:42], st[:, 40:41], AF.Exp, [st], [st])
                            yield
                            c.ts('dve', st[:, 42:43], st[:, 41:42], 1.0, None, ALU.add, None, [st], [st])
                            c.op('dve', lambda e: e.reciprocal(out=st[:, 43:44], in_=st[:, 42:43]), reads=[st], writes=[st])
                            c.tt('dve', st[:, 44:45], st[:, 43:44], st[:, 11:12], ALU.mult, [st], [st])
                            yield
                            c.tt('dve', st[:, 45:46], st[:, 44:45], st[:, 41:42], ALU.mult, [st], [st])
                            c.tt('dve', st[:, 46:47], st[:, 44:45], st[:, 45:46], ALU.subtract, [st], [st])
                            c.ts('dve', st[:, 48:56], st[:, 24:32], st[:, 32:33], st[:, 46:47], ALU.is_ge, ALU.mult, [st], [st])
                            yield
                            c.ts('dve', st[:, 56:64], st[:, 24:32], st[:, 33:34], st[:, 45:46], ALU.is_ge, ALU.mult, [st], [st])
                            c.tt('dve', st[:, 48:56], st[:, 48:56], st[:, 56:64], ALU.add, [st], [st])
                            c.tt('dve', cw[:].rearrange("p (g e) -> p g e", g=4), st[:, 20:24].unsqueeze(2).to_broadcast([128, 4, 8]),
                                 st[:, 48:56].unsqueeze(1).to_broadcast([128, 4, 8]), ALU.mult, [st], [cw])
                            yield
                            c.tr(B[1][0:32, 0:128], cw[:], ident[:], [cw, ident], [B[1]])
                            yield
                            c.cp('act', R(cwT[:, i * 128:(i + 1) * 128]), B[1][0:32, 0:128], [B[1]], [cwT])
                            yield

                    run_threads([tile_chain(0), tile_chain(1)])
                    c.barrier()
            if 'd_tT' in T:
                c.dma(T['d_tT'], tT[:], reads=[tT])
            if 'd_cwT' in T:
                c.dma(T['d_cwT'], cwT[:], reads=[cwT])

            with ExitStack() as esE:
                moe_acc = c.sb(esE, [128, 16, D], F32, 'moe_acc')
                macc = [[Buf(None, 'macc%d_%d' % (t_, n_)) for n_ in range(2)] for t_ in range(16)]
                evt = [c.sb(esE, [128, 512], F32, 'evt%d' % i) for i in range(3)]
                wgb = [c.sb(esE, [128, 8, 256], BF16, 'wgb%d' % i) for i in range(2)]
                wub = [c.sb(esE, [128, 8, 256], BF16, 'wub%d' % i) for i in range(2)]
                wdb = [c.sb(esE, [128, 2, D], BF16, 'wdb%d' % i) for i in range(2)]
                selt = [c.sb(esE, [32, 128], F32, 'selt%d' % i) for i in range(2)]
                sg_ = [c.sb(esE, [128, 512], F32, 'sg%d' % i) for i in range(2)]
                hu_ = [c.sb(esE, [128, 512], F32, 'hu%d' % i) for i in range(2)]
                hid = [c.sb(esE, [128, 2, 512], BF16, 'hid%d' % i) for i in range(2)]
                NEXP = int(os.environ.get('K_NEXP', '32'))
                def moe_gu(e_, tg, k):
                    wg, wu, wd = wgb[e_ % 2], wub[e_ % 2], wdb[e_ % 2]
                    se = selt[e_ % 2]
                    if tg == 0:
                        c.dma(wg[:], moe_wg[e_].rearrange("(kc p) f -> p kc f", p=128), writes=[wg], q='pool')
                        c.dma(wu[:], moe_wu[e_].rearrange("(kc p) f -> p kc f", p=128), writes=[wu], q='pool')
                        c.dma(wd[:], moe_wd[e_].rearrange("(fc p) n -> p fc n", p=128), writes=[wd], q='pool')
                        c.ts('dve', R(se[:]), onesf[0:32, :], ident[0:32, e_:e_ + 1], None, ALU.mult, None, [onesf, ident], [se])
                    tsl = slice(tg * 512, (tg + 1) * 512)
                    hd = hid[k % 2]
                    c.mm(PS[4][:, :], R(se[:]), R(cwT[:, tsl]), True, True, [se, cwT], [PS[4]])
                    for f2 in range(2):
                        fs = slice(f2 * 128, (f2 + 1) * 128)
                        for kc in range(8):
                            c.mm(PS[f2][:, :], wg[:, kc, fs], tT[:, kc, tsl], kc == 0, kc == 7, [wg, tT], [PS[f2]])
                        for kc in range(8):
                            c.mm(PS[2 + f2][:, :], wu[:, kc, fs], tT[:, kc, tsl], kc == 0, kc == 7, [wu, tT], [PS[2 + f2]])
                        c.act(sg_[f2][:], PS[f2][:, :], AF.Silu, [PS[f2]], [sg_[f2]])
                        c.tt('dve', hu_[f2][:], PS[2 + f2][:, :], sg_[f2][:], ALU.mult, [PS[2 + f2], sg_[f2]], [hu_[f2]])
                        c.tt('dve', hd[:, f2, :], PS[4][:, :], hu_[f2][:], ALU.mult, [PS[4], hu_[f2]], [hd])

                def moe_dn(e_, tg, k):
                    wd = wdb[e_ % 2]
                    hd = hid[k % 2]
                    for tt_ in range(4):
                        tile = tg * 4 + tt_
                        for nh in range(2):
                            bank = PS[5 + (tt_ * 2 + nh) % 3]
                            for f2 in range(2):
                                c.mm(bank[:, :], hd[:, f2, tt_ * 128:(tt_ + 1) * 128], wd[:, f2, nh * 512:(nh + 1) * 512], f2 == 0, f2 == 1,
                                     [hd, wd], [bank])
                            hs = slice(nh * 512, (nh + 1) * 512)
                            ma = macc[tile][nh]
                            gi = tt_ * 2 + nh
                            if e_ == 0:
                                c.cp('act', moe_acc[:, tile, hs], bank[:, :], [bank], [ma])
                            elif gi % 2 == 0:
                                c.tt('dve', moe_acc[:, tile, hs], bank[:, :], moe_acc[:, tile, hs], ALU.add, [bank, ma], [ma])
                            else:
                                ev = evt[(gi // 2) % 3]
                                c.cp('act', ev[:], bank[:, :], [bank], [ev])
                                c.tt('pool', moe_acc[:, tile, hs], moe_acc[:, tile, hs], ev[:], ALU.add, [ma, ev], [ma])

                its = [(e_, tg) for e_ in range(NEXP) for tg in range(4)]
                for k, (e_, tg) in enumerate(its):
                    moe_gu(e_, tg, k)
                    if k > 0:
                        moe_dn(its[k - 1][0], its[k - 1][1], k - 1)
                moe_dn(its[-1][0], its[-1][1], len(its) - 1)
                Bg2 = c.sb(esE, [128, D], F32, 'Bg2')
                make_Bg(esE, Bg2, 40)
                gfin = c.sb(esE, [128, D], F32, 'gfin')
                c.dma(gfin[:], gfin_bc, writes=[gfin])
                Hf = [c.sb(esE, [128, D], F32, 'Hf%d' % i) for i in range(2)]
                sf = [c.sb(esE, [128, 4], F32, 'sf%d' % i) for i in range(2)]
                c.barrier()
                Hm = [Buf(wgb[i].t.bitcast(F32).rearrange("p a b -> p (a b)"), 'Hm%d' % i) for i in range(2)]
                sqf2 = [Buf(wub[i].t.rearrange("p a b -> p (a b)"), 'sqf%d' % i) for i in range(2)]

                def fin_g(par_):
                    H_, s_, Hm_, sq_ = Hf[par_], sf[par_], Hm[par_], sqf2[par_]
                    for i in range(par_, 16, 2):
                        c.dma(H_[:], scr_h[i * 128:(i + 1) * 128, :], reads=[scrh_buf], writes=[H_])
                        c.tt('dve', Hm_[:], moe_acc[:, i, :], Bg2[:], ALU.mult, [macc[i][0], macc[i][1], Bg2], [Hm_])
                        yield
                        c.tt('pool', H_[:], H_[:], Hm_[:], ALU.add, [H_, Hm_], [H_])
                        yield
                        c.act(sq_[:, 0:D], H_[:], AF.Square, [H_], [sq_, s_], accum_out=s_[:, 0:1])
                        yield
                        c.ts('dve', s_[:, 1:2], s_[:, 0:1], 1.0 / D, EPS, ALU.mult, ALU.add, [s_], [s_])
                        yield
                        c.act(s_[:, 2:3], s_[:, 1:2], AF.Sqrt, [s_], [s_])
                        yield
                        c.op('dve', lambda e, s_=s_: e.reciprocal(out=s_[:, 3:4], in_=s_[:, 2:3]), reads=[s_], writes=[s_])
                        c.stt(H_[:], H_[:], s_[:, 3:4], gfin[:], ALU.mult, ALU.mult, [H_, s_, gfin], [H_])
                        yield
                        c.dma(out[i * 128:(i + 1) * 128, :], H_[:], reads=[H_])
                        yield

                run_threads([fin_g(0), fin_g(1)])
                c.barrier()

        c.finish()
        print("ninstr", c.ninstr, {k_: v for k_, v in c.cnt.items()})
    return nc


def prep_inputs(inp):
    f = lambda a: np.ascontiguousarray(a, dtype=np.float32)
    fm = lambda v: f(np.asarray(v).reshape(-1, 128).T)
    shared = {
        "ada_w": f(inp["ada_w"][0]),
        "ada_bT": fm(inp["ada_b"][0]),
        "gmixT": fm(inp["norm_mix_g"][0]),
        "gffnT": fm(inp["norm_ffn_g"][0]),
        "gfin_bc": f(np.broadcast_to(inp["final_norm_g"][None, :], (128, D))),
        "w_in": f(inp["w_in"][0]),
        "convT": f(inp["gdn_conv"][0].T.reshape(12, 128, 5).transpose(1, 0, 2)),
        "alog_bc": f(np.broadcast_to(inp["gdn_a_log"][0].reshape(1, 1, 8), (128, 18, 8))),
        "dtb_bc": f(np.broadcast_to(inp["gdn_dt_bias"][0].reshape(1, 1, 8), (128, 18, 8))),
        "onormT": f(inp["gdn_onorm_g"][0].reshape(128, 1)),
        "muT": fm(inp["rwkv_mu"][0]),
        "w0T": f(inp["rwkv_w0"][0].reshape(2, 4, 128).transpose(2, 0, 1)),
        "a0T": f(inp["rwkv_a0"][0].reshape(2, 4, 128).transpose(2, 0, 1)),
        "w2m": f(inp["rwkv_w2"][0].reshape(128, 512)),
        "a2m": f(inp["rwkv_a2"][0].reshape(128, 512)),
        "g2m": f(inp["rwkv_g2"][0]),
        "w_o_a": f(inp["w_o_a"][0]),
        "w_o_b": f(inp["w_o_b"][0]),
        "w_out": f(inp["w_out"][0]),
        "wrt": f(np.concatenate([inp["router_grp"][0], inp["router_exp"][0]], axis=1).reshape(8, 128, 36).transpose(1, 0, 2)),
        "rb_bc": f(np.broadcast_to(np.concatenate([inp["router_grp_b"][0], inp["router_exp_b"][0]])[None, :], (128, 36))),
        "moe_wg": f(inp["moe_w_gate"][0].reshape(32, D, 256)),
        "moe_wu": f(inp["moe_w_up"][0].reshape(32, D, 256)),
        "moe_wd": f(inp["moe_w_down"][0].reshape(32, 256, D)),
        "rwv": f(np.stack([fm(inp["rwkv_k_k"][0]), fm(inp["rwkv_k_a"][0]), fm(inp["rwkv_r_k"][0].reshape(-1)),
                           fm(inp["rwkv_lnx_g"][0]), fm(inp["rwkv_lnx_b"][0])], axis=1)),
    }
    maps = []
    for b in range(NCORES):
        m = dict(shared)
        m["x"] = f(inp["x"][b])
        m["ctx"] = f(inp["ctx"][b])
        m["cT"] = f(np.stack([fm(inp["c"][b]), fm(inp["c_ctx"])], axis=-1))
        maps.append(m)
    return maps


def kernel(**inputs):
    maps = prep_inputs(inputs)
    nc = build()
    res = run_bass_kernel_spmd(nc, maps, core_ids=list(range(NCORES)))
    return np.stack([np.asarray(r["out"]) for r in res.results], axis=0).astype(np.float32)
```

```python
import os
import numpy as np
import concourse.bass as bass
import concourse.mybir as mybir
from concourse.bass_utils import run_bass_kernel_spmd
from concourse.alu_op_type import AluOpType as ALU
from contextlib import ExitStack

F32 = mybir.dt.float32
F32R = mybir.dt.float32r
BF16 = mybir.dt.bfloat16
AF = mybir.ActivationFunctionType
AX = mybir.AxisListType

NCORES = 8
D = 1024
SEQ = 2048
CTX = 256
NTOK = SEQ + CTX
IN_COLS = 6032
A_COLS = 2064
B_COLS = 1920
EPS = 1e-6
LNX_EPS = 1e-5 * 64


class Buf:
    def __init__(self, t, name, psum=False):
        self.t = t
        self.name = name
        self.lw = None
        self.rd = {}
        self.psum = psum
        self.bankrd = None

    def __getitem__(self, idx):
        return self.t[idx]


class Ctx:
    ENG = ['pe', 'dve', 'act', 'pool', 'sp']
    NDMA = 8

    def __init__(self, nc, es):
        self.nc = nc
        self.e = {'pe': nc.tensor, 'dve': nc.vector, 'act': nc.scalar, 'pool': nc.gpsimd, 'sp': nc.sync}
        self.sem = {}
        self.cnt = {}
        for n in self.ENG:
            self.sem[n] = es.enter_context(nc.semaphore('s_' + n))
            self.cnt[n] = 0
        for i in range(self.NDMA):
            n = 'd%d' % i
            self.sem[n] = es.enter_context(nc.semaphore('s_' + n))
            self.cnt[n] = 0
        self.dma_rr = 0
        self.waited = {n: {} for n in self.ENG}
        self.nbuf = 0
        self.ninstr = 0

    def sb(self, es, shape, dt=F32, name=None):
        self.nbuf += 1
        name = (name or 'b') + '_%d' % self.nbuf
        t = es.enter_context(self.nc.sbuf_tensor(name, list(shape), dt))
        return Buf(t, name)

    def ps(self, es, shape, dt=F32, name=None):
        self.nbuf += 1
        name = (name or 'p') + '_%d' % self.nbuf
        t = es.enter_context(self.nc.psum_tensor(name, list(shape), dt))
        return Buf(t, name, psum=True)

    def view(self, buf, name='v'):
        self.nbuf += 1
        return Buf(buf.t, name + '_%d' % self.nbuf)

    def _deps(self, reads, writes):
        deps = {}

        def add(k, v):
            if v > deps.get(k, 0):
                deps[k] = v
        for b in reads:
            if b.lw:
                add(*b.lw)
            if b.psum:
                for k, v in b.rd.items():
                    add(k, v)
                if b.bankrd is not None:
                    for k, v in b.bankrd.items():
                        add(k, v)
        for b in writes:
            if b.lw:
                add(*b.lw)
            for k, v in b.rd.items():
                add(k, v)
        return deps

    def _wait(self, E, deps):
        eng = self.e[E]
        w = self.waited[E]
        nw = 0
        for k, v in deps.items():
            if k == E and E == 'pe' and v > self.cnt['pe']:
                continue
            if w.get(k, 0) >= v:
                continue
            eng.wait_ge(self.sem[k], v)
            nw += 1
            w[k] = v

    def op(self, E, fn, reads=(), writes=(), inc=True):
        deps = self._deps(reads, writes)
        self._wait(E, deps)
        ins = fn(self.e[E])
        self.ninstr += 1
        if inc:
            self.cnt[E] += 1
            ins.then_inc(self.sem[E], 1)
            cval = self.cnt[E]
        else:
            cval = self.cnt[E] + 1
        for b in writes:
            b.lw = (E, cval)
            b.rd = {}
        for b in reads:
            if b not in writes:
                b.rd[E] = max(b.rd.get(E, 0), cval)
            if b.bankrd is not None and E != 'pe':
                b.bankrd[E] = max(b.bankrd.get(E, 0), cval)
        return ins

    def dma(self, out, in_, reads=(), writes=(), q='sp', **kw):
        slot = 'd%d' % self.dma_rr
        self.dma_rr = (self.dma_rr + 1) % self.NDMA
        deps = self._deps(reads, writes)
        if self.cnt[slot] > 0:
            deps[slot] = max(deps.get(slot, 0), self.cnt[slot])
        self._wait(q, deps)
        ins = self.e[q].dma_start(out=out, in_=in_, **kw)
        self.ninstr += 1
        self.cnt[slot] += 16
        ins.then_inc(self.sem[slot], 16)
        cval = self.cnt[slot]
        for b in writes:
            b.lw = (slot, cval)
            b.rd = {}
        for b in reads:
            b.rd[slot] = max(b.rd.get(slot, 0), cval)
        return ins

    def barrier(self):
        for E in self.ENG:
            deps = {k: v for k, v in self.cnt.items() if v > 0 and k != E}
            self._wait(E, deps)

    def finish(self):
        for k in self.sem:
            if k.startswith('d') and self.cnt[k] > 0:
                self.e['sp'].wait_ge(self.sem[k], self.cnt[k])

    def mm(self, out, lhsT, rhs, start, stop, reads, writes, inc=None):
        if inc is None:
            inc = stop
        return self.op('pe', lambda e: e.matmul(out, lhsT=lhsT, rhs=rhs, start=start, stop=stop),
                       reads=reads, writes=writes, inc=inc)

    def tr(self, out, in_, ident, reads, writes, inc=True):
        return self.op('pe', lambda e: e.transpose(out=out, in_=in_, identity=ident), reads=reads, writes=writes, inc=inc)

    def act(self, out, in_, func, reads, writes, E='act', **kw):
        return self.op('act', lambda e: e.activation(out=out, in_=in_, func=func, **kw), reads=reads, writes=writes)

    def ts(self, E, out, in0, s1, s2, op0, op1, reads, writes):
        if op1 is None:
            return self.op(E, lambda e: e.tensor_scalar(out=out, in0=in0, scalar1=s1, scalar2=None, op0=op0), reads=reads, writes=writes)
        return self.op(E, lambda e: e.tensor_scalar(out=out, in0=in0, scalar1=s1, scalar2=s2, op0=op0, op1=op1), reads=reads, writes=writes)

    def tt(self, E, out, in0, in1, op, reads, writes):
        return self.op(E, lambda e: e.tensor_tensor(out=out, in0=in0, in1=in1, op=op), reads=reads, writes=writes)

    def stt(self, out, in0, scalar, in1, op0, op1, reads, writes):
        return self.op('dve', lambda e: e.scalar_tensor_tensor(out=out, in0=in0, scalar=scalar, in1=in1, op0=op0, op1=op1),
                       reads=reads, writes=writes)

    def cp(self, E, out, in_, reads, writes):
        if E == 'act':
            return self.op('act', lambda e: e.copy(out=out, in_=in_), reads=reads, writes=writes)
        return self.op(E, lambda e: e.tensor_copy(out=out, in_=in_), reads=reads, writes=writes)


def R(ap):
    return ap.bitcast(F32R)


def build(dbg=(), stage=99):
    nc = bass.Bass("TRN2", target_bir_lowering=False)
    T = {}

    def din(name, shape, dt=F32):
        T[name] = nc.dram_tensor(name, list(shape), dt, kind="ExternalInput").ap()
        return T[name]

    def dout(name, shape, dt=F32):
        T[name] = nc.dram_tensor(name, list(shape), dt, kind="ExternalOutput").ap()
        return T[name]

    x = din("x", [SEQ, D])
    ctx = din("ctx", [CTX, D])
    cT = din("cT", [128, 8, 2])
    ada_w = din("ada_w", [D, 6 * D])
    ada_bT = din("ada_bT", [128, 48])
    gmixT = din("gmixT", [128, 8])
    gffnT = din("gffnT", [128, 8])
    gfin_bc = din("gfin_bc", [128, D])
    w_in = din("w_in", [D, IN_COLS])
    convT = din("convT", [128, 12, 5])
    alog_bc = din("alog_bc", [128, 18, 8])
    dtb_bc = din("dtb_bc", [128, 18, 8])
    onormT = din("onormT", [128, 1])
    muT = din("muT", [128, 15])
    w0T = din("w0T", [128, 2, 4])
    a0T = din("a0T", [128, 2, 4])
    w2m = din("w2m", [128, 512])
    a2m = din("a2m", [128, 512])
    g2m = din("g2m", [128, 512])
    rwv = din("rwv", [128, 5, 4])
    scr_ob = nc.dram_tensor("scr_ob", [SEQ, 512], BF16, kind="Internal").ap()
    scr_h = nc.dram_tensor("scr_h", [SEQ, D], F32, kind="Internal").ap()
    scr_u = nc.dram_tensor("scr_u", [128, 8, SEQ], BF16, kind="Internal").ap()
    w_o_a = din("w_o_a", [512, D])
    w_o_b = din("w_o_b", [512, D])
    w_out = din("w_out", [D, D])
    wrt = din("wrt", [128, 8, 36])
    rb_bc = din("rb_bc", [128, 36])
    moe_wg = din("moe_wg", [32, 128, 8, 256])
    moe_wu = din("moe_wu", [32, 128, 8, 256])
    moe_wd = din("moe_wd", [32, 128, 2, D])
    out = dout("out", [SEQ, D])
    for name, shape, dt in dbg:
        dout(name, shape, dt)

    with ExitStack() as es:
        c = Ctx(nc, es)
        PS = [c.ps(es, [128, 512], F32, 'ps%d' % i) for i in range(8)]

        def psb(i):
            return PS[i][:].bitcast(BF16)

        bank_reads = [dict() for _ in range(8)]

        class Reg:
            def __init__(self, bank, c0, n, name):
                self.bank, self.c0, self.n = bank, c0, n
                self.buf = PS[bank]

            def ap(self, lo=0, hi=None, rows=slice(None)):
                hi = self.n if hi is None else hi
                return PS[self.bank].t[rows, self.c0 + lo:self.c0 + hi]

        def run_threads(gens):
            gens = list(gens)
            while gens:
                for g_ in list(gens):
                    try:
                        next(g_)
                    except StopIteration:
                        gens.remove(g_)

        def par(*gens):
            gens = list(gens)
            while gens:
                for g_ in list(gens):
                    try:
                        next(g_)
                    except StopIteration:
                        gens.remove(g_)
                        continue
                    yield

        ident = c.sb(es, [128, 128], F32, 'ident')
        identb = c.sb(es, [128, 128], BF16, 'identb')
        onesf = c.sb(es, [128, 128], F32, 'onesf')
        c.op('pool', lambda e: e.memset(ident[:], 0.0), writes=[ident])
        c.op('pool', lambda e: e.affine_select(out=ident[:], in_=ident[:], pattern=[[-1, 128]], compare_op=ALU.not_equal,
                                                fill=1.0, base=0, channel_multiplier=1), reads=[ident], writes=[ident])
        c.cp('dve', identb[:], ident[:], [ident], [identb])
        c.op('pool', lambda e: e.memset(onesf[:], 1.0), writes=[onesf])
        onesb = c.sb(es, [128, 128], BF16, 'onesb')
        c.cp('dve', onesb[:], onesf[:], [onesf], [onesb])
        ones_r = c.sb(es, [128, 128], F32, 'ones_r')
        nones_r = c.sb(es, [128, 128], F32, 'nones_r')
        ident_r = c.sb(es, [128, 128], F32, 'ident_r')
        c.cp('dve', R(ones_r[:]), onesf[:], [onesf], [ones_r])
        c.ts('dve', R(nones_r[:]), onesf[:], -1.0, None, ALU.mult, None, [onesf], [nones_r])
        c.cp('dve', R(ident_r[:]), ident[:], [ident], [ident_r])
        blk = c.sb(es, [128, 128], F32, 'blk')
        c.op('pool', lambda e: e.memset(blk[:], 0.0), writes=[blk])
        c.op('pool', lambda e: e.memset(blk[0:64, 0:64], 1.0), reads=[blk], writes=[blk])
        c.op('pool', lambda e: e.memset(blk[64:128, 64:128], 1.0), reads=[blk], writes=[blk])
        incl = [c.sb(es, [128, 128], F32, 'incl%d' % d) for d in range(2)]
        strict = [c.sb(es, [128, 128], F32, 'strict%d' % d) for d in range(2)]
        incl_r = [c.sb(es, [128, 128], F32, 'inclr%d' % d) for d in range(2)]
        negm_r = [c.sb(es, [128, 128], F32, 'negm%d' % d) for d in range(2)]
        blk_r = c.sb(es, [128, 128], F32, 'blk_r')
        sel_r = [c.sb(es, [128, 128], F32, 'sel%d' % k_) for k_ in range(2)]
        notI = c.sb(es, [128, 128], F32, 'notI')
        for d in range(2):
            pat = [[1, 128]] if d == 0 else [[-1, 128]]
            cm = -1 if d == 0 else 1
            c.op('pool', lambda e, d=d, pat=pat, cm=cm: e.affine_select(out=incl[d][:], in_=blk[:], pattern=pat, compare_op=ALU.is_ge,
                                                                        fill=0.0, base=0, channel_multiplier=cm), reads=[blk], writes=[incl[d]])
            c.op('pool', lambda e, d=d, pat=pat, cm=cm: e.affine_select(out=strict[d][:], in_=blk[:], pattern=pat, compare_op=ALU.is_gt,
                                                                        fill=0.0, base=0, channel_multiplier=cm), reads=[blk], writes=[strict[d]])
            c.cp('dve', R(incl_r[d][:]), incl[d][:], [incl[d]], [incl_r[d]])
            c.ts('dve', R(negm_r[d][:]), incl[d][:], 1.0e5, -1.0e5, ALU.mult, ALU.add, [incl[d]], [negm_r[d]])
        c.cp('dve', R(blk_r[:]), blk[:], [blk], [blk_r])
        c.ts('dve', notI[:], ident[:], -1.0, 1.0, ALU.mult, ALU.add, [ident], [notI])
        zf = c.sb(es, [128, 128], F32, 'zf')
        c.op('pool', lambda e: e.memset(zf[:], 0.0), writes=[zf])
        for k_ in range(2):
            c.cp('dve', R(sel_r[k_][:]), zf[:], [zf], [sel_r[k_]])
            c.cp('dve', R(sel_r[k_][k_ * 64:(k_ + 1) * 64, :]), onesf[k_ * 64:(k_ + 1) * 64, :], [onesf, sel_r[k_]], [sel_r[k_]])

        mT = c.sb(es, [128, 48, 2], F32, 'mT')
        A1g = c.sb(es, [128, 8, 2], F32, 'A1g')
        A2g = c.sb(es, [128, 8, 2], F32, 'A2g')
        oaT = c.sb(es, [128, 4, SEQ], BF16, 'oaT')

        with ExitStack() as es1:
            sT = c.sb(es1, [128, 8, 2], F32, 'sT')
            abT = c.sb(es1, [128, 48], F32, 'abT')
            gm = c.sb(es1, [128, 8], F32, 'gm')
            gf = c.sb(es1, [128, 8], F32, 'gf')
            c.dma(sT[:], cT, writes=[sT])
            c.dma(abT[:], ada_bT, writes=[abT])
            c.dma(gm[:], gmixT, writes=[gm])
            c.dma(gf[:], gffnT, writes=[gf])
            c.act(sT[:], sT[:], AF.Silu, [sT], [sT])
            Wb = [c.sb(es1, [128, 8, 512], F32, 'adaw%d' % i) for i in range(4)]
            ada_v = ada_w.rearrange("(kc p) n -> p kc n", p=128)
            for blk in range(12):
                wb = Wb[blk % 4]
                c.dma(wb[:], ada_v[:, :, blk * 512:(blk + 1) * 512], writes=[wb], q=('sp' if blk % 2 == 0 else 'act'))
                for mc in range(4):
                    col = (blk * 4 + mc) * 2
                    for kc in range(8):
                        c.mm(PS[0][:, col:col + 2], wb[:, kc, mc * 128:(mc + 1) * 128], sT[:, kc, :],
                             kc == 0, kc == 7, [wb, sT], [PS[0]])
            pv = PS[0][:, 0:96].rearrange("p (m s) -> p m s", s=2)
            for s in range(2):
                c.tt('dve', mT[:, :, s], pv[:, :, s], abT[:], ALU.add, [PS[0], abT], [mT])
            for s in range(2):
                c.stt(A1g[:, :, s], mT[:, 8:16, s], 1.0, gm[:], ALU.add, ALU.mult, [mT, gm], [A1g])
                c.stt(A2g[:, :, s], mT[:, 32:40, s], 1.0, gf[:], ALU.add, ALU.mult, [mT, gf], [A2g])
            c.barrier()

        def make_Bg(es_, Bg, base):
            dg = [c.sb(es_, [128, 128], F32, 'dg%d' % i) for i in range(2)]
            for fc in range(8):
                d_ = dg[fc % 2]
                c.ts('dve', d_[:], ident[:], mT[:, base + fc, 0:1], None, ALU.mult, None, [ident, mT], [d_])
                bank = PS[1 + fc // 4]
                c.mm(bank[:, (fc % 4) * 128:(fc % 4 + 1) * 128], onesf[:], d_[:], True, True, [onesf, d_], [bank])
            c.cp('act', Bg[:, 0:512], PS[1][:], [PS[1]], [Bg])
            c.cp('act', Bg[:, 512:1024], PS[2][:], [PS[2]], [Bg])

        scru_buf = Buf(None, 'scr_u_dram')

        def make_uT_g(srcs, s, dst, tok0, Ag, shift_base, bufs, k):
            X = bufs['X'][k % 4]
            xnb = bufs['xnb'][k % 2]
            ss = bufs['ss'][k % 2]
            sq_ = bufs['sq'][k % 2]
            for (p0, p1, ap) in srcs:
                c.dma(X[p0:p1, :], ap, writes=[X], q=('sp' if k % 2 == 0 else 'pool'))
            c.act(sq_[:], X[:], AF.Square, [X], [sq_, ss], accum_out=ss[:, 0:1])
            yield
            c.ts('dve', ss[:, 1:2], ss[:, 0:1], 1.0 / D, EPS, ALU.mult, ALU.add, [ss], [ss])
            yield
            c.act(ss[:, 2:3], ss[:, 1:2], AF.Sqrt, [ss], [ss])
            yield
            c.op('dve', lambda e: e.reciprocal(out=ss[:, 3:4], in_=ss[:, 2:3]), reads=[ss], writes=[ss])
            c.ts('dve', xnb[:], X[:], ss[:, 3:4], None, ALU.mult, None, [X, ss], [xnb])
            yield
            bank = PS[3 + k % 2]
            pb = psb(3 + k % 2)
            for fc in range(8):
                c.tr(pb[:, fc * 128:(fc + 1) * 128], xnb[:, fc * 128:(fc + 1) * 128], identb[:], [xnb, identb], [bank],
                     inc=(fc == 7))
            yield
            for fc in range(8):
                o_ = dst[:, fc, tok0:tok0 + 128]
                i_ = pb[:, fc * 128:(fc + 1) * 128]
                if fc % 2 == 0:
                    c.ts('dve', o_, i_, Ag[:, fc, s:s + 1], mT[:, shift_base + fc, s:s + 1], ALU.mult, ALU.add,
                         [bank, Ag, mT], [dst])
                else:
                    c.act(o_, i_, AF.Identity, [bank, Ag, mT], [dst], scale=Ag[:, fc, s:s + 1],
                          bias=mT[:, shift_base + fc, s:s + 1])
                if fc % 4 == 3:
                    yield

        def run_uT(jobs, bufs):
            def chain(par_):
                for k in range(par_, len(jobs), 2):
                    srcs, s_, dst, tok0 = jobs[k]
                    yield from make_uT_g(srcs, s_, dst, tok0, A1g, 0, bufs, k)
            run_threads([chain(0), chain(1)])

        def uT_bufs(es_):
            return {'X': [c.sb(es_, [128, D], F32, 'X%d' % i) for i in range(4)],
                    'xnb': [c.sb(es_, [128, D], BF16, 'xnb%d' % i) for i in range(2)],
                    'ss': [c.sb(es_, [128, 4], F32, 'ss%d' % i) for i in range(2)],
                    'sq': [c.sb(es_, [128, D], BF16, 'sq%d' % i) for i in range(2)]}

        esG = ExitStack()
        with esG:
            uT_r = c.sb(esG, [128, 8, NTOK], BF16, 'uT_r')
            with ExitStack() as es2:
                bufs = uT_bufs(es2)
                jobs = [([(0, 128, ctx[t * 128:(t + 1) * 128, :])], 1, uT_r, t * 128) for t in range(2)]
                jobs += [([(0, 128, x[t * 128:(t + 1) * 128, :])], 0, uT_r, CTX + t * 128) for t in range(16)]
                run_uT(jobs, bufs)
                for kc in range(8):
                    c.dma(scr_u[:, kc, :], uT_r[:, kc, CTX:NTOK], reads=[uT_r], writes=[scru_buf])
                c.barrier()


            w_in_v = w_in.rearrange("(kc p) n -> p kc n", p=128)
            TBLK = [(0, 256), (256, 768), (768, 1280), (1280, 1792), (1792, 2304)]
            with ExitStack() as es3:
                g_tok = c.sb(es3, [128, 18, 8], F32, 'g_tok')
                b_tok = c.sb(es3, [128, 18, 8], F32, 'b_tok')
                with ExitStack() as es3a:
                    wab = c.sb(es3a, [128, 8, 16], BF16, 'wab')
                    ab = c.sb(es3a, [128, 18, 16], F32, 'ab')
                    alog = c.sb(es3a, [128, 18, 8], F32, 'alog')
                    dtb = c.sb(es3a, [128, 18, 8], F32, 'dtb')
                    t1 = c.sb(es3a, [128, 18, 8], F32, 't1')
                    t2 = c.sb(es3a, [128, 18, 8], F32, 't2')
                    c.dma(wab[:], w_in_v[:, :, 2048:2064], writes=[wab], q='pool')
                    c.dma(alog[:], alog_bc, writes=[alog])
                    c.dma(dtb[:], dtb_bc, writes=[dtb])
                    for t in range(18):
                        bank = PS[t % 2]
                        for kc in range(8):
                            c.mm(bank[:, 0:16], uT_r[:, kc, t * 128:(t + 1) * 128], wab[:, kc, :], kc == 0, kc == 7, [uT_r, wab], [bank])
                        c.cp('act', ab[:, t, :], bank[:, 0:16], [bank], [ab])
                    c.tt('dve', t1[:], ab[:, :, 0:8], dtb[:], ALU.add, [ab, dtb], [t1])
                    c.stt(t2[:], t1[:], -1.0, t1[:], ALU.mult, ALU.max, [t1], [t2])
                    c.act(t2[:], t2[:], AF.Exp, [t2], [t2], scale=-1.0)
                    c.ts('dve', t2[:], t2[:], 1.0, None, ALU.add, None, [t2], [t2])
                    c.act(t2[:], t2[:], AF.Ln, [t2], [t2])
                    c.stt(t1[:], t1[:], 0.0, t2[:], ALU.max, ALU.add, [t1, t2], [t1])
                    c.act(alog[:], alog[:], AF.Exp, [alog], [alog])
                    c.stt(R(g_tok[:]), t1[:], -1.0, alog[:], ALU.mult, ALU.mult, [t1, alog], [g_tok])
                    c.act(b_tok[:], ab[:, :, 8:16], AF.Sigmoid, [ab], [b_tok])
                    c.barrier()

                cv = c.sb(es3, [128, 12, 5], F32, 'cv')
                onm = c.sb(es3, [128, 1], F32, 'onm')
                c.dma(cv[:], convT, writes=[cv])
                c.dma(onm[:], onormT, writes=[onm])
                raws = [c.sb(es3, [128, NTOK], F32, 'raw%d' % i) for i in range(2)]
                accs = [c.sb(es3, [128, NTOK], F32, 'acc%d' % i) for i in range(2)]
                sqs = [c.sb(es3, [128, NTOK], F32, 'sqg%d' % i) for i in range(2)]
                qT = c.sb(es3, [128, NTOK], F32, 'qT')
                kT = c.sb(es3, [128, NTOK], F32, 'kT')
                vT = c.sb(es3, [128, NTOK], F32, 'vT')
                zs = c.sb(es3, [128, SEQ], F32, 'zs')
                o_acc = c.sb(es3, [128, 16, 128], F32, 'o_acc')
                wc = [c.sb(es3, [128, 8, 128], BF16, 'wc%d' % i) for i in range(2)]
                rns = [[c.sb(es3, [128, 512], F32, 'rn%d_%d' % (j, i)) for i in range(2)] for j in range(2)]
                S = [c.sb(es3, [128, 128], F32, 'S%d' % d) for d in range(2)]
                gcs = [c.sb(es3, [128, 8], F32, 'gcs%d' % i) for i in range(2)]
                egc = [c.sb(es3, [128, 8], F32, 'egc%d' % i) for i in range(2)]
                negc = [c.sb(es3, [128, 8], F32, 'negc%d' % i) for i in range(2)]
                ekd = [c.sb(es3, [128, 8], F32, 'ekd%d' % i) for i in range(2)]
                gend = [c.sb(es3, [128, 2, 8], F32, 'gend%d' % i) for i in range(2)]
                k_tok = [c.sb(es3, [128, 128], F32, 'k_tok%d' % i) for i in range(2)]
                v_tok = [c.sb(es3, [128, 128], F32, 'v_tok%d' % i) for i in range(2)]
                NS = 2
                Gt = [c.sb(es3, [128, 128], F32, 'Gt%d' % i) for i in range(NS)]
                Ei = [c.sb(es3, [128, 128], F32, 'Ei%d' % i) for i in range(NS)]
                Es = [c.sb(es3, [128, 128], F32, 'Es%d' % i) for i in range(NS)]
                QKm = [c.sb(es3, [128, 128], F32, 'QKm%d' % i) for i in range(NS)]
                Pb = [[c.sb(es3, [128, 128], F32, 'P%d_%d' % (i, j)) for j in range(2)] for i in range(NS)]
                Qb = [[c.sb(es3, [128, 128], F32, 'Q%d_%d' % (i, j)) for j in range(2)] for i in range(NS)]
                Wb_ = [[c.sb(es3, [128, 128], F32, 'W%d_%d' % (i, j)) for j in range(2)] for i in range(NS)]
                kdec = [c.sb(es3, [128, 128], F32, 'kdec%d' % i) for i in range(NS)]
                Zb = [c.sb(es3, [128, 128], F32, 'Z%d' % i) for i in range(NS)]
                vnew = [c.sb(es3, [128, 128], F32, 'vnew%d' % i) for i in range(NS)]
                otmp = [c.sb(es3, [128, 128], F32, 'otmp%d' % i) for i in range(NS)]
                otmp2 = [c.sb(es3, [128, 128], F32, 'otmp2%d' % i) for i in range(NS)]
                fin = [c.sb(es3, [128, 132], F32, 'fin%d' % i) for i in range(2)]
                wcnt = 0
                unit = 0
                import os
                H_all = c.sb(es3, [128, 18, 32], F32, 'H_all')
                egc_all = c.sb(es3, [128, 18, 8], F32, 'egc_all')
                negc_all = c.sb(es3, [128, 18, 8], F32, 'negc_all')
                ekd_all = c.sb(es3, [128, 18, 8], F32, 'ekd_all')
                gend_all = c.sb(es3, [128, 18, 16], F32, 'gend_all')
                for t in range(18):
                    bankH = PS[t % 2]
                    c.mm(bankH[:, 0:4], R(incl_r[0][:]), R(g_tok[:, t, 0:4]), True, True, [incl_r[0], g_tok], [bankH])
                    c.mm(bankH[:, 4:8], R(incl_r[1][:]), R(g_tok[:, t, 4:8]), True, True, [incl_r[1], g_tok], [bankH])
                    c.mm(bankH[:, 8:16], R(blk_r[:]), R(g_tok[:, t, :]), True, True, [blk_r, g_tok], [bankH])
                    c.mm(bankH[:, 16:24], R(sel_r[0][:]), R(g_tok[:, t, :]), True, True, [sel_r[0], g_tok], [bankH])
                    c.mm(bankH[:, 24:32], R(sel_r[1][:]), R(g_tok[:, t, :]), True, True, [sel_r[1], g_tok], [bankH])
                    c.cp('dve', H_all[:, t, :], bankH[:, 0:32], [bankH], [H_all])
                c.act(egc_all[:], H_all[:, :, 0:8], AF.Exp, [H_all], [egc_all])
                c.ts('dve', negc_all[:], egc_all[:], -1.0, None, ALU.mult, None, [egc_all], [negc_all])
                c.tt('dve', ekd_all[:], H_all[:, :, 8:16], H_all[:, :, 0:8], ALU.subtract, [H_all], [ekd_all])
                c.act(ekd_all[:], ekd_all[:], AF.Exp, [ekd_all], [ekd_all])
                c.act(gend_all[:], H_all[:, :, 16:32], AF.Exp, [H_all], [gend_all])
                NH = int(os.environ.get('K_NH', '4'))
                NSTEP = int(os.environ.get('K_NSTEP', '18'))
                KLAT = int(os.environ.get('K_LAT', '9'))
                for h in range(NH):
                    def proj_g(ci, slot, h=h):
                        col0, dst = [(h * 128, qT), (512 + h * 128, kT), (1024 + h * 128, vT), (1536 + h * 128, zs)][ci]
                        raw, acc, sq = raws[slot], accs[slot], sqs[slot]
                        w_ = wc[slot]
                        pb0 = 4 * slot
                        if h == 0 and ci < 2:
                            c.dma(w_[:], w_in_v[:, :, col0:col0 + 128], writes=[w_], q='pool')
                        for bi, (t0, t1_) in enumerate(TBLK):
                            if ci == 3 and bi == 0:
                                continue
                            bank = PS[pb0 + bi % 2]
                            n = t1_ - t0
                            for kc in range(8):
                                c.mm(bank[:, 0:n], w_[:, kc, :], uT_r[:, kc, t0:t1_], kc == 0, kc == 7, [w_, uT_r], [bank])
                            if ci == 3:
                                c.act(zs[:, t0 - CTX:t1_ - CTX], bank[:, 0:n], AF.Silu, [bank], [zs])
                            else:
                                c.cp('act', raw[:, t0:t1_], bank[:, 0:n], [bank], [raw])
                            yield
                        nh_, nci = (h, ci + 2) if ci < 2 else (h + 1, ci - 2)
                        if nh_ < NH:
                            ncol = [nh_ * 128, 512 + nh_ * 128, 1024 + nh_ * 128, 1536 + nh_ * 128][nci]
                            c.dma(w_[:], w_in_v[:, :, ncol:ncol + 128], writes=[w_], q='pool')
                        if ci == 3:
                            return
                        cch = ci * 4 + h
                        c.ts('dve', acc[:], raw[:], cv[:, cch, 2:3], None, ALU.mult, None, [raw, cv], [acc])
                        yield
                        for kk_ in (0, 1, 3, 4):
                            sft = kk_ - 2
                            for (a_, b_) in ((0, CTX), (CTX, NTOK)):
                                lo = max(a_, a_ - sft)
                                hi = min(b_, b_ - sft)
                                c.stt(acc[:, lo:hi], raw[:, lo + sft:hi + sft], cv[:, cch, kk_:kk_ + 1], acc[:, lo:hi], ALU.mult, ALU.add,
                                      [raw, cv, acc], [acc])
                            yield
                        if ci == 2:
                            c.act(R(vT[:]), acc[:], AF.Silu, [acc], [vT])
                            return
                        c.act(acc[:], acc[:], AF.Silu, [acc], [acc])
                        c.act(R(sq[:]), acc[:], AF.Square, [acc], [sq])
                        yield
                        sc = 128.0 if ci == 0 else 1.0
                        for bi, (t0, t1_) in enumerate(TBLK):
                            bank = PS[pb0 + 2 + bi % 2]
                            n = t1_ - t0
                            r_ = rns[slot][bi % 2]
                            c.mm(bank[:, 0:n], R(ones_r[:]), R(sq[:, t0:t1_]), True, True, [ones_r, sq], [bank])
                            c.ts('dve', r_[:, 0:n], bank[:, 0:n], sc, EPS * sc, ALU.mult, ALU.add, [bank], [r_])
                            c.act(r_[:, 0:n], r_[:, 0:n], AF.Sqrt, [r_], [r_])
                            yield
                            c.op('dve', lambda e, r_=r_, n=n: e.reciprocal(out=r_[:, 0:n], in_=r_[:, 0:n]), reads=[r_], writes=[r_])
                            c.tt('dve', R(dst[:, t0:t1_]), acc[:, t0:t1_], r_[:, 0:n], ALU.mult, [acc, r_], [dst])
                            yield

                    run_threads([proj_g(0, 0), proj_g(1, 1)])
                    run_threads([proj_g(2, 0), proj_g(3, 1)])
                    if 'd_qkv' in T and h == 0:
                        c.dma(T['d_qkv'][0], qT[:], reads=[qT])
                        c.dma(T['d_qkv'][1], kT[:], reads=[kT])
                        c.dma(T['d_qkv'][2], vT[:], reads=[vT])
                    for d in range(2):
                        c.ts('dve', R(S[d][:]), zf[:], 0.0, None, ALU.mult, None, [zf], [S[d]])
                    order_f = list(range(18))
                    order_b = [1, 0] + list(range(17, 1, -1))
                    def gdn_unit_g(d, step):
                        tile = order_f[step] if d == 0 else order_b[step]
                        is_lat = tile >= 2
                        ts0 = tile * 128
                        col = d * 4 + h
                        pi = d
                        u = d
                        X0, X1, X2, X3 = (PS[4 * d + i_] for i_ in range(4))
                        bankT = X3
                        c.tr(bankT[:, 0:128], kT[:, ts0:ts0 + 128], ident[:], [kT, ident], [bankT])
                        c.tr(bankT[:, 128:256], vT[:, ts0:ts0 + 128], ident[:], [vT, ident], [bankT])
                        bankA = X0
                        c.mm(bankA[:, 0:128], R(kT[:, ts0:ts0 + 128]), R(kT[:, ts0:ts0 + 128]), True, True, [kT], [bankA])
                        c.mm(bankA[:, 128:256], R(kT[:, ts0:ts0 + 128]), R(qT[:, ts0:ts0 + 128]), True, True, [kT, qT], [bankA])
                        c.ts('pool', R(Gt[u][:]), incl[d][:], g_tok[:, tile, col:col + 1], None, ALU.mult, None, [incl[d], g_tok], [Gt[u]])
                        yield
                        bankB = X1
                        c.mm(bankB[:, 0:128], R(ones_r[:]), R(Gt[u][:]), True, False, [ones_r, Gt[u]], [bankB])
                        c.mm(bankB[:, 0:128], R(Gt[u][:]), R(nones_r[:]), False, False, [nones_r, Gt[u]], [bankB])
                        c.mm(bankB[:, 0:128], R(ident_r[:]), R(negm_r[d][:]), False, True, [ident_r, negm_r[d]], [bankB])
                        yield
                        c.cp('act', k_tok[pi][:], bankT[:, 0:128], [bankT], [k_tok[pi]])
                        c.cp('act', R(v_tok[pi][:]), bankT[:, 128:256], [bankT], [v_tok[pi]])
                        c.act(Ei[u][:], bankB[:, 0:128], AF.Exp, [bankB], [Ei[u]])
                        yield
                        c.tt('pool', Es[u][:], Ei[u][:], notI[:], ALU.mult, [Ei[u], notI], [Es[u]])
                        P, Q, W = Pb[u], Qb[u], Wb_[u]
                        yield
                        c.stt(R(P[0][:]), bankA[:, 0:128], b_tok[:, tile, col:col + 1], Es[u][:], ALU.mult, ALU.mult,
                              [bankA, b_tok, Es[u]], [P[0]])
                        c.tt('dve', R(QKm[u][:]), bankA[:, 128:256], Ei[u][:], ALU.mult, [bankA, Ei[u]], [QKm[u]])
                        c.ts('dve', R(kdec[u][:]), k_tok[pi][:], ekd_all[:, tile, col:col + 1], None, ALU.mult, None, [k_tok[pi], ekd_all], [kdec[u]])
                        yield
                        bankC = X2
                        bankD = X3
                        c.tr(bankC[:, 0:128], P[0][:], ident[:], [P[0], ident], [bankC])
                        c.tt('pool', R(W[0][:]), ident[:], P[0][:], ALU.subtract, [ident, P[0]], [W[0]])
                        yield
                        c.cp('act', R(Q[0][:]), bankC[:, 0:128], [bankC], [Q[0]])
                        yield
                        for k_ in range(5):
                            a_, b_ = k_ % 2, (k_ + 1) % 2
                            c.mm(bankC[:, 128:256], R(P[a_][:]), R(Q[a_][:]), True, True, [P[a_], Q[a_]], [bankC])
                            if k_ < 4:
                                c.mm(bankD[:, 0:128], R(Q[a_][:]), R(P[a_][:]), True, True, [P[a_], Q[a_]], [bankD])
                            yield
                            c.cp('act', R(Q[b_][:]), bankC[:, 128:256], [bankC], [Q[b_]])
                            if k_ < 4:
                                c.cp('dve', R(P[b_][:]), bankD[:, 0:128], [bankD], [P[b_]])
                            yield
                            c.mm(bankD[:, 128:256], R(Q[b_][:]), R(W[a_][:]), True, True, [Q[b_], W[a_]], [bankD])
                            yield
                            c.tt('dve', R(W[b_][:]), bankD[:, 128:256], W[a_][:], ALU.add, [bankD, W[a_]], [W[b_]])
                            yield
                        Wf = W[1]
                        for cs in ((0, 64) if d == 0 else (64, 0)):
                            sl_ = slice(cs, cs + 64)
                            chunk = cs // 64
                            c.mm(X0[:, 0:128], R(kT[:, ts0:ts0 + 128]), R(S[d][:]), True, True, [kT, S[d]], [X0])
                            if is_lat:
                                c.mm(X0[:, 128:256], R(qT[:, ts0:ts0 + 128]), R(S[d][:]), True, True, [qT, S[d]], [X0])
                            yield
                            c.stt(R(Zb[u][sl_, :]), X0[sl_, 0:128], negc_all[sl_, tile, col:col + 1], v_tok[pi][sl_, :], ALU.mult, ALU.add,
                                  [X0, negc_all, v_tok[pi]], [Zb[u]])
                            if is_lat:
                                c.ts('dve', otmp[u][sl_, :], X0[sl_, 128:256], egc_all[sl_, tile, col:col + 1], None, ALU.mult, None, [X0, egc_all], [otmp[u]])
                            yield
                            c.mm(X1[:, 0:128], R(Wf[sl_, :]), R(Zb[u][sl_, :]), True, True, [Wf, Zb[u]], [X1])
                            yield
                            c.ts('dve', R(vnew[u][sl_, :]), X1[sl_, 0:128], b_tok[sl_, tile, col:col + 1], None, ALU.mult, None,
                                 [X1, b_tok], [vnew[u]])
                            yield
                            c.mm(X3[:, 256:384], R(kdec[u][sl_, :]), R(vnew[u][sl_, :]), True, True, [kdec[u], vnew[u]], [X3])
                            if is_lat:
                                c.mm(X2[:, 0:128], R(QKm[u][sl_, :]), R(vnew[u][sl_, :]), True, True, [QKm[u], vnew[u]], [X2])
                            yield
                            c.stt(R(S[d][:]), S[d][:], gend_all[:, tile, chunk * 8 + col:chunk * 8 + col + 1], X3[:, 256:384], ALU.mult, ALU.add,
                                  [S[d], gend_all, X3], [S[d]])
                            if is_lat:
                                lt = tile - 2
                                if (d == 0) == (lt <= 7):
                                    c.tt('dve', o_acc[sl_, lt, :], X2[sl_, 0:128], otmp[u][sl_, :], ALU.add, [X2, otmp[u]], [o_acc])
                                else:
                                    c.tt('dve', otmp2[u][sl_, :], X2[sl_, 0:128], otmp[u][sl_, :], ALU.add, [X2, otmp[u]], [otmp2[u]])
                                    c.tt('pool', o_acc[sl_, lt, :], o_acc[sl_, lt, :], otmp2[u][sl_, :], ALU.add, [o_acc, otmp2[u]], [o_acc])
                            yield

                    def gdn_dir_g(d):
                        for step in range(NSTEP):
                            yield from gdn_unit_g(d, step)

                    run_threads([gdn_dir_g(0), gdn_dir_g(1)])
                    def gdn_out_g(par_, h=h):
                        f_ = fin[par_]
                        bank = PS[par_]
                        for lt in range(par_, 16, 2):
                            c.act(f_[:, 0:128], o_acc[:, lt, :], AF.Square, [o_acc], [f_], accum_out=f_[:, 128:129])
                            yield
                            c.ts('dve', f_[:, 129:130], f_[:, 128:129], 1.0 / 128, EPS, ALU.mult, ALU.add, [f_], [f_])
                            yield
                            c.act(f_[:, 130:131], f_[:, 129:130], AF.Sqrt, [f_], [f_])
                            yield
                            c.op('dve', lambda e, f_=f_: e.reciprocal(out=f_[:, 131:132], in_=f_[:, 130:131]), reads=[f_], writes=[f_])
                            c.ts('dve', f_[:, 0:128], o_acc[:, lt, :], f_[:, 131:132], None, ALU.mult, None, [o_acc, f_], [f_])
                            yield
                            c.tr(bank[:, 0:128], f_[:, 0:128], ident[:], [f_, ident], [bank])
                            yield
                            c.stt(oaT[:, h, lt * 128:(lt + 1) * 128], bank[:, 0:128], onm[:, 0:1], zs[:, lt * 128:(lt + 1) * 128], ALU.mult, ALU.mult,
                                  [bank, onm, zs], [oaT])
                            yield

                    run_threads([gdn_out_g(0), gdn_out_g(1)])
                c.barrier()
        if 'd_oaT' in T:
            c.dma(T['d_oaT'], oaT[:], reads=[oaT])


        def inv_chain(P, Q, W, bankC, bankD):
            c.tr(bankC[:, 0:128], P[0][:], ident[:], [P[0], ident], [bankC])
            c.cp('act', R(Q[0][:]), bankC[:, 0:128], [bankC], [Q[0]])
            c.tt('pool', R(W[0][:]), ident[:], P[0][:], ALU.subtract, [ident, P[0]], [W[0]])
            for k_ in range(5):
                a_, b_ = k_ % 2, (k_ + 1) % 2
                c.mm(bankC[:, 128:256], R(P[a_][:]), R(Q[a_][:]), True, True, [P[a_], Q[a_]], [bankC])
                if k_ < 4:
                    c.mm(bankD[:, 0:128], R(Q[a_][:]), R(P[a_][:]), True, True, [P[a_], Q[a_]], [bankD])
                c.cp('act', R(Q[b_][:]), bankC[:, 128:256], [bankC], [Q[b_]])
                if k_ < 4:
                    c.cp('dve', R(P[b_][:]), bankD[:, 0:128], [bankD], [P[b_]])
                c.mm(bankD[:, 128:256], R(Q[b_][:]), R(W[a_][:]), True, True, [Q[b_], W[a_]], [bankD])
                c.tt('dve', R(W[b_][:]), bankD[:, 128:256], W[a_][:], ALU.add, [bankD, W[a_]], [W[b_]])
            return W[1]

        BW = 256
        NBLK = NTOK // BW
        with ExitStack() as esR:
            uT_c = c.sb(esR, [128, 8, NTOK], BF16, 'uT_c')
            with ExitStack() as es2:
                bufs = uT_bufs(es2)
                x_cm = x.rearrange("(r w) d -> w r d", w=64)
                jobs = [([(0, 128, ctx[t * 128:(t + 1) * 128, :])], 1, uT_c, t * 128) for t in range(2)]
                jobs += [([(wl * 32, wl * 32 + 32, x_cm[4 * j + wl]) for wl in range(4)], 0, uT_c, CTX + j * 128) for j in range(16)]
                run_uT(jobs, bufs)
                c.barrier()
            mu = c.sb(esR, [128, 15], F32, 'mu')
            hmu = c.sb(esR, [128, 15], F32, 'hmu')
            omu = c.sb(esR, [128, 15], F32, 'omu')
            w0 = c.sb(esR, [128, 2, 4], F32, 'w0')
            a0 = c.sb(esR, [128, 2, 4], F32, 'a0')
            rw = c.sb(esR, [128, 5, 4], F32, 'rw')
            oka = c.sb(esR, [128, 4], F32, 'oka')
            oka2 = c.sb(esR, [128, 4], F32, 'oka2')
            w2b = c.sb(esR, [128, 512], BF16, 'w2b')
            a2b = c.sb(esR, [128, 512], BF16, 'a2b')
            g2b = c.sb(esR, [128, 512], BF16, 'g2b')
            c.dma(mu[:], muT, writes=[mu])
            c.dma(w0[:], w0T, writes=[w0])
            c.dma(a0[:], a0T, writes=[a0])
            c.dma(rw[:], rwv, writes=[rw])
            c.dma(w2b[:], w2m, writes=[w2b], q='pool')
            c.dma(a2b[:], a2m, writes=[a2b], q='pool')
            c.dma(g2b[:], g2m, writes=[g2b], q='pool')
            c.ts('dve', hmu[:], mu[:], 0.5, None, ALU.mult, None, [mu], [hmu])
            c.ts('dve', omu[:], mu[:], -1.0, 1.0, ALU.mult, ALU.add, [mu], [omu])
            c.ts('dve', oka[:], rw[:, 1, :], -1.0, 1.0, ALU.mult, ALU.add, [rw], [oka])
            c.ts('dve', oka2[:], rw[:, 1, :], -2.0, 2.0, ALU.mult, ALU.add, [rw], [oka2])
            cmask = c.sb(esR, [128, BW], F32, 'cmask')
            c.op('pool', lambda e: e.memset(cmask[:], 1.0), writes=[cmask])
            c.op('pool', lambda e: e.memset(cmask[:].rearrange("p (a b) -> p a b", b=64)[:, :, 0:1], 0.0), reads=[cmask], writes=[cmask])
            mskA = [c.sb(esR, [128, 256], F32, 'mskA%d' % d) for d in range(2)]
            mskB = [c.sb(esR, [128, 256], F32, 'mskB%d' % d) for d in range(2)]
            for d in range(2):
                c.ts('dve', mskA[d][:, 0:128], strict[d][:], -1.0, None, ALU.mult, None, [strict[d]], [mskA[d]])
                c.cp('dve', mskA[d][:, 128:256], incl[d][:], [incl[d]], [mskA[d]])
                c.cp('dve', mskB[d][:, 0:128], strict[d][:], [strict[d]], [mskB[d]])
                c.cp('dve', mskB[d][:, 128:256], incl[d][:], [incl[d]], [mskB[d]])

            raw = c.sb(esR, [128, NTOK], F32, 'rraw')
            t1 = c.sb(esR, [128, NTOK], F32, 'rt1')
            twl = c.sb(esR, [128, NTOK], BF16, 'twl')
            alo = c.sb(esR, [128, NTOK], BF16, 'alo')
            sgl = c.sb(esR, [128, NTOK], BF16, 'sgl')
            rb = c.sb(esR, [128, NTOK], BF16, 'rb')
            kb = c.sb(esR, [128, NTOK], BF16, 'kb')
            vb = c.sb(esR, [128, NTOK], BF16, 'vb')
            G1 = c.sb(esR, [128, SEQ], BF16, 'G1')
            G2 = c.sb(esR, [128, SEQ], BF16, 'G2')
            y_acc = c.sb(esR, [128, 16, 128], F32, 'y_acc')
            wcr = [c.sb(esR, [128, 8, 128], BF16, 'wcr%d' % i) for i in range(2)]
            wcn = [0]

            pm_sched = [1536, 1664, 1792]
            for hc_ in range(4):
                pm_sched += [hc_ * 128, 512 + hc_ * 128, 1024 + hc_ * 128]

            def proj_mix(bcol, dst, func, mi):
                n_ = wcn[0]
                wcn[0] += 1
                assert pm_sched[n_] == bcol
                w_ = wcr[n_ % 2]
                if n_ == 0:
                    c.dma(w_[:], w_in_v[:, :, A_COLS + bcol:A_COLS + bcol + 128], writes=[w_], q='pool')
                for bi, (t0, t1_) in enumerate(TBLK):
                    bank = PS[bi % 2]
                    n = t1_ - t0
                    for kc in range(8):
                        c.mm(bank[:, 0:n], w_[:, kc, :], uT_c[:, kc, t0:t1_], kc == 0, kc == 7, [w_, uT_c], [bank])
                    c.cp('act', raw[:, t0:t1_], bank[:, 0:n], [bank], [raw])
                    if bi == 0 and n_ + 1 < len(pm_sched):
                        nb_ = A_COLS + pm_sched[n_ + 1]
                        c.dma(wcr[(n_ + 1) % 2][:], w_in_v[:, :, nb_:nb_ + 128], writes=[wcr[(n_ + 1) % 2]], q='pool')
                for (a_, b_) in ((0, CTX), (CTX, NTOK)):
                    c.tt('pool', t1[:, a_ + 1:b_ - 1], raw[:, a_:b_ - 2], raw[:, a_ + 2:b_], ALU.add, [raw], [t1])
                    c.cp('pool', t1[:, a_:a_ + 1], raw[:, a_ + 1:a_ + 2], [raw], [t1])
                    c.cp('pool', t1[:, b_ - 1:b_], raw[:, b_ - 2:b_ - 1], [raw], [t1])
                c.ts('dve', t1[:], t1[:], hmu[:, mi:mi + 1], None, ALU.mult, None, [t1, hmu], [t1])
                if func is None:
                    c.stt(dst[:], raw[:], omu[:, mi:mi + 1], t1[:], ALU.mult, ALU.add, [raw, omu, t1], [dst])
                else:
                    c.stt(t1[:], raw[:], omu[:, mi:mi + 1], t1[:], ALU.mult, ALU.add, [raw, omu, t1], [t1])
                    c.act(dst[:], t1[:], func, [t1], [dst])

            proj_mix(1536, twl, AF.Tanh, 12)
            proj_mix(1664, alo, None, 13)
            proj_mix(1792, sgl, AF.Sigmoid, 14)

            def blkbuf(name, dt=F32, w=BW):
                return c.sb(esR, [128, w], dt, name)
            DB = []
            for d in range(2):
                g = {}
                arena = (raw, t1)[d]
                for i_, nm in enumerate(('lw', 'icl', 'pre', 'cumd', 'kkn', 'kdir', 'bvec', 'tmpa', 'tmpb', 'opr', 'btT', 'ktT', 'bhT', 'khT')):
                    if i_ < 9:
                        g[nm] = Buf(arena.t[:, i_ * BW:(i_ + 1) * BW], '%s%d' % (nm, d))
                    else:
                        g[nm] = blkbuf('%s%d' % (nm, d))
                g['AR'] = c.sb(esR, [128, 2, BW], F32, 'AR%d' % d)
                g['gC'] = c.sb(esR, [128, BW // 64], F32, 'gC%d' % d)
                for nm in ('Bh_tok', 'Kh_tok', 'Vt'):
                    g[nm] = c.sb(esR, [128, 128], F32, '%s%d' % (nm, d))
                g['BL0'] = Reg(4 * d, 0, 256, 'BL0_%d' % d)
                g['BL1'] = Reg(4 * d + 2, 0, 256, 'BL1_%d' % d)
                g['TL0'] = Reg(4 * d + 1, 0, 128, 'TL0_%d' % d)
                g['TLb'] = Reg(4 * d + 1, 128, 128, 'TLb_%d' % d)
                g['TL1'] = Reg(4 * d + 3, 0, 128, 'TL1_%d' % d)
                g['U'] = []
                for hh in range(2):
                    u = {'bankA': 4 * d + 2 * hh, 'bankB': 4 * d + 2 * hh + 1}
                    sfx = '%d%d' % (d, hh)
                    u['AB1'] = c.sb(esR, [128, 256], F32, 'AB1_' + sfx)
                    u['AB2'] = c.sb(esR, [128, 256], F32, 'AB2_' + sfx)
                    u['XY2'] = c.sb(esR, [128, 128], F32, 'XY2_' + sfx)
                    for nm in ('P', 'Q', 'W'):
                        u[nm] = [c.sb(esR, [128, 128], F32, '%sr%d_%s' % (nm, j, sfx)) for j in range(2)]
                    for nm in ('Xs', 'Us', 'yt', 'yt2', 'Tst'):
                        u[nm] = c.sb(esR, [128, 64], F32, nm + sfx)
                    g['U'].append(u)
                DB.append(g)
            gst2 = [c.sb(esR, [128, 24], F32, 'gst%d' % i) for i in range(2)]
            yn2 = [c.sb(esR, [128, 128], F32, 'yn%d' % i) for i in range(2)]
            obf2 = [c.sb(esR, [128, 128], BF16, 'obf%d' % i) for i in range(2)]
            otf2 = [c.sb(esR, [128, 128], F32, 'otf%d' % i) for i in range(2)]
            NHC = int(os.environ.get('K_NHC', '4'))
            NBS = int(os.environ.get('K_NBS', '9'))
            LG = -0.6065306597126334
            KTG = int(os.environ.get('K_TG', '9'))

            def block_g(d, g, b, hc):
                rowsd = slice(d * 64, d * 64 + 64)
                cs_ = slice(hc * 128, (hc + 1) * 128)
                t0 = b * BW
                tsl = slice(t0, t0 + BW)
                lw, icl, pre, cumd, kkn, kdir, bvec, tmpa, tmpb, opr = (g[n_] for n_ in ('lw', 'icl', 'pre', 'cumd', 'kkn', 'kdir', 'bvec', 'tmpa', 'tmpb', 'opr'))
                AR, btT, ktT, bhT, khT, gC = (g[n_] for n_ in ('AR', 'btT', 'ktT', 'bhT', 'khT', 'gC'))
                BL0, BL1 = g['BL0'], g['BL1']
                c.mm(BL0.ap(), w2b[rowsd, cs_], twl[rowsd, tsl], True, True, [w2b, twl], [BL0.buf])
                c.mm(BL1.ap(), a2b[rowsd, cs_], alo[rowsd, tsl], True, True, [a2b, alo], [BL1.buf])
                c.ts('dve', kkn[:], kb[:, tsl], rw[:, 0, hc:hc + 1], None, ALU.mult, None, [kb, rw], [kkn])
                yield
                c.act(lw[:], BL0.ap(), AF.Sigmoid, [BL0.buf, w0], [lw], bias=w0[:, d, hc:hc + 1])
                c.act(icl[:], BL1.ap(), AF.Sigmoid, [BL1.buf, a0], [icl], bias=a0[:, d, hc:hc + 1])
                c.act(R(opr[:]), kkn[:], AF.Square, [kkn], [opr])
                yield
                c.ts('dve', lw[:], lw[:], LG, None, ALU.mult, None, [lw], [lw])
                c.mm(BL0.ap(), R(blk_r[:]), R(opr[:]), True, True, [blk_r, opr], [BL0.buf])
                c.op('dve', lambda e: e.tensor_tensor_scan(out=pre[:], data0=cmask[:], data1=lw[:], initial=0.0,
                                                           op0=ALU.mult, op1=ALU.add), reads=[cmask, lw], writes=[pre])
                yield
                pre3 = pre[:].rearrange("p (a b) -> p a b", b=64)
                tot_bc = pre3[:, :, 63:64].to_broadcast([128, BW // 64, 64])
                if d == 0:
                    c.cp('pool', cumd[:], pre[:], [pre], [cumd])
                else:
                    c.tt('dve', cumd[:], lw[:], pre[:], ALU.subtract, [lw, pre], [cumd])
                    c.tt('dve', cumd[:].rearrange("p (a b) -> p a b", b=64), cumd[:].rearrange("p (a b) -> p a b", b=64), tot_bc,
                         ALU.add, [cumd, pre], [cumd])
                c.ts('dve', tmpb[:], BL0.ap(), EPS, None, ALU.add, None, [BL0.buf], [tmpb])
                yield
                c.act(gC[:], pre3[:, :, 63], AF.Exp, [pre], [gC])
                c.act(tmpb[:], tmpb[:], AF.Sqrt, [tmpb], [tmpb])
                c.ts('dve', kdir[:], icl[:], rw[:, 1, hc:hc + 1], oka[:, hc:hc + 1], ALU.mult, ALU.add, [icl, rw, oka], [kdir])
                c.tt('dve', kdir[:], kdir[:], kb[:, tsl], ALU.mult, [kdir, kb], [kdir])
                yield
                c.op('dve', lambda e: e.reciprocal(out=tmpb[:], in_=tmpb[:]), reads=[tmpb], writes=[tmpb])
                c.tt('dve', kkn[:], kkn[:], tmpb[:], ALU.mult, [kkn, tmpb], [kkn])
                c.tt('dve', tmpa[:], cumd[:], lw[:], ALU.subtract, [cumd, lw], [tmpa])
                yield
                c.tt('pool', bvec[:], kkn[:], icl[:], ALU.mult, [kkn, icl], [bvec])
                c.act(tmpa[:], tmpa[:], AF.Exp, [tmpa], [tmpa])
                yield
                c.stt(R(AR[:, 0, :]), kkn[:], -1.0, tmpa[:], ALU.mult, ALU.mult, [kkn, tmpa], [AR])
                yield
                c.act(tmpa[:], cumd[:], AF.Exp, [cumd], [tmpa])
                c.tt('dve', tmpb[:].rearrange("p (a b) -> p a b", b=64), cumd[:].rearrange("p (a b) -> p a b", b=64), tot_bc,
                     ALU.subtract, [cumd, pre], [tmpb])
                yield
                c.tt('dve', R(AR[:, 1, :]), rb[:, tsl], tmpa[:], ALU.mult, [rb, tmpa], [AR])
                yield
                c.act(tmpa[:], cumd[:], AF.Exp, [cumd], [tmpa], scale=-1.0)
                c.act(tmpb[:], tmpb[:], AF.Exp, [tmpb], [tmpb], scale=-1.0)
                yield
                c.tt('dve', R(btT[:]), bvec[:], tmpa[:], ALU.mult, [bvec, tmpa], [btT])
                c.tt('pool', R(ktT[:]), kdir[:], tmpa[:], ALU.mult, [kdir, tmpa], [ktT])
                yield
                c.tt('dve', R(bhT[:]), bvec[:], tmpb[:], ALU.mult, [bvec, tmpb], [bhT])
                c.tt('pool', R(khT[:]), kdir[:], tmpb[:], ALU.mult, [kdir, tmpb], [khT])
                yield

            def tile_g(d, g, b, tl):
                lsl_ = slice(tl * 128, tl * 128 + 128)
                gts = (2 * b + tl) * 128
                TL0, TL1, TLb = g['TL0'], g['TL1'], g['TLb']
                bhT, khT, Bh_tok, Kh_tok, Vt = g['bhT'], g['khT'], g['Bh_tok'], g['Kh_tok'], g['Vt']
                c.mm(TL0.ap(), R(bhT[:, lsl_]), R(ident_r[:]), True, True, [bhT, ident_r], [TL0.buf])
                c.mm(TLb.ap(), vb[:, gts:gts + 128], identb[:], True, True, [vb, identb], [TLb.buf])
                c.mm(TL1.ap(), R(khT[:, lsl_]), R(ident_r[:]), True, True, [khT, ident_r], [TL1.buf])
                yield
                c.cp('act', R(Bh_tok[:]), TL0.ap(), [TL0.buf], [Bh_tok])
                c.cp('dve', R(Kh_tok[:]), TL1.ap(), [TL1.buf], [Kh_tok])
                c.cp('act', R(Vt[:]), TLb.ap(), [TLb.buf], [Vt])
                yield

            def unit_g(d, g, u, hh, b, tl):
                rows = slice(hh * 64, hh * 64 + 64)
                tile = 2 * b + tl
                is_lat = tile >= 2
                lt = tile - 2
                lsl_ = slice(tl * 128, tl * 128 + 128)
                AR, btT, ktT, Vt, Bh_tok, Kh_tok, gC = (g[n_] for n_ in ('AR', 'btT', 'ktT', 'Vt', 'Bh_tok', 'Kh_tok', 'gC'))
                AB1, AB2, XY2, Xs, Us, yt, yt2, Tst = (u[n_] for n_ in ('AB1', 'AB2', 'XY2', 'Xs', 'Us', 'yt', 'yt2', 'Tst'))
                P, Q, W = u['P'], u['Q'], u['W']
                A, B = PS[u['bankA']], PS[u['bankB']]
                At, Bt = A.t, B.t
                ARt = AR[rows, :, lsl_]
                c.mm(At[:, 0:256].rearrange("p (a b) -> p a b", a=2), R(btT[rows, lsl_]), R(ARt), True, True, [btT, AR], [A])
                c.mm(Bt[:, 0:256].rearrange("p (a b) -> p a b", a=2), R(ktT[rows, lsl_]), R(ARt), True, True, [ktT, AR], [B])
                yield
                c.tt('dve', R(AB1[:]), At[:, 0:256], mskA[d][:], ALU.mult, [A, mskA[d]], [AB1])
                c.tt('dve', R(AB2[:]), Bt[:, 0:256], mskB[d][:], ALU.mult, [B, mskB[d]], [AB2])
                yield
                if KTG < 3:
                    return
                c.cp('pool', R(P[0][:]), AB1[:, 0:128], [AB1], [P[0]])
                c.mm(At[:, 0:64], R(AB2[:, 0:128]), R(Vt[:, rows]), True, True, [AB2, Vt], [A])
                c.mm(At[:, 64:128], R(AB2[:, 128:256]), R(Vt[:, rows]), True, True, [AB2, Vt], [A])
                yield
                c.cp('act', XY2[:], At[:, 0:128], [A], [XY2])
                c.mm(Bt[:, 0:128], R(P[0][:]), R(ident_r[:]), True, True, [P[0], ident_r], [B])
                c.tt('pool', R(W[0][:]), ident[:], P[0][:], ALU.subtract, [ident, P[0]], [W[0]])
                yield
                c.cp('act', R(Q[0][:]), Bt[:, 0:128], [B], [Q[0]])
                yield
                for k_ in range(5):
                    a_, b_ = k_ % 2, (k_ + 1) % 2
                    c.mm(At[:, 128:256], R(P[a_][:]), R(Q[a_][:]), True, True, [P[a_], Q[a_]], [A])
                    if k_ < 4:
                        c.mm(Bt[:, 0:128], R(Q[a_][:]), R(P[a_][:]), True, True, [P[a_], Q[a_]], [B])
                    yield
                    c.cp('act', R(Q[b_][:]), At[:, 128:256], [A], [Q[b_]])
                    if k_ < 4:
                        c.cp('act', R(P[b_][:]), Bt[:, 0:128], [B], [P[b_]])
                    yield
                    c.mm(Bt[:, 128:256], R(Q[b_][:]), R(W[a_][:]), True, True, [Q[b_], W[a_]], [B])
                    yield
                    c.tt('dve', R(W[b_][:]), Bt[:, 128:256], W[a_][:], ALU.add, [B, W[a_]], [W[b_]])
                    yield
                Wt = W[1]
                if KTG < 4:
                    return
                for cs in ((0, 64) if d == 0 else (64, 0)):
                    sl_ = slice(cs, cs + 64)
                    chunk = tl * 2 + cs // 64
                    c.mm(At[:, 0:64], R(AR[rows, 0, lsl_]), R(Tst[rows, :]), True, True, [AR, Tst], [A])
                    if is_lat:
                        c.mm(At[:, 64:128], R(AR[rows, 1, lsl_]), R(Tst[rows, :]), True, True, [AR, Tst], [A])
                    yield
                    c.tt('dve', R(Xs[sl_, :]), At[sl_, 0:64], XY2[sl_, 0:64], ALU.add, [A, XY2], [Xs])
                    yield
                    c.mm(Bt[:, 0:64], R(Wt[sl_, :]), R(Xs[sl_, :]), True, True, [Wt, Xs], [B])
                    yield
                    c.cp('act', R(Us[sl_, :]), Bt[sl_, 0:64], [B], [Us])
                    yield
                    c.mm(Bt[:, 64:128], R(Bh_tok[sl_, :]), R(Us[sl_, :]), True, False, [Bh_tok, Us], [B])
                    c.mm(Bt[:, 64:128], R(Kh_tok[sl_, :]), R(Vt[sl_, rows]), False, True, [Kh_tok, Vt], [B])
                    if is_lat:
                        c.mm(At[:, 128:192], R(AB1[sl_, 128:256]), R(Us[sl_, :]), True, True, [AB1, Us], [A])
                    yield
                    c.stt(R(Tst[rows, :]), Tst[rows, :], gC[rows, chunk:chunk + 1], Bt[rows, 64:128], ALU.mult, ALU.add,
                          [Tst, gC, B], [Tst])
                    if is_lat:
                        c.tt('dve', yt[sl_, :], At[sl_, 128:192], XY2[sl_, 64:128], ALU.add, [A, XY2], [yt])
                        if (d == 0) == (lt <= 7):
                            c.tt('dve', y_acc[sl_, lt, rows], At[sl_, 64:128], yt[sl_, :], ALU.add, [A, yt], [y_acc])
                        else:
                            c.tt('dve', yt2[sl_, :], At[sl_, 64:128], yt[sl_, :], ALU.add, [A, yt], [yt2])
                            c.tt('pool', y_acc[sl_, lt, rows], y_acc[sl_, lt, rows], yt2[sl_, :], ALU.add, [y_acc, yt2], [y_acc])
                    yield

            def dir_g(d, hc):
                g = DB[d]
                for hh in range(2):
                    c.ts('dve', R(g['U'][hh]['Tst'][:]), zf[:, 0:64], 0.0, None, ALU.mult, None, [zf], [g['U'][hh]['Tst']])
                border = list(range(NBLK)) if d == 0 else [0] + list(range(NBLK - 1, 0, -1))
                for b in border[:NBS]:
                    yield from block_g(d, g, b, hc)
                    for tl in ((0, 1) if d == 0 else (1, 0)):
                        if KTG < 1:
                            continue
                        yield from tile_g(d, g, b, tl)
                        if KTG < 2:
                            continue
                        yield from par(unit_g(d, g, g['U'][0], 0, b, tl), unit_g(d, g, g['U'][1], 1, b, tl))

            ob_hc = c.sb(esR, [128, 16, 128], BF16, 'ob_hc')
            scr_ob_v = scr_ob.rearrange("(lt p) cc -> p lt cc", p=128)
            scrw = Buf(None, 'scrw')
            for hc in range(NHC):
                proj_mix(hc * 128, rb, None, hc)
                proj_mix(512 + hc * 128, kb, None, 4 + hc)
                proj_mix(1024 + hc * 128, vb, None, 8 + hc)
                c.barrier()
                cs_ = slice(hc * 128, (hc + 1) * 128)
                def prepass_g(par_, hc=hc, cs_=cs_):
                    g_ = DB[par_]
                    icl, icl1, tmpa, opr = g_['icl'], g_['pre'], g_['tmpa'], g_['opr']
                    P0, P3, P1, P2 = (PS[4 * par_ + j] for j in range(4))
                    for b in range(1 + par_, NBLK, 2):
                        t0 = b * BW
                        tsl = slice(t0, t0 + BW)
                        lsl = slice(t0 - CTX, t0 - CTX + BW)
                        c.mm(P0[:, 0:BW], a2b[0:64, cs_], alo[0:64, tsl], True, True, [a2b, alo], [P0])
                        c.mm(P3[:, 0:BW], a2b[64:128, cs_], alo[64:128, tsl], True, True, [a2b, alo], [P3])
                        c.mm(P2[:, 0:BW], g2b[:, cs_], sgl[:, tsl], True, True, [g2b, sgl], [P2])
                        yield
                        c.act(icl[:], P0[:, 0:BW], AF.Sigmoid, [P0, a0], [icl], bias=a0[:, 0, hc:hc + 1])
                        c.act(icl1[:], P3[:, 0:BW], AF.Sigmoid, [P3, a0], [icl1], bias=a0[:, 1, hc:hc + 1])
                        c.cp('act', G1[:, lsl], P2[:, 0:BW], [P2], [G1])
                        yield
                        c.tt('dve', tmpa[:], icl[:], icl1[:], ALU.add, [icl, icl1], [tmpa])
                        c.ts('dve', tmpa[:], tmpa[:], rw[:, 1, hc:hc + 1], oka2[:, hc:hc + 1], ALU.mult, ALU.add, [tmpa, rw, oka2], [tmpa])
                        yield
                        c.tt('dve', tmpa[:], tmpa[:], kb[:, tsl], ALU.mult, [tmpa, kb], [tmpa])
                        c.tt('dve', tmpa[:], tmpa[:], rb[:, tsl], ALU.mult, [tmpa, rb], [tmpa])
                        yield
                        c.ts('dve', R(opr[:]), tmpa[:], rw[:, 2, hc:hc + 1], None, ALU.mult, None, [tmpa, rw], [opr])
                        yield
                        c.mm(P1[:, 0:BW], R(blk_r[:]), R(opr[:]), True, True, [blk_r, opr], [P1])
                        yield
                        c.tt('dve', tmpa[:], P1[:, 0:BW], vb[:, tsl], ALU.mult, [P1, vb], [tmpa])
                        yield
                        c.stt(G2[:, lsl], tmpa[:], rw[:, 4, hc:hc + 1], P2[:, 0:BW], ALU.add, ALU.mult, [tmpa, rw, P2], [G2])
                        yield

                run_threads([prepass_g(0), prepass_g(1)])
                c.barrier()
                run_threads([dir_g(0, hc), dir_g(1, hc)])
                c.barrier()
                def rwkv_out_g(par_, hc=hc, cs_=cs_):
                    gst, yn, obf, otf = gst2[par_], yn2[par_], obf2[par_], otf2[par_]
                    bkA, bkB = PS[par_], PS[2 + par_]
                    pbk = psb(2 + par_)
                    for lt in range(par_, 0 if os.environ.get('K_NOOUT') else 16, 2):
                        for hh in range(2):
                            rows = slice(hh * 64, hh * 64 + 64)
                            c.op('dve', lambda e, hh=hh, rows=rows: e.bn_stats(out=gst[:, hh * 6:(hh + 1) * 6], in_=y_acc[:, lt, rows]), reads=[y_acc], writes=[gst])
                            c.op('dve', lambda e, hh=hh: e.bn_aggr(out=gst[:, 12 + hh * 2:14 + hh * 2], in_=gst[:, hh * 6:(hh + 1) * 6]), reads=[gst], writes=[gst])
                        yield
                        c.ts('dve', gst[:, 16:18], gst[:, 13:16:2], LNX_EPS, None, ALU.add, None, [gst], [gst])
                        yield
                        c.act(gst[:, 16:18], gst[:, 16:18], AF.Sqrt, [gst], [gst])
                        yield
                        c.op('dve', lambda e: e.reciprocal(out=gst[:, 18:20], in_=gst[:, 16:18]), reads=[gst], writes=[gst])
                        for hh in range(2):
                            rows = slice(hh * 64, hh * 64 + 64)
                            c.ts('dve', yn[:, rows], y_acc[:, lt, rows], gst[:, 12 + hh * 2:13 + hh * 2], gst[:, 18 + hh:19 + hh], ALU.subtract, ALU.mult,
                                 [y_acc, gst], [yn])
                        yield
                        c.tr(bkA[:, 0:128], yn[:], ident[:], [yn, ident], [bkA])
                        yield
                        lsl = slice(lt * 128, (lt + 1) * 128)
                        c.stt(otf[:], bkA[:, 0:128], rw[:, 3, hc:hc + 1], G1[:, lsl], ALU.mult, ALU.mult, [bkA, rw, G1], [otf])
                        c.tt('dve', obf[:], otf[:], G2[:, lsl], ALU.add, [otf, G2], [obf])
                        yield
                        c.tr(pbk[:, 0:128], obf[:], identb[:], [obf, identb], [bkB])
                        yield
                        c.cp('act', ob_hc[:, lt, :], pbk[:, 0:128], [bkB], [ob_hc])
                        yield

                run_threads([rwkv_out_g(0), rwkv_out_g(1)])
                for q4 in range(4):
                    c.dma(scr_ob_v[:, q4 * 4:(q4 + 1) * 4, cs_], ob_hc[:, q4 * 4:(q4 + 1) * 4, :], reads=[ob_hc], writes=[scrw])
            c.barrier()

        scrob_buf = scrw
        scrh_buf = Buf(None, 'scr_h_dram')
        esF = ExitStack()
        with esF:
            tT = c.sb(esF, [128, 8, SEQ], BF16, 'tT')
            cwT = c.sb(esF, [32, SEQ], F32, 'cwT')
            with ExitStack() as esY:
                yT = c.sb(esY, [128, 8, SEQ], BF16, 'yT')
                with ExitStack() as esM1:
                    woa = c.sb(esM1, [128, 4, D], BF16, 'woa')
                    wob = c.sb(esM1, [128, 4, D], BF16, 'wob')
                    c.dma(woa[:], w_o_a.rearrange("(cc p) n -> p cc n", p=128), writes=[woa], q='pool')
                    c.dma(wob[:], w_o_b.rearrange("(cc p) n -> p cc n", p=128), writes=[wob], q='pool')
                    uTb = c.sb(esM1, [128, 8, 512], BF16, 'uTb')
                    obTb = c.sb(esM1, [128, 4, 512], BF16, 'obTb')
                    obt = [c.sb(esM1, [128, 512], BF16, 'obt%d' % i) for i in range(2)]
                    wg_ = [c.sb(esM1, [128, 8, 128], BF16, 'wgm%d' % i) for i in range(4)]
                    sga2 = [c.sb(esM1, [128, 512], F32, 'sga%d' % i) for i in range(2)]
                    sgb2 = [c.sb(esM1, [128, 512], F32, 'sgb%d' % i) for i in range(2)]
                    ya2 = [c.sb(esM1, [128, 512], F32, 'ya%d' % i) for i in range(2)]
                    yb2 = [c.sb(esM1, [128, 512], F32, 'yb%d' % i) for i in range(2)]
                    ob_v = scr_ob.rearrange("(w r) cc -> r w cc", r=32)
                    k = 0
                    wn = 0
                    for tb in range(4):
                        c.dma(uTb[:], scr_u[:, :, tb * 512:(tb + 1) * 512], reads=[scru_buf], writes=[uTb])
                        for j in range(4):
                            i = tb * 4 + j
                            o_ = obt[i % 2]
                            c.dma(o_[0:64, :], ob_v[2 * i], reads=[scrob_buf], writes=[o_])
                            c.dma(o_[64:128, :], ob_v[2 * i + 1], reads=[scrob_buf], writes=[o_])
                            pb = psb(5 + i % 2)
                            for hc in range(4):
                                c.tr(pb[:, hc * 128:(hc + 1) * 128], o_[:, hc * 128:(hc + 1) * 128], identb[:], [o_, identb], [PS[5 + i % 2]],
                                     inc=(hc == 3))
                            c.cp('act', obTb[:, :, j * 128:(j + 1) * 128], pb[:, 0:512].rearrange("p (a b) -> p a b", a=4), [PS[5 + i % 2]], [obTb])
                        tsl = slice(tb * 512, (tb + 1) * 512)
                        for fc in range(8):
                            wa_ = wg_[wn % 4]
                            wb_ = wg_[(wn + 1) % 4]
                            wn += 2
                            ga0 = A_COLS + B_COLS + fc * 128
                            c.dma(wa_[:], w_in_v[:, :, ga0:ga0 + 128], writes=[wa_], q='pool')
                            c.dma(wb_[:], w_in_v[:, :, ga0 + D:ga0 + D + 128], writes=[wb_], q='pool')
                            fsl = slice(fc * 128, (fc + 1) * 128)
                            fp_ = fc % 2
                            Q0, Q1, Q2, Q3 = (PS[4 * fp_ + j_] for j_ in range(4))
                            sga, sgb, ya, yb = sga2[fp_], sgb2[fp_], ya2[fp_], yb2[fp_]
                            for kc in range(8):
                                c.mm(Q2[:, :], wa_[:, kc, :], uTb[:, kc, :], kc == 0, kc == 7, [wa_, uTb], [Q2])
                            for kc in range(8):
                                c.mm(Q3[:, :], wb_[:, kc, :], uTb[:, kc, :], kc == 0, kc == 7, [wb_, uTb], [Q3])
                            for cc in range(4):
                                c.mm(Q0[:, :], woa[:, cc, fsl], oaT[:, cc, tsl], cc == 0, cc == 3, [woa, oaT], [Q0])
                            for cc in range(4):
                                c.mm(Q1[:, :], wob[:, cc, fsl], obTb[:, cc, :], cc == 0, cc == 3, [wob, obTb], [Q1])
                            c.act(sga[:], Q2[:, :], AF.Sigmoid, [Q2], [sga])
                            c.act(sgb[:], Q3[:, :], AF.Sigmoid, [Q3], [sgb])
                            c.tt('dve', ya[:], Q0[:, :], sga[:], ALU.mult, [Q0, sga], [ya])
                            c.tt('dve', yb[:], Q1[:, :], sgb[:], ALU.mult, [Q1, sgb], [yb])
                            c.tt('pool', yT[:, fc, tsl], ya[:], yb[:], ALU.add, [ya, yb], [yT])
                    c.barrier()
                if 'd_yT' in T:
                    c.dma(T['d_yT'], yT[:], reads=[yT])
                with ExitStack() as esM2:
                    wout = c.sb(esM2, [128, 8, D], BF16, 'wout')
                    c.dma(wout[:, 0:4, :], w_out.rearrange("(cc p) n -> p cc n", p=128)[:, 0:4, :], writes=[wout], q='pool')
                    c.dma(wout[:, 4:8, :], w_out.rearrange("(cc p) n -> p cc n", p=128)[:, 4:8, :], writes=[wout], q='pool')
                    Bg1 = c.sb(esM2, [128, D], F32, 'Bg1')
                    make_Bg(esM2, Bg1, 16)
                    wr32 = c.sb(esM2, [128, 8, 36], F32, 'wr32')
                    rbb = c.sb(esM2, [128, 36], F32, 'rbb')
                    c.dma(wr32[:], wrt, writes=[wr32])
                    c.dma(rbb[:], rb_bc, writes=[rbb])
                    Xh = [c.sb(esM2, [128, D], F32, 'Xh%d' % i) for i in range(2)]
                    Hh = [c.sb(esM2, [128, D], F32, 'Hh%d' % i) for i in range(2)]
                    hn2 = [c.sb(esM2, [128, D], F32, 'hn%d' % i) for i in range(2)]
                    sqh2 = [c.sb(esM2, [128, D], BF16, 'sqh%d' % i) for i in range(2)]
                    t322 = [c.sb(esM2, [128, 8, 128], F32, 't32%d' % i) for i in range(2)]
                    st2 = [c.sb(esM2, [128, 64], F32, 'st%d' % i) for i in range(2)]
                    lg2 = [c.sb(esM2, [128, 36], F32, 'lg%d' % i) for i in range(2)]
                    tm32 = [c.sb(esM2, [128, 32], F32, 'tm3%d' % i) for i in range(2)]
                    cw2 = [c.sb(esM2, [128, 32], F32, 'cw%d' % i) for i in range(2)]

                    def tile_chain(t):
                        hn, sqh, t32, st, lg, tm3, cw = hn2[t], sqh2[t], t322[t], st2[t], lg2[t], tm32[t], cw2[t]
                        B = [PS[4 * t + j] for j in range(4)]
                        X_, H_ = Xh[t], Hh[t]
                        for i in range(t, 16, 2):
                            c.dma(X_[:], x[i * 128:(i + 1) * 128, :], writes=[X_])
                            for nh in range(2):
                                bank = B[nh]
                                for fc in range(8):
                                    c.mm(bank[:, :], yT[:, fc, i * 128:(i + 1) * 128], wout[:, fc, nh * 512:(nh + 1) * 512], fc == 0, fc == 7, [yT, wout], [bank])
                                hs = slice(nh * 512, (nh + 1) * 512)
                                c.tt('dve', H_[:, hs], bank[:, :], Bg1[:, hs], ALU.mult, [bank, Bg1], [H_])
                                yield
                            c.tt('pool', H_[:], H_[:], X_[:], ALU.add, [H_, X_], [H_])
                            yield
                            c.dma(scr_h[i * 128:(i + 1) * 128, :], H_[:], reads=[H_], writes=[scrh_buf])
                            c.act(sqh[:], H_[:], AF.Square, [H_], [sqh, st], accum_out=st[:, 0:1])
                            yield
                            c.ts('dve', st[:, 1:2], st[:, 0:1], 1.0 / D, EPS, ALU.mult, ALU.add, [st], [st])
                            c.act(st[:, 2:3], st[:, 1:2], AF.Sqrt, [st], [st])
                            yield
                            c.op('dve', lambda e: e.reciprocal(out=st[:, 3:4], in_=st[:, 2:3]), reads=[st], writes=[st])
                            c.ts('dve', hn[:], H_[:], st[:, 3:4], None, ALU.mult, None, [H_, st], [hn])
                            yield
                            for fc in range(8):
                                bank = B[2 + fc // 4]
                                c.tr(bank[:, (fc % 4) * 128:(fc % 4 + 1) * 128], hn[:, fc * 128:(fc + 1) * 128], ident[:], [hn, ident], [bank],
                                     inc=(fc % 4 == 3))
                            yield
                            for fc in range(8):
                                bank = B[2 + fc // 4]
                                i_ = bank[:, (fc % 4) * 128:(fc % 4 + 1) * 128]
                                c.ts('dve', t32[:, fc, :], i_, A2g[:, fc, 0:1], mT[:, 24 + fc, 0:1], ALU.mult, ALU.add, [bank, A2g, mT], [t32])
                                c.cp('act', tT[:, fc, i * 128:(i + 1) * 128], t32[:, fc, :], [t32], [tT])
                                if fc % 4 == 3:
                                    yield
                            for kc in range(8):
                                c.mm(B[0][:, 0:36], t32[:, kc, :], wr32[:, kc, :], kc == 0, kc == 7, [t32, wr32], [B[0]])
                            yield
                            c.tt('dve', lg[:], B[0][:, 0:36], rbb[:], ALU.add, [B[0], rbb], [lg])
                            c.op('dve', lambda e: e.tensor_reduce(out=st[:, 8:9], in_=lg[:, 0:4], axis=AX.X, op=ALU.max), reads=[lg], writes=[st])
                            c.ts('dve', st[:, 9:10], st[:, 8:9], -1.0, None, ALU.mult, None, [st], [st])
                            yield
                            c.act(st[:, 16:20], lg[:, 0:4], AF.Exp, [lg, st], [st], bias=st[:, 9:10], accum_out=st[:, 10:11])
                            yield
                            c.op('dve', lambda e: e.reciprocal(out=st[:, 11:12], in_=st[:, 10:11]), reads=[st], writes=[st])
                            c.ts('dve', st[:, 20:24], lg[:, 0:4], st[:, 8:9], None, ALU.is_ge, None, [lg, st], [st])
                            c.tt('dve', tm3[:].rearrange("p (g e) -> p g e", g=4), lg[:, 4:36].rearrange("p (g e) -> p g e", g=4),
                                 st[:, 20:24].unsqueeze(2).to_broadcast([128, 4, 8]), ALU.mult, [lg, st], [tm3])
                            yield
                            c.op('dve', lambda e: e.tensor_reduce(out=st[:, 24:32], in_=tm3[:].rearrange("p (g e) -> p e g", g=4), axis=AX.X, op=ALU.add),
                                 reads=[tm3], writes=[st])
                            c.op('dve', lambda e: e.max(out=st[:, 32:40], in_=st[:, 24:32]), reads=[st], writes=[st])
                            c.tt('dve', st[:, 40:41], st[:, 33:34], st[:, 32:33], ALU.subtract, [st], [st])
                            yield
                            c.act(st[:, 41:42], st[:, 40:41], AF.Exp, [st], [st])
                            yield
                            c.ts('dve', st[:, 42:43], st[:, 41:42], 1.0, None, ALU.add, None, [st], [st])
                            c.op('dve', lambda e: e.reciprocal(out=st[:, 43:44], in_=st[:, 42:43]), reads=[st], writes=[st])
                            c.tt('dve', st[:, 44:45], st[:, 43:44], st[:, 11:12], ALU.mult, [st], [st])
                            yield
                            c.tt('dve', st[:, 45:46], st[:, 44:45], st[:, 41:42], ALU.mult, [st], [st])
                            c.tt('dve', st[:, 46:47], st[:, 44:45], st[:, 45:46], ALU.subtract, [st], [st])
                            c.ts('dve', st[:, 48:56], st[:, 24:32], st[:, 32:33], st[:, 46:47], ALU.is_ge, ALU.mult, [st], [st])
                            yield
                            c.ts('dve', st[:, 56:64], st[:, 24:32], st[:, 33:34], st[:, 45:46], ALU.is_ge, ALU.mult, [st], [st])
                            c.tt('dve', st[:, 48:56], st[:, 48:56], st[:, 56:64], ALU.add, [st], [st])
                            c.tt('dve', cw[:].rearrange("p (g e) -> p g e", g=4), st[:, 20:24].unsqueeze(2).to_broadcast([128, 4, 8]),
                                 st[:, 48:56].unsqueeze(1).to_broadcast([128, 4, 8]), ALU.mult, [st], [cw])
                            yield
                            c.tr(B[1][0:32, 0:128], cw[:], ident[:], [cw, ident], [B[1]])
                            yield
                            c.cp('act', R(cwT[:, i * 128:(i + 1) * 128]), B[1][0:32, 0:128], [B[1]], [cwT])
                            yield

                    run_threads([tile_chain(0), tile_chain(1)])
                    c.barrier()
            if 'd_tT' in T:
                c.dma(T['d_tT'], tT[:], reads=[tT])
            if 'd_cwT' in T:
                c.dma(T['d_cwT'], cwT[:], reads=[cwT])

            with ExitStack() as esE:
                moe_acc = c.sb(esE, [128, 16, D], F32, 'moe_acc')
                macc = [[Buf(None, 'macc%d_%d' % (t_, n_)) for n_ in range(2)] for t_ in range(16)]
                evt = [c.sb(esE, [128, 512], F32, 'evt%d' % i) for i in range(3)]
                wgb = [c.sb(esE, [128, 8, 256], BF16, 'wgb%d' % i) for i in range(2)]
                wub = [c.sb(esE, [128, 8, 256], BF16, 'wub%d' % i) for i in range(2)]
                wdb = [c.sb(esE, [128, 2, D], BF16, 'wdb%d' % i) for i in range(2)]
                selt = [c.sb(esE, [32, 128], F32, 'selt%d' % i) for i in range(2)]
                sg_ = [c.sb(esE, [128, 512], F32, 'sg%d' % i) for i in range(2)]
                hu_ = [c.sb(esE, [128, 512], F32, 'hu%d' % i) for i in range(2)]
                hid = [c.sb(esE, [128, 2, 512], BF16, 'hid%d' % i) for i in range(2)]
                NEXP = int(os.environ.get('K_NEXP', '32'))
                def moe_gu(e_, tg, k):
                    wg, wu, wd = wgb[e_ % 2], wub[e_ % 2], wdb[e_ % 2]
                    se = selt[e_ % 2]
                    if tg == 0:
                        c.dma(wg[:], moe_wg[e_], writes=[wg], q='pool')
                        c.dma(wu[:], moe_wu[e_], writes=[wu], q='pool')
                        c.dma(wd[:], moe_wd[e_], writes=[wd], q='pool')
                        c.ts('dve', R(se[:]), onesf[0:32, :], ident[0:32, e_:e_ + 1], None, ALU.mult, None, [onesf, ident], [se])
                    tsl = slice(tg * 512, (tg + 1) * 512)
                    hd = hid[k % 2]
                    c.mm(PS[4][:, :], R(se[:]), R(cwT[:, tsl]), True, True, [se, cwT], [PS[4]])
                    for f2 in range(2):
                        fs = slice(f2 * 128, (f2 + 1) * 128)
                        for kc in range(8):
                            c.mm(PS[f2][:, :], wg[:, kc, fs], tT[:, kc, tsl], kc == 0, kc == 7, [wg, tT], [PS[f2]])
                        for kc in range(8):
                            c.mm(PS[2 + f2][:, :], wu[:, kc, fs], tT[:, kc, tsl], kc == 0, kc == 7, [wu, tT], [PS[2 + f2]])
                        c.act(sg_[f2][:], PS[f2][:, :], AF.Silu, [PS[f2]], [sg_[f2]])
                        c.tt('dve', hu_[f2][:], PS[2 + f2][:, :], sg_[f2][:], ALU.mult, [PS[2 + f2], sg_[f2]], [hu_[f2]])
                        c.tt('dve', hd[:, f2, :], PS[4][:, :], hu_[f2][:], ALU.mult, [PS[4], hu_[f2]], [hd])

                def moe_dn(e_, tg, k):
                    wd = wdb[e_ % 2]
                    hd = hid[k % 2]
                    for tt_ in range(4):
                        tile = tg * 4 + tt_
                        for nh in range(2):
                            bank = PS[5 + (tt_ * 2 + nh) % 3]
                            for f2 in range(2):
                                c.mm(bank[:, :], hd[:, f2, tt_ * 128:(tt_ + 1) * 128], wd[:, f2, nh * 512:(nh + 1) * 512], f2 == 0, f2 == 1,
                                     [hd, wd], [bank])
                            hs = slice(nh * 512, (nh + 1) * 512)
                            ma = macc[tile][nh]
                            gi = tt_ * 2 + nh
                            if e_ == 0:
                                c.cp('act', moe_acc[:, tile, hs], bank[:, :], [bank], [ma])
                            elif gi % 2 == 0:
                                c.tt('dve', moe_acc[:, tile, hs], bank[:, :], moe_acc[:, tile, hs], ALU.add, [bank, ma], [ma])
                            else:
                                ev = evt[(gi // 2) % 3]
                                c.cp('act', ev[:], bank[:, :], [bank], [ev])
                                c.tt('pool', moe_acc[:, tile, hs], moe_acc[:, tile, hs], ev[:], ALU.add, [ma, ev], [ma])

                its = [(e_, tg) for e_ in range(NEXP) for tg in range(4)]
                for k, (e_, tg) in enumerate(its):
                    moe_gu(e_, tg, k)
                    if k > 0:
                        moe_dn(its[k - 1][0], its[k - 1][1], k - 1)
                moe_dn(its[-1][0], its[-1][1], len(its) - 1)
                Bg2 = c.sb(esE, [128, D], F32, 'Bg2')
                make_Bg(esE, Bg2, 40)
                gfin = c.sb(esE, [128, D], F32, 'gfin')
                c.dma(gfin[:], gfin_bc, writes=[gfin])
                Hf = [c.sb(esE, [128, D], F32, 'Hf%d' % i) for i in range(2)]
                sf = [c.sb(esE, [128, 4], F32, 'sf%d' % i) for i in range(2)]
                c.barrier()
                Hm = [Buf(wgb[i].t.bitcast(F32).rearrange("p a b -> p (a b)"), 'Hm%d' % i) for i in range(2)]
                sqf2 = [Buf(wub[i].t.rearrange("p a b -> p (a b)"), 'sqf%d' % i) for i in range(2)]

                def fin_g(par_):
                    H_, s_, Hm_, sq_ = Hf[par_], sf[par_], Hm[par_], sqf2[par_]
                    for i in range(par_, 16, 2):
                        c.dma(H_[:], scr_h[i * 128:(i + 1) * 128, :], reads=[scrh_buf], writes=[H_])
                        c.tt('dve', Hm_[:], moe_acc[:, i, :], Bg2[:], ALU.mult, [macc[i][0], macc[i][1], Bg2], [Hm_])
                        yield
                        c.tt('pool', H_[:], H_[:], Hm_[:], ALU.add, [H_, Hm_], [H_])
                        yield
                        c.act(sq_[:, 0:D], H_[:], AF.Square, [H_], [sq_, s_], accum_out=s_[:, 0:1])
                        yield
                        c.ts('dve', s_[:, 1:2], s_[:, 0:1], 1.0 / D, EPS, ALU.mult, ALU.add, [s_], [s_])
                        yield
                        c.act(s_[:, 2:3], s_[:, 1:2], AF.Sqrt, [s_], [s_])
                        yield
                        c.op('dve', lambda e, s_=s_: e.reciprocal(out=s_[:, 3:4], in_=s_[:, 2:3]), reads=[s_], writes=[s_])
                        c.stt(H_[:], H_[:], s_[:, 3:4], gfin[:], ALU.mult, ALU.mult, [H_, s_, gfin], [H_])
                        yield
                        c.dma(out[i * 128:(i + 1) * 128, :], H_[:], reads=[H_])
                        yield

                run_threads([fin_g(0), fin_g(1)])
                c.barrier()

        c.finish()
        print("ninstr", c.ninstr, {k_: v for k_, v in c.cnt.items()})
    return nc


def prep_inputs(inp):
    f = lambda a: np.ascontiguousarray(a, dtype=np.float32)
    fm = lambda v: f(np.asarray(v).reshape(-1, 128).T)
    shared = {
        "ada_w": f(inp["ada_w"][0]),
        "ada_bT": fm(inp["ada_b"][0]),
        "gmixT": fm(inp["norm_mix_g"][0]),
        "gffnT": fm(inp["norm_ffn_g"][0]),
        "gfin_bc": f(np.broadcast_to(inp["final_norm_g"][None, :], (128, D))),
        "w_in": f(inp["w_in"][0]),
        "convT": f(inp["gdn_conv"][0].T.reshape(12, 128, 5).transpose(1, 0, 2)),
        "alog_bc": f(np.broadcast_to(inp["gdn_a_log"][0].reshape(1, 1, 8), (128, 18, 8))),
        "dtb_bc": f(np.broadcast_to(inp["gdn_dt_bias"][0].reshape(1, 1, 8), (128, 18, 8))),
        "onormT": f(inp["gdn_onorm_g"][0].reshape(128, 1)),
        "muT": fm(inp["rwkv_mu"][0]),
        "w0T": f(inp["rwkv_w0"][0].reshape(2, 4, 128).transpose(2, 0, 1)),
        "a0T": f(inp["rwkv_a0"][0].reshape(2, 4, 128).transpose(2, 0, 1)),
        "w2m": f(inp["rwkv_w2"][0].reshape(128, 512)),
        "a2m": f(inp["rwkv_a2"][0].reshape(128, 512)),
        "g2m": f(inp["rwkv_g2"][0]),
        "w_o_a": f(inp["w_o_a"][0]),
        "w_o_b": f(inp["w_o_b"][0]),
        "w_out": f(inp["w_out"][0]),
        "wrt": f(np.concatenate([inp["router_grp"][0], inp["router_exp"][0]], axis=1).reshape(8, 128, 36).transpose(1, 0, 2)),
        "rb_bc": f(np.broadcast_to(np.concatenate([inp["router_grp_b"][0], inp["router_exp_b"][0]])[None, :], (128, 36))),
        "moe_wg": f(np.asarray(inp["moe_w_gate"][0]).reshape(32, 8, 128, 256).transpose(0, 2, 1, 3)),
        "moe_wu": f(np.asarray(inp["moe_w_up"][0]).reshape(32, 8, 128, 256).transpose(0, 2, 1, 3)),
        "moe_wd": f(np.asarray(inp["moe_w_down"][0]).reshape(32, 2, 128, D).transpose(0, 2, 1, 3)),
        "rwv": f(np.stack([fm(inp["rwkv_k_k"][0]), fm(inp["rwkv_k_a"][0]), fm(inp["rwkv_r_k"][0].reshape(-1)),
                           fm(inp["rwkv_lnx_g"][0]), fm(inp["rwkv_lnx_b"][0])], axis=1)),
    }
    maps = []
    for b in range(NCORES):
        m = dict(shared)
        m["x"] = f(inp["x"][b])
        m["ctx"] = f(inp["ctx"][b])
        m["cT"] = f(np.stack([fm(inp["c"][b]), fm(inp["c_ctx"])], axis=-1))
        maps.append(m)
    return maps


def kernel(**inputs):
    maps = prep_inputs(inputs)
    nc = build()
    res = run_bass_kernel_spmd(nc, maps, core_ids=list(range(NCORES)))
    return np.stack([np.asarray(r["out"]) for r in res.results], axis=0).astype(np.float32)
```

```python
import os
import numpy as np
import concourse.bass as bass
import concourse.mybir as mybir
from concourse.bass_utils import run_bass_kernel_spmd
from concourse.alu_op_type import AluOpType as ALU
from contextlib import ExitStack

F32 = mybir.dt.float32
F32R = mybir.dt.float32r
BF16 = mybir.dt.bfloat16
AF = mybir.ActivationFunctionType
AX = mybir.AxisListType

NCORES = 8
D = 1024
SEQ = 2048
CTX = 256
NTOK = SEQ + CTX
IN_COLS = 6032
A_COLS = 2064
B_COLS = 1920
EPS = 1e-6
LNX_EPS = 1e-5 * 64


class Buf:
    def __init__(self, t, name, psum=False):
        self.t = t
        self.name = name
        self.lw = None
        self.rd = {}
        self.psum = psum
        self.bankrd = None

    def __getitem__(self, idx):
        return self.t[idx]


class Ctx:
    ENG = ['pe', 'dve', 'act', 'pool', 'sp']
    NDMA = 8

    def __init__(self, nc, es):
        self.nc = nc
        self.e = {'pe': nc.tensor, 'dve': nc.vector, 'act': nc.scalar, 'pool': nc.gpsimd, 'sp': nc.sync}
        self.sem = {}
        self.cnt = {}
        for n in self.ENG:
            self.sem[n] = es.enter_context(nc.semaphore('s_' + n))
            self.cnt[n] = 0
        for i in range(self.NDMA):
            n = 'd%d' % i
            self.sem[n] = es.enter_context(nc.semaphore('s_' + n))
            self.cnt[n] = 0
        self.dma_rr = 0
        self.waited = {n: {} for n in self.ENG}
        self.nbuf = 0
        self.ninstr = 0

    def sb(self, es, shape, dt=F32, name=None):
        self.nbuf += 1
        name = (name or 'b') + '_%d' % self.nbuf
        t = es.enter_context(self.nc.sbuf_tensor(name, list(shape), dt))
        return Buf(t, name)

    def ps(self, es, shape, dt=F32, name=None):
        self.nbuf += 1
        name = (name or 'p') + '_%d' % self.nbuf
        t = es.enter_context(self.nc.psum_tensor(name, list(shape), dt))
        return Buf(t, name, psum=True)

    def view(self, buf, name='v'):
        self.nbuf += 1
        return Buf(buf.t, name + '_%d' % self.nbuf)

    def _deps(self, reads, writes):
        deps = {}

        def add(k, v):
            if v > deps.get(k, 0):
                deps[k] = v
        for b in reads:
            if b.lw:
                add(*b.lw)
            if b.psum:
                for k, v in b.rd.items():
                    add(k, v)
                if b.bankrd is not None:
                    for k, v in b.bankrd.items():
                        add(k, v)
        for b in writes:
            if b.lw:
                add(*b.lw)
            for k, v in b.rd.items():
                add(k, v)
        return deps

    def _wait(self, E, deps):
        eng = self.e[E]
        w = self.waited[E]
        nw = 0
        for k, v in deps.items():
            if k == E and E == 'pe' and v > self.cnt['pe']:
                continue
            if w.get(k, 0) >= v:
                continue
            eng.wait_ge(self.sem[k], v)
            nw += 1
            w[k] = v

    def op(self, E, fn, reads=(), writes=(), inc=True):
        deps = self._deps(reads, writes)
        self._wait(E, deps)
        ins = fn(self.e[E])
        self.ninstr += 1
        if inc:
            self.cnt[E] += 1
            ins.then_inc(self.sem[E], 1)
            cval = self.cnt[E]
        else:
            cval = self.cnt[E] + 1
        for b in writes:
            b.lw = (E, cval)
            b.rd = {}
        for b in reads:
            if b not in writes:
                b.rd[E] = max(b.rd.get(E, 0), cval)
            if b.bankrd is not None and E != 'pe':
                b.bankrd[E] = max(b.bankrd.get(E, 0), cval)
        return ins

    def dma(self, out, in_, reads=(), writes=(), q='sp', **kw):
        slot = 'd%d' % self.dma_rr
        self.dma_rr = (self.dma_rr + 1) % self.NDMA
        deps = self._deps(reads, writes)
        if self.cnt[slot] > 0:
            deps[slot] = max(deps.get(slot, 0), self.cnt[slot])
        self._wait(q, deps)
        ins = self.e[q].dma_start(out=out, in_=in_, **kw)
        self.ninstr += 1
        self.cnt[slot] += 16
        ins.then_inc(self.sem[slot], 16)
        cval = self.cnt[slot]
        for b in writes:
            b.lw = (slot, cval)
            b.rd = {}
        for b in reads:
            b.rd[slot] = max(b.rd.get(slot, 0), cval)
        return ins

    def barrier(self):
        for E in self.ENG:
            deps = {k: v for k, v in self.cnt.items() if v > 0 and k != E}
            self._wait(E, deps)

    def finish(self):
        for k in self.sem:
            if k.startswith('d') and self.cnt[k] > 0:
                self.e['sp'].wait_ge(self.sem[k], self.cnt[k])

    def mm(self, out, lhsT, rhs, start, stop, reads, writes, inc=None):
        if inc is None:
            inc = stop
        return self.op('pe', lambda e: e.matmul(out, lhsT=lhsT, rhs=rhs, start=start, stop=stop),
                       reads=reads, writes=writes, inc=inc)

    def tr(self, out, in_, ident, reads, writes, inc=True):
        return self.op('pe', lambda e: e.transpose(out=out, in_=in_, identity=ident), reads=reads, writes=writes, inc=inc)

    def act(self, out, in_, func, reads, writes, E='act', **kw):
        return self.op('act', lambda e: e.activation(out=out, in_=in_, func=func, **kw), reads=reads, writes=writes)

    def ts(self, E, out, in0, s1, s2, op0, op1, reads, writes):
        if op1 is None:
            return self.op(E, lambda e: e.tensor_scalar(out=out, in0=in0, scalar1=s1, scalar2=None, op0=op0), reads=reads, writes=writes)
        return self.op(E, lambda e: e.tensor_scalar(out=out, in0=in0, scalar1=s1, scalar2=s2, op0=op0, op1=op1), reads=reads, writes=writes)

    def tt(self, E, out, in0, in1, op, reads, writes):
        return self.op(E, lambda e: e.tensor_tensor(out=out, in0=in0, in1=in1, op=op), reads=reads, writes=writes)

    def stt(self, out, in0, scalar, in1, op0, op1, reads, writes):
        return self.op('dve', lambda e: e.scalar_tensor_tensor(out=out, in0=in0, scalar=scalar, in1=in1, op0=op0, op1=op1),
                       reads=reads, writes=writes)

    def cp(self, E, out, in_, reads, writes):
        if E == 'act':
            return self.op('act', lambda e: e.copy(out=out, in_=in_), reads=reads, writes=writes)
        return self.op(E, lambda e: e.tensor_copy(out=out, in_=in_), reads=reads, writes=writes)


def R(ap):
    return ap.bitcast(F32R)


def build(dbg=(), stage=99):
    nc = bass.Bass("TRN2", target_bir_lowering=False)
    T = {}

    def din(name, shape, dt=F32):
        T[name] = nc.dram_tensor(name, list(shape), dt, kind="ExternalInput").ap()
        return T[name]

    def dout(name, shape, dt=F32):
        T[name] = nc.dram_tensor(name, list(shape), dt, kind="ExternalOutput").ap()
        return T[name]

    x = din("x", [SEQ, D])
    ctx = din("ctx", [CTX, D])
    cT = din("cT", [128, 8, 2])
    ada_w = din("ada_w", [D, 6 * D])
    ada_bT = din("ada_bT", [128, 48])
    gmixT = din("gmixT", [128, 8])
    gffnT = din("gffnT", [128, 8])
    gfin_bc = din("gfin_bc", [128, D])
    w_in = din("w_in", [D, IN_COLS])
    convT = din("convT", [128, 12, 5])
    alog_bc = din("alog_bc", [128, 18, 8])
    dtb_bc = din("dtb_bc", [128, 18, 8])
    onormT = din("onormT", [128, 1])
    muT = din("muT", [128, 15])
    w0T = din("w0T", [128, 2, 4])
    a0T = din("a0T", [128, 2, 4])
    w2m = din("w2m", [128, 512])
    a2m = din("a2m", [128, 512])
    g2m = din("g2m", [128, 512])
    rwv = din("rwv", [128, 5, 4])
    scr_ob = nc.dram_tensor("scr_ob", [SEQ, 512], BF16, kind="Internal").ap()
    scr_h = nc.dram_tensor("scr_h", [SEQ, D], F32, kind="Internal").ap()
    scr_u = nc.dram_tensor("scr_u", [128, 8, SEQ], BF16, kind="Internal").ap()
    w_o_a = din("w_o_a", [512, D])
    w_o_b = din("w_o_b", [512, D])
    w_out = din("w_out", [D, D])
    wrt = din("wrt", [128, 8, 36])
    rb_bc = din("rb_bc", [128, 36])
    moe_wg = din("moe_wg", [32, 128, 8, 256])
    moe_wu = din("moe_wu", [32, 128, 8, 256])
    moe_wd = din("moe_wd", [32, 128, 2, D])
    out = dout("out", [SEQ, D])
    for name, shape, dt in dbg:
        dout(name, shape, dt)

    with ExitStack() as es:
        c = Ctx(nc, es)
        PS = [c.ps(es, [128, 512], F32, 'ps%d' % i) for i in range(8)]

        def psb(i):
            return PS[i][:].bitcast(BF16)

        bank_reads = [dict() for _ in range(8)]

        class Reg:
            def __init__(self, bank, c0, n, name):
                self.bank, self.c0, self.n = bank, c0, n
                self.buf = PS[bank]

            def ap(self, lo=0, hi=None, rows=slice(None)):
                hi = self.n if hi is None else hi
                return PS[self.bank].t[rows, self.c0 + lo:self.c0 + hi]

        def run_threads(gens):
            gens = list(gens)
            while gens:
                for g_ in list(gens):
                    try:
                        next(g_)
                    except StopIteration:
                        gens.remove(g_)

        def par(*gens):
            gens = list(gens)
            while gens:
                for g_ in list(gens):
                    try:
                        next(g_)
                    except StopIteration:
                        gens.remove(g_)
                        continue
                    yield

        ident = c.sb(es, [128, 128], F32, 'ident')
        identb = c.sb(es, [128, 128], BF16, 'identb')
        onesf = c.sb(es, [128, 128], F32, 'onesf')
        c.op('pool', lambda e: e.memset(ident[:], 0.0), writes=[ident])
        c.op('pool', lambda e: e.affine_select(out=ident[:], in_=ident[:], pattern=[[-1, 128]], compare_op=ALU.not_equal,
                                                fill=1.0, base=0, channel_multiplier=1), reads=[ident], writes=[ident])
        c.cp('dve', identb[:], ident[:], [ident], [identb])
        c.op('pool', lambda e: e.memset(onesf[:], 1.0), writes=[onesf])
        onesb = c.sb(es, [128, 128], BF16, 'onesb')
        c.cp('dve', onesb[:], onesf[:], [onesf], [onesb])
        ones_r = c.sb(es, [128, 128], F32, 'ones_r')
        nones_r = c.sb(es, [128, 128], F32, 'nones_r')
        ident_r = c.sb(es, [128, 128], F32, 'ident_r')
        c.cp('dve', R(ones_r[:]), onesf[:], [onesf], [ones_r])
        c.ts('dve', R(nones_r[:]), onesf[:], -1.0, None, ALU.mult, None, [onesf], [nones_r])
        c.cp('dve', R(ident_r[:]), ident[:], [ident], [ident_r])
        blk = c.sb(es, [128, 128], F32, 'blk')
        c.op('pool', lambda e: e.memset(blk[:], 0.0), writes=[blk])
        c.op('pool', lambda e: e.memset(blk[0:64, 0:64], 1.0), reads=[blk], writes=[blk])
        c.op('pool', lambda e: e.memset(blk[64:128, 64:128], 1.0), reads=[blk], writes=[blk])
        incl = [c.sb(es, [128, 128], F32, 'incl%d' % d) for d in range(2)]
        strict = [c.sb(es, [128, 128], F32, 'strict%d' % d) for d in range(2)]
        incl_r = [c.sb(es, [128, 128], F32, 'inclr%d' % d) for d in range(2)]
        negm_r = [c.sb(es, [128, 128], F32, 'negm%d' % d) for d in range(2)]
        blk_r = c.sb(es, [128, 128], F32, 'blk_r')
        sel_r = [c.sb(es, [128, 128], F32, 'sel%d' % k_) for k_ in range(2)]
        notI = c.sb(es, [128, 128], F32, 'notI')
        for d in range(2):
            pat = [[1, 128]] if d == 0 else [[-1, 128]]
            cm = -1 if d == 0 else 1
            c.op('pool', lambda e, d=d, pat=pat, cm=cm: e.affine_select(out=incl[d][:], in_=blk[:], pattern=pat, compare_op=ALU.is_ge,
                                                                        fill=0.0, base=0, channel_multiplier=cm), reads=[blk], writes=[incl[d]])
            c.op('pool', lambda e, d=d, pat=pat, cm=cm: e.affine_select(out=strict[d][:], in_=blk[:], pattern=pat, compare_op=ALU.is_gt,
                                                                        fill=0.0, base=0, channel_multiplier=cm), reads=[blk], writes=[strict[d]])
            c.cp('dve', R(incl_r[d][:]), incl[d][:], [incl[d]], [incl_r[d]])
            c.ts('dve', R(negm_r[d][:]), incl[d][:], 1.0e5, -1.0e5, ALU.mult, ALU.add, [incl[d]], [negm_r[d]])
        c.cp('dve', R(blk_r[:]), blk[:], [blk], [blk_r])
        c.ts('dve', notI[:], ident[:], -1.0, 1.0, ALU.mult, ALU.add, [ident], [notI])
        zf = c.sb(es, [128, 128], F32, 'zf')
        c.op('pool', lambda e: e.memset(zf[:], 0.0), writes=[zf])
        for k_ in range(2):
            c.cp('dve', R(sel_r[k_][:]), zf[:], [zf], [sel_r[k_]])
            c.cp('dve', R(sel_r[k_][k_ * 64:(k_ + 1) * 64, :]), onesf[k_ * 64:(k_ + 1) * 64, :], [onesf, sel_r[k_]], [sel_r[k_]])

        mT = c.sb(es, [128, 48, 2], F32, 'mT')
        A1g = c.sb(es, [128, 8, 2], F32, 'A1g')
        A2g = c.sb(es, [128, 8, 2], F32, 'A2g')
        oaT = c.sb(es, [128, 4, SEQ], BF16, 'oaT')

        with ExitStack() as es1:
            sT = c.sb(es1, [128, 8, 2], F32, 'sT')
            abT = c.sb(es1, [128, 48], F32, 'abT')
            gm = c.sb(es1, [128, 8], F32, 'gm')
            gf = c.sb(es1, [128, 8], F32, 'gf')
            c.dma(sT[:], cT, writes=[sT])
            c.dma(abT[:], ada_bT, writes=[abT])
            c.dma(gm[:], gmixT, writes=[gm])
            c.dma(gf[:], gffnT, writes=[gf])
            c.act(sT[:], sT[:], AF.Silu, [sT], [sT])
            Wb = [c.sb(es1, [128, 8, 512], F32, 'adaw%d' % i) for i in range(4)]
            ada_v = ada_w.rearrange("(kc p) n -> p kc n", p=128)
            for blk in range(12):
                wb = Wb[blk % 4]
                c.dma(wb[:], ada_v[:, :, blk * 512:(blk + 1) * 512], writes=[wb], q=('sp' if blk % 2 == 0 else 'act'))
                for mc in range(4):
                    col = (blk * 4 + mc) * 2
                    for kc in range(8):
                        c.mm(PS[0][:, col:col + 2], wb[:, kc, mc * 128:(mc + 1) * 128], sT[:, kc, :],
                             kc == 0, kc == 7, [wb, sT], [PS[0]])
            pv = PS[0][:, 0:96].rearrange("p (m s) -> p m s", s=2)
            for s in range(2):
                c.tt('dve', mT[:, :, s], pv[:, :, s], abT[:], ALU.add, [PS[0], abT], [mT])
            for s in range(2):
                c.stt(A1g[:, :, s], mT[:, 8:16, s], 1.0, gm[:], ALU.add, ALU.mult, [mT, gm], [A1g])
                c.stt(A2g[:, :, s], mT[:, 32:40, s], 1.0, gf[:], ALU.add, ALU.mult, [mT, gf], [A2g])
            c.barrier()

        def make_Bg(es_, Bg, base):
            dg = [c.sb(es_, [128, 128], F32, 'dg%d' % i) for i in range(2)]
            for fc in range(8):
                d_ = dg[fc % 2]
                c.ts('dve', d_[:], ident[:], mT[:, base + fc, 0:1], None, ALU.mult, None, [ident, mT], [d_])
                bank = PS[1 + fc // 4]
                c.mm(bank[:, (fc % 4) * 128:(fc % 4 + 1) * 128], onesf[:], d_[:], True, True, [onesf, d_], [bank])
            c.cp('act', Bg[:, 0:512], PS[1][:], [PS[1]], [Bg])
            c.cp('act', Bg[:, 512:1024], PS[2][:], [PS[2]], [Bg])

        scru_buf = Buf(None, 'scr_u_dram')

        def make_uT_g(srcs, s, dst, tok0, Ag, shift_base, bufs, k):
            X = bufs['X'][k % 4]
            xnb = bufs['xnb'][k % 2]
            ss = bufs['ss'][k % 2]
            sq_ = bufs['sq'][k % 2]
            for (p0, p1, ap) in srcs:
                c.dma(X[p0:p1, :], ap, writes=[X], q=('sp' if k % 2 == 0 else 'pool'))
            c.act(sq_[:], X[:], AF.Square, [X], [sq_, ss], accum_out=ss[:, 0:1])
            yield
            c.ts('dve', ss[:, 1:2], ss[:, 0:1], 1.0 / D, EPS, ALU.mult, ALU.add, [ss], [ss])
            yield
            c.act(ss[:, 2:3], ss[:, 1:2], AF.Sqrt, [ss], [ss])
            yield
            c.op('dve', lambda e: e.reciprocal(out=ss[:, 3:4], in_=ss[:, 2:3]), reads=[ss], writes=[ss])
            c.ts('dve', xnb[:], X[:], ss[:, 3:4], None, ALU.mult, None, [X, ss], [xnb])
            yield
            bank = PS[3 + k % 2]
            pb = psb(3 + k % 2)
            for fc in range(8):
                c.tr(pb[:, fc * 128:(fc + 1) * 128], xnb[:, fc * 128:(fc + 1) * 128], identb[:], [xnb, identb], [bank],
                     inc=(fc == 7))
            yield
            for fc in range(8):
                o_ = dst[:, fc, tok0:tok0 + 128]
                i_ = pb[:, fc * 128:(fc + 1) * 128]
                if fc % 2 == 0:
                    c.ts('dve', o_, i_, Ag[:, fc, s:s + 1], mT[:, shift_base + fc, s:s + 1], ALU.mult, ALU.add,
                         [bank, Ag, mT], [dst])
                else:
                    c.act(o_, i_, AF.Identity, [bank, Ag, mT], [dst], scale=Ag[:, fc, s:s + 1],
                          bias=mT[:, shift_base + fc, s:s + 1])
                if fc % 4 == 3:
                    yield

        def run_uT(jobs, bufs):
            def chain(par_):
                for k in range(par_, len(jobs), 2):
                    srcs, s_, dst, tok0 = jobs[k]
                    yield from make_uT_g(srcs, s_, dst, tok0, A1g, 0, bufs, k)
            run_threads([chain(0), chain(1)])

        def uT_bufs(es_):
            return {'X': [c.sb(es_, [128, D], F32, 'X%d' % i) for i in range(4)],
                    'xnb': [c.sb(es_, [128, D], BF16, 'xnb%d' % i) for i in range(2)],
                    'ss': [c.sb(es_, [128, 4], F32, 'ss%d' % i) for i in range(2)],
                    'sq': [c.sb(es_, [128, D], BF16, 'sq%d' % i) for i in range(2)]}

        esG = ExitStack()
        with esG:
            uT_r = c.sb(esG, [128, 8, NTOK], BF16, 'uT_r')
            with ExitStack() as es2:
                bufs = uT_bufs(es2)
                jobs = [([(0, 128, ctx[t * 128:(t + 1) * 128, :])], 1, uT_r, t * 128) for t in range(2)]
                jobs += [([(0, 128, x[t * 128:(t + 1) * 128, :])], 0, uT_r, CTX + t * 128) for t in range(16)]
                run_uT(jobs, bufs)
                for kc in range(8):
                    c.dma(scr_u[:, kc, :], uT_r[:, kc, CTX:NTOK], reads=[uT_r], writes=[scru_buf])
                c.barrier()


            w_in_v = w_in.rearrange("(kc p) n -> p kc n", p=128)
            TBLK = [(0, 256), (256, 768), (768, 1280), (1280, 1792), (1792, 2304)]
            with ExitStack() as es3:
                g_tok = c.sb(es3, [128, 18, 8], F32, 'g_tok')
                b_tok = c.sb(es3, [128, 18, 8], F32, 'b_tok')
                with ExitStack() as es3a:
                    wab = c.sb(es3a, [128, 8, 16], BF16, 'wab')
                    ab = c.sb(es3a, [128, 18, 16], F32, 'ab')
                    alog = c.sb(es3a, [128, 18, 8], F32, 'alog')
                    dtb = c.sb(es3a, [128, 18, 8], F32, 'dtb')
                    t1 = c.sb(es3a, [128, 18, 8], F32, 't1')
                    t2 = c.sb(es3a, [128, 18, 8], F32, 't2')
                    c.dma(wab[:], w_in_v[:, :, 2048:2064], writes=[wab], q='pool')
                    c.dma(alog[:], alog_bc, writes=[alog])
                    c.dma(dtb[:], dtb_bc, writes=[dtb])
                    for t in range(18):
                        bank = PS[t % 2]
                        for kc in range(8):
                            c.mm(bank[:, 0:16], uT_r[:, kc, t * 128:(t + 1) * 128], wab[:, kc, :], kc == 0, kc == 7, [uT_r, wab], [bank])
                        c.cp('act', ab[:, t, :], bank[:, 0:16], [bank], [ab])
                    c.tt('dve', t1[:], ab[:, :, 0:8], dtb[:], ALU.add, [ab, dtb], [t1])
                    c.stt(t2[:], t1[:], -1.0, t1[:], ALU.mult, ALU.max, [t1], [t2])
                    c.act(t2[:], t2[:], AF.Exp, [t2], [t2], scale=-1.0)
                    c.ts('dve', t2[:], t2[:], 1.0, None, ALU.add, None, [t2], [t2])
                    c.act(t2[:], t2[:], AF.Ln, [t2], [t2])
                    c.stt(t1[:], t1[:], 0.0, t2[:], ALU.max, ALU.add, [t1, t2], [t1])
                    c.act(alog[:], alog[:], AF.Exp, [alog], [alog])
                    c.stt(R(g_tok[:]), t1[:], -1.0, alog[:], ALU.mult, ALU.mult, [t1, alog], [g_tok])
                    c.act(b_tok[:], ab[:, :, 8:16], AF.Sigmoid, [ab], [b_tok])
                    c.barrier()

                cv = c.sb(es3, [128, 12, 5], F32, 'cv')
                onm = c.sb(es3, [128, 1], F32, 'onm')
                c.dma(cv[:], convT, writes=[cv])
                c.dma(onm[:], onormT, writes=[onm])
                raws = [c.sb(es3, [128, NTOK], F32, 'raw%d' % i) for i in range(2)]
                accs = [c.sb(es3, [128, NTOK], F32, 'acc%d' % i) for i in range(2)]
                sqs = [c.sb(es3, [128, NTOK], F32, 'sqg%d' % i) for i in range(2)]
                qT = c.sb(es3, [128, NTOK], F32, 'qT')
                kT = c.sb(es3, [128, NTOK], F32, 'kT')
                vT = c.sb(es3, [128, NTOK], F32, 'vT')
                zs = c.sb(es3, [128, SEQ], F32, 'zs')
                o_acc = c.sb(es3, [128, 16, 128], F32, 'o_acc')
                wc = [c.sb(es3, [128, 8, 128], BF16, 'wc%d' % i) for i in range(2)]
                rns = [[c.sb(es3, [128, 512], F32, 'rn%d_%d' % (j, i)) for i in range(2)] for j in range(2)]
                S = [c.sb(es3, [128, 128], F32, 'S%d' % d) for d in range(2)]
                gcs = [c.sb(es3, [128, 8], F32, 'gcs%d' % i) for i in range(2)]
                egc = [c.sb(es3, [128, 8], F32, 'egc%d' % i) for i in range(2)]
                negc = [c.sb(es3, [128, 8], F32, 'negc%d' % i) for i in range(2)]
                ekd = [c.sb(es3, [128, 8], F32, 'ekd%d' % i) for i in range(2)]
                gend = [c.sb(es3, [128, 2, 8], F32, 'gend%d' % i) for i in range(2)]
                k_tok = [c.sb(es3, [128, 128], F32, 'k_tok%d' % i) for i in range(2)]
                v_tok = [c.sb(es3, [128, 128], F32, 'v_tok%d' % i) for i in range(2)]
                NS = 2
                Gt = [c.sb(es3, [128, 128], F32, 'Gt%d' % i) for i in range(NS)]
                Ei = [c.sb(es3, [128, 128], F32, 'Ei%d' % i) for i in range(NS)]
                Es = [c.sb(es3, [128, 128], F32, 'Es%d' % i) for i in range(NS)]
                QKm = [c.sb(es3, [128, 128], F32, 'QKm%d' % i) for i in range(NS)]
                Pb = [[c.sb(es3, [128, 128], F32, 'P%d_%d' % (i, j)) for j in range(2)] for i in range(NS)]
                Qb = [[c.sb(es3, [128, 128], F32, 'Q%d_%d' % (i, j)) for j in range(2)] for i in range(NS)]
                Wb_ = [[c.sb(es3, [128, 128], F32, 'W%d_%d' % (i, j)) for j in range(2)] for i in range(NS)]
                kdec = [c.sb(es3, [128, 128], F32, 'kdec%d' % i) for i in range(NS)]
                Zb = [c.sb(es3, [128, 128], F32, 'Z%d' % i) for i in range(NS)]
                vnew = [c.sb(es3, [128, 128], F32, 'vnew%d' % i) for i in range(NS)]
                otmp = [c.sb(es3, [128, 128], F32, 'otmp%d' % i) for i in range(NS)]
                otmp2 = [c.sb(es3, [128, 128], F32, 'otmp2%d' % i) for i in range(NS)]
                fin = [c.sb(es3, [128, 132], F32, 'fin%d' % i) for i in range(2)]
                wcnt = 0
                unit = 0
                import os
                H_all = c.sb(es3, [128, 18, 32], F32, 'H_all')
                egc_all = c.sb(es3, [128, 18, 8], F32, 'egc_all')
                negc_all = c.sb(es3, [128, 18, 8], F32, 'negc_all')
                ekd_all = c.sb(es3, [128, 18, 8], F32, 'ekd_all')
                gend_all = c.sb(es3, [128, 18, 16], F32, 'gend_all')
                for t in range(18):
                    bankH = PS[t % 2]
                    c.mm(bankH[:, 0:4], R(incl_r[0][:]), R(g_tok[:, t, 0:4]), True, True, [incl_r[0], g_tok], [bankH])
                    c.mm(bankH[:, 4:8], R(incl_r[1][:]), R(g_tok[:, t, 4:8]), True, True, [incl_r[1], g_tok], [bankH])
                    c.mm(bankH[:, 8:16], R(blk_r[:]), R(g_tok[:, t, :]), True, True, [blk_r, g_tok], [bankH])
                    c.mm(bankH[:, 16:24], R(sel_r[0][:]), R(g_tok[:, t, :]), True, True, [sel_r[0], g_tok], [bankH])
                    c.mm(bankH[:, 24:32], R(sel_r[1][:]), R(g_tok[:, t, :]), True, True, [sel_r[1], g_tok], [bankH])
                    c.cp('dve', H_all[:, t, :], bankH[:, 0:32], [bankH], [H_all])
                c.act(egc_all[:], H_all[:, :, 0:8], AF.Exp, [H_all], [egc_all])
                c.ts('dve', negc_all[:], egc_all[:], -1.0, None, ALU.mult, None, [egc_all], [negc_all])
                c.tt('dve', ekd_all[:], H_all[:, :, 8:16], H_all[:, :, 0:8], ALU.subtract, [H_all], [ekd_all])
                c.act(ekd_all[:], ekd_all[:], AF.Exp, [ekd_all], [ekd_all])
                c.act(gend_all[:], H_all[:, :, 16:32], AF.Exp, [H_all], [gend_all])
                NH = int(os.environ.get('K_NH', '4'))
                NSTEP = int(os.environ.get('K_NSTEP', '18'))
                KLAT = int(os.environ.get('K_LAT', '9'))
                for h in range(NH):
                    def proj_g(ci, slot, h=h):
                        col0, dst = [(h * 128, qT), (512 + h * 128, kT), (1024 + h * 128, vT), (1536 + h * 128, zs)][ci]
                        raw, acc, sq = raws[slot], accs[slot], sqs[slot]
                        w_ = wc[slot]
                        pb0 = 4 * slot
                        if h == 0 and ci < 2:
                            c.dma(w_[:], w_in_v[:, :, col0:col0 + 128], writes=[w_], q='pool')
                        for bi, (t0, t1_) in enumerate(TBLK):
                            if ci == 3 and bi == 0:
                                continue
                            bank = PS[pb0 + bi % 2]
                            n = t1_ - t0
                            for kc in range(8):
                                c.mm(bank[:, 0:n], w_[:, kc, :], uT_r[:, kc, t0:t1_], kc == 0, kc == 7, [w_, uT_r], [bank])
                            if ci == 3:
                                c.act(zs[:, t0 - CTX:t1_ - CTX], bank[:, 0:n], AF.Silu, [bank], [zs])
                            else:
                                c.cp('act', raw[:, t0:t1_], bank[:, 0:n], [bank], [raw])
                            yield
                        nh_, nci = (h, ci + 2) if ci < 2 else (h + 1, ci - 2)
                        if nh_ < NH:
                            ncol = [nh_ * 128, 512 + nh_ * 128, 1024 + nh_ * 128, 1536 + nh_ * 128][nci]
                            c.dma(w_[:], w_in_v[:, :, ncol:ncol + 128], writes=[w_], q='pool')
                        if ci == 3:
                            return
                        cch = ci * 4 + h
                        c.ts('dve', acc[:], raw[:], cv[:, cch, 2:3], None, ALU.mult, None, [raw, cv], [acc])
                        yield
                        for kk_ in (0, 1, 3, 4):
                            sft = kk_ - 2
                            for (a_, b_) in ((0, CTX), (CTX, NTOK)):
                                lo = max(a_, a_ - sft)
                                hi = min(b_, b_ - sft)
                                c.stt(acc[:, lo:hi], raw[:, lo + sft:hi + sft], cv[:, cch, kk_:kk_ + 1], acc[:, lo:hi], ALU.mult, ALU.add,
                                      [raw, cv, acc], [acc])
                            yield
                        if ci == 2:
                            c.act(R(vT[:]), acc[:], AF.Silu, [acc], [vT])
                            return
                        c.act(acc[:], acc[:], AF.Silu, [acc], [acc])
                        c.act(R(sq[:]), acc[:], AF.Square, [acc], [sq])
                        yield
                        sc = 128.0 if ci == 0 else 1.0
                        for bi, (t0, t1_) in enumerate(TBLK):
                            bank = PS[pb0 + 2 + bi % 2]
                            n = t1_ - t0
                            r_ = rns[slot][bi % 2]
                            c.mm(bank[:, 0:n], R(ones_r[:]), R(sq[:, t0:t1_]), True, True, [ones_r, sq], [bank])
                            c.ts('dve', r_[:, 0:n], bank[:, 0:n], sc, EPS * sc, ALU.mult, ALU.add, [bank], [r_])
                            c.act(r_[:, 0:n], r_[:, 0:n], AF.Sqrt, [r_], [r_])
                            yield
                            c.op('dve', lambda e, r_=r_, n=n: e.reciprocal(out=r_[:, 0:n], in_=r_[:, 0:n]), reads=[r_], writes=[r_])
                            c.tt('dve', R(dst[:, t0:t1_]), acc[:, t0:t1_], r_[:, 0:n], ALU.mult, [acc, r_], [dst])
                            yield

                    run_threads([proj_g(0, 0), proj_g(1, 1)])
                    run_threads([proj_g(2, 0), proj_g(3, 1)])
                    if 'd_qkv' in T and h == 0:
                        c.dma(T['d_qkv'][0], qT[:], reads=[qT])
                        c.dma(T['d_qkv'][1], kT[:], reads=[kT])
                        c.dma(T['d_qkv'][2], vT[:], reads=[vT])
                    for d in range(2):
                        c.ts('dve', R(S[d][:]), zf[:], 0.0, None, ALU.mult, None, [zf], [S[d]])
                    order_f = list(range(18))
                    order_b = [1, 0] + list(range(17, 1, -1))
                    def gdn_unit_g(d, step):
                        tile = order_f[step] if d == 0 else order_b[step]
                        is_lat = tile >= 2
                        ts0 = tile * 128
                        col = d * 4 + h
                        pi = d
                        u = d
                        X0, X1, X2, X3 = (PS[4 * d + i_] for i_ in range(4))
                        bankT = X3
                        c.tr(bankT[:, 0:128], kT[:, ts0:ts0 + 128], ident[:], [kT, ident], [bankT])
                        c.tr(bankT[:, 128:256], vT[:, ts0:ts0 + 128], ident[:], [vT, ident], [bankT])
                        bankA = X0
                        c.mm(bankA[:, 0:128], R(kT[:, ts0:ts0 + 128]), R(kT[:, ts0:ts0 + 128]), True, True, [kT], [bankA])
                        c.mm(bankA[:, 128:256], R(kT[:, ts0:ts0 + 128]), R(qT[:, ts0:ts0 + 128]), True, True, [kT, qT], [bankA])
                        c.ts('pool', R(Gt[u][:]), incl[d][:], g_tok[:, tile, col:col + 1], None, ALU.mult, None, [incl[d], g_tok], [Gt[u]])
                        yield
                        bankB = X1
                        c.mm(bankB[:, 0:128], R(ones_r[:]), R(Gt[u][:]), True, False, [ones_r, Gt[u]], [bankB])
                        c.mm(bankB[:, 0:128], R(Gt[u][:]), R(nones_r[:]), False, False, [nones_r, Gt[u]], [bankB])
                        c.mm(bankB[:, 0:128], R(ident_r[:]), R(negm_r[d][:]), False, True, [ident_r, negm_r[d]], [bankB])
                        yield
                        c.cp('act', k_tok[pi][:], bankT[:, 0:128], [bankT], [k_tok[pi]])
                        c.cp('act', R(v_tok[pi][:]), bankT[:, 128:256], [bankT], [v_tok[pi]])
                        c.act(Ei[u][:], bankB[:, 0:128], AF.Exp, [bankB], [Ei[u]])
                        yield
                        c.tt('pool', Es[u][:], Ei[u][:], notI[:], ALU.mult, [Ei[u], notI], [Es[u]])
                        P, Q, W = Pb[u], Qb[u], Wb_[u]
                        yield
                        c.stt(R(P[0][:]), bankA[:, 0:128], b_tok[:, tile, col:col + 1], Es[u][:], ALU.mult, ALU.mult,
                              [bankA, b_tok, Es[u]], [P[0]])
                        c.tt('dve', R(QKm[u][:]), bankA[:, 128:256], Ei[u][:], ALU.mult, [bankA, Ei[u]], [QKm[u]])
                        c.ts('dve', R(kdec[u][:]), k_tok[pi][:], ekd_all[:, tile, col:col + 1], None, ALU.mult, None, [k_tok[pi], ekd_all], [kdec[u]])
                        yield
                        bankC = X2
                        bankD = X3
                        c.tr(bankC[:, 0:128], P[0][:], ident[:], [P[0], ident], [bankC])
                        c.tt('pool', R(W[0][:]), ident[:], P[0][:], ALU.subtract, [ident, P[0]], [W[0]])
                        yield
                        c.cp('act', R(Q[0][:]), bankC[:, 0:128], [bankC], [Q[0]])
                        yield
                        for k_ in range(5):
                            a_, b_ = k_ % 2, (k_ + 1) % 2
                            c.mm(bankC[:, 128:256], R(P[a_][:]), R(Q[a_][:]), True, True, [P[a_], Q[a_]], [bankC])
                            if k_ < 4:
                                c.mm(bankD[:, 0:128], R(Q[a_][:]), R(P[a_][:]), True, True, [P[a_], Q[a_]], [bankD])
                            yield
                            c.cp('act', R(Q[b_][:]), bankC[:, 128:256], [bankC], [Q[b_]])
                            if k_ < 4:
                                c.cp('dve', R(P[b_][:]), bankD[:, 0:128], [bankD], [P[b_]])
                            yield
                            c.mm(bankD[:, 128:256], R(Q[b_][:]), R(W[a_][:]), True, True, [Q[b_], W[a_]], [bankD])
                            yield
                            c.tt('dve', R(W[b_][:]), bankD[:, 128:256], W[a_][:], ALU.add, [bankD, W[a_]], [W[b_]])
                            yield
                        Wf = W[1]
                        for cs in ((0, 64) if d == 0 else (64, 0)):
                            sl_ = slice(cs, cs + 64)
                            chunk = cs // 64
                            c.mm(X0[:, 0:128], R(kT[:, ts0:ts0 + 128]), R(S[d][:]), True, True, [kT, S[d]], [X0])
                            if is_lat:
                                c.mm(X0[:, 128:256], R(qT[:, ts0:ts0 + 128]), R(S[d][:]), True, True, [qT, S[d]], [X0])
                            yield
                            c.stt(R(Zb[u][sl_, :]), X0[sl_, 0:128], negc_all[sl_, tile, col:col + 1], v_tok[pi][sl_, :], ALU.mult, ALU.add,
                                  [X0, negc_all, v_tok[pi]], [Zb[u]])
                            if is_lat:
                                c.ts('dve', otmp[u][sl_, :], X0[sl_, 128:256], egc_all[sl_, tile, col:col + 1], None, ALU.mult, None, [X0, egc_all], [otmp[u]])
                            yield
                            c.mm(X1[:, 0:128], R(Wf[sl_, :]), R(Zb[u][sl_, :]), True, True, [Wf, Zb[u]], [X1])
                            yield
                            c.ts('dve', R(vnew[u][sl_, :]), X1[sl_, 0:128], b_tok[sl_, tile, col:col + 1], None, ALU.mult, None,
                                 [X1, b_tok], [vnew[u]])
                            yield
                            c.mm(X3[:, 256:384], R(kdec[u][sl_, :]), R(vnew[u][sl_, :]), True, True, [kdec[u], vnew[u]], [X3])
                            if is_lat:
                                c.mm(X2[:, 0:128], R(QKm[u][sl_, :]), R(vnew[u][sl_, :]), True, True, [QKm[u], vnew[u]], [X2])
                            yield
                            c.stt(R(S[d][:]), S[d][:], gend_all[:, tile, chunk * 8 + col:chunk * 8 + col + 1], X3[:, 256:384], ALU.mult, ALU.add,
                                  [S[d], gend_all, X3], [S[d]])
                            if is_lat:
                                lt = tile - 2
                                if (d == 0) == (lt <= 7):
                                    c.tt('dve', o_acc[sl_, lt, :], X2[sl_, 0:128], otmp[u][sl_, :], ALU.add, [X2, otmp[u]], [o_acc])
                                else:
                                    c.tt('dve', otmp2[u][sl_, :], X2[sl_, 0:128], otmp[u][sl_, :], ALU.add, [X2, otmp[u]], [otmp2[u]])
                                    c.tt('pool', o_acc[sl_, lt, :], o_acc[sl_, lt, :], otmp2[u][sl_, :], ALU.add, [o_acc, otmp2[u]], [o_acc])
                            yield

                    def gdn_dir_g(d):
                        for step in range(NSTEP):
                            yield from gdn_unit_g(d, step)

                    run_threads([gdn_dir_g(0), gdn_dir_g(1)])
                    def gdn_out_g(par_, h=h):
                        f_ = fin[par_]
                        bank = PS[par_]
                        for lt in range(par_, 16, 2):
                            c.act(f_[:, 0:128], o_acc[:, lt, :], AF.Square, [o_acc], [f_], accum_out=f_[:, 128:129])
                            yield
                            c.ts('dve', f_[:, 129:130], f_[:, 128:129], 1.0 / 128, EPS, ALU.mult, ALU.add, [f_], [f_])
                            yield
                            c.act(f_[:, 130:131], f_[:, 129:130], AF.Sqrt, [f_], [f_])
                            yield
                            c.op('dve', lambda e, f_=f_: e.reciprocal(out=f_[:, 131:132], in_=f_[:, 130:131]), reads=[f_], writes=[f_])
                            c.ts('dve', f_[:, 0:128], o_acc[:, lt, :], f_[:, 131:132], None, ALU.mult, None, [o_acc, f_], [f_])
                            yield
                            c.tr(bank[:, 0:128], f_[:, 0:128], ident[:], [f_, ident], [bank])
                            yield
                            c.stt(oaT[:, h, lt * 128:(lt + 1) * 128], bank[:, 0:128], onm[:, 0:1], zs[:, lt * 128:(lt + 1) * 128], ALU.mult, ALU.mult,
                                  [bank, onm, zs], [oaT])
                            yield

                    run_threads([gdn_out_g(0), gdn_out_g(1)])
                c.barrier()
        if 'd_oaT' in T:
            c.dma(T['d_oaT'], oaT[:], reads=[oaT])


        def inv_chain(P, Q, W, bankC, bankD):
            c.tr(bankC[:, 0:128], P[0][:], ident[:], [P[0], ident], [bankC])
            c.cp('act', R(Q[0][:]), bankC[:, 0:128], [bankC], [Q[0]])
            c.tt('pool', R(W[0][:]), ident[:], P[0][:], ALU.subtract, [ident, P[0]], [W[0]])
            for k_ in range(5):
                a_, b_ = k_ % 2, (k_ + 1) % 2
                c.mm(bankC[:, 128:256], R(P[a_][:]), R(Q[a_][:]), True, True, [P[a_], Q[a_]], [bankC])
                if k_ < 4:
                    c.mm(bankD[:, 0:128], R(Q[a_][:]), R(P[a_][:]), True, True, [P[a_], Q[a_]], [bankD])
                c.cp('act', R(Q[b_][:]), bankC[:, 128:256], [bankC], [Q[b_]])
                if k_ < 4:
                    c.cp('dve', R(P[b_][:]), bankD[:, 0:128], [bankD], [P[b_]])
                c.mm(bankD[:, 128:256], R(Q[b_][:]), R(W[a_][:]), True, True, [Q[b_], W[a_]], [bankD])
                c.tt('dve', R(W[b_][:]), bankD[:, 128:256], W[a_][:], ALU.add, [bankD, W[a_]], [W[b_]])
            return W[1]

        BW = 256
        NBLK = NTOK // BW
        with ExitStack() as esR:
            uT_c = c.sb(esR, [128, 8, NTOK], BF16, 'uT_c')
            with ExitStack() as es2:
                bufs = uT_bufs(es2)
                x_cm = x.rearrange("(r w) d -> w r d", w=64)
                jobs = [([(0, 128, ctx[t * 128:(t + 1) * 128, :])], 1, uT_c, t * 128) for t in range(2)]
                jobs += [([(wl * 32, wl * 32 + 32, x_cm[4 * j + wl]) for wl in range(4)], 0, uT_c, CTX + j * 128) for j in range(16)]
                run_uT(jobs, bufs)
                c.barrier()
            mu = c.sb(esR, [128, 15], F32, 'mu')
            hmu = c.sb(esR, [128, 15], F32, 'hmu')
            omu = c.sb(esR, [128, 15], F32, 'omu')
            w0 = c.sb(esR, [128, 2, 4], F32, 'w0')
            a0 = c.sb(esR, [128, 2, 4], F32, 'a0')
            rw = c.sb(esR, [128, 5, 4], F32, 'rw')
            oka = c.sb(esR, [128, 4], F32, 'oka')
            oka2 = c.sb(esR, [128, 4], F32, 'oka2')
            w2b = c.sb(esR, [128, 512], BF16, 'w2b')
            a2b = c.sb(esR, [128, 512], BF16, 'a2b')
            g2b = c.sb(esR, [128, 512], BF16, 'g2b')
            c.dma(mu[:], muT, writes=[mu])
            c.dma(w0[:], w0T, writes=[w0])
            c.dma(a0[:], a0T, writes=[a0])
            c.dma(rw[:], rwv, writes=[rw])
            c.dma(w2b[:], w2m, writes=[w2b], q='pool')
            c.dma(a2b[:], a2m, writes=[a2b], q='pool')
            c.dma(g2b[:], g2m, writes=[g2b], q='pool')
            c.ts('dve', hmu[:], mu[:], 0.5, None, ALU.mult, None, [mu], [hmu])
            c.ts('dve', omu[:], mu[:], -1.0, 1.0, ALU.mult, ALU.add, [mu], [omu])
            c.ts('dve', oka[:], rw[:, 1, :], -1.0, 1.0, ALU.mult, ALU.add, [rw], [oka])
            c.ts('dve', oka2[:], rw[:, 1, :], -2.0, 2.0, ALU.mult, ALU.add, [rw], [oka2])
            cmask = c.sb(esR, [128, BW], F32, 'cmask')
            c.op('pool', lambda e: e.memset(cmask[:], 1.0), writes=[cmask])
            c.op('pool', lambda e: e.memset(cmask[:].rearrange("p (a b) -> p a b", b=64)[:, :, 0:1], 0.0), reads=[cmask], writes=[cmask])
            mskA = [c.sb(esR, [128, 256], F32, 'mskA%d' % d) for d in range(2)]
            mskB = [c.sb(esR, [128, 256], F32, 'mskB%d' % d) for d in range(2)]
            for d in range(2):
                c.ts('dve', mskA[d][:, 0:128], strict[d][:], -1.0, None, ALU.mult, None, [strict[d]], [mskA[d]])
                c.cp('dve', mskA[d][:, 128:256], incl[d][:], [incl[d]], [mskA[d]])
                c.cp('dve', mskB[d][:, 0:128], strict[d][:], [strict[d]], [mskB[d]])
                c.cp('dve', mskB[d][:, 128:256], incl[d][:], [incl[d]], [mskB[d]])

            raw = c.sb(esR, [128, NTOK], F32, 'rraw')
            t1 = c.sb(esR, [128, NTOK], F32, 'rt1')
            twl = c.sb(esR, [128, NTOK], BF16, 'twl')
            alo = c.sb(esR, [128, NTOK], BF16, 'alo')
            sgl = c.sb(esR, [128, NTOK], BF16, 'sgl')
            rb = c.sb(esR, [128, NTOK], BF16, 'rb')
            kb = c.sb(esR, [128, NTOK], BF16, 'kb')
            vb = c.sb(esR, [128, NTOK], BF16, 'vb')
            G1 = c.sb(esR, [128, SEQ], BF16, 'G1')
            G2 = c.sb(esR, [128, SEQ], BF16, 'G2')
            y_acc = c.sb(esR, [128, 16, 128], F32, 'y_acc')
            wcr = [c.sb(esR, [128, 8, 128], BF16, 'wcr%d' % i) for i in range(2)]
            wcn = [0]

            pm_sched = [1536, 1664, 1792]
            for hc_ in range(4):
                pm_sched += [hc_ * 128, 512 + hc_ * 128, 1024 + hc_ * 128]

            def proj_mix(bcol, dst, func, mi):
                n_ = wcn[0]
                wcn[0] += 1
                assert pm_sched[n_] == bcol
                w_ = wcr[n_ % 2]
                if n_ == 0:
                    c.dma(w_[:], w_in_v[:, :, A_COLS + bcol:A_COLS + bcol + 128], writes=[w_], q='pool')
                for bi, (t0, t1_) in enumerate(TBLK):
                    bank = PS[bi % 2]
                    n = t1_ - t0
                    for kc in range(8):
                        c.mm(bank[:, 0:n], w_[:, kc, :], uT_c[:, kc, t0:t1_], kc == 0, kc == 7, [w_, uT_c], [bank])
                    c.cp('act', raw[:, t0:t1_], bank[:, 0:n], [bank], [raw])
                    if bi == 0 and n_ + 1 < len(pm_sched):
                        nb_ = A_COLS + pm_sched[n_ + 1]
                        c.dma(wcr[(n_ + 1) % 2][:], w_in_v[:, :, nb_:nb_ + 128], writes=[wcr[(n_ + 1) % 2]], q='pool')
                for (a_, b_) in ((0, CTX), (CTX, NTOK)):
                    c.tt('pool', t1[:, a_ + 1:b_ - 1], raw[:, a_:b_ - 2], raw[:, a_ + 2:b_], ALU.add, [raw], [t1])
                    c.cp('pool', t1[:, a_:a_ + 1], raw[:, a_ + 1:a_ + 2], [raw], [t1])
                    c.cp('pool', t1[:, b_ - 1:b_], raw[:, b_ - 2:b_ - 1], [raw], [t1])
                c.ts('dve', t1[:], t1[:], hmu[:, mi:mi + 1], None, ALU.mult, None, [t1, hmu], [t1])
                if func is None:
                    c.stt(dst[:], raw[:], omu[:, mi:mi + 1], t1[:], ALU.mult, ALU.add, [raw, omu, t1], [dst])
                else:
                    c.stt(t1[:], raw[:], omu[:, mi:mi + 1], t1[:], ALU.mult, ALU.add, [raw, omu, t1], [t1])
                    c.act(dst[:], t1[:], func, [t1], [dst])

            proj_mix(1536, twl, AF.Tanh, 12)
            proj_mix(1664, alo, None, 13)
            proj_mix(1792, sgl, AF.Sigmoid, 14)

            def blkbuf(name, dt=F32, w=BW):
                return c.sb(esR, [128, w], dt, name)
            DB = []
            for d in range(2):
                g = {}
                arena = (raw, t1)[d]
                for i_, nm in enumerate(('lw', 'icl', 'pre', 'cumd', 'kkn', 'kdir', 'bvec', 'tmpa', 'tmpb', 'opr', 'btT', 'ktT', 'bhT', 'khT')):
                    if i_ < 9:
                        g[nm] = Buf(arena.t[:, i_ * BW:(i_ + 1) * BW], '%s%d' % (nm, d))
                    else:
                        g[nm] = blkbuf('%s%d' % (nm, d))
                g['AR'] = c.sb(esR, [128, 2, BW], F32, 'AR%d' % d)
                g['gC'] = c.sb(esR, [128, BW // 64], F32, 'gC%d' % d)
                for nm in ('Bh_tok', 'Kh_tok', 'Vt'):
                    g[nm] = c.sb(esR, [128, 128], F32, '%s%d' % (nm, d))
                g['BL0'] = Reg(4 * d, 0, 256, 'BL0_%d' % d)
                g['BL1'] = Reg(4 * d + 2, 0, 256, 'BL1_%d' % d)
                g['TL0'] = Reg(4 * d + 1, 0, 128, 'TL0_%d' % d)
                g['TLb'] = Reg(4 * d + 1, 128, 128, 'TLb_%d' % d)
                g['TL1'] = Reg(4 * d + 3, 0, 128, 'TL1_%d' % d)
                g['U'] = []
                for hh in range(2):
                    u = {'bankA': 4 * d + 2 * hh, 'bankB': 4 * d + 2 * hh + 1}
                    sfx = '%d%d' % (d, hh)
                    u['AB1'] = c.sb(esR, [128, 256], F32, 'AB1_' + sfx)
                    u['AB2'] = c.sb(esR, [128, 256], F32, 'AB2_' + sfx)
                    u['XY2'] = c.sb(esR, [128, 128], F32, 'XY2_' + sfx)
                    for nm in ('P', 'Q', 'W'):
                        u[nm] = [c.sb(esR, [128, 128], F32, '%sr%d_%s' % (nm, j, sfx)) for j in range(2)]
                    for nm in ('Xs', 'Us', 'yt', 'yt2', 'Tst'):
                        u[nm] = c.sb(esR, [128, 64], F32, nm + sfx)
                    g['U'].append(u)
                DB.append(g)
            gst2 = [c.sb(esR, [128, 24], F32, 'gst%d' % i) for i in range(2)]
            yn2 = [c.sb(esR, [128, 128], F32, 'yn%d' % i) for i in range(2)]
            obf2 = [c.sb(esR, [128, 128], BF16, 'obf%d' % i) for i in range(2)]
            otf2 = [c.sb(esR, [128, 128], F32, 'otf%d' % i) for i in range(2)]
            NHC = int(os.environ.get('K_NHC', '4'))
            NBS = int(os.environ.get('K_NBS', '9'))
            LG = -0.6065306597126334
            KTG = int(os.environ.get('K_TG', '9'))

            def block_g(d, g, b, hc):
                rowsd = slice(d * 64, d * 64 + 64)
                cs_ = slice(hc * 128, (hc + 1) * 128)
                t0 = b * BW
                tsl = slice(t0, t0 + BW)
                lw, icl, pre, cumd, kkn, kdir, bvec, tmpa, tmpb, opr = (g[n_] for n_ in ('lw', 'icl', 'pre', 'cumd', 'kkn', 'kdir', 'bvec', 'tmpa', 'tmpb', 'opr'))
                AR, btT, ktT, bhT, khT, gC = (g[n_] for n_ in ('AR', 'btT', 'ktT', 'bhT', 'khT', 'gC'))
                BL0, BL1 = g['BL0'], g['BL1']
                c.mm(BL0.ap(), w2b[rowsd, cs_], twl[rowsd, tsl], True, True, [w2b, twl], [BL0.buf])
                c.mm(BL1.ap(), a2b[rowsd, cs_], alo[rowsd, tsl], True, True, [a2b, alo], [BL1.buf])
                c.ts('dve', kkn[:], kb[:, tsl], rw[:, 0, hc:hc + 1], None, ALU.mult, None, [kb, rw], [kkn])
                yield
                c.act(lw[:], BL0.ap(), AF.Sigmoid, [BL0.buf, w0], [lw], bias=w0[:, d, hc:hc + 1])
                c.act(icl[:], BL1.ap(), AF.Sigmoid, [BL1.buf, a0], [icl], bias=a0[:, d, hc:hc + 1])
                c.act(R(opr[:]), kkn[:], AF.Square, [kkn], [opr])
                yield
                c.ts('dve', lw[:], lw[:], LG, None, ALU.mult, None, [lw], [lw])
                c.mm(BL0.ap(), R(blk_r[:]), R(opr[:]), True, True, [blk_r, opr], [BL0.buf])
                c.op('dve', lambda e: e.tensor_tensor_scan(out=pre[:], data0=cmask[:], data1=lw[:], initial=0.0,
                                                           op0=ALU.mult, op1=ALU.add), reads=[cmask, lw], writes=[pre])
                yield
                pre3 = pre[:].rearrange("p (a b) -> p a b", b=64)
                tot_bc = pre3[:, :, 63:64].to_broadcast([128, BW // 64, 64])
                if d == 0:
                    c.cp('pool', cumd[:], pre[:], [pre], [cumd])
                else:
                    c.tt('dve', cumd[:], lw[:], pre[:], ALU.subtract, [lw, pre], [cumd])
                    c.tt('dve', cumd[:].rearrange("p (a b) -> p a b", b=64), cumd[:].rearrange("p (a b) -> p a b", b=64), tot_bc,
                         ALU.add, [cumd, pre], [cumd])
                c.ts('dve', tmpb[:], BL0.ap(), EPS, None, ALU.add, None, [BL0.buf], [tmpb])
                yield
                c.act(gC[:], pre3[:, :, 63], AF.Exp, [pre], [gC])
                c.act(tmpb[:], tmpb[:], AF.Sqrt, [tmpb], [tmpb])
                c.ts('dve', kdir[:], icl[:], rw[:, 1, hc:hc + 1], oka[:, hc:hc + 1], ALU.mult, ALU.add, [icl, rw, oka], [kdir])
                c.tt('dve', kdir[:], kdir[:], kb[:, tsl], ALU.mult, [kdir, kb], [kdir])
                yield
                c.op('dve', lambda e: e.reciprocal(out=tmpb[:], in_=tmpb[:]), reads=[tmpb], writes=[tmpb])
                c.tt('dve', kkn[:], kkn[:], tmpb[:], ALU.mult, [kkn, tmpb], [kkn])
                c.tt('dve', tmpa[:], cumd[:], lw[:], ALU.subtract, [cumd, lw], [tmpa])
                yield
                c.tt('pool', bvec[:], kkn[:], icl[:], ALU.mult, [kkn, icl], [bvec])
                c.act(tmpa[:], tmpa[:], AF.Exp, [tmpa], [tmpa])
                yield
                c.stt(R(AR[:, 0, :]), kkn[:], -1.0, tmpa[:], ALU.mult, ALU.mult, [kkn, tmpa], [AR])
                yield
                c.act(tmpa[:], cumd[:], AF.Exp, [cumd], [tmpa])
                c.tt('dve', tmpb[:].rearrange("p (a b) -> p a b", b=64), cumd[:].rearrange("p (a b) -> p a b", b=64), tot_bc,
                     ALU.subtract, [cumd, pre], [tmpb])
                yield
                c.tt('dve', R(AR[:, 1, :]), rb[:, tsl], tmpa[:], ALU.mult, [rb, tmpa], [AR])
                yield
                c.act(tmpa[:], cumd[:], AF.Exp, [cumd], [tmpa], scale=-1.0)
                c.act(tmpb[:], tmpb[:], AF.Exp, [tmpb], [tmpb], scale=-1.0)
                yield
                c.tt('dve', R(btT[:]), bvec[:], tmpa[:], ALU.mult, [bvec, tmpa], [btT])
                c.tt('pool', R(ktT[:]), kdir[:], tmpa[:], ALU.mult, [kdir, tmpa], [ktT])
                yield
                c.tt('dve', R(bhT[:]), bvec[:], tmpb[:], ALU.mult, [bvec, tmpb], [bhT])
                c.tt('pool', R(khT[:]), kdir[:], tmpb[:], ALU.mult, [kdir, tmpb], [khT])
                yield

            def tile_g(d, g, b, tl):
                lsl_ = slice(tl * 128, tl * 128 + 128)
                gts = (2 * b + tl) * 128
                TL0, TL1, TLb = g['TL0'], g['TL1'], g['TLb']
                bhT, khT, Bh_tok, Kh_tok, Vt = g['bhT'], g['khT'], g['Bh_tok'], g['Kh_tok'], g['Vt']
                c.mm(TL0.ap(), R(bhT[:, lsl_]), R(ident_r[:]), True, True, [bhT, ident_r], [TL0.buf])
                c.mm(TLb.ap(), vb[:, gts:gts + 128], identb[:], True, True, [vb, identb], [TLb.buf])
                c.mm(TL1.ap(), R(khT[:, lsl_]), R(ident_r[:]), True, True, [khT, ident_r], [TL1.buf])
                yield
                c.cp('act', R(Bh_tok[:]), TL0.ap(), [TL0.buf], [Bh_tok])
                c.cp('dve', R(Kh_tok[:]), TL1.ap(), [TL1.buf], [Kh_tok])
                c.cp('act', R(Vt[:]), TLb.ap(), [TLb.buf], [Vt])
                yield

            def unit_g(d, g, u, hh, b, tl):
                rows = slice(hh * 64, hh * 64 + 64)
                tile = 2 * b + tl
                is_lat = tile >= 2
                lt = tile - 2
                lsl_ = slice(tl * 128, tl * 128 + 128)
                AR, btT, ktT, Vt, Bh_tok, Kh_tok, gC = (g[n_] for n_ in ('AR', 'btT', 'ktT', 'Vt', 'Bh_tok', 'Kh_tok', 'gC'))
                AB1, AB2, XY2, Xs, Us, yt, yt2, Tst = (u[n_] for n_ in ('AB1', 'AB2', 'XY2', 'Xs', 'Us', 'yt', 'yt2', 'Tst'))
                P, Q, W = u['P'], u['Q'], u['W']
                A, B = PS[u['bankA']], PS[u['bankB']]
                At, Bt = A.t, B.t
                ARt = AR[rows, :, lsl_]
                c.mm(At[:, 0:256].rearrange("p (a b) -> p a b", a=2), R(btT[rows, lsl_]), R(ARt), True, True, [btT, AR], [A])
                c.mm(Bt[:, 0:256].rearrange("p (a b) -> p a b", a=2), R(ktT[rows, lsl_]), R(ARt), True, True, [ktT, AR], [B])
                yield
                c.tt('dve', R(AB1[:]), At[:, 0:256], mskA[d][:], ALU.mult, [A, mskA[d]], [AB1])
                c.tt('dve', R(AB2[:]), Bt[:, 0:256], mskB[d][:], ALU.mult, [B, mskB[d]], [AB2])
                yield
                if KTG < 3:
                    return
                c.cp('pool', R(P[0][:]), AB1[:, 0:128], [AB1], [P[0]])
                c.mm(At[:, 0:64], R(AB2[:, 0:128]), R(Vt[:, rows]), True, True, [AB2, Vt], [A])
                c.mm(At[:, 64:128], R(AB2[:, 128:256]), R(Vt[:, rows]), True, True, [AB2, Vt], [A])
                yield
                c.cp('act', XY2[:], At[:, 0:128], [A], [XY2])
                c.mm(Bt[:, 0:128], R(P[0][:]), R(ident_r[:]), True, True, [P[0], ident_r], [B])
                c.tt('pool', R(W[0][:]), ident[:], P[0][:], ALU.subtract, [ident, P[0]], [W[0]])
                yield
                c.cp('act', R(Q[0][:]), Bt[:, 0:128], [B], [Q[0]])
                yield
                for k_ in range(5):
                    a_, b_ = k_ % 2, (k_ + 1) % 2
                    c.mm(At[:, 128:256], R(P[a_][:]), R(Q[a_][:]), True, True, [P[a_], Q[a_]], [A])
                    if k_ < 4:
                        c.mm(Bt[:, 0:128], R(Q[a_][:]), R(P[a_][:]), True, True, [P[a_], Q[a_]], [B])
                    yield
                    c.cp('act', R(Q[b_][:]), At[:, 128:256], [A], [Q[b_]])
                    if k_ < 4:
                        c.cp('act', R(P[b_][:]), Bt[:, 0:128], [B], [P[b_]])
                    yield
                    c.mm(Bt[:, 128:256], R(Q[b_][:]), R(W[a_][:]), True, True, [Q[b_], W[a_]], [B])
                    yield
                    c.tt('dve', R(W[b_][:]), Bt[:, 128:256], W[a_][:], ALU.add, [B, W[a_]], [W[b_]])
                    yield
                Wt = W[1]
                if KTG < 4:
                    return
                for cs in ((0, 64) if d == 0 else (64, 0)):
                    sl_ = slice(cs, cs + 64)
                    chunk = tl * 2 + cs // 64
                    c.mm(At[:, 0:64], R(AR[rows, 0, lsl_]), R(Tst[rows, :]), True, True, [AR, Tst], [A])
                    if is_lat:
                        c.mm(At[:, 64:128], R(AR[rows, 1, lsl_]), R(Tst[rows, :]), True, True, [AR, Tst], [A])
                    yield
                    c.tt('dve', R(Xs[sl_, :]), At[sl_, 0:64], XY2[sl_, 0:64], ALU.add, [A, XY2], [Xs])
                    yield
                    c.mm(Bt[:, 0:64], R(Wt[sl_, :]), R(Xs[sl_, :]), True, True, [Wt, Xs], [B])
                    yield
                    c.cp('act', R(Us[sl_, :]), Bt[sl_, 0:64], [B], [Us])
                    yield
                    c.mm(Bt[:, 64:128], R(Bh_tok[sl_, :]), R(Us[sl_, :]), True, False, [Bh_tok, Us], [B])
                    c.mm(Bt[:, 64:128], R(Kh_tok[sl_, :]), R(Vt[sl_, rows]), False, True, [Kh_tok, Vt], [B])
                    if is_lat:
                        c.mm(At[:, 128:192], R(AB1[sl_, 128:256]), R(Us[sl_, :]), True, True, [AB1, Us], [A])
                    yield
                    c.stt(R(Tst[rows, :]), Tst[rows, :], gC[rows, chunk:chunk + 1], Bt[rows, 64:128], ALU.mult, ALU.add,
                          [Tst, gC, B], [Tst])
                    if is_lat:
                        c.tt('dve', yt[sl_, :], At[sl_, 128:192], XY2[sl_, 64:128], ALU.add, [A, XY2], [yt])
                        if (d == 0) == (lt <= 7):
                            c.tt('dve', y_acc[sl_, lt, rows], At[sl_, 64:128], yt[sl_, :], ALU.add, [A, yt], [y_acc])
                        else:
                            c.tt('dve', yt2[sl_, :], At[sl_, 64:128], yt[sl_, :], ALU.add, [A, yt], [yt2])
                            c.tt('pool', y_acc[sl_, lt, rows], y_acc[sl_, lt, rows], yt2[sl_, :], ALU.add, [y_acc, yt2], [y_acc])
                    yield

            def dir_g(d, hc):
                g = DB[d]
                for hh in range(2):
                    c.ts('dve', R(g['U'][hh]['Tst'][:]), zf[:, 0:64], 0.0, None, ALU.mult, None, [zf], [g['U'][hh]['Tst']])
                border = list(range(NBLK)) if d == 0 else [0] + list(range(NBLK - 1, 0, -1))
                for b in border[:NBS]:
                    yield from block_g(d, g, b, hc)
                    for tl in ((0, 1) if d == 0 else (1, 0)):
                        if KTG < 1:
                            continue
                        yield from tile_g(d, g, b, tl)
                        if KTG < 2:
                            continue
                        yield from par(unit_g(d, g, g['U'][0], 0, b, tl), unit_g(d, g, g['U'][1], 1, b, tl))

            ob_hc = c.sb(esR, [128, 16, 128], BF16, 'ob_hc')
            scr_ob_v = scr_ob.rearrange("(lt p) cc -> p lt cc", p=128)
            scrw = Buf(None, 'scrw')
            for hc in range(NHC):
                proj_mix(hc * 128, rb, None, hc)
                proj_mix(512 + hc * 128, kb, None, 4 + hc)
                proj_mix(1024 + hc * 128, vb, None, 8 + hc)
                c.barrier()
                cs_ = slice(hc * 128, (hc + 1) * 128)
                def prepass_g(par_, hc=hc, cs_=cs_):
                    g_ = DB[par_]
                    icl, icl1, tmpa, opr = g_['icl'], g_['pre'], g_['tmpa'], g_['opr']
                    P0, P3, P1, P2 = (PS[4 * par_ + j] for j in range(4))
                    for b in range(1 + par_, NBLK, 2):
                        t0 = b * BW
                        tsl = slice(t0, t0 + BW)
                        lsl = slice(t0 - CTX, t0 - CTX + BW)
                        c.mm(P0[:, 0:BW], a2b[0:64, cs_], alo[0:64, tsl], True, True, [a2b, alo], [P0])
                        c.mm(P3[:, 0:BW], a2b[64:128, cs_], alo[64:128, tsl], True, True, [a2b, alo], [P3])
                        c.mm(P2[:, 0:BW], g2b[:, cs_], sgl[:, tsl], True, True, [g2b, sgl], [P2])
                        yield
                        c.act(icl[:], P0[:, 0:BW], AF.Sigmoid, [P0, a0], [icl], bias=a0[:, 0, hc:hc + 1])
                        c.act(icl1[:], P3[:, 0:BW], AF.Sigmoid, [P3, a0], [icl1], bias=a0[:, 1, hc:hc + 1])
                        c.cp('act', G1[:, lsl], P2[:, 0:BW], [P2], [G1])
                        yield
                        c.tt('dve', tmpa[:], icl[:], icl1[:], ALU.add, [icl, icl1], [tmpa])
                        c.ts('dve', tmpa[:], tmpa[:], rw[:, 1, hc:hc + 1], oka2[:, hc:hc + 1], ALU.mult, ALU.add, [tmpa, rw, oka2], [tmpa])
                        yield
                        c.tt('dve', tmpa[:], tmpa[:], kb[:, tsl], ALU.mult, [tmpa, kb], [tmpa])
                        c.tt('dve', tmpa[:], tmpa[:], rb[:, tsl], ALU.mult, [tmpa, rb], [tmpa])
                        yield
                        c.ts('dve', R(opr[:]), tmpa[:], rw[:, 2, hc:hc + 1], None, ALU.mult, None, [tmpa, rw], [opr])
                        yield
                        c.mm(P1[:, 0:BW], R(blk_r[:]), R(opr[:]), True, True, [blk_r, opr], [P1])
                        yield
                        c.tt('dve', tmpa[:], P1[:, 0:BW], vb[:, tsl], ALU.mult, [P1, vb], [tmpa])
                        yield
                        c.stt(G2[:, lsl], tmpa[:], rw[:, 4, hc:hc + 1], P2[:, 0:BW], ALU.add, ALU.mult, [tmpa, rw, P2], [G2])
                        yield

                run_threads([prepass_g(0), prepass_g(1)])
                c.barrier()
                run_threads([dir_g(0, hc), dir_g(1, hc)])
                c.barrier()
                def rwkv_out_g(par_, hc=hc, cs_=cs_):
                    gst, yn, obf, otf = gst2[par_], yn2[par_], obf2[par_], otf2[par_]
                    bkA, bkB = PS[par_], PS[2 + par_]
                    pbk = psb(2 + par_)
                    for lt in range(par_, 0 if os.environ.get('K_NOOUT') else 16, 2):
                        for hh in range(2):
                            rows = slice(hh * 64, hh * 64 + 64)
                            c.op('dve', lambda e, hh=hh, rows=rows: e.bn_stats(out=gst[:, hh * 6:(hh + 1) * 6], in_=y_acc[:, lt, rows]), reads=[y_acc], writes=[gst])
                            c.op('dve', lambda e, hh=hh: e.bn_aggr(out=gst[:, 12 + hh * 2:14 + hh * 2], in_=gst[:, hh * 6:(hh + 1) * 6]), reads=[gst], writes=[gst])
                        yield
                        c.ts('dve', gst[:, 16:18], gst[:, 13:16:2], LNX_EPS, None, ALU.add, None, [gst], [gst])
                        yield
                        c.act(gst[:, 16:18], gst[:, 16:18], AF.Sqrt, [gst], [gst])
                        yield
                        c.op('dve', lambda e: e.reciprocal(out=gst[:, 18:20], in_=gst[:, 16:18]), reads=[gst], writes=[gst])
                        for hh in range(2):
                            rows = slice(hh * 64, hh * 64 + 64)
                            c.ts('dve', yn[:, rows], y_acc[:, lt, rows], gst[:, 12 + hh * 2:13 + hh * 2], gst[:, 18 + hh:19 + hh], ALU.subtract, ALU.mult,
                                 [y_acc, gst], [yn])
                        yield
                        c.tr(bkA[:, 0:128], yn[:], ident[:], [yn, ident], [bkA])
                        yield
                        lsl = slice(lt * 128, (lt + 1) * 128)
                        c.stt(otf[:], bkA[:, 0:128], rw[:, 3, hc:hc + 1], G1[:, lsl], ALU.mult, ALU.mult, [bkA, rw, G1], [otf])
                        c.tt('dve', obf[:], otf[:], G2[:, lsl], ALU.add, [otf, G2], [obf])
                        yield
                        c.tr(pbk[:, 0:128], obf[:], identb[:], [obf, identb], [bkB])
                        yield
                        c.cp('act', ob_hc[:, lt, :], pbk[:, 0:128], [bkB], [ob_hc])
                        yield

                run_threads([rwkv_out_g(0), rwkv_out_g(1)])
                for q4 in range(4):
                    c.dma(scr_ob_v[:, q4 * 4:(q4 + 1) * 4, cs_], ob_hc[:, q4 * 4:(q4 + 1) * 4, :], reads=[ob_hc], writes=[scrw])
            c.barrier()

        scrob_buf = scrw
        scrh_buf = Buf(None, 'scr_h_dram')
        esF = ExitStack()
        with esF:
            tT = c.sb(esF, [128, 8, SEQ], BF16, 'tT')
            cwT = c.sb(esF, [32, SEQ], F32, 'cwT')
            with ExitStack() as esY:
                yT = c.sb(esY, [128, 8, SEQ], BF16, 'yT')
                with ExitStack() as esM1:
                    woa = c.sb(esM1, [128, 4, D], BF16, 'woa')
                    wob = c.sb(esM1, [128, 4, D], BF16, 'wob')
                    c.dma(woa[:], w_o_a.rearrange("(cc p) n -> p cc n", p=128), writes=[woa], q='pool')
                    c.dma(wob[:], w_o_b.rearrange("(cc p) n -> p cc n", p=128), writes=[wob], q='pool')
                    uTb = c.sb(esM1, [128, 8, 512], BF16, 'uTb')
                    obTb = c.sb(esM1, [128, 4, 512], BF16, 'obTb')
                    obt = [c.sb(esM1, [128, 512], BF16, 'obt%d' % i) for i in range(2)]
                    wg_ = [c.sb(esM1, [128, 8, 128], BF16, 'wgm%d' % i) for i in range(4)]
                    sga2 = [c.sb(esM1, [128, 512], F32, 'sga%d' % i) for i in range(2)]
                    sgb2 = [c.sb(esM1, [128, 512], F32, 'sgb%d' % i) for i in range(2)]
                    ya2 = [c.sb(esM1, [128, 512], F32, 'ya%d' % i) for i in range(2)]
                    yb2 = [c.sb(esM1, [128, 512], F32, 'yb%d' % i) for i in range(2)]
                    ob_v = scr_ob.rearrange("(w r) cc -> r w cc", r=32)
                    k = 0
                    wn = 0
                    for tb in range(4):
                        c.dma(uTb[:], scr_u[:, :, tb * 512:(tb + 1) * 512], reads=[scru_buf], writes=[uTb])
                        for j in range(4):
                            i = tb * 4 + j
                            o_ = obt[i % 2]
                            c.dma(o_[0:64, :], ob_v[2 * i], reads=[scrob_buf], writes=[o_])
                            c.dma(o_[64:128, :], ob_v[2 * i + 1], reads=[scrob_buf], writes=[o_])
                            pb = psb(5 + i % 2)
                            for hc in range(4):
                                c.tr(pb[:, hc * 128:(hc + 1) * 128], o_[:, hc * 128:(hc + 1) * 128], identb[:], [o_, identb], [PS[5 + i % 2]],
                                     inc=(hc == 3))
                            c.cp('act', obTb[:, :, j * 128:(j + 1) * 128], pb[:, 0:512].rearrange("p (a b) -> p a b", a=4), [PS[5 + i % 2]], [obTb])
                        tsl = slice(tb * 512, (tb + 1) * 512)
                        for fc in range(8):
                            wa_ = wg_[wn % 4]
                            wb_ = wg_[(wn + 1) % 4]
                            wn += 2
                            ga0 = A_COLS + B_COLS + fc * 128
                            c.dma(wa_[:], w_in_v[:, :, ga0:ga0 + 128], writes=[wa_], q='pool')
                            c.dma(wb_[:], w_in_v[:, :, ga0 + D:ga0 + D + 128], writes=[wb_], q='pool')
                            fsl = slice(fc * 128, (fc + 1) * 128)
                            fp_ = fc % 2
                            Q0, Q1, Q2, Q3 = (PS[4 * fp_ + j_] for j_ in range(4))
                            sga, sgb, ya, yb = sga2[fp_], sgb2[fp_], ya2[fp_], yb2[fp_]
                            for kc in range(8):
                                c.mm(Q2[:, :], wa_[:, kc, :], uTb[:, kc, :], kc == 0, kc == 7, [wa_, uTb], [Q2])
                            for kc in range(8):
                                c.mm(Q3[:, :], wb_[:, kc, :], uTb[:, kc, :], kc == 0, kc == 7, [wb_, uTb], [Q3])
                            for cc in range(4):
                                c.mm(Q0[:, :], woa[:, cc, fsl], oaT[:, cc, tsl], cc == 0, cc == 3, [woa, oaT], [Q0])
                            for cc in range(4):
                                c.mm(Q1[:, :], wob[:, cc, fsl], obTb[:, cc, :], cc == 0, cc == 3, [wob, obTb], [Q1])
                            c.act(sga[:], Q2[:, :], AF.Sigmoid, [Q2], [sga])
                            c.act(sgb[:], Q3[:, :], AF.Sigmoid, [Q3], [sgb])
                            c.tt('dve', ya[:], Q0[:, :], sga[:], ALU.mult, [Q0, sga], [ya])
                            c.tt('dve', yb[:], Q1[:, :], sgb[:], ALU.mult, [Q1, sgb], [yb])
                            c.tt('pool', yT[:, fc, tsl], ya[:], yb[:], ALU.add, [ya, yb], [yT])
                    c.barrier()
                if 'd_yT' in T:
                    c.dma(T['d_yT'], yT[:], reads=[yT])
                with ExitStack() as esM2:
                    wout = c.sb(esM2, [128, 8, D], BF16, 'wout')
                    c.dma(wout[:, 0:4, :], w_out.rearrange("(cc p) n -> p cc n", p=128)[:, 0:4, :], writes=[wout], q='pool')
                    c.dma(wout[:, 4:8, :], w_out.rearrange("(cc p) n -> p cc n", p=128)[:, 4:8, :], writes=[wout], q='pool')
                    Bg1 = c.sb(esM2, [128, D], F32, 'Bg1')
                    make_Bg(esM2, Bg1, 16)
                    wr32 = c.sb(esM2, [128, 8, 36], F32, 'wr32')
                    rbb = c.sb(esM2, [128, 36], F32, 'rbb')
                    c.dma(wr32[:], wrt, writes=[wr32])
                    c.dma(rbb[:], rb_bc, writes=[rbb])
                    Xh = [c.sb(esM2, [128, D], F32, 'Xh%d' % i) for i in range(2)]
                    Hh = [c.sb(esM2, [128, D], F32, 'Hh%d' % i) for i in range(2)]
                    hn2 = [c.sb(esM2, [128, D], F32, 'hn%d' % i) for i in range(2)]
                    sqh2 = [c.sb(esM2, [128, D], BF16, 'sqh%d' % i) for i in range(2)]
                    t322 = [c.sb(esM2, [128, 8, 128], F32, 't32%d' % i) for i in range(2)]
                    st2 = [c.sb(esM2, [128, 64], F32, 'st%d' % i) for i in range(2)]
                    lg2 = [c.sb(esM2, [128, 36], F32, 'lg%d' % i) for i in range(2)]
                    tm32 = [c.sb(esM2, [128, 32], F32, 'tm3%d' % i) for i in range(2)]
                    cw2 = [c.sb(esM2, [128, 32], F32, 'cw%d' % i) for i in range(2)]

                    def tile_chain(t):
                        hn, sqh, t32, st, lg, tm3, cw = hn2[t], sqh2[t], t322[t], st2[t], lg2[t], tm32[t], cw2[t]
                        B = [PS[4 * t + j] for j in range(4)]
                        X_, H_ = Xh[t], Hh[t]
                        for i in range(t, 16, 2):
                            c.dma(X_[:], x[i * 128:(i + 1) * 128, :], writes=[X_])
                            for nh in range(2):
                                bank = B[nh]
                                for fc in range(8):
                                    c.mm(bank[:, :], yT[:, fc, i * 128:(i + 1) * 128], wout[:, fc, nh * 512:(nh + 1) * 512], fc == 0, fc == 7, [yT, wout], [bank])
                                hs = slice(nh * 512, (nh + 1) * 512)
                                c.tt('dve', H_[:, hs], bank[:, :], Bg1[:, hs], ALU.mult, [bank, Bg1], [H_])
                                yield
                            c.tt('pool', H_[:], H_[:], X_[:], ALU.add, [H_, X_], [H_])
                            yield
                            c.dma(scr_h[i * 128:(i + 1) * 128, :], H_[:], reads=[H_], writes=[scrh_buf])
                            c.act(sqh[:], H_[:], AF.Square, [H_], [sqh, st], accum_out=st[:, 0:1])
                            yield
                            c.ts('dve', st[:, 1:2], st[:, 0:1], 1.0 / D, EPS, ALU.mult, ALU.add, [st], [st])
                            c.act(st[:, 2:3], st[:, 1:2], AF.Sqrt, [st], [st])
                            yield
                            c.op('dve', lambda e: e.reciprocal(out=st[:, 3:4], in_=st[:, 2:3]), reads=[st], writes=[st])
                            c.ts('dve', hn[:], H_[:], st[:, 3:4], None, ALU.mult, None, [H_, st], [hn])
                            yield
                            for fc in range(8):
                                bank = B[2 + fc // 4]
                                c.tr(bank[:, (fc % 4) * 128:(fc % 4 + 1) * 128], hn[:, fc * 128:(fc + 1) * 128], ident[:], [hn, ident], [bank],
                                     inc=(fc % 4 == 3))
                            yield
                            for fc in range(8):
                                bank = B[2 + fc // 4]
                                i_ = bank[:, (fc % 4) * 128:(fc % 4 + 1) * 128]
                                c.ts('dve', t32[:, fc, :], i_, A2g[:, fc, 0:1], mT[:, 24 + fc, 0:1], ALU.mult, ALU.add, [bank, A2g, mT], [t32])
                                c.cp('act', tT[:, fc, i * 128:(i + 1) * 128], t32[:, fc, :], [t32], [tT])
                                if fc % 4 == 3:
                                    yield
                            for kc in range(8):
                                c.mm(B[0][:, 0:36], t32[:, kc, :], wr32[:, kc, :], kc == 0, kc == 7, [t32, wr32], [B[0]])
                            yield
                            c.tt('dve', lg[:], B[0][:, 0:36], rbb[:], ALU.add, [B[0], rbb], [lg])
                            c.op('dve', lambda e: e.tensor_reduce(out=st[:, 8:9], in_=lg[:, 0:4], axis=AX.X, op=ALU.max), reads=[lg], writes=[st])
                            c.ts('dve', st[:, 9:10], st[:, 8:9], -1.0, None, ALU.mult, None, [st], [st])
                            yield
                            c.act(st[:, 16:20], lg[:, 0:4], AF.Exp, [lg, st], [st], bias=st[:, 9:10], accum_out=st[:, 10:11])
                            yield
                            c.op('dve', lambda e: e.reciprocal(out=st[:, 11:12], in_=st[:, 10:11]), reads=[st], writes=[st])
                            c.ts('dve', st[:, 20:24], lg[:, 0:4], st[:, 8:9], None, ALU.is_ge, None, [lg, st], [st])
                            c.tt('dve', tm3[:].rearrange("p (g e) -> p g e", g=4), lg[:, 4:36].rearrange("p (g e) -> p g e", g=4),
                                 st[:, 20:24].unsqueeze(2).to_broadcast([128, 4, 8]), ALU.mult, [lg, st], [tm3])
                            yield
                            c.op('dve', lambda e: e.tensor_reduce(out=st[:, 24:32], in_=tm3[:].rearrange("p (g e) -> p e g", g=4), axis=AX.X, op=ALU.add),
                                 reads=[tm3], writes=[st])
                            c.op('dve', lambda e: e.max(out=st[:, 32:40], in_=st[:, 24:32]), reads=[st], writes=[st])
                            c.tt('dve', st[:, 40:41], st[:, 33:34], st[:, 32:33], ALU.subtract, [st], [st])
                            yield
                            c.act(st[:, 41:42], st[:, 40:41], AF.Exp, [st], [st])
                            yield
                            c.ts('dve', st[:, 42:43], st[:, 41:42], 1.0, None, ALU.add, None, [st], [st])
                            c.op('dve', lambda e: e.reciprocal(out=st[:, 43:44], in_=st[:, 42:43]), reads=[st], writes=[st])
                            c.tt('dve', st[:, 44:45], st[:, 43:44], st[:, 11:12], ALU.mult, [st], [st])
                            yield
                            c.tt('dve', st[:, 45:46], st[:, 44:45], st[:, 41:42], ALU.mult, [st], [st])
                            c.tt('dve', st[:, 46:47], st[:, 44:45], st[:, 45:46], ALU.subtract, [st], [st])
                            c.ts('dve', st[:, 48:56], st[:, 24:32], st[:, 32:33], st[:, 46:47], ALU.is_ge, ALU.mult, [st], [st])
                            yield
                            c.ts('dve', st[:, 56:64], st[:, 24:32], st[:, 33:34], st[:, 45:46], ALU.is_ge, ALU.mult, [st], [st])
                            c.tt('dve', st[:, 48:56], st[:, 48:56], st[:, 56:64], ALU.add, [st], [st])
                            c.tt('dve', cw[:].rearrange("p (g e) -> p g e", g=4), st[:, 20:24].unsqueeze(2).to_broadcast([128, 4, 8]),
                                 st[:, 48:56].unsqueeze(1).to_broadcast([128, 4, 8]), ALU.mult, [st], [cw])
                            yield
                            c.tr(B[1][0:32, 0:128], cw[:], ident[:], [cw, ident], [B[1]])
                            yield
                            c.cp('act', R(cwT[:, i * 128:(i + 1) * 128]), B[1][0:32, 0:128], [B[1]], [cwT])
                            yield

                    run_threads([tile_chain(0), tile_chain(1)])
                    c.barrier()
            if 'd_tT' in T:
                c.dma(T['d_tT'], tT[:], reads=[tT])
            if 'd_cwT' in T:
                c.dma(T['d_cwT'], cwT[:], reads=[cwT])

            with ExitStack() as esE:
                moe_acc = c.sb(esE, [128, 16, D], F32, 'moe_acc')
                macc = [[Buf(None, 'macc%d_%d' % (t_, n_)) for n_ in range(2)] for t_ in range(16)]
                evt = [c.sb(esE, [128, 512], F32, 'evt%d' % i) for i in range(3)]
                wgb = [c.sb(esE, [128, 8, 256], BF16, 'wgb%d' % i) for i in range(2)]
                wub = [c.sb(esE, [128, 8, 256], BF16, 'wub%d' % i) for i in range(2)]
                wdb = [c.sb(esE, [128, 2, D], BF16, 'wdb%d' % i) for i in range(2)]
                selt = [c.sb(esE, [32, 128], F32, 'selt%d' % i) for i in range(2)]
                sg_ = [c.sb(esE, [128, 512], F32, 'sg%d' % i) for i in range(2)]
                hu_ = [c.sb(esE, [128, 512], F32, 'hu%d' % i) for i in range(2)]
                hid = [c.sb(esE, [128, 2, 512], BF16, 'hid%d' % i) for i in range(2)]
                NEXP = int(os.environ.get('K_NEXP', '32'))
                def moe_gu(e_, tg, k):
                    wg, wu, wd = wgb[e_ % 2], wub[e_ % 2], wdb[e_ % 2]
                    se = selt[e_ % 2]
                    if tg == 0:
                        c.dma(wg[:], moe_wg[e_], writes=[wg], q='pool')
                        c.dma(wu[:], moe_wu[e_], writes=[wu], q='pool')
                        c.dma(wd[:], moe_wd[e_], writes=[wd], q='pool')
                        c.ts('dve', R(se[:]), onesf[0:32, :], ident[0:32, e_:e_ + 1], None, ALU.mult, None, [onesf, ident], [se])
                    tsl = slice(tg * 512, (tg + 1) * 512)
                    hd = hid[k % 2]
                    c.mm(PS[4][:, :], R(se[:]), R(cwT[:, tsl]), True, True, [se, cwT], [PS[4]])
                    for f2 in range(2):
                        fs = slice(f2 * 128, (f2 + 1) * 128)
                        for kc in range(8):
                            c.mm(PS[f2][:, :], wg[:, kc, fs], tT[:, kc, tsl], kc == 0, kc == 7, [wg, tT], [PS[f2]])
                        for kc in range(8):
                            c.mm(PS[2 + f2][:, :], wu[:, kc, fs], tT[:, kc, tsl], kc == 0, kc == 7, [wu, tT], [PS[2 + f2]])
                        c.act(sg_[f2][:], PS[f2][:, :], AF.Silu, [PS[f2]], [sg_[f2]])
                        c.tt('dve', hu_[f2][:], PS[2 + f2][:, :], sg_[f2][:], ALU.mult, [PS[2 + f2], sg_[f2]], [hu_[f2]])
                        c.tt('dve', hd[:, f2, :], PS[4][:, :], hu_[f2][:], ALU.mult, [PS[4], hu_[f2]], [hd])

                MOE_SPLIT = False

                def moe_dn(e_, tg, k):
                    wd = wdb[e_ % 2]
                    hd = hid[k % 2]
                    for tt_ in range(4):
                        tile = tg * 4 + tt_
                        for nh in range(2):
                            bank = PS[5 + (tt_ * 2 + nh) % 3]
                            for f2 in range(2):
                                c.mm(bank[:, :], hd[:, f2, tt_ * 128:(tt_ + 1) * 128], wd[:, f2, nh * 512:(nh + 1) * 512], f2 == 0, f2 == 1,
                                     [hd, wd], [bank])
                            hs = slice(nh * 512, (nh + 1) * 512)
                            ma = macc[tile][nh]
                            gi = tt_ * 2 + nh
                            if e_ == 0:
                                c.cp('act', moe_acc[:, tile, hs], bank[:, :], [bank], [ma])
                            elif gi % 2 == 0 or not MOE_SPLIT:
                                c.tt('dve', moe_acc[:, tile, hs], bank[:, :], moe_acc[:, tile, hs], ALU.add, [bank, ma], [ma])
                            else:
                                ev = evt[(gi // 2) % 3]
                                c.cp('act', ev[:], bank[:, :], [bank], [ev])
                                c.tt('pool', moe_acc[:, tile, hs], moe_acc[:, tile, hs], ev[:], ALU.add, [ma, ev], [ma])

                its = [(e_, tg) for e_ in range(NEXP) for tg in range(4)]
                for k, (e_, tg) in enumerate(its):
                    moe_gu(e_, tg, k)
                    if k > 0:
                        moe_dn(its[k - 1][0], its[k - 1][1], k - 1)
                moe_dn(its[-1][0], its[-1][1], len(its) - 1)
                Bg2 = c.sb(esE, [128, D], F32, 'Bg2')
                make_Bg(esE, Bg2, 40)
                gfin = c.sb(esE, [128, D], F32, 'gfin')
                c.dma(gfin[:], gfin_bc, writes=[gfin])
                Hf = [c.sb(esE, [128, D], F32, 'Hf%d' % i) for i in range(2)]
                sf = [c.sb(esE, [128, 4], F32, 'sf%d' % i) for i in range(2)]
                c.barrier()
                Hm = [Buf(wgb[i].t.bitcast(F32).rearrange("p a b -> p (a b)"), 'Hm%d' % i) for i in range(2)]
                sqf2 = [Buf(wub[i].t.rearrange("p a b -> p (a b)"), 'sqf%d' % i) for i in range(2)]

                def fin_g(par_):
                    H_, s_, Hm_, sq_ = Hf[par_], sf[par_], Hm[par_], sqf2[par_]
                    for i in range(par_, 16, 2):
                        c.dma(H_[:], scr_h[i * 128:(i + 1) * 128, :], reads=[scrh_buf], writes=[H_])
                        c.tt('dve', Hm_[:], moe_acc[:, i, :], Bg2[:], ALU.mult, [macc[i][0], macc[i][1], Bg2], [Hm_])
                        yield
                        c.tt('pool', H_[:], H_[:], Hm_[:], ALU.add, [H_, Hm_], [H_])
                        yield
                        c.act(sq_[:, 0:D], H_[:], AF.Square, [H_], [sq_, s_], accum_out=s_[:, 0:1])
                        yield
                        c.ts('dve', s_[:, 1:2], s_[:, 0:1], 1.0 / D, EPS, ALU.mult, ALU.add, [s_], [s_])
                        yield
                        c.act(s_[:, 2:3], s_[:, 1:2], AF.Sqrt, [s_], [s_])
                        yield
                        c.op('dve', lambda e, s_=s_: e.reciprocal(out=s_[:, 3:4], in_=s_[:, 2:3]), reads=[s_], writes=[s_])
                        c.stt(H_[:], H_[:], s_[:, 3:4], gfin[:], ALU.mult, ALU.mult, [H_, s_, gfin], [H_])
                        yield
                        c.dma(out[i * 128:(i + 1) * 128, :], H_[:], reads=[H_])
                        yield

                run_threads([fin_g(0), fin_g(1)])
                c.barrier()

        c.finish()
        print("ninstr", c.ninstr, {k_: v for k_, v in c.cnt.items()})
    return nc


def prep_inputs(inp):
    f = lambda a: np.ascontiguousarray(a, dtype=np.float32)
    fm = lambda v: f(np.asarray(v).reshape(-1, 128).T)
    shared = {
        "ada_w": f(inp["ada_w"][0]),
        "ada_bT": fm(inp["ada_b"][0]),
        "gmixT": fm(inp["norm_mix_g"][0]),
        "gffnT": fm(inp["norm_ffn_g"][0]),
        "gfin_bc": f(np.broadcast_to(inp["final_norm_g"][None, :], (128, D))),
        "w_in": f(inp["w_in"][0]),
        "convT": f(inp["gdn_conv"][0].T.reshape(12, 128, 5).transpose(1, 0, 2)),
        "alog_bc": f(np.broadcast_to(inp["gdn_a_log"][0].reshape(1, 1, 8), (128, 18, 8))),
        "dtb_bc": f(np.broadcast_to(inp["gdn_dt_bias"][0].reshape(1, 1, 8), (128, 18, 8))),
        "onormT": f(inp["gdn_onorm_g"][0].reshape(128, 1)),
        "muT": fm(inp["rwkv_mu"][0]),
        "w0T": f(inp["rwkv_w0"][0].reshape(2, 4, 128).transpose(2, 0, 1)),
        "a0T": f(inp["rwkv_a0"][0].reshape(2, 4, 128).transpose(2, 0, 1)),
        "w2m": f(inp["rwkv_w2"][0].reshape(128, 512)),
        "a2m": f(inp["rwkv_a2"][0].reshape(128, 512)),
        "g2m": f(inp["rwkv_g2"][0]),
        "w_o_a": f(inp["w_o_a"][0]),
        "w_o_b": f(inp["w_o_b"][0]),
        "w_out": f(inp["w_out"][0]),
        "wrt": f(np.concatenate([inp["router_grp"][0], inp["router_exp"][0]], axis=1).reshape(8, 128, 36).transpose(1, 0, 2)),
        "rb_bc": f(np.broadcast_to(np.concatenate([inp["router_grp_b"][0], inp["router_exp_b"][0]])[None, :], (128, 36))),
        "moe_wg": f(np.asarray(inp["moe_w_gate"][0]).reshape(32, 8, 128, 256).transpose(0, 2, 1, 3)),
        "moe_wu": f(np.asarray(inp["moe_w_up"][0]).reshape(32, 8, 128, 256).transpose(0, 2, 1, 3)),
        "moe_wd": f(np.asarray(inp["moe_w_down"][0]).reshape(32, 2, 128, D).transpose(0, 2, 1, 3)),
        "rwv": f(np.stack([fm(inp["rwkv_k_k"][0]), fm(inp["rwkv_k_a"][0]), fm(inp["rwkv_r_k"][0].reshape(-1)),
                           fm(inp["rwkv_lnx_g"][0]), fm(inp["rwkv_lnx_b"][0])], axis=1)),
    }
    maps = []
    for b in range(NCORES):
        m = dict(shared)
        m["x"] = f(inp["x"][b])
        m["ctx"] = f(inp["ctx"][b])
        m["cT"] = f(np.stack([fm(inp["c"][b]), fm(inp["c_ctx"])], axis=-1))
        maps.append(m)
    return maps


def kernel(**inputs):
    maps = prep_inputs(inputs)
    nc = build()
    res = run_bass_kernel_spmd(nc, maps, core_ids=list(range(NCORES)))
    return np.stack([np.asarray(r["out"]) for r in res.results], axis=0).astype(np.float32)
```

```python
import os
import numpy as np
import concourse.bass as bass
import concourse.mybir as mybir
from concourse.bass_utils import run_bass_kernel_spmd
from concourse.alu_op_type import AluOpType as ALU
from contextlib import ExitStack

F32 = mybir.dt.float32
F32R = mybir.dt.float32r
BF16 = mybir.dt.bfloat16
AF = mybir.ActivationFunctionType
AX = mybir.AxisListType

NCORES = 8
W_CHUNKS = [128 * j for j in range(16)] + [2064 + 128 * j for j in range(15)] + [3984 + 128 * j for j in range(16)]
D = 1024
SEQ = 2048
CTX = 256
NTOK = SEQ + CTX
IN_COLS = 6032
A_COLS = 2064
B_COLS = 1920
EPS = 1e-6
LNX_EPS = 1e-5 * 64


class Buf:
    def __init__(self, t, name, psum=False):
        self.t = t
        self.name = name
        self.lw = None
        self.rd = {}
        self.psum = psum
        self.bankrd = None

    def __getitem__(self, idx):
        return self.t[idx]


class Ctx:
    ENG = ['pe', 'dve', 'act', 'pool', 'sp']
    NDMA = 8

    def __init__(self, nc, es):
        self.nc = nc
        self.e = {'pe': nc.tensor, 'dve': nc.vector, 'act': nc.scalar, 'pool': nc.gpsimd, 'sp': nc.sync}
        self.sem = {}
        self.cnt = {}
        for n in self.ENG:
            self.sem[n] = es.enter_context(nc.semaphore('s_' + n))
            self.cnt[n] = 0
        for i in range(self.NDMA):
            n = 'd%d' % i
            self.sem[n] = es.enter_context(nc.semaphore('s_' + n))
            self.cnt[n] = 0
        self.dma_rr = 0
        self.waited = {n: {} for n in self.ENG}
        self.nbuf = 0
        self.ninstr = 0

    def sb(self, es, shape, dt=F32, name=None):
        self.nbuf += 1
        name = (name or 'b') + '_%d' % self.nbuf
        t = es.enter_context(self.nc.sbuf_tensor(name, list(shape), dt))
        return Buf(t, name)

    def ps(self, es, shape, dt=F32, name=None):
        self.nbuf += 1
        name = (name or 'p') + '_%d' % self.nbuf
        t = es.enter_context(self.nc.psum_tensor(name, list(shape), dt))
        return Buf(t, name, psum=True)

    def view(self, buf, name='v'):
        self.nbuf += 1
        return Buf(buf.t, name + '_%d' % self.nbuf)

    def _deps(self, reads, writes):
        deps = {}

        def add(k, v):
            if v > deps.get(k, 0):
                deps[k] = v
        for b in reads:
            if b.lw:
                add(*b.lw)
            if b.psum:
                for k, v in b.rd.items():
                    add(k, v)
                if b.bankrd is not None:
                    for k, v in b.bankrd.items():
                        add(k, v)
        for b in writes:
            if b.lw:
                add(*b.lw)
            for k, v in b.rd.items():
                add(k, v)
        return deps

    def _wait(self, E, deps):
        eng = self.e[E]
        w = self.waited[E]
        nw = 0
        for k, v in deps.items():
            if k == E and E == 'pe' and v > self.cnt['pe']:
                continue
            if w.get(k, 0) >= v:
                continue
            eng.wait_ge(self.sem[k], v)
            nw += 1
            w[k] = v

    def op(self, E, fn, reads=(), writes=(), inc=True):
        deps = self._deps(reads, writes)
        self._wait(E, deps)
        ins = fn(self.e[E])
        self.ninstr += 1
        if inc:
            self.cnt[E] += 1
            ins.then_inc(self.sem[E], 1)
            cval = self.cnt[E]
        else:
            cval = self.cnt[E] + 1
        for b in writes:
            b.lw = (E, cval)
            b.rd = {}
        for b in reads:
            if b not in writes:
                b.rd[E] = max(b.rd.get(E, 0), cval)
            if b.bankrd is not None and E != 'pe':
                b.bankrd[E] = max(b.bankrd.get(E, 0), cval)
        return ins

    def dma(self, out, in_, reads=(), writes=(), q='sp', **kw):
        slot = 'd%d' % self.dma_rr
        self.dma_rr = (self.dma_rr + 1) % self.NDMA
        deps = self._deps(reads, writes)
        if self.cnt[slot] > 0:
            deps[slot] = max(deps.get(slot, 0), self.cnt[slot])
        self._wait(q, deps)
        ins = self.e[q].dma_start(out=out, in_=in_, **kw)
        self.ninstr += 1
        self.cnt[slot] += 16
        ins.then_inc(self.sem[slot], 16)
        cval = self.cnt[slot]
        for b in writes:
            b.lw = (slot, cval)
            b.rd = {}
        for b in reads:
            b.rd[slot] = max(b.rd.get(slot, 0), cval)
        return ins

    def barrier(self):
        for E in self.ENG:
            deps = {k: v for k, v in self.cnt.items() if v > 0 and k != E}
            self._wait(E, deps)

    def finish(self):
        for k in self.sem:
            if k.startswith('d') and self.cnt[k] > 0:
                self.e['sp'].wait_ge(self.sem[k], self.cnt[k])

    def mm(self, out, lhsT, rhs, start, stop, reads, writes, inc=None):
        if inc is None:
            inc = stop
        return self.op('pe', lambda e: e.matmul(out, lhsT=lhsT, rhs=rhs, start=start, stop=stop),
                       reads=reads, writes=writes, inc=inc)

    def tr(self, out, in_, ident, reads, writes, inc=True):
        return self.op('pe', lambda e: e.transpose(out=out, in_=in_, identity=ident), reads=reads, writes=writes, inc=inc)

    def act(self, out, in_, func, reads, writes, E='act', **kw):
        return self.op('act', lambda e: e.activation(out=out, in_=in_, func=func, **kw), reads=reads, writes=writes)

    def ts(self, E, out, in0, s1, s2, op0, op1, reads, writes):
        if op1 is None:
            return self.op(E, lambda e: e.tensor_scalar(out=out, in0=in0, scalar1=s1, scalar2=None, op0=op0), reads=reads, writes=writes)
        return self.op(E, lambda e: e.tensor_scalar(out=out, in0=in0, scalar1=s1, scalar2=s2, op0=op0, op1=op1), reads=reads, writes=writes)

    def tt(self, E, out, in0, in1, op, reads, writes):
        return self.op(E, lambda e: e.tensor_tensor(out=out, in0=in0, in1=in1, op=op), reads=reads, writes=writes)

    def stt(self, out, in0, scalar, in1, op0, op1, reads, writes):
        return self.op('dve', lambda e: e.scalar_tensor_tensor(out=out, in0=in0, scalar=scalar, in1=in1, op0=op0, op1=op1),
                       reads=reads, writes=writes)

    def cp(self, E, out, in_, reads, writes):
        if E == 'act':
            return self.op('act', lambda e: e.copy(out=out, in_=in_), reads=reads, writes=writes)
        return self.op(E, lambda e: e.tensor_copy(out=out, in_=in_), reads=reads, writes=writes)


def R(ap):
    return ap.bitcast(F32R)


def build(dbg=(), stage=99):
    nc = bass.Bass("TRN2", target_bir_lowering=False)
    T = {}

    def din(name, shape, dt=F32):
        T[name] = nc.dram_tensor(name, list(shape), dt, kind="ExternalInput").ap()
        return T[name]

    def dout(name, shape, dt=F32):
        T[name] = nc.dram_tensor(name, list(shape), dt, kind="ExternalOutput").ap()
        return T[name]

    x = din("x", [SEQ, D])
    ctx = din("ctx", [CTX, D])
    cT = din("cT", [128, 8, 2])
    ada_w = din("ada_w", [D, 6 * D])
    ada_bT = din("ada_bT", [128, 48])
    gmixT = din("gmixT", [128, 8])
    gffnT = din("gffnT", [128, 8])
    gfin_bc = din("gfin_bc", [128, D])
    w_in = din("w_in", [D, IN_COLS])
    w_in_c = din("w_in_c", [len(W_CHUNKS), 128, 8, 128])
    cidx = {c0: i for i, c0 in enumerate(W_CHUNKS)}
    convT = din("convT", [128, 12, 5])
    alog_bc = din("alog_bc", [128, 18, 8])
    dtb_bc = din("dtb_bc", [128, 18, 8])
    onormT = din("onormT", [128, 1])
    muT = din("muT", [128, 15])
    w0T = din("w0T", [128, 2, 4])
    a0T = din("a0T", [128, 2, 4])
    w2m = din("w2m", [128, 512])
    a2m = din("a2m", [128, 512])
    g2m = din("g2m", [128, 512])
    rwv = din("rwv", [128, 5, 4])
    scr_ob = nc.dram_tensor("scr_ob", [SEQ, 512], BF16, kind="Internal").ap()
    scr_h = nc.dram_tensor("scr_h", [SEQ, D], F32, kind="Internal").ap()
    scr_u = nc.dram_tensor("scr_u", [128, 8, SEQ], BF16, kind="Internal").ap()
    w_o_a = din("w_o_a", [512, D])
    w_o_b = din("w_o_b", [512, D])
    w_out = din("w_out", [D, D])
    wrt = din("wrt", [128, 8, 36])
    rb_bc = din("rb_bc", [128, 36])
    moe_wg = din("moe_wg", [32, 128, 8, 256])
    moe_wu = din("moe_wu", [32, 128, 8, 256])
    moe_wd = din("moe_wd", [32, 128, 2, D])
    out = dout("out", [SEQ, D])
    for name, shape, dt in dbg:
        dout(name, shape, dt)

    with ExitStack() as es:
        c = Ctx(nc, es)
        PS = [c.ps(es, [128, 512], F32, 'ps%d' % i) for i in range(8)]

        def psb(i):
            return PS[i][:].bitcast(BF16)

        bank_reads = [dict() for _ in range(8)]

        class Reg:
            def __init__(self, bank, c0, n, name):
                self.bank, self.c0, self.n = bank, c0, n
                self.buf = PS[bank]

            def ap(self, lo=0, hi=None, rows=slice(None)):
                hi = self.n if hi is None else hi
                return PS[self.bank].t[rows, self.c0 + lo:self.c0 + hi]

        def run_threads(gens):
            gens = list(gens)
            while gens:
                for g_ in list(gens):
                    try:
                        next(g_)
                    except StopIteration:
                        gens.remove(g_)

        def par(*gens):
            gens = list(gens)
            while gens:
                for g_ in list(gens):
                    try:
                        next(g_)
                    except StopIteration:
                        gens.remove(g_)
                        continue
                    yield

        ident = c.sb(es, [128, 128], F32, 'ident')
        identb = c.sb(es, [128, 128], BF16, 'identb')
        onesf = c.sb(es, [128, 128], F32, 'onesf')
        c.op('pool', lambda e: e.memset(ident[:], 0.0), writes=[ident])
        c.op('pool', lambda e: e.affine_select(out=ident[:], in_=ident[:], pattern=[[-1, 128]], compare_op=ALU.not_equal,
                                                fill=1.0, base=0, channel_multiplier=1), reads=[ident], writes=[ident])
        c.cp('dve', identb[:], ident[:], [ident], [identb])
        c.op('pool', lambda e: e.memset(onesf[:], 1.0), writes=[onesf])
        onesb = c.sb(es, [128, 128], BF16, 'onesb')
        c.cp('dve', onesb[:], onesf[:], [onesf], [onesb])
        ones_r = c.sb(es, [128, 128], F32, 'ones_r')
        nones_r = c.sb(es, [128, 128], F32, 'nones_r')
        ident_r = c.sb(es, [128, 128], F32, 'ident_r')
        c.cp('dve', R(ones_r[:]), onesf[:], [onesf], [ones_r])
        c.ts('dve', R(nones_r[:]), onesf[:], -1.0, None, ALU.mult, None, [onesf], [nones_r])
        c.cp('dve', R(ident_r[:]), ident[:], [ident], [ident_r])
        blk = c.sb(es, [128, 128], F32, 'blk')
        c.op('pool', lambda e: e.memset(blk[:], 0.0), writes=[blk])
        c.op('pool', lambda e: e.memset(blk[0:64, 0:64], 1.0), reads=[blk], writes=[blk])
        c.op('pool', lambda e: e.memset(blk[64:128, 64:128], 1.0), reads=[blk], writes=[blk])
        incl = [c.sb(es, [128, 128], F32, 'incl%d' % d) for d in range(2)]
        strict = [c.sb(es, [128, 128], F32, 'strict%d' % d) for d in range(2)]
        incl_r = [c.sb(es, [128, 128], F32, 'inclr%d' % d) for d in range(2)]
        negm_r = [c.sb(es, [128, 128], F32, 'negm%d' % d) for d in range(2)]
        blk_r = c.sb(es, [128, 128], F32, 'blk_r')
        sel_r = [c.sb(es, [128, 128], F32, 'sel%d' % k_) for k_ in range(2)]
        notI = c.sb(es, [128, 128], F32, 'notI')
        for d in range(2):
            pat = [[1, 128]] if d == 0 else [[-1, 128]]
            cm = -1 if d == 0 else 1
            c.op('pool', lambda e, d=d, pat=pat, cm=cm: e.affine_select(out=incl[d][:], in_=blk[:], pattern=pat, compare_op=ALU.is_ge,
                                                                        fill=0.0, base=0, channel_multiplier=cm), reads=[blk], writes=[incl[d]])
            c.op('pool', lambda e, d=d, pat=pat, cm=cm: e.affine_select(out=strict[d][:], in_=blk[:], pattern=pat, compare_op=ALU.is_gt,
                                                                        fill=0.0, base=0, channel_multiplier=cm), reads=[blk], writes=[strict[d]])
            c.cp('dve', R(incl_r[d][:]), incl[d][:], [incl[d]], [incl_r[d]])
            c.ts('dve', R(negm_r[d][:]), incl[d][:], 1.0e5, -1.0e5, ALU.mult, ALU.add, [incl[d]], [negm_r[d]])
        c.cp('dve', R(blk_r[:]), blk[:], [blk], [blk_r])
        c.ts('dve', notI[:], ident[:], -1.0, 1.0, ALU.mult, ALU.add, [ident], [notI])
        zf = c.sb(es, [128, 128], F32, 'zf')
        c.op('pool', lambda e: e.memset(zf[:], 0.0), writes=[zf])
        for k_ in range(2):
            c.cp('dve', R(sel_r[k_][:]), zf[:], [zf], [sel_r[k_]])
            c.cp('dve', R(sel_r[k_][k_ * 64:(k_ + 1) * 64, :]), onesf[k_ * 64:(k_ + 1) * 64, :], [onesf, sel_r[k_]], [sel_r[k_]])

        mT = c.sb(es, [128, 48, 2], F32, 'mT')
        A1g = c.sb(es, [128, 8, 2], F32, 'A1g')
        A2g = c.sb(es, [128, 8, 2], F32, 'A2g')
        oaT = c.sb(es, [128, 4, SEQ], BF16, 'oaT')

        with ExitStack() as es1:
            sT = c.sb(es1, [128, 8, 2], F32, 'sT')
            abT = c.sb(es1, [128, 48], F32, 'abT')
            gm = c.sb(es1, [128, 8], F32, 'gm')
            gf = c.sb(es1, [128, 8], F32, 'gf')
            c.dma(sT[:], cT, writes=[sT])
            c.dma(abT[:], ada_bT, writes=[abT])
            c.dma(gm[:], gmixT, writes=[gm])
            c.dma(gf[:], gffnT, writes=[gf])
            c.act(sT[:], sT[:], AF.Silu, [sT], [sT])
            Wb = [c.sb(es1, [128, 8, 512], F32, 'adaw%d' % i) for i in range(4)]
            ada_v = ada_w.rearrange("(kc p) n -> p kc n", p=128)
            for blk in range(12):
                wb = Wb[blk % 4]
                c.dma(wb[:], ada_v[:, :, blk * 512:(blk + 1) * 512], writes=[wb], q=('sp' if blk % 2 == 0 else 'act'))
                for mc in range(4):
                    col = (blk * 4 + mc) * 2
                    for kc in range(8):
                        c.mm(PS[0][:, col:col + 2], wb[:, kc, mc * 128:(mc + 1) * 128], sT[:, kc, :],
                             kc == 0, kc == 7, [wb, sT], [PS[0]])
            pv = PS[0][:, 0:96].rearrange("p (m s) -> p m s", s=2)
            for s in range(2):
                c.tt('dve', mT[:, :, s], pv[:, :, s], abT[:], ALU.add, [PS[0], abT], [mT])
            for s in range(2):
                c.stt(A1g[:, :, s], mT[:, 8:16, s], 1.0, gm[:], ALU.add, ALU.mult, [mT, gm], [A1g])
                c.stt(A2g[:, :, s], mT[:, 32:40, s], 1.0, gf[:], ALU.add, ALU.mult, [mT, gf], [A2g])
            c.barrier()

        def make_Bg(es_, Bg, base):
            dg = [c.sb(es_, [128, 128], F32, 'dg%d' % i) for i in range(2)]
            for fc in range(8):
                d_ = dg[fc % 2]
                c.ts('dve', d_[:], ident[:], mT[:, base + fc, 0:1], None, ALU.mult, None, [ident, mT], [d_])
                bank = PS[1 + fc // 4]
                c.mm(bank[:, (fc % 4) * 128:(fc % 4 + 1) * 128], onesf[:], d_[:], True, True, [onesf, d_], [bank])
            c.cp('act', Bg[:, 0:512], PS[1][:], [PS[1]], [Bg])
            c.cp('act', Bg[:, 512:1024], PS[2][:], [PS[2]], [Bg])

        scru_buf = Buf(None, 'scr_u_dram')

        def make_uT_g(srcs, s, dst, tok0, Ag, shift_base, bufs, k):
            X = bufs['X'][k % 4]
            xnb = bufs['xnb'][k % 2]
            ss = bufs['ss'][k % 2]
            sq_ = bufs['sq'][k % 2]
            for (p0, p1, ap) in srcs:
                c.dma(X[p0:p1, :], ap, writes=[X], q=('sp' if k % 2 == 0 else 'pool'))
            c.act(sq_[:], X[:], AF.Square, [X], [sq_, ss], accum_out=ss[:, 0:1])
            yield
            c.ts('dve', ss[:, 1:2], ss[:, 0:1], 1.0 / D, EPS, ALU.mult, ALU.add, [ss], [ss])
            yield
            c.act(ss[:, 2:3], ss[:, 1:2], AF.Sqrt, [ss], [ss])
            yield
            c.op('dve', lambda e: e.reciprocal(out=ss[:, 3:4], in_=ss[:, 2:3]), reads=[ss], writes=[ss])
            c.ts('dve', xnb[:], X[:], ss[:, 3:4], None, ALU.mult, None, [X, ss], [xnb])
            yield
            bank = PS[3 + k % 2]
            pb = psb(3 + k % 2)
            for fc in range(8):
                c.tr(pb[:, fc * 128:(fc + 1) * 128], xnb[:, fc * 128:(fc + 1) * 128], identb[:], [xnb, identb], [bank],
                     inc=(fc == 7))
            yield
            for fc in range(8):
                o_ = dst[:, fc, tok0:tok0 + 128]
                i_ = pb[:, fc * 128:(fc + 1) * 128]
                if fc % 2 == 0:
                    c.ts('dve', o_, i_, Ag[:, fc, s:s + 1], mT[:, shift_base + fc, s:s + 1], ALU.mult, ALU.add,
                         [bank, Ag, mT], [dst])
                else:
                    c.act(o_, i_, AF.Identity, [bank, Ag, mT], [dst], scale=Ag[:, fc, s:s + 1],
                          bias=mT[:, shift_base + fc, s:s + 1])
                if fc % 4 == 3:
                    yield

        def run_uT(jobs, bufs):
            def chain(par_):
                for k in range(par_, len(jobs), 2):
                    srcs, s_, dst, tok0 = jobs[k]
                    yield from make_uT_g(srcs, s_, dst, tok0, A1g, 0, bufs, k)
            run_threads([chain(0), chain(1)])

        def uT_bufs(es_):
            return {'X': [c.sb(es_, [128, D], F32, 'X%d' % i) for i in range(4)],
                    'xnb': [c.sb(es_, [128, D], BF16, 'xnb%d' % i) for i in range(2)],
                    'ss': [c.sb(es_, [128, 4], F32, 'ss%d' % i) for i in range(2)],
                    'sq': [c.sb(es_, [128, D], BF16, 'sq%d' % i) for i in range(2)]}

        esG = ExitStack()
        with esG:
            uT_r = c.sb(esG, [128, 8, NTOK], BF16, 'uT_r')
            with ExitStack() as es2:
                bufs = uT_bufs(es2)
                jobs = [([(0, 128, ctx[t * 128:(t + 1) * 128, :])], 1, uT_r, t * 128) for t in range(2)]
                jobs += [([(0, 128, x[t * 128:(t + 1) * 128, :])], 0, uT_r, CTX + t * 128) for t in range(16)]
                run_uT(jobs, bufs)
                for kc in range(8):
                    c.dma(scr_u[:, kc, :], uT_r[:, kc, CTX:NTOK], reads=[uT_r], writes=[scru_buf])
                c.barrier()


            w_in_v = w_in.rearrange("(kc p) n -> p kc n", p=128)
            TBLK = [(0, 256), (256, 768), (768, 1280), (1280, 1792), (1792, 2304)]
            with ExitStack() as es3:
                g_tok = c.sb(es3, [128, 18, 8], F32, 'g_tok')
                b_tok = c.sb(es3, [128, 18, 8], F32, 'b_tok')
                with ExitStack() as es3a:
                    wab = c.sb(es3a, [128, 8, 16], BF16, 'wab')
                    ab = c.sb(es3a, [128, 18, 16], F32, 'ab')
                    alog = c.sb(es3a, [128, 18, 8], F32, 'alog')
                    dtb = c.sb(es3a, [128, 18, 8], F32, 'dtb')
                    t1 = c.sb(es3a, [128, 18, 8], F32, 't1')
                    t2 = c.sb(es3a, [128, 18, 8], F32, 't2')
                    c.dma(wab[:], w_in_v[:, :, 2048:2064], writes=[wab], q='pool')
                    c.dma(alog[:], alog_bc, writes=[alog])
                    c.dma(dtb[:], dtb_bc, writes=[dtb])
                    for t in range(18):
                        bank = PS[t % 2]
                        for kc in range(8):
                            c.mm(bank[:, 0:16], uT_r[:, kc, t * 128:(t + 1) * 128], wab[:, kc, :], kc == 0, kc == 7, [uT_r, wab], [bank])
                        c.cp('act', ab[:, t, :], bank[:, 0:16], [bank], [ab])
                    c.tt('dve', t1[:], ab[:, :, 0:8], dtb[:], ALU.add, [ab, dtb], [t1])
                    c.stt(t2[:], t1[:], -1.0, t1[:], ALU.mult, ALU.max, [t1], [t2])
                    c.act(t2[:], t2[:], AF.Exp, [t2], [t2], scale=-1.0)
                    c.ts('dve', t2[:], t2[:], 1.0, None, ALU.add, None, [t2], [t2])
                    c.act(t2[:], t2[:], AF.Ln, [t2], [t2])
                    c.stt(t1[:], t1[:], 0.0, t2[:], ALU.max, ALU.add, [t1, t2], [t1])
                    c.act(alog[:], alog[:], AF.Exp, [alog], [alog])
                    c.stt(R(g_tok[:]), t1[:], -1.0, alog[:], ALU.mult, ALU.mult, [t1, alog], [g_tok])
                    c.act(b_tok[:], ab[:, :, 8:16], AF.Sigmoid, [ab], [b_tok])
                    c.barrier()

                cv = c.sb(es3, [128, 12, 5], F32, 'cv')
                onm = c.sb(es3, [128, 1], F32, 'onm')
                c.dma(cv[:], convT, writes=[cv])
                c.dma(onm[:], onormT, writes=[onm])
                raws = [c.sb(es3, [128, NTOK], F32, 'raw%d' % i) for i in range(2)]
                accs = [c.sb(es3, [128, NTOK], F32, 'acc%d' % i) for i in range(2)]
                sqs = [c.sb(es3, [128, NTOK], F32, 'sqg%d' % i) for i in range(2)]
                qT = c.sb(es3, [128, NTOK], F32, 'qT')
                kT = c.sb(es3, [128, NTOK], F32, 'kT')
                vT = c.sb(es3, [128, NTOK], F32, 'vT')
                zs = c.sb(es3, [128, SEQ], F32, 'zs')
                o_acc = c.sb(es3, [128, 16, 128], F32, 'o_acc')
                wc = [c.sb(es3, [128, 8, 128], BF16, 'wc%d' % i) for i in range(2)]
                rns = [[c.sb(es3, [128, 512], F32, 'rn%d_%d' % (j, i)) for i in range(2)] for j in range(2)]
                S = [c.sb(es3, [128, 128], F32, 'S%d' % d) for d in range(2)]
                gcs = [c.sb(es3, [128, 8], F32, 'gcs%d' % i) for i in range(2)]
                egc = [c.sb(es3, [128, 8], F32, 'egc%d' % i) for i in range(2)]
                negc = [c.sb(es3, [128, 8], F32, 'negc%d' % i) for i in range(2)]
                ekd = [c.sb(es3, [128, 8], F32, 'ekd%d' % i) for i in range(2)]
                gend = [c.sb(es3, [128, 2, 8], F32, 'gend%d' % i) for i in range(2)]
                k_tok = [c.sb(es3, [128, 128], F32, 'k_tok%d' % i) for i in range(2)]
                v_tok = [c.sb(es3, [128, 128], F32, 'v_tok%d' % i) for i in range(2)]
                NS = 2
                Gt = [c.sb(es3, [128, 128], F32, 'Gt%d' % i) for i in range(NS)]
                Ei = [c.sb(es3, [128, 128], F32, 'Ei%d' % i) for i in range(NS)]
                Es = [c.sb(es3, [128, 128], F32, 'Es%d' % i) for i in range(NS)]
                QKm = [c.sb(es3, [128, 128], F32, 'QKm%d' % i) for i in range(NS)]
                Pb = [[c.sb(es3, [128, 128], F32, 'P%d_%d' % (i, j)) for j in range(2)] for i in range(NS)]
                Qb = [[c.sb(es3, [128, 128], F32, 'Q%d_%d' % (i, j)) for j in range(2)] for i in range(NS)]
                Wb_ = [[c.sb(es3, [128, 128], F32, 'W%d_%d' % (i, j)) for j in range(2)] for i in range(NS)]
                kdec = [c.sb(es3, [128, 128], F32, 'kdec%d' % i) for i in range(NS)]
                Zb = [c.sb(es3, [128, 128], F32, 'Z%d' % i) for i in range(NS)]
                vnew = [c.sb(es3, [128, 128], F32, 'vnew%d' % i) for i in range(NS)]
                otmp = [c.sb(es3, [128, 128], F32, 'otmp%d' % i) for i in range(NS)]
                otmp2 = [c.sb(es3, [128, 128], F32, 'otmp2%d' % i) for i in range(NS)]
                fin = [c.sb(es3, [128, 132], F32, 'fin%d' % i) for i in range(2)]
                wcnt = 0
                unit = 0
                import os
                H_all = c.sb(es3, [128, 18, 32], F32, 'H_all')
                egc_all = c.sb(es3, [128, 18, 8], F32, 'egc_all')
                negc_all = c.sb(es3, [128, 18, 8], F32, 'negc_all')
                ekd_all = c.sb(es3, [128, 18, 8], F32, 'ekd_all')
                gend_all = c.sb(es3, [128, 18, 16], F32, 'gend_all')
                for t in range(18):
                    bankH = PS[t % 2]
                    c.mm(bankH[:, 0:4], R(incl_r[0][:]), R(g_tok[:, t, 0:4]), True, True, [incl_r[0], g_tok], [bankH])
                    c.mm(bankH[:, 4:8], R(incl_r[1][:]), R(g_tok[:, t, 4:8]), True, True, [incl_r[1], g_tok], [bankH])
                    c.mm(bankH[:, 8:16], R(blk_r[:]), R(g_tok[:, t, :]), True, True, [blk_r, g_tok], [bankH])
                    c.mm(bankH[:, 16:24], R(sel_r[0][:]), R(g_tok[:, t, :]), True, True, [sel_r[0], g_tok], [bankH])
                    c.mm(bankH[:, 24:32], R(sel_r[1][:]), R(g_tok[:, t, :]), True, True, [sel_r[1], g_tok], [bankH])
                    c.cp('dve', H_all[:, t, :], bankH[:, 0:32], [bankH], [H_all])
                c.act(egc_all[:], H_all[:, :, 0:8], AF.Exp, [H_all], [egc_all])
                c.ts('dve', negc_all[:], egc_all[:], -1.0, None, ALU.mult, None, [egc_all], [negc_all])
                c.tt('dve', ekd_all[:], H_all[:, :, 8:16], H_all[:, :, 0:8], ALU.subtract, [H_all], [ekd_all])
                c.act(ekd_all[:], ekd_all[:], AF.Exp, [ekd_all], [ekd_all])
                c.act(gend_all[:], H_all[:, :, 16:32], AF.Exp, [H_all], [gend_all])
                NH = int(os.environ.get('K_NH', '4'))
                NSTEP = int(os.environ.get('K_NSTEP', '18'))
                KLAT = int(os.environ.get('K_LAT', '9'))
                for h in range(NH):
                    def proj_g(ci, slot, h=h):
                        col0, dst = [(h * 128, qT), (512 + h * 128, kT), (1024 + h * 128, vT), (1536 + h * 128, zs)][ci]
                        raw, acc, sq = raws[slot], accs[slot], sqs[slot]
                        w_ = wc[slot]
                        pb0 = 4 * slot
                        if h == 0 and ci < 2:
                            c.dma(w_[:], w_in_c[cidx[col0]], writes=[w_], q='pool')
                        for bi, (t0, t1_) in enumerate(TBLK):
                            if ci == 3 and bi == 0:
                                continue
                            bank = PS[pb0 + bi % 2]
                            n = t1_ - t0
                            for kc in range(8):
                                c.mm(bank[:, 0:n], w_[:, kc, :], uT_r[:, kc, t0:t1_], kc == 0, kc == 7, [w_, uT_r], [bank])
                            if ci == 3:
                                c.act(zs[:, t0 - CTX:t1_ - CTX], bank[:, 0:n], AF.Silu, [bank], [zs])
                            else:
                                c.cp('act', raw[:, t0:t1_], bank[:, 0:n], [bank], [raw])
                            yield
                        nh_, nci = (h, ci + 2) if ci < 2 else (h + 1, ci - 2)
                        if nh_ < NH:
                            ncol = [nh_ * 128, 512 + nh_ * 128, 1024 + nh_ * 128, 1536 + nh_ * 128][nci]
                            c.dma(w_[:], w_in_c[cidx[ncol]], writes=[w_], q='pool')
                        if ci == 3:
                            return
                        cch = ci * 4 + h
                        c.ts('dve', acc[:], raw[:], cv[:, cch, 2:3], None, ALU.mult, None, [raw, cv], [acc])
                        yield
                        for kk_ in (0, 1, 3, 4):
                            sft = kk_ - 2
                            for (a_, b_) in ((0, CTX), (CTX, NTOK)):
                                lo = max(a_, a_ - sft)
                                hi = min(b_, b_ - sft)
                                c.stt(acc[:, lo:hi], raw[:, lo + sft:hi + sft], cv[:, cch, kk_:kk_ + 1], acc[:, lo:hi], ALU.mult, ALU.add,
                                      [raw, cv, acc], [acc])
                            yield
                        if ci == 2:
                            c.act(R(vT[:]), acc[:], AF.Silu, [acc], [vT])
                            return
                        c.act(acc[:], acc[:], AF.Silu, [acc], [acc])
                        c.act(R(sq[:]), acc[:], AF.Square, [acc], [sq])
                        yield
                        sc = 128.0 if ci == 0 else 1.0
                        for bi, (t0, t1_) in enumerate(TBLK):
                            bank = PS[pb0 + 2 + bi % 2]
                            n = t1_ - t0
                            r_ = rns[slot][bi % 2]
                            c.mm(bank[:, 0:n], R(ones_r[:]), R(sq[:, t0:t1_]), True, True, [ones_r, sq], [bank])
                            c.ts('dve', r_[:, 0:n], bank[:, 0:n], sc, EPS * sc, ALU.mult, ALU.add, [bank], [r_])
                            c.act(r_[:, 0:n], r_[:, 0:n], AF.Sqrt, [r_], [r_])
                            yield
                            c.op('dve', lambda e, r_=r_, n=n: e.reciprocal(out=r_[:, 0:n], in_=r_[:, 0:n]), reads=[r_], writes=[r_])
                            c.tt('dve', R(dst[:, t0:t1_]), acc[:, t0:t1_], r_[:, 0:n], ALU.mult, [acc, r_], [dst])
                            yield

                    run_threads([proj_g(0, 0), proj_g(1, 1)])
                    run_threads([proj_g(2, 0), proj_g(3, 1)])
                    if 'd_qkv' in T and h == 0:
                        c.dma(T['d_qkv'][0], qT[:], reads=[qT])
                        c.dma(T['d_qkv'][1], kT[:], reads=[kT])
                        c.dma(T['d_qkv'][2], vT[:], reads=[vT])
                    for d in range(2):
                        c.ts('dve', R(S[d][:]), zf[:], 0.0, None, ALU.mult, None, [zf], [S[d]])
                    order_f = list(range(18))
                    order_b = [1, 0] + list(range(17, 1, -1))
                    def gdn_unit_g(d, step):
                        tile = order_f[step] if d == 0 else order_b[step]
                        is_lat = tile >= 2
                        ts0 = tile * 128
                        col = d * 4 + h
                        pi = d
                        u = d
                        X0, X1, X2, X3 = (PS[4 * d + i_] for i_ in range(4))
                        bankT = X3
                        c.tr(bankT[:, 0:128], kT[:, ts0:ts0 + 128], ident[:], [kT, ident], [bankT])
                        c.tr(bankT[:, 128:256], vT[:, ts0:ts0 + 128], ident[:], [vT, ident], [bankT])
                        bankA = X0
                        c.mm(bankA[:, 0:128], R(kT[:, ts0:ts0 + 128]), R(kT[:, ts0:ts0 + 128]), True, True, [kT], [bankA])
                        c.mm(bankA[:, 128:256], R(kT[:, ts0:ts0 + 128]), R(qT[:, ts0:ts0 + 128]), True, True, [kT, qT], [bankA])
                        c.ts('pool', R(Gt[u][:]), incl[d][:], g_tok[:, tile, col:col + 1], None, ALU.mult, None, [incl[d], g_tok], [Gt[u]])
                        yield
                        bankB = X1
                        c.mm(bankB[:, 0:128], R(ones_r[:]), R(Gt[u][:]), True, False, [ones_r, Gt[u]], [bankB])
                        c.mm(bankB[:, 0:128], R(Gt[u][:]), R(nones_r[:]), False, False, [nones_r, Gt[u]], [bankB])
                        c.mm(bankB[:, 0:128], R(ident_r[:]), R(negm_r[d][:]), False, True, [ident_r, negm_r[d]], [bankB])
                        yield
                        c.cp('act', k_tok[pi][:], bankT[:, 0:128], [bankT], [k_tok[pi]])
                        c.cp('act', R(v_tok[pi][:]), bankT[:, 128:256], [bankT], [v_tok[pi]])
                        c.act(Ei[u][:], bankB[:, 0:128], AF.Exp, [bankB], [Ei[u]])
                        yield
                        c.tt('pool', Es[u][:], Ei[u][:], notI[:], ALU.mult, [Ei[u], notI], [Es[u]])
                        P, Q, W = Pb[u], Qb[u], Wb_[u]
                        yield
                        c.stt(R(P[0][:]), bankA[:, 0:128], b_tok[:, tile, col:col + 1], Es[u][:], ALU.mult, ALU.mult,
                              [bankA, b_tok, Es[u]], [P[0]])
                        c.tt('dve', R(QKm[u][:]), bankA[:, 128:256], Ei[u][:], ALU.mult, [bankA, Ei[u]], [QKm[u]])
                        c.ts('dve', R(kdec[u][:]), k_tok[pi][:], ekd_all[:, tile, col:col + 1], None, ALU.mult, None, [k_tok[pi], ekd_all], [kdec[u]])
                        yield
                        bankC = X2
                        bankD = X3
                        c.tr(bankC[:, 0:128], P[0][:], ident[:], [P[0], ident], [bankC])
                        c.tt('pool', R(W[0][:]), ident[:], P[0][:], ALU.subtract, [ident, P[0]], [W[0]])
                        yield
                        c.cp('act', R(Q[0][:]), bankC[:, 0:128], [bankC], [Q[0]])
                        yield
                        for k_ in range(5):
                            a_, b_ = k_ % 2, (k_ + 1) % 2
                            c.mm(bankC[:, 128:256], R(P[a_][:]), R(Q[a_][:]), True, True, [P[a_], Q[a_]], [bankC])
                            if k_ < 4:
                                c.mm(bankD[:, 0:128], R(Q[a_][:]), R(P[a_][:]), True, True, [P[a_], Q[a_]], [bankD])
                            yield
                            c.cp('act', R(Q[b_][:]), bankC[:, 128:256], [bankC], [Q[b_]])
                            if k_ < 4:
                                c.cp('dve', R(P[b_][:]), bankD[:, 0:128], [bankD], [P[b_]])
                            yield
                            c.mm(bankD[:, 128:256], R(Q[b_][:]), R(W[a_][:]), True, True, [Q[b_], W[a_]], [bankD])
                            yield
                            c.tt('dve', R(W[b_][:]), bankD[:, 128:256], W[a_][:], ALU.add, [bankD, W[a_]], [W[b_]])
                            yield
                        Wf = W[1]
                        for cs in ((0, 64) if d == 0 else (64, 0)):
                            sl_ = slice(cs, cs + 64)
                            chunk = cs // 64
                            c.mm(X0[:, 0:128], R(kT[:, ts0:ts0 + 128]), R(S[d][:]), True, True, [kT, S[d]], [X0])
                            if is_lat:
                                c.mm(X0[:, 128:256], R(qT[:, ts0:ts0 + 128]), R(S[d][:]), True, True, [qT, S[d]], [X0])
                            yield
                            c.stt(R(Zb[u][sl_, :]), X0[sl_, 0:128], negc_all[sl_, tile, col:col + 1], v_tok[pi][sl_, :], ALU.mult, ALU.add,
                                  [X0, negc_all, v_tok[pi]], [Zb[u]])
                            if is_lat:
                                c.ts('dve', otmp[u][sl_, :], X0[sl_, 128:256], egc_all[sl_, tile, col:col + 1], None, ALU.mult, None, [X0, egc_all], [otmp[u]])
                            yield
                            c.mm(X1[:, 0:128], R(Wf[sl_, :]), R(Zb[u][sl_, :]), True, True, [Wf, Zb[u]], [X1])
                            yield
                            c.ts('dve', R(vnew[u][sl_, :]), X1[sl_, 0:128], b_tok[sl_, tile, col:col + 1], None, ALU.mult, None,
                                 [X1, b_tok], [vnew[u]])
                            yield
                            c.mm(X3[:, 256:384], R(kdec[u][sl_, :]), R(vnew[u][sl_, :]), True, True, [kdec[u], vnew[u]], [X3])
                            if is_lat:
                                c.mm(X2[:, 0:128], R(QKm[u][sl_, :]), R(vnew[u][sl_, :]), True, True, [QKm[u], vnew[u]], [X2])
                            yield
                            c.stt(R(S[d][:]), S[d][:], gend_all[:, tile, chunk * 8 + col:chunk * 8 + col + 1], X3[:, 256:384], ALU.mult, ALU.add,
                                  [S[d], gend_all, X3], [S[d]])
                            if is_lat:
                                lt = tile - 2
                                if (d == 0) == (lt <= 7):
                                    c.tt('dve', o_acc[sl_, lt, :], X2[sl_, 0:128], otmp[u][sl_, :], ALU.add, [X2, otmp[u]], [o_acc])
                                else:
                                    c.tt('dve', otmp2[u][sl_, :], X2[sl_, 0:128], otmp[u][sl_, :], ALU.add, [X2, otmp[u]], [otmp2[u]])
                                    c.tt('pool', o_acc[sl_, lt, :], o_acc[sl_, lt, :], otmp2[u][sl_, :], ALU.add, [o_acc, otmp2[u]], [o_acc])
                            yield

                    def gdn_dir_g(d):
                        for step in range(NSTEP):
                            yield from gdn_unit_g(d, step)

                    run_threads([gdn_dir_g(0), gdn_dir_g(1)])
                    def gdn_out_g(par_, h=h):
                        f_ = fin[par_]
                        bank = PS[par_]
                        for lt in range(par_, 16, 2):
                            c.act(f_[:, 0:128], o_acc[:, lt, :], AF.Square, [o_acc], [f_], accum_out=f_[:, 128:129])
                            yield
                            c.ts('dve', f_[:, 129:130], f_[:, 128:129], 1.0 / 128, EPS, ALU.mult, ALU.add, [f_], [f_])
                            yield
                            c.act(f_[:, 130:131], f_[:, 129:130], AF.Sqrt, [f_], [f_])
                            yield
                            c.op('dve', lambda e, f_=f_: e.reciprocal(out=f_[:, 131:132], in_=f_[:, 130:131]), reads=[f_], writes=[f_])
                            c.ts('dve', f_[:, 0:128], o_acc[:, lt, :], f_[:, 131:132], None, ALU.mult, None, [o_acc, f_], [f_])
                            yield
                            c.tr(bank[:, 0:128], f_[:, 0:128], ident[:], [f_, ident], [bank])
                            yield
                            c.stt(oaT[:, h, lt * 128:(lt + 1) * 128], bank[:, 0:128], onm[:, 0:1], zs[:, lt * 128:(lt + 1) * 128], ALU.mult, ALU.mult,
                                  [bank, onm, zs], [oaT])
                            yield

                    run_threads([gdn_out_g(0), gdn_out_g(1)])
                c.barrier()
        if 'd_oaT' in T:
            c.dma(T['d_oaT'], oaT[:], reads=[oaT])


        def inv_chain(P, Q, W, bankC, bankD):
            c.tr(bankC[:, 0:128], P[0][:], ident[:], [P[0], ident], [bankC])
            c.cp('act', R(Q[0][:]), bankC[:, 0:128], [bankC], [Q[0]])
            c.tt('pool', R(W[0][:]), ident[:], P[0][:], ALU.subtract, [ident, P[0]], [W[0]])
            for k_ in range(5):
                a_, b_ = k_ % 2, (k_ + 1) % 2
                c.mm(bankC[:, 128:256], R(P[a_][:]), R(Q[a_][:]), True, True, [P[a_], Q[a_]], [bankC])
                if k_ < 4:
                    c.mm(bankD[:, 0:128], R(Q[a_][:]), R(P[a_][:]), True, True, [P[a_], Q[a_]], [bankD])
                c.cp('act', R(Q[b_][:]), bankC[:, 128:256], [bankC], [Q[b_]])
                if k_ < 4:
                    c.cp('dve', R(P[b_][:]), bankD[:, 0:128], [bankD], [P[b_]])
                c.mm(bankD[:, 128:256], R(Q[b_][:]), R(W[a_][:]), True, True, [Q[b_], W[a_]], [bankD])
                c.tt('dve', R(W[b_][:]), bankD[:, 128:256], W[a_][:], ALU.add, [bankD, W[a_]], [W[b_]])
            return W[1]

        BW = 256
        NBLK = NTOK // BW
        with ExitStack() as esR:
            uT_c = c.sb(esR, [128, 8, NTOK], BF16, 'uT_c')
            with ExitStack() as es2:
                bufs = uT_bufs(es2)
                x_cm = x.rearrange("(r w) d -> w r d", w=64)
                jobs = [([(0, 128, ctx[t * 128:(t + 1) * 128, :])], 1, uT_c, t * 128) for t in range(2)]
                jobs += [([(wl * 32, wl * 32 + 32, x_cm[4 * j + wl]) for wl in range(4)], 0, uT_c, CTX + j * 128) for j in range(16)]
                run_uT(jobs, bufs)
                c.barrier()
            mu = c.sb(esR, [128, 15], F32, 'mu')
            hmu = c.sb(esR, [128, 15], F32, 'hmu')
            omu = c.sb(esR, [128, 15], F32, 'omu')
            w0 = c.sb(esR, [128, 2, 4], F32, 'w0')
            a0 = c.sb(esR, [128, 2, 4], F32, 'a0')
            rw = c.sb(esR, [128, 5, 4], F32, 'rw')
            oka = c.sb(esR, [128, 4], F32, 'oka')
            oka2 = c.sb(esR, [128, 4], F32, 'oka2')
            w2b = c.sb(esR, [128, 512], BF16, 'w2b')
            a2b = c.sb(esR, [128, 512], BF16, 'a2b')
            g2b = c.sb(esR, [128, 512], BF16, 'g2b')
            c.dma(mu[:], muT, writes=[mu])
            c.dma(w0[:], w0T, writes=[w0])
            c.dma(a0[:], a0T, writes=[a0])
            c.dma(rw[:], rwv, writes=[rw])
            c.dma(w2b[:], w2m, writes=[w2b], q='pool')
            c.dma(a2b[:], a2m, writes=[a2b], q='pool')
            c.dma(g2b[:], g2m, writes=[g2b], q='pool')
            c.ts('dve', hmu[:], mu[:], 0.5, None, ALU.mult, None, [mu], [hmu])
            c.ts('dve', omu[:], mu[:], -1.0, 1.0, ALU.mult, ALU.add, [mu], [omu])
            c.ts('dve', oka[:], rw[:, 1, :], -1.0, 1.0, ALU.mult, ALU.add, [rw], [oka])
            c.ts('dve', oka2[:], rw[:, 1, :], -2.0, 2.0, ALU.mult, ALU.add, [rw], [oka2])
            cmask = c.sb(esR, [128, BW], F32, 'cmask')
            c.op('pool', lambda e: e.memset(cmask[:], 1.0), writes=[cmask])
            c.op('pool', lambda e: e.memset(cmask[:].rearrange("p (a b) -> p a b", b=64)[:, :, 0:1], 0.0), reads=[cmask], writes=[cmask])
            mskA = [c.sb(esR, [128, 256], F32, 'mskA%d' % d) for d in range(2)]
            mskB = [c.sb(esR, [128, 256], F32, 'mskB%d' % d) for d in range(2)]
            for d in range(2):
                c.ts('dve', mskA[d][:, 0:128], strict[d][:], -1.0, None, ALU.mult, None, [strict[d]], [mskA[d]])
                c.cp('dve', mskA[d][:, 128:256], incl[d][:], [incl[d]], [mskA[d]])
                c.cp('dve', mskB[d][:, 0:128], strict[d][:], [strict[d]], [mskB[d]])
                c.cp('dve', mskB[d][:, 128:256], incl[d][:], [incl[d]], [mskB[d]])

            raw = c.sb(esR, [128, NTOK], F32, 'rraw')
            t1 = c.sb(esR, [128, NTOK], F32, 'rt1')
            twl = c.sb(esR, [128, NTOK], BF16, 'twl')
            alo = c.sb(esR, [128, NTOK], BF16, 'alo')
            sgl = c.sb(esR, [128, NTOK], BF16, 'sgl')
            rb = c.sb(esR, [128, NTOK], BF16, 'rb')
            kb = c.sb(esR, [128, NTOK], BF16, 'kb')
            vb = c.sb(esR, [128, NTOK], BF16, 'vb')
            G1 = c.sb(esR, [128, SEQ], BF16, 'G1')
            G2 = c.sb(esR, [128, SEQ], BF16, 'G2')
            y_acc = c.sb(esR, [128, 16, 128], F32, 'y_acc')
            wcr = [c.sb(esR, [128, 8, 128], BF16, 'wcr%d' % i) for i in range(2)]
            wcn = [0]

            pm_sched = [1536, 1664, 1792]
            for hc_ in range(4):
                pm_sched += [hc_ * 128, 512 + hc_ * 128, 1024 + hc_ * 128]

            def proj_mix(bcol, dst, func, mi):
                n_ = wcn[0]
                wcn[0] += 1
                assert pm_sched[n_] == bcol
                w_ = wcr[n_ % 2]
                if n_ == 0:
                    c.dma(w_[:], w_in_c[cidx[A_COLS + bcol]], writes=[w_], q='pool')
                for bi, (t0, t1_) in enumerate(TBLK):
                    bank = PS[bi % 2]
                    n = t1_ - t0
                    for kc in range(8):
                        c.mm(bank[:, 0:n], w_[:, kc, :], uT_c[:, kc, t0:t1_], kc == 0, kc == 7, [w_, uT_c], [bank])
                    c.cp('act', raw[:, t0:t1_], bank[:, 0:n], [bank], [raw])
                    if bi == 0 and n_ + 1 < len(pm_sched):
                        nb_ = A_COLS + pm_sched[n_ + 1]
                        c.dma(wcr[(n_ + 1) % 2][:], w_in_c[cidx[nb_]], writes=[wcr[(n_ + 1) % 2]], q='pool')
                for (a_, b_) in ((0, CTX), (CTX, NTOK)):
                    c.tt('pool', t1[:, a_ + 1:b_ - 1], raw[:, a_:b_ - 2], raw[:, a_ + 2:b_], ALU.add, [raw], [t1])
                    c.cp('pool', t1[:, a_:a_ + 1], raw[:, a_ + 1:a_ + 2], [raw], [t1])
                    c.cp('pool', t1[:, b_ - 1:b_], raw[:, b_ - 2:b_ - 1], [raw], [t1])
                c.ts('dve', t1[:], t1[:], hmu[:, mi:mi + 1], None, ALU.mult, None, [t1, hmu], [t1])
                if func is None:
                    c.stt(dst[:], raw[:], omu[:, mi:mi + 1], t1[:], ALU.mult, ALU.add, [raw, omu, t1], [dst])
                else:
                    c.stt(t1[:], raw[:], omu[:, mi:mi + 1], t1[:], ALU.mult, ALU.add, [raw, omu, t1], [t1])
                    c.act(dst[:], t1[:], func, [t1], [dst])

            proj_mix(1536, twl, AF.Tanh, 12)
            proj_mix(1664, alo, None, 13)
            proj_mix(1792, sgl, AF.Sigmoid, 14)

            def blkbuf(name, dt=F32, w=BW):
                return c.sb(esR, [128, w], dt, name)
            DB = []
            for d in range(2):
                g = {}
                arena = (raw, t1)[d]
                for i_, nm in enumerate(('lw', 'icl', 'pre', 'cumd', 'kkn', 'kdir', 'bvec', 'tmpa', 'tmpb', 'opr', 'btT', 'ktT', 'bhT', 'khT')):
                    if i_ < 9:
                        g[nm] = Buf(arena.t[:, i_ * BW:(i_ + 1) * BW], '%s%d' % (nm, d))
                    else:
                        g[nm] = blkbuf('%s%d' % (nm, d))
                g['AR'] = c.sb(esR, [128, 2, BW], F32, 'AR%d' % d)
                g['gC'] = c.sb(esR, [128, BW // 64], F32, 'gC%d' % d)
                for nm in ('Bh_tok', 'Kh_tok', 'Vt'):
                    g[nm] = c.sb(esR, [128, 128], F32, '%s%d' % (nm, d))
                g['BL0'] = Reg(4 * d, 0, 256, 'BL0_%d' % d)
                g['BL1'] = Reg(4 * d + 2, 0, 256, 'BL1_%d' % d)
                g['TL0'] = Reg(4 * d + 1, 0, 128, 'TL0_%d' % d)
                g['TLb'] = Reg(4 * d + 1, 128, 128, 'TLb_%d' % d)
                g['TL1'] = Reg(4 * d + 3, 0, 128, 'TL1_%d' % d)
                g['U'] = []
                for hh in range(2):
                    u = {'bankA': 4 * d + 2 * hh, 'bankB': 4 * d + 2 * hh + 1}
                    sfx = '%d%d' % (d, hh)
                    u['AB1'] = c.sb(esR, [128, 256], F32, 'AB1_' + sfx)
                    u['AB2'] = c.sb(esR, [128, 256], F32, 'AB2_' + sfx)
                    u['XY2'] = c.sb(esR, [128, 128], F32, 'XY2_' + sfx)
                    for nm in ('P', 'Q', 'W'):
                        u[nm] = [c.sb(esR, [128, 128], F32, '%sr%d_%s' % (nm, j, sfx)) for j in range(2)]
                    for nm in ('Xs', 'Us', 'yt', 'yt2', 'Tst'):
                        u[nm] = c.sb(esR, [128, 64], F32, nm + sfx)
                    g['U'].append(u)
                DB.append(g)
            gst2 = [c.sb(esR, [128, 24], F32, 'gst%d' % i) for i in range(2)]
            yn2 = [c.sb(esR, [128, 128], F32, 'yn%d' % i) for i in range(2)]
            obf2 = [c.sb(esR, [128, 128], BF16, 'obf%d' % i) for i in range(2)]
            otf2 = [c.sb(esR, [128, 128], F32, 'otf%d' % i) for i in range(2)]
            NHC = int(os.environ.get('K_NHC', '4'))
            NBS = int(os.environ.get('K_NBS', '9'))
            LG = -0.6065306597126334
            KTG = int(os.environ.get('K_TG', '9'))

            def block_g(d, g, b, hc):
                rowsd = slice(d * 64, d * 64 + 64)
                cs_ = slice(hc * 128, (hc + 1) * 128)
                t0 = b * BW
                tsl = slice(t0, t0 + BW)
                lw, icl, pre, cumd, kkn, kdir, bvec, tmpa, tmpb, opr = (g[n_] for n_ in ('lw', 'icl', 'pre', 'cumd', 'kkn', 'kdir', 'bvec', 'tmpa', 'tmpb', 'opr'))
                AR, btT, ktT, bhT, khT, gC = (g[n_] for n_ in ('AR', 'btT', 'ktT', 'bhT', 'khT', 'gC'))
                BL0, BL1 = g['BL0'], g['BL1']
                c.mm(BL0.ap(), w2b[rowsd, cs_], twl[rowsd, tsl], True, True, [w2b, twl], [BL0.buf])
                c.mm(BL1.ap(), a2b[rowsd, cs_], alo[rowsd, tsl], True, True, [a2b, alo], [BL1.buf])
                c.ts('dve', kkn[:], kb[:, tsl], rw[:, 0, hc:hc + 1], None, ALU.mult, None, [kb, rw], [kkn])
                yield
                c.act(lw[:], BL0.ap(), AF.Sigmoid, [BL0.buf, w0], [lw], bias=w0[:, d, hc:hc + 1])
                c.act(icl[:], BL1.ap(), AF.Sigmoid, [BL1.buf, a0], [icl], bias=a0[:, d, hc:hc + 1])
                c.act(R(opr[:]), kkn[:], AF.Square, [kkn], [opr])
                yield
                c.ts('dve', lw[:], lw[:], LG, None, ALU.mult, None, [lw], [lw])
                c.mm(BL0.ap(), R(blk_r[:]), R(opr[:]), True, True, [blk_r, opr], [BL0.buf])
                c.op('dve', lambda e: e.tensor_tensor_scan(out=pre[:], data0=cmask[:], data1=lw[:], initial=0.0,
                                                           op0=ALU.mult, op1=ALU.add), reads=[cmask, lw], writes=[pre])
                yield
                pre3 = pre[:].rearrange("p (a b) -> p a b", b=64)
                tot_bc = pre3[:, :, 63:64].to_broadcast([128, BW // 64, 64])
                if d == 0:
                    c.cp('pool', cumd[:], pre[:], [pre], [cumd])
                else:
                    c.tt('dve', cumd[:], lw[:], pre[:], ALU.subtract, [lw, pre], [cumd])
                    c.tt('dve', cumd[:].rearrange("p (a b) -> p a b", b=64), cumd[:].rearrange("p (a b) -> p a b", b=64), tot_bc,
                         ALU.add, [cumd, pre], [cumd])
                c.ts('dve', tmpb[:], BL0.ap(), EPS, None, ALU.add, None, [BL0.buf], [tmpb])
                yield
                c.act(gC[:], pre3[:, :, 63], AF.Exp, [pre], [gC])
                c.act(tmpb[:], tmpb[:], AF.Sqrt, [tmpb], [tmpb])
                c.ts('dve', kdir[:], icl[:], rw[:, 1, hc:hc + 1], oka[:, hc:hc + 1], ALU.mult, ALU.add, [icl, rw, oka], [kdir])
                c.tt('dve', kdir[:], kdir[:], kb[:, tsl], ALU.mult, [kdir, kb], [kdir])
                yield
                c.op('dve', lambda e: e.reciprocal(out=tmpb[:], in_=tmpb[:]), reads=[tmpb], writes=[tmpb])
                c.tt('dve', kkn[:], kkn[:], tmpb[:], ALU.mult, [kkn, tmpb], [kkn])
                c.tt('dve', tmpa[:], cumd[:], lw[:], ALU.subtract, [cumd, lw], [tmpa])
                yield
                c.tt('pool', bvec[:], kkn[:], icl[:], ALU.mult, [kkn, icl], [bvec])
                c.act(tmpa[:], tmpa[:], AF.Exp, [tmpa], [tmpa])
                yield
                c.stt(R(AR[:, 0, :]), kkn[:], -1.0, tmpa[:], ALU.mult, ALU.mult, [kkn, tmpa], [AR])
                yield
                c.act(tmpa[:], cumd[:], AF.Exp, [cumd], [tmpa])
                c.tt('dve', tmpb[:].rearrange("p (a b) -> p a b", b=64), cumd[:].rearrange("p (a b) -> p a b", b=64), tot_bc,
                     ALU.subtract, [cumd, pre], [tmpb])
                yield
                c.tt('dve', R(AR[:, 1, :]), rb[:, tsl], tmpa[:], ALU.mult, [rb, tmpa], [AR])
                yield
                c.act(tmpa[:], cumd[:], AF.Exp, [cumd], [tmpa], scale=-1.0)
                c.act(tmpb[:], tmpb[:], AF.Exp, [tmpb], [tmpb], scale=-1.0)
                yield
                c.tt('dve', R(btT[:]), bvec[:], tmpa[:], ALU.mult, [bvec, tmpa], [btT])
                c.tt('pool', R(ktT[:]), kdir[:], tmpa[:], ALU.mult, [kdir, tmpa], [ktT])
                yield
                c.tt('dve', R(bhT[:]), bvec[:], tmpb[:], ALU.mult, [bvec, tmpb], [bhT])
                c.tt('pool', R(khT[:]), kdir[:], tmpb[:], ALU.mult, [kdir, tmpb], [khT])
                yield

            def tile_g(d, g, b, tl):
                lsl_ = slice(tl * 128, tl * 128 + 128)
                gts = (2 * b + tl) * 128
                TL0, TL1, TLb = g['TL0'], g['TL1'], g['TLb']
                bhT, khT, Bh_tok, Kh_tok, Vt = g['bhT'], g['khT'], g['Bh_tok'], g['Kh_tok'], g['Vt']
                c.mm(TL0.ap(), R(bhT[:, lsl_]), R(ident_r[:]), True, True, [bhT, ident_r], [TL0.buf])
                c.mm(TLb.ap(), vb[:, gts:gts + 128], identb[:], True, True, [vb, identb], [TLb.buf])
                c.mm(TL1.ap(), R(khT[:, lsl_]), R(ident_r[:]), True, True, [khT, ident_r], [TL1.buf])
                yield
                c.cp('act', R(Bh_tok[:]), TL0.ap(), [TL0.buf], [Bh_tok])
                c.cp('dve', R(Kh_tok[:]), TL1.ap(), [TL1.buf], [Kh_tok])
                c.cp('act', R(Vt[:]), TLb.ap(), [TLb.buf], [Vt])
                yield

            def unit_g(d, g, u, hh, b, tl):
                rows = slice(hh * 64, hh * 64 + 64)
                tile = 2 * b + tl
                is_lat = tile >= 2
                lt = tile - 2
                lsl_ = slice(tl * 128, tl * 128 + 128)
                AR, btT, ktT, Vt, Bh_tok, Kh_tok, gC = (g[n_] for n_ in ('AR', 'btT', 'ktT', 'Vt', 'Bh_tok', 'Kh_tok', 'gC'))
                AB1, AB2, XY2, Xs, Us, yt, yt2, Tst = (u[n_] for n_ in ('AB1', 'AB2', 'XY2', 'Xs', 'Us', 'yt', 'yt2', 'Tst'))
                P, Q, W = u['P'], u['Q'], u['W']
                A, B = PS[u['bankA']], PS[u['bankB']]
                At, Bt = A.t, B.t
                ARt = AR[rows, :, lsl_]
                c.mm(At[:, 0:256].rearrange("p (a b) -> p a b", a=2), R(btT[rows, lsl_]), R(ARt), True, True, [btT, AR], [A])
                c.mm(Bt[:, 0:256].rearrange("p (a b) -> p a b", a=2), R(ktT[rows, lsl_]), R(ARt), True, True, [ktT, AR], [B])
                yield
                c.tt('dve', R(AB1[:]), At[:, 0:256], mskA[d][:], ALU.mult, [A, mskA[d]], [AB1])
                c.tt('dve', R(AB2[:]), Bt[:, 0:256], mskB[d][:], ALU.mult, [B, mskB[d]], [AB2])
                yield
                if KTG < 3:
                    return
                c.cp('pool', R(P[0][:]), AB1[:, 0:128], [AB1], [P[0]])
                c.mm(At[:, 0:64], R(AB2[:, 0:128]), R(Vt[:, rows]), True, True, [AB2, Vt], [A])
                c.mm(At[:, 64:128], R(AB2[:, 128:256]), R(Vt[:, rows]), True, True, [AB2, Vt], [A])
                yield
                c.cp('act', XY2[:], At[:, 0:128], [A], [XY2])
                c.mm(Bt[:, 0:128], R(P[0][:]), R(ident_r[:]), True, True, [P[0], ident_r], [B])
                c.tt('pool', R(W[0][:]), ident[:], P[0][:], ALU.subtract, [ident, P[0]], [W[0]])
                yield
                c.cp('act', R(Q[0][:]), Bt[:, 0:128], [B], [Q[0]])
                yield
                for k_ in range(5):
                    a_, b_ = k_ % 2, (k_ + 1) % 2
                    c.mm(At[:, 128:256], R(P[a_][:]), R(Q[a_][:]), True, True, [P[a_], Q[a_]], [A])
                    if k_ < 4:
                        c.mm(Bt[:, 0:128], R(Q[a_][:]), R(P[a_][:]), True, True, [P[a_], Q[a_]], [B])
                    yield
                    c.cp('act', R(Q[b_][:]), At[:, 128:256], [A], [Q[b_]])
                    if k_ < 4:
                        c.cp('act', R(P[b_][:]), Bt[:, 0:128], [B], [P[b_]])
                    yield
                    c.mm(Bt[:, 128:256], R(Q[b_][:]), R(W[a_][:]), True, True, [Q[b_], W[a_]], [B])
                    yield
                    c.tt('dve', R(W[b_][:]), Bt[:, 128:256], W[a_][:], ALU.add, [B, W[a_]], [W[b_]])
                    yield
                Wt = W[1]
                if KTG < 4:
                    return
                for cs in ((0, 64) if d == 0 else (64, 0)):
                    sl_ = slice(cs, cs + 64)
                    chunk = tl * 2 + cs // 64
                    c.mm(At[:, 0:64], R(AR[rows, 0, lsl_]), R(Tst[rows, :]), True, True, [AR, Tst], [A])
                    if is_lat:
                        c.mm(At[:, 64:128], R(AR[rows, 1, lsl_]), R(Tst[rows, :]), True, True, [AR, Tst], [A])
                    yield
                    c.tt('dve', R(Xs[sl_, :]), At[sl_, 0:64], XY2[sl_, 0:64], ALU.add, [A, XY2], [Xs])
                    yield
                    c.mm(Bt[:, 0:64], R(Wt[sl_, :]), R(Xs[sl_, :]), True, True, [Wt, Xs], [B])
                    yield
                    c.cp('act', R(Us[sl_, :]), Bt[sl_, 0:64], [B], [Us])
                    yield
                    c.mm(Bt[:, 64:128], R(Bh_tok[sl_, :]), R(Us[sl_, :]), True, False, [Bh_tok, Us], [B])
                    c.mm(Bt[:, 64:128], R(Kh_tok[sl_, :]), R(Vt[sl_, rows]), False, True, [Kh_tok, Vt], [B])
                    if is_lat:
                        c.mm(At[:, 128:192], R(AB1[sl_, 128:256]), R(Us[sl_, :]), True, True, [AB1, Us], [A])
                    yield
                    c.stt(R(Tst[rows, :]), Tst[rows, :], gC[rows, chunk:chunk + 1], Bt[rows, 64:128], ALU.mult, ALU.add,
                          [Tst, gC, B], [Tst])
                    if is_lat:
                        c.tt('dve', yt[sl_, :], At[sl_, 128:192], XY2[sl_, 64:128], ALU.add, [A, XY2], [yt])
                        if (d == 0) == (lt <= 7):
                            c.tt('dve', y_acc[sl_, lt, rows], At[sl_, 64:128], yt[sl_, :], ALU.add, [A, yt], [y_acc])
                        else:
                            c.tt('dve', yt2[sl_, :], At[sl_, 64:128], yt[sl_, :], ALU.add, [A, yt], [yt2])
                            c.tt('pool', y_acc[sl_, lt, rows], y_acc[sl_, lt, rows], yt2[sl_, :], ALU.add, [y_acc, yt2], [y_acc])
                    yield

            def dir_g(d, hc):
                g = DB[d]
                for hh in range(2):
                    c.ts('dve', R(g['U'][hh]['Tst'][:]), zf[:, 0:64], 0.0, None, ALU.mult, None, [zf], [g['U'][hh]['Tst']])
                border = list(range(NBLK)) if d == 0 else [0] + list(range(NBLK - 1, 0, -1))
                for b in border[:NBS]:
                    yield from block_g(d, g, b, hc)
                    for tl in ((0, 1) if d == 0 else (1, 0)):
                        if KTG < 1:
                            continue
                        yield from tile_g(d, g, b, tl)
                        if KTG < 2:
                            continue
                        yield from par(unit_g(d, g, g['U'][0], 0, b, tl), unit_g(d, g, g['U'][1], 1, b, tl))

            ob_hc = c.sb(esR, [128, 16, 128], BF16, 'ob_hc')
            scr_ob_v = scr_ob.rearrange("(lt p) cc -> p lt cc", p=128)
            scrw = Buf(None, 'scrw')
            for hc in range(NHC):
                proj_mix(hc * 128, rb, None, hc)
                proj_mix(512 + hc * 128, kb, None, 4 + hc)
                proj_mix(1024 + hc * 128, vb, None, 8 + hc)
                c.barrier()
                cs_ = slice(hc * 128, (hc + 1) * 128)
                def prepass_g(par_, hc=hc, cs_=cs_):
                    g_ = DB[par_]
                    icl, icl1, tmpa, opr = g_['icl'], g_['pre'], g_['tmpa'], g_['opr']
                    P0, P3, P1, P2 = (PS[4 * par_ + j] for j in range(4))
                    for b in range(1 + par_, NBLK, 2):
                        t0 = b * BW
                        tsl = slice(t0, t0 + BW)
                        lsl = slice(t0 - CTX, t0 - CTX + BW)
                        c.mm(P0[:, 0:BW], a2b[0:64, cs_], alo[0:64, tsl], True, True, [a2b, alo], [P0])
                        c.mm(P3[:, 0:BW], a2b[64:128, cs_], alo[64:128, tsl], True, True, [a2b, alo], [P3])
                        c.mm(P2[:, 0:BW], g2b[:, cs_], sgl[:, tsl], True, True, [g2b, sgl], [P2])
                        yield
                        c.act(icl[:], P0[:, 0:BW], AF.Sigmoid, [P0, a0], [icl], bias=a0[:, 0, hc:hc + 1])
                        c.act(icl1[:], P3[:, 0:BW], AF.Sigmoid, [P3, a0], [icl1], bias=a0[:, 1, hc:hc + 1])
                        c.cp('act', G1[:, lsl], P2[:, 0:BW], [P2], [G1])
                        yield
                        c.tt('dve', tmpa[:], icl[:], icl1[:], ALU.add, [icl, icl1], [tmpa])
                        c.ts('dve', tmpa[:], tmpa[:], rw[:, 1, hc:hc + 1], oka2[:, hc:hc + 1], ALU.mult, ALU.add, [tmpa, rw, oka2], [tmpa])
                        yield
                        c.tt('dve', tmpa[:], tmpa[:], kb[:, tsl], ALU.mult, [tmpa, kb], [tmpa])
                        c.tt('dve', tmpa[:], tmpa[:], rb[:, tsl], ALU.mult, [tmpa, rb], [tmpa])
                        yield
                        c.ts('dve', R(opr[:]), tmpa[:], rw[:, 2, hc:hc + 1], None, ALU.mult, None, [tmpa, rw], [opr])
                        yield
                        c.mm(P1[:, 0:BW], R(blk_r[:]), R(opr[:]), True, True, [blk_r, opr], [P1])
                        yield
                        c.tt('dve', tmpa[:], P1[:, 0:BW], vb[:, tsl], ALU.mult, [P1, vb], [tmpa])
                        yield
                        c.stt(G2[:, lsl], tmpa[:], rw[:, 4, hc:hc + 1], P2[:, 0:BW], ALU.add, ALU.mult, [tmpa, rw, P2], [G2])
                        yield

                run_threads([prepass_g(0), prepass_g(1)])
                c.barrier()
                run_threads([dir_g(0, hc), dir_g(1, hc)])
                c.barrier()
                def rwkv_out_g(par_, hc=hc, cs_=cs_):
                    gst, yn, obf, otf = gst2[par_], yn2[par_], obf2[par_], otf2[par_]
                    bkA, bkB = PS[par_], PS[2 + par_]
                    pbk = psb(2 + par_)
                    for lt in range(par_, 0 if os.environ.get('K_NOOUT') else 16, 2):
                        for hh in range(2):
                            rows = slice(hh * 64, hh * 64 + 64)
                            c.op('dve', lambda e, hh=hh, rows=rows: e.bn_stats(out=gst[:, hh * 6:(hh + 1) * 6], in_=y_acc[:, lt, rows]), reads=[y_acc], writes=[gst])
                            c.op('dve', lambda e, hh=hh: e.bn_aggr(out=gst[:, 12 + hh * 2:14 + hh * 2], in_=gst[:, hh * 6:(hh + 1) * 6]), reads=[gst], writes=[gst])
                        yield
                        c.ts('dve', gst[:, 16:18], gst[:, 13:16:2], LNX_EPS, None, ALU.add, None, [gst], [gst])
                        yield
                        c.act(gst[:, 16:18], gst[:, 16:18], AF.Sqrt, [gst], [gst])
                        yield
                        c.op('dve', lambda e: e.reciprocal(out=gst[:, 18:20], in_=gst[:, 16:18]), reads=[gst], writes=[gst])
                        for hh in range(2):
                            rows = slice(hh * 64, hh * 64 + 64)
                            c.ts('dve', yn[:, rows], y_acc[:, lt, rows], gst[:, 12 + hh * 2:13 + hh * 2], gst[:, 18 + hh:19 + hh], ALU.subtract, ALU.mult,
                                 [y_acc, gst], [yn])
                        yield
                        c.tr(bkA[:, 0:128], yn[:], ident[:], [yn, ident], [bkA])
                        yield
                        lsl = slice(lt * 128, (lt + 1) * 128)
                        c.stt(otf[:], bkA[:, 0:128], rw[:, 3, hc:hc + 1], G1[:, lsl], ALU.mult, ALU.mult, [bkA, rw, G1], [otf])
                        c.tt('dve', obf[:], otf[:], G2[:, lsl], ALU.add, [otf, G2], [obf])
                        yield
                        c.tr(pbk[:, 0:128], obf[:], identb[:], [obf, identb], [bkB])
                        yield
                        c.cp('act', ob_hc[:, lt, :], pbk[:, 0:128], [bkB], [ob_hc])
                        yield

                run_threads([rwkv_out_g(0), rwkv_out_g(1)])
                for q4 in range(4):
                    c.dma(scr_ob_v[:, q4 * 4:(q4 + 1) * 4, cs_], ob_hc[:, q4 * 4:(q4 + 1) * 4, :], reads=[ob_hc], writes=[scrw])
            c.barrier()

        scrob_buf = scrw
        scrh_buf = Buf(None, 'scr_h_dram')
        esF = ExitStack()
        with esF:
            tT = c.sb(esF, [128, 8, SEQ], BF16, 'tT')
            cwT = c.sb(esF, [32, SEQ], F32, 'cwT')
            with ExitStack() as esY:
                yT = c.sb(esY, [128, 8, SEQ], BF16, 'yT')
                with ExitStack() as esM1:
                    woa = c.sb(esM1, [128, 4, D], BF16, 'woa')
                    wob = c.sb(esM1, [128, 4, D], BF16, 'wob')
                    c.dma(woa[:], w_o_a.rearrange("(cc p) n -> p cc n", p=128), writes=[woa], q='pool')
                    c.dma(wob[:], w_o_b.rearrange("(cc p) n -> p cc n", p=128), writes=[wob], q='pool')
                    uTb = c.sb(esM1, [128, 8, 512], BF16, 'uTb')
                    obTb = c.sb(esM1, [128, 4, 512], BF16, 'obTb')
                    obt = [c.sb(esM1, [128, 512], BF16, 'obt%d' % i) for i in range(2)]
                    wg_ = [c.sb(esM1, [128, 8, 128], BF16, 'wgm%d' % i) for i in range(4)]
                    sga2 = [c.sb(esM1, [128, 512], F32, 'sga%d' % i) for i in range(2)]
                    sgb2 = [c.sb(esM1, [128, 512], F32, 'sgb%d' % i) for i in range(2)]
                    ya2 = [c.sb(esM1, [128, 512], F32, 'ya%d' % i) for i in range(2)]
                    yb2 = [c.sb(esM1, [128, 512], F32, 'yb%d' % i) for i in range(2)]
                    ob_v = scr_ob.rearrange("(w r) cc -> r w cc", r=32)
                    k = 0
                    wn = 0
                    for tb in range(4):
                        c.dma(uTb[:], scr_u[:, :, tb * 512:(tb + 1) * 512], reads=[scru_buf], writes=[uTb])
                        for j in range(4):
                            i = tb * 4 + j
                            o_ = obt[i % 2]
                            c.dma(o_[0:64, :], ob_v[2 * i], reads=[scrob_buf], writes=[o_])
                            c.dma(o_[64:128, :], ob_v[2 * i + 1], reads=[scrob_buf], writes=[o_])
                            pb = psb(5 + i % 2)
                            for hc in range(4):
                                c.tr(pb[:, hc * 128:(hc + 1) * 128], o_[:, hc * 128:(hc + 1) * 128], identb[:], [o_, identb], [PS[5 + i % 2]],
                                     inc=(hc == 3))
                            c.cp('act', obTb[:, :, j * 128:(j + 1) * 128], pb[:, 0:512].rearrange("p (a b) -> p a b", a=4), [PS[5 + i % 2]], [obTb])
                        tsl = slice(tb * 512, (tb + 1) * 512)
                        for fc in range(8):
                            wa_ = wg_[wn % 4]
                            wb_ = wg_[(wn + 1) % 4]
                            wn += 2
                            ga0 = A_COLS + B_COLS + fc * 128
                            c.dma(wa_[:], w_in_c[cidx[ga0]], writes=[wa_], q='pool')
                            c.dma(wb_[:], w_in_c[cidx[ga0 + D]], writes=[wb_], q='pool')
                            fsl = slice(fc * 128, (fc + 1) * 128)
                            fp_ = fc % 2
                            Q0, Q1, Q2, Q3 = (PS[4 * fp_ + j_] for j_ in range(4))
                            sga, sgb, ya, yb = sga2[fp_], sgb2[fp_], ya2[fp_], yb2[fp_]
                            for kc in range(8):
                                c.mm(Q2[:, :], wa_[:, kc, :], uTb[:, kc, :], kc == 0, kc == 7, [wa_, uTb], [Q2])
                            for kc in range(8):
                                c.mm(Q3[:, :], wb_[:, kc, :], uTb[:, kc, :], kc == 0, kc == 7, [wb_, uTb], [Q3])
                            for cc in range(4):
                                c.mm(Q0[:, :], woa[:, cc, fsl], oaT[:, cc, tsl], cc == 0, cc == 3, [woa, oaT], [Q0])
                            for cc in range(4):
                                c.mm(Q1[:, :], wob[:, cc, fsl], obTb[:, cc, :], cc == 0, cc == 3, [wob, obTb], [Q1])
                            c.act(sga[:], Q2[:, :], AF.Sigmoid, [Q2], [sga])
                            c.act(sgb[:], Q3[:, :], AF.Sigmoid, [Q3], [sgb])
                            c.tt('dve', ya[:], Q0[:, :], sga[:], ALU.mult, [Q0, sga], [ya])
                            c.tt('dve', yb[:], Q1[:, :], sgb[:], ALU.mult, [Q1, sgb], [yb])
                            c.tt('dve', yT[:, fc, tsl], ya[:], yb[:], ALU.add, [ya, yb], [yT])
                    c.barrier()
                if 'd_yT' in T:
                    c.dma(T['d_yT'], yT[:], reads=[yT])
                with ExitStack() as esM2:
                    wout = c.sb(esM2, [128, 8, D], BF16, 'wout')
                    c.dma(wout[:, 0:4, :], w_out.rearrange("(cc p) n -> p cc n", p=128)[:, 0:4, :], writes=[wout], q='pool')
                    c.dma(wout[:, 4:8, :], w_out.rearrange("(cc p) n -> p cc n", p=128)[:, 4:8, :], writes=[wout], q='pool')
                    Bg1 = c.sb(esM2, [128, D], F32, 'Bg1')
                    make_Bg(esM2, Bg1, 16)
                    wr32 = c.sb(esM2, [128, 8, 36], F32, 'wr32')
                    rbb = c.sb(esM2, [128, 36], F32, 'rbb')
                    c.dma(wr32[:], wrt, writes=[wr32])
                    c.dma(rbb[:], rb_bc, writes=[rbb])
                    Xh = [c.sb(esM2, [128, D], F32, 'Xh%d' % i) for i in range(2)]
                    Hh = [c.sb(esM2, [128, D], F32, 'Hh%d' % i) for i in range(2)]
                    hn2 = [c.sb(esM2, [128, D], F32, 'hn%d' % i) for i in range(2)]
                    sqh2 = [c.sb(esM2, [128, D], BF16, 'sqh%d' % i) for i in range(2)]
                    t322 = [c.sb(esM2, [128, 8, 128], F32, 't32%d' % i) for i in range(2)]
                    st2 = [c.sb(esM2, [128, 64], F32, 'st%d' % i) for i in range(2)]
                    lg2 = [c.sb(esM2, [128, 36], F32, 'lg%d' % i) for i in range(2)]
                    tm32 = [c.sb(esM2, [128, 32], F32, 'tm3%d' % i) for i in range(2)]
                    cw2 = [c.sb(esM2, [128, 32], F32, 'cw%d' % i) for i in range(2)]

                    def tile_chain(t):
                        hn, sqh, t32, st, lg, tm3, cw = hn2[t], sqh2[t], t322[t], st2[t], lg2[t], tm32[t], cw2[t]
                        B = [PS[4 * t + j] for j in range(4)]
                        X_, H_ = Xh[t], Hh[t]
                        for i in range(t, 16, 2):
                            c.dma(X_[:], x[i * 128:(i + 1) * 128, :], writes=[X_])
                            for nh in range(2):
                                bank = B[nh]
                                for fc in range(8):
                                    c.mm(bank[:, :], yT[:, fc, i * 128:(i + 1) * 128], wout[:, fc, nh * 512:(nh + 1) * 512], fc == 0, fc == 7, [yT, wout], [bank])
                                hs = slice(nh * 512, (nh + 1) * 512)
                                c.tt('dve', H_[:, hs], bank[:, :], Bg1[:, hs], ALU.mult, [bank, Bg1], [H_])
                                yield
                            c.tt('pool', H_[:], H_[:], X_[:], ALU.add, [H_, X_], [H_])
                            yield
                            c.dma(scr_h[i * 128:(i + 1) * 128, :], H_[:], reads=[H_], writes=[scrh_buf])
                            c.act(sqh[:], H_[:], AF.Square, [H_], [sqh, st], accum_out=st[:, 0:1])
                            yield
                            c.ts('dve', st[:, 1:2], st[:, 0:1], 1.0 / D, EPS, ALU.mult, ALU.add, [st], [st])
                            c.act(st[:, 2:3], st[:, 1:2], AF.Sqrt, [st], [st])
                            yield
                            c.op('dve', lambda e: e.reciprocal(out=st[:, 3:4], in_=st[:, 2:3]), reads=[st], writes=[st])
                            c.ts('dve', hn[:], H_[:], st[:, 3:4], None, ALU.mult, None, [H_, st], [hn])
                            yield
                            for fc in range(8):
                                bank = B[2 + fc // 4]
                                c.tr(bank[:, (fc % 4) * 128:(fc % 4 + 1) * 128], hn[:, fc * 128:(fc + 1) * 128], ident[:], [hn, ident], [bank],
                                     inc=(fc % 4 == 3))
                            yield
                            for fc in range(8):
                                bank = B[2 + fc // 4]
                                i_ = bank[:, (fc % 4) * 128:(fc % 4 + 1) * 128]
                                c.ts('dve', t32[:, fc, :], i_, A2g[:, fc, 0:1], mT[:, 24 + fc, 0:1], ALU.mult, ALU.add, [bank, A2g, mT], [t32])
                                c.cp('act', tT[:, fc, i * 128:(i + 1) * 128], t32[:, fc, :], [t32], [tT])
                                if fc % 4 == 3:
                                    yield
                            for kc in range(8):
                                c.mm(B[0][:, 0:36], t32[:, kc, :], wr32[:, kc, :], kc == 0, kc == 7, [t32, wr32], [B[0]])
                            yield
                            c.tt('dve', lg[:], B[0][:, 0:36], rbb[:], ALU.add, [B[0], rbb], [lg])
                            c.op('dve', lambda e: e.tensor_reduce(out=st[:, 8:9], in_=lg[:, 0:4], axis=AX.X, op=ALU.max), reads=[lg], writes=[st])
                            c.ts('dve', st[:, 9:10], st[:, 8:9], -1.0, None, ALU.mult, None, [st], [st])
                            yield
                            c.act(st[:, 16:20], lg[:, 0:4], AF.Exp, [lg, st], [st], bias=st[:, 9:10], accum_out=st[:, 10:11])
                            yield
                            c.op('dve', lambda e: e.reciprocal(out=st[:, 11:12], in_=st[:, 10:11]), reads=[st], writes=[st])
                            c.ts('dve', st[:, 20:24], lg[:, 0:4], st[:, 8:9], None, ALU.is_ge, None, [lg, st], [st])
                            c.tt('dve', tm3[:].rearrange("p (g e) -> p g e", g=4), lg[:, 4:36].rearrange("p (g e) -> p g e", g=4),
                                 st[:, 20:24].unsqueeze(2).to_broadcast([128, 4, 8]), ALU.mult, [lg, st], [tm3])
                            yield
                            c.op('dve', lambda e: e.tensor_reduce(out=st[:, 24:32], in_=tm3[:].rearrange("p (g e) -> p e g", g=4), axis=AX.X, op=ALU.add),
                                 reads=[tm3], writes=[st])
                            c.op('dve', lambda e: e.max(out=st[:, 32:40], in_=st[:, 24:32]), reads=[st], writes=[st])
                            c.tt('dve', st[:, 40:41], st[:, 33:34], st[:, 32:33], ALU.subtract, [st], [st])
                            yield
                            c.act(st[:, 41:42], st[:, 40:41], AF.Exp, [st], [st])
                            yield
                            c.ts('dve', st[:, 42:43], st[:, 41:42], 1.0, None, ALU.add, None, [st], [st])
                            c.op('dve', lambda e: e.reciprocal(out=st[:, 43:44], in_=st[:, 42:43]), reads=[st], writes=[st])
                            c.tt('dve', st[:, 44:45], st[:, 43:44], st[:, 11:12], ALU.mult, [st], [st])
                            yield
                            c.tt('dve', st[:, 45:46], st[:, 44:45], st[:, 41:42], ALU.mult, [st], [st])
                            c.tt('dve', st[:, 46:47], st[:, 44:45], st[:, 45:46], ALU.subtract, [st], [st])
                            c.ts('dve', st[:, 48:56], st[:, 24:32], st[:, 32:33], st[:, 46:47], ALU.is_ge, ALU.mult, [st], [st])
                            yield
                            c.ts('dve', st[:, 56:64], st[:, 24:32], st[:, 33:34], st[:, 45:46], ALU.is_ge, ALU.mult, [st], [st])
                            c.tt('dve', st[:, 48:56], st[:, 48:56], st[:, 56:64], ALU.add, [st], [st])
                            c.tt('dve', cw[:].rearrange("p (g e) -> p g e", g=4), st[:, 20:24].unsqueeze(2).to_broadcast([128, 4, 8]),
                                 st[:, 48:56].unsqueeze(1).to_broadcast([128, 4, 8]), ALU.mult, [st], [cw])
                            yield
                            c.tr(B[1][0:32, 0:128], cw[:], ident[:], [cw, ident], [B[1]])
                            yield
                            c.cp('act', R(cwT[:, i * 128:(i + 1) * 128]), B[1][0:32, 0:128], [B[1]], [cwT])
                            yield

                    run_threads([tile_chain(0), tile_chain(1)])
                    c.barrier()
            if 'd_tT' in T:
                c.dma(T['d_tT'], tT[:], reads=[tT])
            if 'd_cwT' in T:
                c.dma(T['d_cwT'], cwT[:], reads=[cwT])

            with ExitStack() as esE:
                moe_acc = c.sb(esE, [128, 16, D], F32, 'moe_acc')
                macc = [[Buf(None, 'macc%d_%d' % (t_, n_)) for n_ in range(2)] for t_ in range(16)]
                evt = [c.sb(esE, [128, 512], F32, 'evt%d' % i) for i in range(3)]
                wgb = [c.sb(esE, [128, 8, 256], BF16, 'wgb%d' % i) for i in range(2)]
                wub = [c.sb(esE, [128, 8, 256], BF16, 'wub%d' % i) for i in range(2)]
                wdb = [c.sb(esE, [128, 2, D], BF16, 'wdb%d' % i) for i in range(2)]
                selt = [c.sb(esE, [32, 128], F32, 'selt%d' % i) for i in range(2)]
                sg_ = [c.sb(esE, [128, 512], F32, 'sg%d' % i) for i in range(2)]
                hu_ = [c.sb(esE, [128, 512], F32, 'hu%d' % i) for i in range(2)]
                hid = [c.sb(esE, [128, 2, 512], BF16, 'hid%d' % i) for i in range(2)]
                NEXP = int(os.environ.get('K_NEXP', '32'))
                def moe_gu(e_, tg, k):
                    wg, wu, wd = wgb[e_ % 2], wub[e_ % 2], wdb[e_ % 2]
                    se = selt[e_ % 2]
                    if tg == 0:
                        c.dma(wg[:], moe_wg[e_], writes=[wg], q='pool')
                        c.dma(wu[:], moe_wu[e_], writes=[wu], q='pool')
                        c.dma(wd[:], moe_wd[e_], writes=[wd], q='pool')
                        c.ts('dve', R(se[:]), onesf[0:32, :], ident[0:32, e_:e_ + 1], None, ALU.mult, None, [onesf, ident], [se])
                    tsl = slice(tg * 512, (tg + 1) * 512)
                    hd = hid[k % 2]
                    c.mm(PS[4][:, :], R(se[:]), R(cwT[:, tsl]), True, True, [se, cwT], [PS[4]])
                    for f2 in range(2):
                        fs = slice(f2 * 128, (f2 + 1) * 128)
                        for kc in range(8):
                            c.mm(PS[f2][:, :], wg[:, kc, fs], tT[:, kc, tsl], kc == 0, kc == 7, [wg, tT], [PS[f2]])
                        for kc in range(8):
                            c.mm(PS[2 + f2][:, :], wu[:, kc, fs], tT[:, kc, tsl], kc == 0, kc == 7, [wu, tT], [PS[2 + f2]])
                        c.act(sg_[f2][:], PS[f2][:, :], AF.Silu, [PS[f2]], [sg_[f2]])
                        c.tt('dve', hu_[f2][:], PS[2 + f2][:, :], sg_[f2][:], ALU.mult, [PS[2 + f2], sg_[f2]], [hu_[f2]])
                        c.tt('dve', hd[:, f2, :], PS[4][:, :], hu_[f2][:], ALU.mult, [PS[4], hu_[f2]], [hd])

                MOE_SPLIT = False

                def moe_dn(e_, tg, k):
                    wd = wdb[e_ % 2]
                    hd = hid[k % 2]
                    for tt_ in range(4):
                        tile = tg * 4 + tt_
                        for nh in range(2):
                            bank = PS[5 + (tt_ * 2 + nh) % 3]
                            for f2 in range(2):
                                c.mm(bank[:, :], hd[:, f2, tt_ * 128:(tt_ + 1) * 128], wd[:, f2, nh * 512:(nh + 1) * 512], f2 == 0, f2 == 1,
                                     [hd, wd], [bank])
                            hs = slice(nh * 512, (nh + 1) * 512)
                            ma = macc[tile][nh]
                            gi = tt_ * 2 + nh
                            if e_ == 0:
                                c.cp('act', moe_acc[:, tile, hs], bank[:, :], [bank], [ma])
                            elif gi % 2 == 0 or not MOE_SPLIT:
                                c.tt('dve', moe_acc[:, tile, hs], bank[:, :], moe_acc[:, tile, hs], ALU.add, [bank, ma], [ma])
                            else:
                                ev = evt[(gi // 2) % 3]
                                c.cp('act', ev[:], bank[:, :], [bank], [ev])
                                c.tt('pool', moe_acc[:, tile, hs], moe_acc[:, tile, hs], ev[:], ALU.add, [ma, ev], [ma])

                its = [(e_, tg) for e_ in range(NEXP) for tg in range(4)]
                for k, (e_, tg) in enumerate(its):
                    moe_gu(e_, tg, k)
                    if k > 0:
                        moe_dn(its[k - 1][0], its[k - 1][1], k - 1)
                moe_dn(its[-1][0], its[-1][1], len(its) - 1)
                Bg2 = c.sb(esE, [128, D], F32, 'Bg2')
                make_Bg(esE, Bg2, 40)
                gfin = c.sb(esE, [128, D], F32, 'gfin')
                c.dma(gfin[:], gfin_bc, writes=[gfin])
                Hf = [c.sb(esE, [128, D], F32, 'Hf%d' % i) for i in range(2)]
                sf = [c.sb(esE, [128, 4], F32, 'sf%d' % i) for i in range(2)]
                c.barrier()
                Hm = [Buf(wgb[i].t.bitcast(F32).rearrange("p a b -> p (a b)"), 'Hm%d' % i) for i in range(2)]
                sqf2 = [Buf(wub[i].t.rearrange("p a b -> p (a b)"), 'sqf%d' % i) for i in range(2)]

                def fin_g(par_):
                    H_, s_, Hm_, sq_ = Hf[par_], sf[par_], Hm[par_], sqf2[par_]
                    for i in range(par_, 16, 2):
                        c.dma(H_[:], scr_h[i * 128:(i + 1) * 128, :], reads=[scrh_buf], writes=[H_])
                        c.tt('dve', Hm_[:], moe_acc[:, i, :], Bg2[:], ALU.mult, [macc[i][0], macc[i][1], Bg2], [Hm_])
                        yield
                        c.tt('pool', H_[:], H_[:], Hm_[:], ALU.add, [H_, Hm_], [H_])
                        yield
                        c.act(sq_[:, 0:D], H_[:], AF.Square, [H_], [sq_, s_], accum_out=s_[:, 0:1])
                        yield
                        c.ts('dve', s_[:, 1:2], s_[:, 0:1], 1.0 / D, EPS, ALU.mult, ALU.add, [s_], [s_])
                        yield
                        c.act(s_[:, 2:3], s_[:, 1:2], AF.Sqrt, [s_], [s_])
                        yield
                        c.op('dve', lambda e, s_=s_: e.reciprocal(out=s_[:, 3:4], in_=s_[:, 2:3]), reads=[s_], writes=[s_])
                        c.stt(H_[:], H_[:], s_[:, 3:4], gfin[:], ALU.mult, ALU.mult, [H_, s_, gfin], [H_])
                        yield
                        c.dma(out[i * 128:(i + 1) * 128, :], H_[:], reads=[H_])
                        yield

                run_threads([fin_g(0), fin_g(1)])
                c.barrier()

        c.finish()
        print("ninstr", c.ninstr, {k_: v for k_, v in c.cnt.items()})
    return nc


def prep_inputs(inp):
    f = lambda a: np.ascontiguousarray(a, dtype=np.float32)
    fm = lambda v: f(np.asarray(v).reshape(-1, 128).T)
    shared = {
        "ada_w": f(inp["ada_w"][0]),
        "ada_bT": fm(inp["ada_b"][0]),
        "gmixT": fm(inp["norm_mix_g"][0]),
        "gffnT": fm(inp["norm_ffn_g"][0]),
        "gfin_bc": f(np.broadcast_to(inp["final_norm_g"][None, :], (128, D))),
        "w_in": f(inp["w_in"][0]),
        "w_in_c": f(np.stack([np.asarray(inp["w_in"][0])[:, c0:c0 + 128].reshape(8, 128, 128).transpose(1, 0, 2) for c0 in W_CHUNKS])),
        "convT": f(inp["gdn_conv"][0].T.reshape(12, 128, 5).transpose(1, 0, 2)),
        "alog_bc": f(np.broadcast_to(inp["gdn_a_log"][0].reshape(1, 1, 8), (128, 18, 8))),
        "dtb_bc": f(np.broadcast_to(inp["gdn_dt_bias"][0].reshape(1, 1, 8), (128, 18, 8))),
        "onormT": f(inp["gdn_onorm_g"][0].reshape(128, 1)),
        "muT": fm(inp["rwkv_mu"][0]),
        "w0T": f(inp["rwkv_w0"][0].reshape(2, 4, 128).transpose(2, 0, 1)),
        "a0T": f(inp["rwkv_a0"][0].reshape(2, 4, 128).transpose(2, 0, 1)),
        "w2m": f(inp["rwkv_w2"][0].reshape(128, 512)),
        "a2m": f(inp["rwkv_a2"][0].reshape(128, 512)),
        "g2m": f(inp["rwkv_g2"][0]),
        "w_o_a": f(inp["w_o_a"][0]),
        "w_o_b": f(inp["w_o_b"][0]),
        "w_out": f(inp["w_out"][0]),
        "wrt": f(np.concatenate([inp["router_grp"][0], inp["router_exp"][0]], axis=1).reshape(8, 128, 36).transpose(1, 0, 2)),
        "rb_bc": f(np.broadcast_to(np.concatenate([inp["router_grp_b"][0], inp["router_exp_b"][0]])[None, :], (128, 36))),
        "moe_wg": f(np.asarray(inp["moe_w_gate"][0]).reshape(32, 8, 128, 256).transpose(0, 2, 1, 3)),
        "moe_wu": f(np.asarray(inp["moe_w_up"][0]).reshape(32, 8, 128, 256).transpose(0, 2, 1, 3)),
        "moe_wd": f(np.asarray(inp["moe_w_down"][0]).reshape(32, 2, 128, D).transpose(0, 2, 1, 3)),
        "rwv": f(np.stack([fm(inp["rwkv_k_k"][0]), fm(inp["rwkv_k_a"][0]), fm(inp["rwkv_r_k"][0].reshape(-1)),
                           fm(inp["rwkv_lnx_g"][0]), fm(inp["rwkv_lnx_b"][0])], axis=1)),
    }
    maps = []
    for b in range(NCORES):
        m = dict(shared)
        m["x"] = f(inp["x"][b])
        m["ctx"] = f(inp["ctx"][b])
        m["cT"] = f(np.stack([fm(inp["c"][b]), fm(inp["c_ctx"])], axis=-1))
        maps.append(m)
    return maps


def kernel(**inputs):
    maps = prep_inputs(inputs)
    nc = build()
    res = run_bass_kernel_spmd(nc, maps, core_ids=list(range(NCORES)))
    return np.stack([np.asarray(r["out"]) for r in res.results], axis=0).astype(np.float32)
```

```python
import os
import numpy as np
import concourse.bass as bass
import concourse.mybir as mybir
from concourse.bass_utils import run_bass_kernel_spmd
from concourse.alu_op_type import AluOpType as ALU
from contextlib import ExitStack

F32 = mybir.dt.float32
F32R = mybir.dt.float32r
BF16 = mybir.dt.bfloat16
AF = mybir.ActivationFunctionType
AX = mybir.AxisListType

NCORES = 8
W_CHUNKS = [128 * j for j in range(16)] + [2064 + 128 * j for j in range(15)] + [3984 + 128 * j for j in range(16)]
D = 1024
SEQ = 2048
CTX = 256
NTOK = SEQ + CTX
IN_COLS = 6032
A_COLS = 2064
B_COLS = 1920
EPS = 1e-6
LNX_EPS = 1e-5 * 64


class Buf:
    def __init__(self, t, name, psum=False):
        self.t = t
        self.name = name
        self.lw = None
        self.rd = {}
        self.psum = psum
        self.bankrd = None

    def __getitem__(self, idx):
        return self.t[idx]


class Ctx:
    ENG = ['pe', 'dve', 'act', 'pool', 'sp']
    NDMA = 8

    def __init__(self, nc, es):
        self.nc = nc
        self.e = {'pe': nc.tensor, 'dve': nc.vector, 'act': nc.scalar, 'pool': nc.gpsimd, 'sp': nc.sync}
        self.sem = {}
        self.cnt = {}
        for n in self.ENG:
            self.sem[n] = es.enter_context(nc.semaphore('s_' + n))
            self.cnt[n] = 0
        for i in range(self.NDMA):
            n = 'd%d' % i
            self.sem[n] = es.enter_context(nc.semaphore('s_' + n))
            self.cnt[n] = 0
        self.dma_rr = 0
        self.waited = {n: {} for n in self.ENG}
        self.nbuf = 0
        self.ninstr = 0

    def sb(self, es, shape, dt=F32, name=None):
        self.nbuf += 1
        name = (name or 'b') + '_%d' % self.nbuf
        t = es.enter_context(self.nc.sbuf_tensor(name, list(shape), dt))
        return Buf(t, name)

    def ps(self, es, shape, dt=F32, name=None):
        self.nbuf += 1
        name = (name or 'p') + '_%d' % self.nbuf
        t = es.enter_context(self.nc.psum_tensor(name, list(shape), dt))
        return Buf(t, name, psum=True)

    def view(self, buf, name='v'):
        self.nbuf += 1
        return Buf(buf.t, name + '_%d' % self.nbuf)

    def _deps(self, reads, writes):
        deps = {}

        def add(k, v):
            if v > deps.get(k, 0):
                deps[k] = v
        for b in reads:
            if b.lw:
                add(*b.lw)
            if b.psum:
                for k, v in b.rd.items():
                    add(k, v)
                if b.bankrd is not None:
                    for k, v in b.bankrd.items():
                        add(k, v)
        for b in writes:
            if b.lw:
                add(*b.lw)
            for k, v in b.rd.items():
                add(k, v)
        return deps

    def _wait(self, E, deps):
        eng = self.e[E]
        w = self.waited[E]
        nw = 0
        for k, v in deps.items():
            if k == E and E == 'pe' and v > self.cnt['pe']:
                continue
            if w.get(k, 0) >= v:
                continue
            eng.wait_ge(self.sem[k], v)
            nw += 1
            w[k] = v

    def op(self, E, fn, reads=(), writes=(), inc=True):
        deps = self._deps(reads, writes)
        self._wait(E, deps)
        ins = fn(self.e[E])
        self.ninstr += 1
        if inc:
            self.cnt[E] += 1
            ins.then_inc(self.sem[E], 1)
            cval = self.cnt[E]
        else:
            cval = self.cnt[E] + 1
        for b in writes:
            b.lw = (E, cval)
            b.rd = {}
        for b in reads:
            if b not in writes:
                b.rd[E] = max(b.rd.get(E, 0), cval)
            if b.bankrd is not None and E != 'pe':
                b.bankrd[E] = max(b.bankrd.get(E, 0), cval)
        return ins

    def dma(self, out, in_, reads=(), writes=(), q='sp', **kw):
        slot = 'd%d' % self.dma_rr
        self.dma_rr = (self.dma_rr + 1) % self.NDMA
        deps = self._deps(reads, writes)
        if self.cnt[slot] > 0:
            deps[slot] = max(deps.get(slot, 0), self.cnt[slot])
        self._wait(q, deps)
        ins = self.e[q].dma_start(out=out, in_=in_, **kw)
        self.ninstr += 1
        self.cnt[slot] += 16
        ins.then_inc(self.sem[slot], 16)
        cval = self.cnt[slot]
        for b in writes:
            b.lw = (slot, cval)
            b.rd = {}
        for b in reads:
            b.rd[slot] = max(b.rd.get(slot, 0), cval)
        return ins

    def barrier(self):
        for E in self.ENG:
            deps = {k: v for k, v in self.cnt.items() if v > 0 and k != E}
            self._wait(E, deps)

    def finish(self):
        for k in self.sem:
            if k.startswith('d') and self.cnt[k] > 0:
                self.e['sp'].wait_ge(self.sem[k], self.cnt[k])

    def mm(self, out, lhsT, rhs, start, stop, reads, writes, inc=None):
        if inc is None:
            inc = stop
        return self.op('pe', lambda e: e.matmul(out, lhsT=lhsT, rhs=rhs, start=start, stop=stop),
                       reads=reads, writes=writes, inc=inc)

    def tr(self, out, in_, ident, reads, writes, inc=True):
        return self.op('pe', lambda e: e.transpose(out=out, in_=in_, identity=ident), reads=reads, writes=writes, inc=inc)

    def act(self, out, in_, func, reads, writes, E='act', **kw):
        return self.op('act', lambda e: e.activation(out=out, in_=in_, func=func, **kw), reads=reads, writes=writes)

    def ts(self, E, out, in0, s1, s2, op0, op1, reads, writes):
        if op1 is None:
            return self.op(E, lambda e: e.tensor_scalar(out=out, in0=in0, scalar1=s1, scalar2=None, op0=op0), reads=reads, writes=writes)
        return self.op(E, lambda e: e.tensor_scalar(out=out, in0=in0, scalar1=s1, scalar2=s2, op0=op0, op1=op1), reads=reads, writes=writes)

    def tt(self, E, out, in0, in1, op, reads, writes):
        return self.op(E, lambda e: e.tensor_tensor(out=out, in0=in0, in1=in1, op=op), reads=reads, writes=writes)

    def stt(self, out, in0, scalar, in1, op0, op1, reads, writes):
        return self.op('dve', lambda e: e.scalar_tensor_tensor(out=out, in0=in0, scalar=scalar, in1=in1, op0=op0, op1=op1),
                       reads=reads, writes=writes)

    def cp(self, E, out, in_, reads, writes):
        if E == 'act':
            return self.op('act', lambda e: e.copy(out=out, in_=in_), reads=reads, writes=writes)
        return self.op(E, lambda e: e.tensor_copy(out=out, in_=in_), reads=reads, writes=writes)


def R(ap):
    return ap.bitcast(F32R)


def build(dbg=(), stage=99):
    nc = bass.Bass("TRN2", target_bir_lowering=False)
    T = {}

    def din(name, shape, dt=F32):
        T[name] = nc.dram_tensor(name, list(shape), dt, kind="ExternalInput").ap()
        return T[name]

    def dout(name, shape, dt=F32):
        T[name] = nc.dram_tensor(name, list(shape), dt, kind="ExternalOutput").ap()
        return T[name]

    x = din("x", [SEQ, D])
    ctx = din("ctx", [CTX, D])
    cT = din("cT", [128, 8, 2])
    ada_w = din("ada_w", [D, 6 * D])
    ada_bT = din("ada_bT", [128, 48])
    gmixT = din("gmixT", [128, 8])
    gffnT = din("gffnT", [128, 8])
    gfin_bc = din("gfin_bc", [128, D])
    w_in = din("w_in", [D, IN_COLS])
    w_in_c = din("w_in_c", [len(W_CHUNKS), 128, 8, 128])
    cidx = {c0: i for i, c0 in enumerate(W_CHUNKS)}
    convT = din("convT", [128, 12, 5])
    alog_bc = din("alog_bc", [128, 18, 8])
    dtb_bc = din("dtb_bc", [128, 18, 8])
    onormT = din("onormT", [128, 1])
    muT = din("muT", [128, 15])
    w0T = din("w0T", [128, 2, 4])
    a0T = din("a0T", [128, 2, 4])
    w2m = din("w2m", [128, 512])
    a2m = din("a2m", [128, 512])
    g2m = din("g2m", [128, 512])
    rwv = din("rwv", [128, 5, 4])
    scr_ob = nc.dram_tensor("scr_ob", [SEQ, 512], BF16, kind="Internal").ap()
    scr_h = nc.dram_tensor("scr_h", [SEQ, D], F32, kind="Internal").ap()
    scr_u = nc.dram_tensor("scr_u", [128, 8, SEQ], BF16, kind="Internal").ap()
    w_o_a = din("w_o_a", [512, D])
    w_o_b = din("w_o_b", [512, D])
    w_out = din("w_out", [D, D])
    wrt = din("wrt", [128, 8, 36])
    rb_bc = din("rb_bc", [128, 36])
    moe_wg = din("moe_wg", [32, 128, 8, 256])
    moe_wu = din("moe_wu", [32, 128, 8, 256])
    moe_wd = din("moe_wd", [32, 128, 2, D])
    out = dout("out", [SEQ, D])
    for name, shape, dt in dbg:
        dout(name, shape, dt)

    with ExitStack() as es:
        c = Ctx(nc, es)
        PS = [c.ps(es, [128, 512], F32, 'ps%d' % i) for i in range(8)]

        def psb(i):
            return PS[i][:].bitcast(BF16)

        bank_reads = [dict() for _ in range(8)]

        class Reg:
            def __init__(self, bank, c0, n, name):
                self.bank, self.c0, self.n = bank, c0, n
                self.buf = PS[bank]

            def ap(self, lo=0, hi=None, rows=slice(None)):
                hi = self.n if hi is None else hi
                return PS[self.bank].t[rows, self.c0 + lo:self.c0 + hi]

        def run_threads(gens):
            gens = list(gens)
            while gens:
                for g_ in list(gens):
                    try:
                        next(g_)
                    except StopIteration:
                        gens.remove(g_)

        def par(*gens):
            gens = list(gens)
            while gens:
                for g_ in list(gens):
                    try:
                        next(g_)
                    except StopIteration:
                        gens.remove(g_)
                        continue
                    yield

        ident = c.sb(es, [128, 128], F32, 'ident')
        identb = c.sb(es, [128, 128], BF16, 'identb')
        onesf = c.sb(es, [128, 128], F32, 'onesf')
        c.op('pool', lambda e: e.memset(ident[:], 0.0), writes=[ident])
        c.op('pool', lambda e: e.affine_select(out=ident[:], in_=ident[:], pattern=[[-1, 128]], compare_op=ALU.not_equal,
                                                fill=1.0, base=0, channel_multiplier=1), reads=[ident], writes=[ident])
        c.cp('dve', identb[:], ident[:], [ident], [identb])
        c.op('pool', lambda e: e.memset(onesf[:], 1.0), writes=[onesf])
        onesb = c.sb(es, [128, 128], BF16, 'onesb')
        c.cp('dve', onesb[:], onesf[:], [onesf], [onesb])
        ones_r = c.sb(es, [128, 128], F32, 'ones_r')
        nones_r = c.sb(es, [128, 128], F32, 'nones_r')
        ident_r = c.sb(es, [128, 128], F32, 'ident_r')
        c.cp('dve', R(ones_r[:]), onesf[:], [onesf], [ones_r])
        c.ts('dve', R(nones_r[:]), onesf[:], -1.0, None, ALU.mult, None, [onesf], [nones_r])
        c.cp('dve', R(ident_r[:]), ident[:], [ident], [ident_r])
        blk = c.sb(es, [128, 128], F32, 'blk')
        c.op('pool', lambda e: e.memset(blk[:], 0.0), writes=[blk])
        c.op('pool', lambda e: e.memset(blk[0:64, 0:64], 1.0), reads=[blk], writes=[blk])
        c.op('pool', lambda e: e.memset(blk[64:128, 64:128], 1.0), reads=[blk], writes=[blk])
        incl = [c.sb(es, [128, 128], F32, 'incl%d' % d) for d in range(2)]
        strict = [c.sb(es, [128, 128], F32, 'strict%d' % d) for d in range(2)]
        incl_r = [c.sb(es, [128, 128], F32, 'inclr%d' % d) for d in range(2)]
        negm_r = [c.sb(es, [128, 128], F32, 'negm%d' % d) for d in range(2)]
        blk_r = c.sb(es, [128, 128], F32, 'blk_r')
        sel_r = [c.sb(es, [128, 128], F32, 'sel%d' % k_) for k_ in range(2)]
        notI = c.sb(es, [128, 128], F32, 'notI')
        for d in range(2):
            pat = [[1, 128]] if d == 0 else [[-1, 128]]
            cm = -1 if d == 0 else 1
            c.op('pool', lambda e, d=d, pat=pat, cm=cm: e.affine_select(out=incl[d][:], in_=blk[:], pattern=pat, compare_op=ALU.is_ge,
                                                                        fill=0.0, base=0, channel_multiplier=cm), reads=[blk], writes=[incl[d]])
            c.op('pool', lambda e, d=d, pat=pat, cm=cm: e.affine_select(out=strict[d][:], in_=blk[:], pattern=pat, compare_op=ALU.is_gt,
                                                                        fill=0.0, base=0, channel_multiplier=cm), reads=[blk], writes=[strict[d]])
            c.cp('dve', R(incl_r[d][:]), incl[d][:], [incl[d]], [incl_r[d]])
            c.ts('dve', R(negm_r[d][:]), incl[d][:], 1.0e5, -1.0e5, ALU.mult, ALU.add, [incl[d]], [negm_r[d]])
        c.cp('dve', R(blk_r[:]), blk[:], [blk], [blk_r])
        c.ts('dve', notI[:], ident[:], -1.0, 1.0, ALU.mult, ALU.add, [ident], [notI])
        zf = c.sb(es, [128, 128], F32, 'zf')
        c.op('pool', lambda e: e.memset(zf[:], 0.0), writes=[zf])
        for k_ in range(2):
            c.cp('dve', R(sel_r[k_][:]), zf[:], [zf], [sel_r[k_]])
            c.cp('dve', R(sel_r[k_][k_ * 64:(k_ + 1) * 64, :]), onesf[k_ * 64:(k_ + 1) * 64, :], [onesf, sel_r[k_]], [sel_r[k_]])

        mT = c.sb(es, [128, 48, 2], F32, 'mT')
        A1g = c.sb(es, [128, 8, 2], F32, 'A1g')
        A2g = c.sb(es, [128, 8, 2], F32, 'A2g')
        oaT = c.sb(es, [128, 4, SEQ], BF16, 'oaT')

        with ExitStack() as es1:
            sT = c.sb(es1, [128, 8, 2], F32, 'sT')
            abT = c.sb(es1, [128, 48], F32, 'abT')
            gm = c.sb(es1, [128, 8], F32, 'gm')
            gf = c.sb(es1, [128, 8], F32, 'gf')
            c.dma(sT[:], cT, writes=[sT])
            c.dma(abT[:], ada_bT, writes=[abT])
            c.dma(gm[:], gmixT, writes=[gm])
            c.dma(gf[:], gffnT, writes=[gf])
            c.act(sT[:], sT[:], AF.Silu, [sT], [sT])
            Wb = [c.sb(es1, [128, 8, 512], F32, 'adaw%d' % i) for i in range(4)]
            ada_v = ada_w.rearrange("(kc p) n -> p kc n", p=128)
            for blk in range(12):
                wb = Wb[blk % 4]
                c.dma(wb[:], ada_v[:, :, blk * 512:(blk + 1) * 512], writes=[wb], q=('sp' if blk % 2 == 0 else 'act'))
                for mc in range(4):
                    col = (blk * 4 + mc) * 2
                    for kc in range(8):
                        c.mm(PS[0][:, col:col + 2], wb[:, kc, mc * 128:(mc + 1) * 128], sT[:, kc, :],
                             kc == 0, kc == 7, [wb, sT], [PS[0]])
            pv = PS[0][:, 0:96].rearrange("p (m s) -> p m s", s=2)
            for s in range(2):
                c.tt('dve', mT[:, :, s], pv[:, :, s], abT[:], ALU.add, [PS[0], abT], [mT])
            for s in range(2):
                c.stt(A1g[:, :, s], mT[:, 8:16, s], 1.0, gm[:], ALU.add, ALU.mult, [mT, gm], [A1g])
                c.stt(A2g[:, :, s], mT[:, 32:40, s], 1.0, gf[:], ALU.add, ALU.mult, [mT, gf], [A2g])
            c.barrier()

        def make_Bg(es_, Bg, base):
            dg = [c.sb(es_, [128, 128], F32, 'dg%d' % i) for i in range(2)]
            for fc in range(8):
                d_ = dg[fc % 2]
                c.ts('dve', d_[:], ident[:], mT[:, base + fc, 0:1], None, ALU.mult, None, [ident, mT], [d_])
                bank = PS[1 + fc // 4]
                c.mm(bank[:, (fc % 4) * 128:(fc % 4 + 1) * 128], onesf[:], d_[:], True, True, [onesf, d_], [bank])
            c.cp('act', Bg[:, 0:512], PS[1][:], [PS[1]], [Bg])
            c.cp('act', Bg[:, 512:1024], PS[2][:], [PS[2]], [Bg])

        scru_buf = Buf(None, 'scr_u_dram')

        def make_uT_g(srcs, s, dst, tok0, Ag, shift_base, bufs, k):
            X = bufs['X'][k % 4]
            xnb = bufs['xnb'][k % 2]
            ss = bufs['ss'][k % 2]
            sq_ = bufs['sq'][k % 2]
            for (p0, p1, ap) in srcs:
                c.dma(X[p0:p1, :], ap, writes=[X], q=('sp' if k % 2 == 0 else 'pool'))
            c.act(sq_[:], X[:], AF.Square, [X], [sq_, ss], accum_out=ss[:, 0:1])
            yield
            c.ts('dve', ss[:, 1:2], ss[:, 0:1], 1.0 / D, EPS, ALU.mult, ALU.add, [ss], [ss])
            yield
            c.act(ss[:, 2:3], ss[:, 1:2], AF.Sqrt, [ss], [ss])
            yield
            c.op('dve', lambda e: e.reciprocal(out=ss[:, 3:4], in_=ss[:, 2:3]), reads=[ss], writes=[ss])
            c.ts('dve', xnb[:], X[:], ss[:, 3:4], None, ALU.mult, None, [X, ss], [xnb])
            yield
            bank = PS[3 + k % 2]
            pb = psb(3 + k % 2)
            for fc in range(8):
                c.tr(pb[:, fc * 128:(fc + 1) * 128], xnb[:, fc * 128:(fc + 1) * 128], identb[:], [xnb, identb], [bank],
                     inc=(fc == 7))
            yield
            for fc in range(8):
                o_ = dst[:, fc, tok0:tok0 + 128]
                i_ = pb[:, fc * 128:(fc + 1) * 128]
                if fc % 2 == 0:
                    c.ts('dve', o_, i_, Ag[:, fc, s:s + 1], mT[:, shift_base + fc, s:s + 1], ALU.mult, ALU.add,
                         [bank, Ag, mT], [dst])
                else:
                    c.act(o_, i_, AF.Identity, [bank, Ag, mT], [dst], scale=Ag[:, fc, s:s + 1],
                          bias=mT[:, shift_base + fc, s:s + 1])
                if fc % 4 == 3:
                    yield

        def run_uT(jobs, bufs):
            def chain(par_):
                for k in range(par_, len(jobs), 2):
                    srcs, s_, dst, tok0 = jobs[k]
                    yield from make_uT_g(srcs, s_, dst, tok0, A1g, 0, bufs, k)
            run_threads([chain(0), chain(1)])

        def uT_bufs(es_):
            return {'X': [c.sb(es_, [128, D], F32, 'X%d' % i) for i in range(4)],
                    'xnb': [c.sb(es_, [128, D], BF16, 'xnb%d' % i) for i in range(2)],
                    'ss': [c.sb(es_, [128, 4], F32, 'ss%d' % i) for i in range(2)],
                    'sq': [c.sb(es_, [128, D], BF16, 'sq%d' % i) for i in range(2)]}

        esG = ExitStack()
        with esG:
            uT_r = c.sb(esG, [128, 8, NTOK], BF16, 'uT_r')
            with ExitStack() as es2:
                bufs = uT_bufs(es2)
                jobs = [([(0, 128, ctx[t * 128:(t + 1) * 128, :])], 1, uT_r, t * 128) for t in range(2)]
                jobs += [([(0, 128, x[t * 128:(t + 1) * 128, :])], 0, uT_r, CTX + t * 128) for t in range(16)]
                run_uT(jobs, bufs)
                for kc in range(8):
                    c.dma(scr_u[:, kc, :], uT_r[:, kc, CTX:NTOK], reads=[uT_r], writes=[scru_buf])
                c.barrier()


            w_in_v = w_in.rearrange("(kc p) n -> p kc n", p=128)
            TBLK = [(0, 256), (256, 768), (768, 1280), (1280, 1792), (1792, 2304)]
            with ExitStack() as es3:
                g_tok = c.sb(es3, [128, 18, 8], F32, 'g_tok')
                b_tok = c.sb(es3, [128, 18, 8], F32, 'b_tok')
                with ExitStack() as es3a:
                    wab = c.sb(es3a, [128, 8, 16], BF16, 'wab')
                    ab = c.sb(es3a, [128, 18, 16], F32, 'ab')
                    alog = c.sb(es3a, [128, 18, 8], F32, 'alog')
                    dtb = c.sb(es3a, [128, 18, 8], F32, 'dtb')
                    t1 = c.sb(es3a, [128, 18, 8], F32, 't1')
                    t2 = c.sb(es3a, [128, 18, 8], F32, 't2')
                    c.dma(wab[:], w_in_v[:, :, 2048:2064], writes=[wab], q='pool')
                    c.dma(alog[:], alog_bc, writes=[alog])
                    c.dma(dtb[:], dtb_bc, writes=[dtb])
                    for t in range(18):
                        bank = PS[t % 2]
                        for kc in range(8):
                            c.mm(bank[:, 0:16], uT_r[:, kc, t * 128:(t + 1) * 128], wab[:, kc, :], kc == 0, kc == 7, [uT_r, wab], [bank])
                        c.cp('act', ab[:, t, :], bank[:, 0:16], [bank], [ab])
                    c.tt('dve', t1[:], ab[:, :, 0:8], dtb[:], ALU.add, [ab, dtb], [t1])
                    c.stt(t2[:], t1[:], -1.0, t1[:], ALU.mult, ALU.max, [t1], [t2])
                    c.act(t2[:], t2[:], AF.Exp, [t2], [t2], scale=-1.0)
                    c.ts('dve', t2[:], t2[:], 1.0, None, ALU.add, None, [t2], [t2])
                    c.act(t2[:], t2[:], AF.Ln, [t2], [t2])
                    c.stt(t1[:], t1[:], 0.0, t2[:], ALU.max, ALU.add, [t1, t2], [t1])
                    c.act(alog[:], alog[:], AF.Exp, [alog], [alog])
                    c.stt(R(g_tok[:]), t1[:], -1.0, alog[:], ALU.mult, ALU.mult, [t1, alog], [g_tok])
                    c.act(b_tok[:], ab[:, :, 8:16], AF.Sigmoid, [ab], [b_tok])
                    c.barrier()

                cv = c.sb(es3, [128, 12, 5], F32, 'cv')
                onm = c.sb(es3, [128, 1], F32, 'onm')
                c.dma(cv[:], convT, writes=[cv])
                c.dma(onm[:], onormT, writes=[onm])
                raws = [c.sb(es3, [128, NTOK], F32, 'raw%d' % i) for i in range(2)]
                accs = [c.sb(es3, [128, NTOK], F32, 'acc%d' % i) for i in range(2)]
                sqs = [c.sb(es3, [128, NTOK], F32, 'sqg%d' % i) for i in range(2)]
                qT = c.sb(es3, [128, NTOK], F32, 'qT')
                kT = c.sb(es3, [128, NTOK], F32, 'kT')
                vT = c.sb(es3, [128, NTOK], F32, 'vT')
                zs = c.sb(es3, [128, SEQ], F32, 'zs')
                o_acc = c.sb(es3, [128, 16, 128], F32, 'o_acc')
                wc = [c.sb(es3, [128, 8, 128], BF16, 'wc%d' % i) for i in range(2)]
                rns = [[c.sb(es3, [128, 512], F32, 'rn%d_%d' % (j, i)) for i in range(2)] for j in range(2)]
                S = [c.sb(es3, [128, 128], F32, 'S%d' % d) for d in range(2)]
                gcs = [c.sb(es3, [128, 8], F32, 'gcs%d' % i) for i in range(2)]
                egc = [c.sb(es3, [128, 8], F32, 'egc%d' % i) for i in range(2)]
                negc = [c.sb(es3, [128, 8], F32, 'negc%d' % i) for i in range(2)]
                ekd = [c.sb(es3, [128, 8], F32, 'ekd%d' % i) for i in range(2)]
                gend = [c.sb(es3, [128, 2, 8], F32, 'gend%d' % i) for i in range(2)]
                k_tok = [c.sb(es3, [128, 128], F32, 'k_tok%d' % i) for i in range(2)]
                v_tok = [c.sb(es3, [128, 128], F32, 'v_tok%d' % i) for i in range(2)]
                NS = 2
                Gt = [c.sb(es3, [128, 128], F32, 'Gt%d' % i) for i in range(NS)]
                Ei = [c.sb(es3, [128, 128], F32, 'Ei%d' % i) for i in range(NS)]
                Es = [c.sb(es3, [128, 128], F32, 'Es%d' % i) for i in range(NS)]
                QKm = [c.sb(es3, [128, 128], F32, 'QKm%d' % i) for i in range(NS)]
                Pb = [[c.sb(es3, [128, 128], F32, 'P%d_%d' % (i, j)) for j in range(2)] for i in range(NS)]
                Qb = [[c.sb(es3, [128, 128], F32, 'Q%d_%d' % (i, j)) for j in range(2)] for i in range(NS)]
                Wb_ = [[c.sb(es3, [128, 128], F32, 'W%d_%d' % (i, j)) for j in range(2)] for i in range(NS)]
                kdec = [c.sb(es3, [128, 128], F32, 'kdec%d' % i) for i in range(NS)]
                Zb = [c.sb(es3, [128, 128], F32, 'Z%d' % i) for i in range(NS)]
                vnew = [c.sb(es3, [128, 128], F32, 'vnew%d' % i) for i in range(NS)]
                otmp = [c.sb(es3, [128, 128], F32, 'otmp%d' % i) for i in range(NS)]
                otmp2 = [c.sb(es3, [128, 128], F32, 'otmp2%d' % i) for i in range(NS)]
                fin = [c.sb(es3, [128, 132], F32, 'fin%d' % i) for i in range(2)]
                wcnt = 0
                unit = 0
                import os
                H_all = c.sb(es3, [128, 18, 32], F32, 'H_all')
                egc_all = c.sb(es3, [128, 18, 8], F32, 'egc_all')
                negc_all = c.sb(es3, [128, 18, 8], F32, 'negc_all')
                ekd_all = c.sb(es3, [128, 18, 8], F32, 'ekd_all')
                gend_all = c.sb(es3, [128, 18, 16], F32, 'gend_all')
                for t in range(18):
                    bankH = PS[t % 2]
                    c.mm(bankH[:, 0:4], R(incl_r[0][:]), R(g_tok[:, t, 0:4]), True, True, [incl_r[0], g_tok], [bankH])
                    c.mm(bankH[:, 4:8], R(incl_r[1][:]), R(g_tok[:, t, 4:8]), True, True, [incl_r[1], g_tok], [bankH])
                    c.mm(bankH[:, 8:16], R(blk_r[:]), R(g_tok[:, t, :]), True, True, [blk_r, g_tok], [bankH])
                    c.mm(bankH[:, 16:24], R(sel_r[0][:]), R(g_tok[:, t, :]), True, True, [sel_r[0], g_tok], [bankH])
                    c.mm(bankH[:, 24:32], R(sel_r[1][:]), R(g_tok[:, t, :]), True, True, [sel_r[1], g_tok], [bankH])
                    c.cp('dve', H_all[:, t, :], bankH[:, 0:32], [bankH], [H_all])
                c.act(egc_all[:], H_all[:, :, 0:8], AF.Exp, [H_all], [egc_all])
                c.ts('dve', negc_all[:], egc_all[:], -1.0, None, ALU.mult, None, [egc_all], [negc_all])
                c.tt('dve', ekd_all[:], H_all[:, :, 8:16], H_all[:, :, 0:8], ALU.subtract, [H_all], [ekd_all])
                c.act(ekd_all[:], ekd_all[:], AF.Exp, [ekd_all], [ekd_all])
                c.act(gend_all[:], H_all[:, :, 16:32], AF.Exp, [H_all], [gend_all])
                NH = int(os.environ.get('K_NH', '4'))
                NSTEP = int(os.environ.get('K_NSTEP', '18'))
                KLAT = int(os.environ.get('K_LAT', '9'))
                for h in range(NH):
                    def proj_g(ci, slot, h=h):
                        col0, dst = [(h * 128, qT), (512 + h * 128, kT), (1024 + h * 128, vT), (1536 + h * 128, zs)][ci]
                        raw, acc, sq = raws[slot], accs[slot], sqs[slot]
                        w_ = wc[slot]
                        pb0 = 4 * slot
                        if h == 0 and ci < 2:
                            c.dma(w_[:], w_in_c[cidx[col0]], writes=[w_], q='pool')
                        for bi, (t0, t1_) in enumerate(TBLK):
                            if ci == 3 and bi == 0:
                                continue
                            bank = PS[pb0 + bi % 2]
                            n = t1_ - t0
                            for kc in range(8):
                                c.mm(bank[:, 0:n], w_[:, kc, :], uT_r[:, kc, t0:t1_], kc == 0, kc == 7, [w_, uT_r], [bank])
                            if ci == 3:
                                c.act(zs[:, t0 - CTX:t1_ - CTX], bank[:, 0:n], AF.Silu, [bank], [zs])
                            else:
                                c.cp('act', raw[:, t0:t1_], bank[:, 0:n], [bank], [raw])
                            yield
                        nh_, nci = (h, ci + 2) if ci < 2 else (h + 1, ci - 2)
                        if nh_ < NH:
                            ncol = [nh_ * 128, 512 + nh_ * 128, 1024 + nh_ * 128, 1536 + nh_ * 128][nci]
                            c.dma(w_[:], w_in_c[cidx[ncol]], writes=[w_], q='pool')
                        if ci == 3:
                            return
                        cch = ci * 4 + h
                        c.ts('dve', acc[:], raw[:], cv[:, cch, 2:3], None, ALU.mult, None, [raw, cv], [acc])
                        yield
                        for kk_ in (0, 1, 3, 4):
                            sft = kk_ - 2
                            for (a_, b_) in ((0, CTX), (CTX, NTOK)):
                                lo = max(a_, a_ - sft)
                                hi = min(b_, b_ - sft)
                                c.stt(acc[:, lo:hi], raw[:, lo + sft:hi + sft], cv[:, cch, kk_:kk_ + 1], acc[:, lo:hi], ALU.mult, ALU.add,
                                      [raw, cv, acc], [acc])
                            yield
                        if ci == 2:
                            c.act(R(vT[:]), acc[:], AF.Silu, [acc], [vT])
                            return
                        c.act(acc[:], acc[:], AF.Silu, [acc], [acc])
                        c.act(R(sq[:]), acc[:], AF.Square, [acc], [sq])
                        yield
                        sc = 128.0 if ci == 0 else 1.0
                        for bi, (t0, t1_) in enumerate(TBLK):
                            bank = PS[pb0 + 2 + bi % 2]
                            n = t1_ - t0
                            r_ = rns[slot][bi % 2]
                            c.mm(bank[:, 0:n], R(ones_r[:]), R(sq[:, t0:t1_]), True, True, [ones_r, sq], [bank])
                            c.ts('dve', r_[:, 0:n], bank[:, 0:n], sc, EPS * sc, ALU.mult, ALU.add, [bank], [r_])
                            c.act(r_[:, 0:n], r_[:, 0:n], AF.Sqrt, [r_], [r_])
                            yield
                            c.op('dve', lambda e, r_=r_, n=n: e.reciprocal(out=r_[:, 0:n], in_=r_[:, 0:n]), reads=[r_], writes=[r_])
                            c.tt('dve', R(dst[:, t0:t1_]), acc[:, t0:t1_], r_[:, 0:n], ALU.mult, [acc, r_], [dst])
                            yield

                    run_threads([proj_g(0, 0), proj_g(1, 1)])
                    run_threads([proj_g(2, 0), proj_g(3, 1)])
                    if 'd_qkv' in T and h == 0:
                        c.dma(T['d_qkv'][0], qT[:], reads=[qT])
                        c.dma(T['d_qkv'][1], kT[:], reads=[kT])
                        c.dma(T['d_qkv'][2], vT[:], reads=[vT])
                    for d in range(2):
                        c.ts('dve', R(S[d][:]), zf[:], 0.0, None, ALU.mult, None, [zf], [S[d]])
                    order_f = list(range(18))
                    order_b = [1, 0] + list(range(17, 1, -1))
                    def gdn_unit_g(d, step):
                        tile = order_f[step] if d == 0 else order_b[step]
                        is_lat = tile >= 2
                        ts0 = tile * 128
                        col = d * 4 + h
                        pi = d
                        u = d
                        X0, X1, X2, X3 = (PS[4 * d + i_] for i_ in range(4))
                        bankT = X3
                        c.tr(bankT[:, 0:128], kT[:, ts0:ts0 + 128], ident[:], [kT, ident], [bankT])
                        c.tr(bankT[:, 128:256], vT[:, ts0:ts0 + 128], ident[:], [vT, ident], [bankT])
                        bankA = X0
                        c.mm(bankA[:, 0:128], R(kT[:, ts0:ts0 + 128]), R(kT[:, ts0:ts0 + 128]), True, True, [kT], [bankA])
                        c.mm(bankA[:, 128:256], R(kT[:, ts0:ts0 + 128]), R(qT[:, ts0:ts0 + 128]), True, True, [kT, qT], [bankA])
                        c.ts('pool', R(Gt[u][:]), incl[d][:], g_tok[:, tile, col:col + 1], None, ALU.mult, None, [incl[d], g_tok], [Gt[u]])
                        yield
                        bankB = X1
                        c.mm(bankB[:, 0:128], R(ones_r[:]), R(Gt[u][:]), True, False, [ones_r, Gt[u]], [bankB])
                        c.mm(bankB[:, 0:128], R(Gt[u][:]), R(nones_r[:]), False, False, [nones_r, Gt[u]], [bankB])
                        c.mm(bankB[:, 0:128], R(ident_r[:]), R(negm_r[d][:]), False, True, [ident_r, negm_r[d]], [bankB])
                        yield
                        c.cp('act', k_tok[pi][:], bankT[:, 0:128], [bankT], [k_tok[pi]])
                        c.cp('act', R(v_tok[pi][:]), bankT[:, 128:256], [bankT], [v_tok[pi]])
                        c.act(Ei[u][:], bankB[:, 0:128], AF.Exp, [bankB], [Ei[u]])
                        yield
                        c.tt('pool', Es[u][:], Ei[u][:], notI[:], ALU.mult, [Ei[u], notI], [Es[u]])
                        P, Q, W = Pb[u], Qb[u], Wb_[u]
                        yield
                        c.stt(R(P[0][:]), bankA[:, 0:128], b_tok[:, tile, col:col + 1], Es[u][:], ALU.mult, ALU.mult,
                              [bankA, b_tok, Es[u]], [P[0]])
                        c.tt('dve', R(QKm[u][:]), bankA[:, 128:256], Ei[u][:], ALU.mult, [bankA, Ei[u]], [QKm[u]])
                        c.ts('dve', R(kdec[u][:]), k_tok[pi][:], ekd_all[:, tile, col:col + 1], None, ALU.mult, None, [k_tok[pi], ekd_all], [kdec[u]])
                        yield
                        bankC = X2
                        bankD = X3
                        c.tr(bankC[:, 0:128], P[0][:], ident[:], [P[0], ident], [bankC])
                        c.tt('pool', R(W[0][:]), ident[:], P[0][:], ALU.subtract, [ident, P[0]], [W[0]])
                        yield
                        c.cp('act', R(Q[0][:]), bankC[:, 0:128], [bankC], [Q[0]])
                        yield
                        for k_ in range(5):
                            a_, b_ = k_ % 2, (k_ + 1) % 2
                            c.mm(bankC[:, 128:256], R(P[a_][:]), R(Q[a_][:]), True, True, [P[a_], Q[a_]], [bankC])
                            if k_ < 4:
                                c.mm(bankD[:, 0:128], R(Q[a_][:]), R(P[a_][:]), True, True, [P[a_], Q[a_]], [bankD])
                            yield
                            c.cp('act', R(Q[b_][:]), bankC[:, 128:256], [bankC], [Q[b_]])
                            if k_ < 4:
                                c.cp('dve', R(P[b_][:]), bankD[:, 0:128], [bankD], [P[b_]])
                            yield
                            c.mm(bankD[:, 128:256], R(Q[b_][:]), R(W[a_][:]), True, True, [Q[b_], W[a_]], [bankD])
                            yield
                            c.tt('dve', R(W[b_][:]), bankD[:, 128:256], W[a_][:], ALU.add, [bankD, W[a_]], [W[b_]])
                            yield
                        Wf = W[1]
                        for cs in ((0, 64) if d == 0 else (64, 0)):
                            sl_ = slice(cs, cs + 64)
                            chunk = cs // 64
                            c.mm(X0[:, 0:128], R(kT[:, ts0:ts0 + 128]), R(S[d][:]), True, True, [kT, S[d]], [X0])
                            if is_lat:
                                c.mm(X0[:, 128:256], R(qT[:, ts0:ts0 + 128]), R(S[d][:]), True, True, [qT, S[d]], [X0])
                            yield
                            c.stt(R(Zb[u][sl_, :]), X0[sl_, 0:128], negc_all[sl_, tile, col:col + 1], v_tok[pi][sl_, :], ALU.mult, ALU.add,
                                  [X0, negc_all, v_tok[pi]], [Zb[u]])
                            if is_lat:
                                c.ts('dve', otmp[u][sl_, :], X0[sl_, 128:256], egc_all[sl_, tile, col:col + 1], None, ALU.mult, None, [X0, egc_all], [otmp[u]])
                            yield
                            c.mm(X1[:, 0:128], R(Wf[sl_, :]), R(Zb[u][sl_, :]), True, True, [Wf, Zb[u]], [X1])
                            yield
                            c.ts('dve', R(vnew[u][sl_, :]), X1[sl_, 0:128], b_tok[sl_, tile, col:col + 1], None, ALU.mult, None,
                                 [X1, b_tok], [vnew[u]])
                            yield
                            c.mm(X3[:, 256:384], R(kdec[u][sl_, :]), R(vnew[u][sl_, :]), True, True, [kdec[u], vnew[u]], [X3])
                            if is_lat:
                                c.mm(X2[:, 0:128], R(QKm[u][sl_, :]), R(vnew[u][sl_, :]), True, True, [QKm[u], vnew[u]], [X2])
                            yield
                            c.stt(R(S[d][:]), S[d][:], gend_all[:, tile, chunk * 8 + col:chunk * 8 + col + 1], X3[:, 256:384], ALU.mult, ALU.add,
                                  [S[d], gend_all, X3], [S[d]])
                            if is_lat:
                                lt = tile - 2
                                if (d == 0) == (lt <= 7):
                                    c.tt('dve', o_acc[sl_, lt, :], X2[sl_, 0:128], otmp[u][sl_, :], ALU.add, [X2, otmp[u]], [o_acc])
                                else:
                                    c.tt('dve', otmp2[u][sl_, :], X2[sl_, 0:128], otmp[u][sl_, :], ALU.add, [X2, otmp[u]], [otmp2[u]])
                                    c.tt('pool', o_acc[sl_, lt, :], o_acc[sl_, lt, :], otmp2[u][sl_, :], ALU.add, [o_acc, otmp2[u]], [o_acc])
                            yield

                    def gdn_dir_g(d):
                        for step in range(NSTEP):
                            yield from gdn_unit_g(d, step)

                    run_threads([gdn_dir_g(0), gdn_dir_g(1)])
                    def gdn_out_g(par_, h=h):
                        f_ = fin[par_]
                        bank = PS[par_]
                        for lt in range(par_, 16, 2):
                            c.act(f_[:, 0:128], o_acc[:, lt, :], AF.Square, [o_acc], [f_], accum_out=f_[:, 128:129])
                            yield
                            c.ts('dve', f_[:, 129:130], f_[:, 128:129], 1.0 / 128, EPS, ALU.mult, ALU.add, [f_], [f_])
                            yield
                            c.act(f_[:, 130:131], f_[:, 129:130], AF.Sqrt, [f_], [f_])
                            yield
                            c.op('dve', lambda e, f_=f_: e.reciprocal(out=f_[:, 131:132], in_=f_[:, 130:131]), reads=[f_], writes=[f_])
                            c.ts('dve', f_[:, 0:128], o_acc[:, lt, :], f_[:, 131:132], None, ALU.mult, None, [o_acc, f_], [f_])
                            yield
                            c.tr(bank[:, 0:128], f_[:, 0:128], ident[:], [f_, ident], [bank])
                            yield
                            c.stt(oaT[:, h, lt * 128:(lt + 1) * 128], bank[:, 0:128], onm[:, 0:1], zs[:, lt * 128:(lt + 1) * 128], ALU.mult, ALU.mult,
                                  [bank, onm, zs], [oaT])
                            yield

                    run_threads([gdn_out_g(0), gdn_out_g(1)])
                c.barrier()
        if 'd_oaT' in T:
            c.dma(T['d_oaT'], oaT[:], reads=[oaT])


        def inv_chain(P, Q, W, bankC, bankD):
            c.tr(bankC[:, 0:128], P[0][:], ident[:], [P[0], ident], [bankC])
            c.cp('act', R(Q[0][:]), bankC[:, 0:128], [bankC], [Q[0]])
            c.tt('pool', R(W[0][:]), ident[:], P[0][:], ALU.subtract, [ident, P[0]], [W[0]])
            for k_ in range(5):
                a_, b_ = k_ % 2, (k_ + 1) % 2
                c.mm(bankC[:, 128:256], R(P[a_][:]), R(Q[a_][:]), True, True, [P[a_], Q[a_]], [bankC])
                if k_ < 4:
                    c.mm(bankD[:, 0:128], R(Q[a_][:]), R(P[a_][:]), True, True, [P[a_], Q[a_]], [bankD])
                c.cp('act', R(Q[b_][:]), bankC[:, 128:256], [bankC], [Q[b_]])
                if k_ < 4:
                    c.cp('dve', R(P[b_][:]), bankD[:, 0:128], [bankD], [P[b_]])
                c.mm(bankD[:, 128:256], R(Q[b_][:]), R(W[a_][:]), True, True, [Q[b_], W[a_]], [bankD])
                c.tt('dve', R(W[b_][:]), bankD[:, 128:256], W[a_][:], ALU.add, [bankD, W[a_]], [W[b_]])
            return W[1]

        BW = 256
        NBLK = NTOK // BW
        with ExitStack() as esR:
            uT_c = c.sb(esR, [128, 8, NTOK], BF16, 'uT_c')
            with ExitStack() as es2:
                bufs = uT_bufs(es2)
                x_cm = x.rearrange("(r w) d -> w r d", w=64)
                jobs = [([(0, 128, ctx[t * 128:(t + 1) * 128, :])], 1, uT_c, t * 128) for t in range(2)]
                jobs += [([(wl * 32, wl * 32 + 32, x_cm[4 * j + wl]) for wl in range(4)], 0, uT_c, CTX + j * 128) for j in range(16)]
                run_uT(jobs, bufs)
                c.barrier()
            mu = c.sb(esR, [128, 15], F32, 'mu')
            hmu = c.sb(esR, [128, 15], F32, 'hmu')
            omu = c.sb(esR, [128, 15], F32, 'omu')
            w0 = c.sb(esR, [128, 2, 4], F32, 'w0')
            a0 = c.sb(esR, [128, 2, 4], F32, 'a0')
            rw = c.sb(esR, [128, 5, 4], F32, 'rw')
            oka = c.sb(esR, [128, 4], F32, 'oka')
            oka2 = c.sb(esR, [128, 4], F32, 'oka2')
            w2b = c.sb(esR, [128, 512], BF16, 'w2b')
            a2b = c.sb(esR, [128, 512], BF16, 'a2b')
            g2b = c.sb(esR, [128, 512], BF16, 'g2b')
            c.dma(mu[:], muT, writes=[mu])
            c.dma(w0[:], w0T, writes=[w0])
            c.dma(a0[:], a0T, writes=[a0])
            c.dma(rw[:], rwv, writes=[rw])
            c.dma(w2b[:], w2m, writes=[w2b], q='pool')
            c.dma(a2b[:], a2m, writes=[a2b], q='pool')
            c.dma(g2b[:], g2m, writes=[g2b], q='pool')
            c.ts('dve', hmu[:], mu[:], 0.5, None, ALU.mult, None, [mu], [hmu])
            c.ts('dve', omu[:], mu[:], -1.0, 1.0, ALU.mult, ALU.add, [mu], [omu])
            c.ts('dve', oka[:], rw[:, 1, :], -1.0, 1.0, ALU.mult, ALU.add, [rw], [oka])
            c.ts('dve', oka2[:], rw[:, 1, :], -2.0, 2.0, ALU.mult, ALU.add, [rw], [oka2])
            cmask = c.sb(esR, [128, BW], F32, 'cmask')
            c.op('pool', lambda e: e.memset(cmask[:], 1.0), writes=[cmask])
            c.op('pool', lambda e: e.memset(cmask[:].rearrange("p (a b) -> p a b", b=64)[:, :, 0:1], 0.0), reads=[cmask], writes=[cmask])
            mskA = [c.sb(esR, [128, 256], F32, 'mskA%d' % d) for d in range(2)]
            mskB = [c.sb(esR, [128, 256], F32, 'mskB%d' % d) for d in range(2)]
            for d in range(2):
                c.ts('dve', mskA[d][:, 0:128], strict[d][:], -1.0, None, ALU.mult, None, [strict[d]], [mskA[d]])
                c.cp('dve', mskA[d][:, 128:256], incl[d][:], [incl[d]], [mskA[d]])
                c.cp('dve', mskB[d][:, 0:128], strict[d][:], [strict[d]], [mskB[d]])
                c.cp('dve', mskB[d][:, 128:256], incl[d][:], [incl[d]], [mskB[d]])

            raw = c.sb(esR, [128, NTOK], F32, 'rraw')
            t1 = c.sb(esR, [128, NTOK], F32, 'rt1')
            twl = c.sb(esR, [128, NTOK], BF16, 'twl')
            alo = c.sb(esR, [128, NTOK], BF16, 'alo')
            sgl = c.sb(esR, [128, NTOK], BF16, 'sgl')
            rb = c.sb(esR, [128, NTOK], BF16, 'rb')
            kb = c.sb(esR, [128, NTOK], BF16, 'kb')
            vb = c.sb(esR, [128, NTOK], BF16, 'vb')
            G1 = c.sb(esR, [128, SEQ], BF16, 'G1')
            G2 = c.sb(esR, [128, SEQ], BF16, 'G2')
            y_acc = c.sb(esR, [128, 16, 128], F32, 'y_acc')
            wcr = [c.sb(esR, [128, 8, 128], BF16, 'wcr%d' % i) for i in range(2)]
            wcn = [0]

            pm_sched = [1536, 1664, 1792]
            for hc_ in range(4):
                pm_sched += [hc_ * 128, 512 + hc_ * 128, 1024 + hc_ * 128]

            def proj_mix(bcol, dst, func, mi):
                n_ = wcn[0]
                wcn[0] += 1
                assert pm_sched[n_] == bcol
                w_ = wcr[n_ % 2]
                if n_ == 0:
                    c.dma(w_[:], w_in_c[cidx[A_COLS + bcol]], writes=[w_], q='pool')
                for bi, (t0, t1_) in enumerate(TBLK):
                    bank = PS[bi % 2]
                    n = t1_ - t0
                    for kc in range(8):
                        c.mm(bank[:, 0:n], w_[:, kc, :], uT_c[:, kc, t0:t1_], kc == 0, kc == 7, [w_, uT_c], [bank])
                    c.cp('act', raw[:, t0:t1_], bank[:, 0:n], [bank], [raw])
                    if bi == 0 and n_ + 1 < len(pm_sched):
                        nb_ = A_COLS + pm_sched[n_ + 1]
                        c.dma(wcr[(n_ + 1) % 2][:], w_in_c[cidx[nb_]], writes=[wcr[(n_ + 1) % 2]], q='pool')
                for (a_, b_) in ((0, CTX), (CTX, NTOK)):
                    c.tt('dve', t1[:, a_ + 1:b_ - 1], raw[:, a_:b_ - 2], raw[:, a_ + 2:b_], ALU.add, [raw], [t1])
                    c.cp('dve', t1[:, a_:a_ + 1], raw[:, a_ + 1:a_ + 2], [raw], [t1])
                    c.cp('dve', t1[:, b_ - 1:b_], raw[:, b_ - 2:b_ - 1], [raw], [t1])
                c.ts('dve', t1[:], t1[:], hmu[:, mi:mi + 1], None, ALU.mult, None, [t1, hmu], [t1])
                if func is None:
                    c.stt(dst[:], raw[:], omu[:, mi:mi + 1], t1[:], ALU.mult, ALU.add, [raw, omu, t1], [dst])
                else:
                    c.stt(t1[:], raw[:], omu[:, mi:mi + 1], t1[:], ALU.mult, ALU.add, [raw, omu, t1], [t1])
                    c.act(dst[:], t1[:], func, [t1], [dst])

            proj_mix(1536, twl, AF.Tanh, 12)
            proj_mix(1664, alo, None, 13)
            proj_mix(1792, sgl, AF.Sigmoid, 14)

            def blkbuf(name, dt=F32, w=BW):
                return c.sb(esR, [128, w], dt, name)
            DB = []
            for d in range(2):
                g = {}
                arena = (raw, t1)[d]
                for i_, nm in enumerate(('lw', 'icl', 'pre', 'cumd', 'kkn', 'kdir', 'bvec', 'tmpa', 'tmpb', 'opr', 'btT', 'ktT', 'bhT', 'khT')):
                    if i_ < 9:
                        g[nm] = Buf(arena.t[:, i_ * BW:(i_ + 1) * BW], '%s%d' % (nm, d))
                    else:
                        g[nm] = blkbuf('%s%d' % (nm, d))
                g['AR'] = c.sb(esR, [128, 2, BW], F32, 'AR%d' % d)
                g['gC'] = c.sb(esR, [128, BW // 64], F32, 'gC%d' % d)
                for nm in ('Bh_tok', 'Kh_tok', 'Vt'):
                    g[nm] = c.sb(esR, [128, 128], F32, '%s%d' % (nm, d))
                g['BL0'] = Reg(4 * d, 0, 256, 'BL0_%d' % d)
                g['BL1'] = Reg(4 * d + 2, 0, 256, 'BL1_%d' % d)
                g['TL0'] = Reg(4 * d + 1, 0, 128, 'TL0_%d' % d)
                g['TLb'] = Reg(4 * d + 1, 128, 128, 'TLb_%d' % d)
                g['TL1'] = Reg(4 * d + 3, 0, 128, 'TL1_%d' % d)
                g['U'] = []
                for hh in range(2):
                    u = {'bankA': 4 * d + 2 * hh, 'bankB': 4 * d + 2 * hh + 1}
                    sfx = '%d%d' % (d, hh)
                    u['AB1'] = c.sb(esR, [128, 256], F32, 'AB1_' + sfx)
                    u['AB2'] = c.sb(esR, [128, 256], F32, 'AB2_' + sfx)
                    u['XY2'] = c.sb(esR, [128, 128], F32, 'XY2_' + sfx)
                    for nm in ('P', 'Q', 'W'):
                        u[nm] = [c.sb(esR, [128, 128], F32, '%sr%d_%s' % (nm, j, sfx)) for j in range(2)]
                    for nm in ('Xs', 'Us', 'yt', 'yt2', 'Tst'):
                        u[nm] = c.sb(esR, [128, 64], F32, nm + sfx)
                    g['U'].append(u)
                DB.append(g)
            gst2 = [c.sb(esR, [128, 24], F32, 'gst%d' % i) for i in range(2)]
            yn2 = [c.sb(esR, [128, 128], F32, 'yn%d' % i) for i in range(2)]
            obf2 = [c.sb(esR, [128, 128], BF16, 'obf%d' % i) for i in range(2)]
            otf2 = [c.sb(esR, [128, 128], F32, 'otf%d' % i) for i in range(2)]
            NHC = int(os.environ.get('K_NHC', '4'))
            NBS = int(os.environ.get('K_NBS', '9'))
            LG = -0.6065306597126334
            KTG = int(os.environ.get('K_TG', '9'))

            def block_g(d, g, b, hc):
                rowsd = slice(d * 64, d * 64 + 64)
                cs_ = slice(hc * 128, (hc + 1) * 128)
                t0 = b * BW
                tsl = slice(t0, t0 + BW)
                lw, icl, pre, cumd, kkn, kdir, bvec, tmpa, tmpb, opr = (g[n_] for n_ in ('lw', 'icl', 'pre', 'cumd', 'kkn', 'kdir', 'bvec', 'tmpa', 'tmpb', 'opr'))
                AR, btT, ktT, bhT, khT, gC = (g[n_] for n_ in ('AR', 'btT', 'ktT', 'bhT', 'khT', 'gC'))
                BL0, BL1 = g['BL0'], g['BL1']
                c.mm(BL0.ap(), w2b[rowsd, cs_], twl[rowsd, tsl], True, True, [w2b, twl], [BL0.buf])
                c.mm(BL1.ap(), a2b[rowsd, cs_], alo[rowsd, tsl], True, True, [a2b, alo], [BL1.buf])
                c.ts('dve', kkn[:], kb[:, tsl], rw[:, 0, hc:hc + 1], None, ALU.mult, None, [kb, rw], [kkn])
                yield
                c.act(lw[:], BL0.ap(), AF.Sigmoid, [BL0.buf, w0], [lw], bias=w0[:, d, hc:hc + 1])
                c.act(icl[:], BL1.ap(), AF.Sigmoid, [BL1.buf, a0], [icl], bias=a0[:, d, hc:hc + 1])
                c.act(R(opr[:]), kkn[:], AF.Square, [kkn], [opr])
                yield
                c.ts('dve', lw[:], lw[:], LG, None, ALU.mult, None, [lw], [lw])
                c.mm(BL0.ap(), R(blk_r[:]), R(opr[:]), True, True, [blk_r, opr], [BL0.buf])
                c.op('dve', lambda e: e.tensor_tensor_scan(out=pre[:], data0=cmask[:], data1=lw[:], initial=0.0,
                                                           op0=ALU.mult, op1=ALU.add), reads=[cmask, lw], writes=[pre])
                yield
                pre3 = pre[:].rearrange("p (a b) -> p a b", b=64)
                tot_bc = pre3[:, :, 63:64].to_broadcast([128, BW // 64, 64])
                if d == 0:
                    c.cp('pool', cumd[:], pre[:], [pre], [cumd])
                else:
                    c.tt('dve', cumd[:], lw[:], pre[:], ALU.subtract, [lw, pre], [cumd])
                    c.tt('dve', cumd[:].rearrange("p (a b) -> p a b", b=64), cumd[:].rearrange("p (a b) -> p a b", b=64), tot_bc,
                         ALU.add, [cumd, pre], [cumd])
                c.ts('dve', tmpb[:], BL0.ap(), EPS, None, ALU.add, None, [BL0.buf], [tmpb])
                yield
                c.act(gC[:], pre3[:, :, 63], AF.Exp, [pre], [gC])
                c.act(tmpb[:], tmpb[:], AF.Sqrt, [tmpb], [tmpb])
                c.ts('dve', kdir[:], icl[:], rw[:, 1, hc:hc + 1], oka[:, hc:hc + 1], ALU.mult, ALU.add, [icl, rw, oka], [kdir])
                c.tt('dve', kdir[:], kdir[:], kb[:, tsl], ALU.mult, [kdir, kb], [kdir])
                yield
                c.op('dve', lambda e: e.reciprocal(out=tmpb[:], in_=tmpb[:]), reads=[tmpb], writes=[tmpb])
                c.tt('dve', kkn[:], kkn[:], tmpb[:], ALU.mult, [kkn, tmpb], [kkn])
                c.tt('dve', tmpa[:], cumd[:], lw[:], ALU.subtract, [cumd, lw], [tmpa])
                yield
                c.tt('pool', bvec[:], kkn[:], icl[:], ALU.mult, [kkn, icl], [bvec])
                c.act(tmpa[:], tmpa[:], AF.Exp, [tmpa], [tmpa])
                yield
                c.stt(R(AR[:, 0, :]), kkn[:], -1.0, tmpa[:], ALU.mult, ALU.mult, [kkn, tmpa], [AR])
                yield
                c.act(tmpa[:], cumd[:], AF.Exp, [cumd], [tmpa])
                c.tt('dve', tmpb[:].rearrange("p (a b) -> p a b", b=64), cumd[:].rearrange("p (a b) -> p a b", b=64), tot_bc,
                     ALU.subtract, [cumd, pre], [tmpb])
                yield
                c.tt('dve', R(AR[:, 1, :]), rb[:, tsl], tmpa[:], ALU.mult, [rb, tmpa], [AR])
                yield
                c.act(tmpa[:], cumd[:], AF.Exp, [cumd], [tmpa], scale=-1.0)
                c.act(tmpb[:], tmpb[:], AF.Exp, [tmpb], [tmpb], scale=-1.0)
                yield
                c.tt('dve', R(btT[:]), bvec[:], tmpa[:], ALU.mult, [bvec, tmpa], [btT])
                c.tt('pool', R(ktT[:]), kdir[:], tmpa[:], ALU.mult, [kdir, tmpa], [ktT])
                yield
                c.tt('dve', R(bhT[:]), bvec[:], tmpb[:], ALU.mult, [bvec, tmpb], [bhT])
                c.tt('pool', R(khT[:]), kdir[:], tmpb[:], ALU.mult, [kdir, tmpb], [khT])
                yield

            def tile_g(d, g, b, tl):
                lsl_ = slice(tl * 128, tl * 128 + 128)
                gts = (2 * b + tl) * 128
                TL0, TL1, TLb = g['TL0'], g['TL1'], g['TLb']
                bhT, khT, Bh_tok, Kh_tok, Vt = g['bhT'], g['khT'], g['Bh_tok'], g['Kh_tok'], g['Vt']
                c.mm(TL0.ap(), R(bhT[:, lsl_]), R(ident_r[:]), True, True, [bhT, ident_r], [TL0.buf])
                c.mm(TLb.ap(), vb[:, gts:gts + 128], identb[:], True, True, [vb, identb], [TLb.buf])
                c.mm(TL1.ap(), R(khT[:, lsl_]), R(ident_r[:]), True, True, [khT, ident_r], [TL1.buf])
                yield
                c.cp('act', R(Bh_tok[:]), TL0.ap(), [TL0.buf], [Bh_tok])
                c.cp('dve', R(Kh_tok[:]), TL1.ap(), [TL1.buf], [Kh_tok])
                c.cp('act', R(Vt[:]), TLb.ap(), [TLb.buf], [Vt])
                yield

            def unit_g(d, g, u, hh, b, tl):
                rows = slice(hh * 64, hh * 64 + 64)
                tile = 2 * b + tl
                is_lat = tile >= 2
                lt = tile - 2
                lsl_ = slice(tl * 128, tl * 128 + 128)
                AR, btT, ktT, Vt, Bh_tok, Kh_tok, gC = (g[n_] for n_ in ('AR', 'btT', 'ktT', 'Vt', 'Bh_tok', 'Kh_tok', 'gC'))
                AB1, AB2, XY2, Xs, Us, yt, yt2, Tst = (u[n_] for n_ in ('AB1', 'AB2', 'XY2', 'Xs', 'Us', 'yt', 'yt2', 'Tst'))
                P, Q, W = u['P'], u['Q'], u['W']
                A, B = PS[u['bankA']], PS[u['bankB']]
                At, Bt = A.t, B.t
                ARt = AR[rows, :, lsl_]
                c.mm(At[:, 0:256].rearrange("p (a b) -> p a b", a=2), R(btT[rows, lsl_]), R(ARt), True, True, [btT, AR], [A])
                c.mm(Bt[:, 0:256].rearrange("p (a b) -> p a b", a=2), R(ktT[rows, lsl_]), R(ARt), True, True, [ktT, AR], [B])
                yield
                c.tt('dve', R(AB1[:]), At[:, 0:256], mskA[d][:], ALU.mult, [A, mskA[d]], [AB1])
                c.tt('dve', R(AB2[:]), Bt[:, 0:256], mskB[d][:], ALU.mult, [B, mskB[d]], [AB2])
                yield
                if KTG < 3:
                    return
                c.cp('pool', R(P[0][:]), AB1[:, 0:128], [AB1], [P[0]])
                c.mm(At[:, 0:64], R(AB2[:, 0:128]), R(Vt[:, rows]), True, True, [AB2, Vt], [A])
                c.mm(At[:, 64:128], R(AB2[:, 128:256]), R(Vt[:, rows]), True, True, [AB2, Vt], [A])
                yield
                c.cp('act', XY2[:], At[:, 0:128], [A], [XY2])
                c.mm(Bt[:, 0:128], R(P[0][:]), R(ident_r[:]), True, True, [P[0], ident_r], [B])
                c.tt('pool', R(W[0][:]), ident[:], P[0][:], ALU.subtract, [ident, P[0]], [W[0]])
                yield
                c.cp('act', R(Q[0][:]), Bt[:, 0:128], [B], [Q[0]])
                yield
                for k_ in range(5):
                    a_, b_ = k_ % 2, (k_ + 1) % 2
                    c.mm(At[:, 128:256], R(P[a_][:]), R(Q[a_][:]), True, True, [P[a_], Q[a_]], [A])
                    if k_ < 4:
                        c.mm(Bt[:, 0:128], R(Q[a_][:]), R(P[a_][:]), True, True, [P[a_], Q[a_]], [B])
                    yield
                    c.cp('act', R(Q[b_][:]), At[:, 128:256], [A], [Q[b_]])
                    if k_ < 4:
                        c.cp('act', R(P[b_][:]), Bt[:, 0:128], [B], [P[b_]])
                    yield
                    c.mm(Bt[:, 128:256], R(Q[b_][:]), R(W[a_][:]), True, True, [Q[b_], W[a_]], [B])
                    yield
                    c.tt('dve', R(W[b_][:]), Bt[:, 128:256], W[a_][:], ALU.add, [B, W[a_]], [W[b_]])
                    yield
                Wt = W[1]
                if KTG < 4:
                    return
                for cs in ((0, 64) if d == 0 else (64, 0)):
                    sl_ = slice(cs, cs + 64)
                    chunk = tl * 2 + cs // 64
                    c.mm(At[:, 0:64], R(AR[rows, 0, lsl_]), R(Tst[rows, :]), True, True, [AR, Tst], [A])
                    if is_lat:
                        c.mm(At[:, 64:128], R(AR[rows, 1, lsl_]), R(Tst[rows, :]), True, True, [AR, Tst], [A])
                    yield
                    c.tt('dve', R(Xs[sl_, :]), At[sl_, 0:64], XY2[sl_, 0:64], ALU.add, [A, XY2], [Xs])
                    yield
                    c.mm(Bt[:, 0:64], R(Wt[sl_, :]), R(Xs[sl_, :]), True, True, [Wt, Xs], [B])
                    yield
                    c.cp('act', R(Us[sl_, :]), Bt[sl_, 0:64], [B], [Us])
                    yield
                    c.mm(Bt[:, 64:128], R(Bh_tok[sl_, :]), R(Us[sl_, :]), True, False, [Bh_tok, Us], [B])
                    c.mm(Bt[:, 64:128], R(Kh_tok[sl_, :]), R(Vt[sl_, rows]), False, True, [Kh_tok, Vt], [B])
                    if is_lat:
                        c.mm(At[:, 128:192], R(AB1[sl_, 128:256]), R(Us[sl_, :]), True, True, [AB1, Us], [A])
                    yield
                    c.stt(R(Tst[rows, :]), Tst[rows, :], gC[rows, chunk:chunk + 1], Bt[rows, 64:128], ALU.mult, ALU.add,
                          [Tst, gC, B], [Tst])
                    if is_lat:
                        c.tt('dve', yt[sl_, :], At[sl_, 128:192], XY2[sl_, 64:128], ALU.add, [A, XY2], [yt])
                        if (d == 0) == (lt <= 7):
                            c.tt('dve', y_acc[sl_, lt, rows], At[sl_, 64:128], yt[sl_, :], ALU.add, [A, yt], [y_acc])
                        else:
                            c.tt('dve', yt2[sl_, :], At[sl_, 64:128], yt[sl_, :], ALU.add, [A, yt], [yt2])
                            c.tt('pool', y_acc[sl_, lt, rows], y_acc[sl_, lt, rows], yt2[sl_, :], ALU.add, [y_acc, yt2], [y_acc])
                    yield

            def dir_g(d, hc):
                g = DB[d]
                for hh in range(2):
                    c.ts('dve', R(g['U'][hh]['Tst'][:]), zf[:, 0:64], 0.0, None, ALU.mult, None, [zf], [g['U'][hh]['Tst']])
                border = list(range(NBLK)) if d == 0 else [0] + list(range(NBLK - 1, 0, -1))
                for b in border[:NBS]:
                    yield from block_g(d, g, b, hc)
                    for tl in ((0, 1) if d == 0 else (1, 0)):
                        if KTG < 1:
                            continue
                        yield from tile_g(d, g, b, tl)
                        if KTG < 2:
                            continue
                        yield from par(unit_g(d, g, g['U'][0], 0, b, tl), unit_g(d, g, g['U'][1], 1, b, tl))

            ob_hc = c.sb(esR, [128, 16, 128], BF16, 'ob_hc')
            scr_ob_v = scr_ob.rearrange("(lt p) cc -> p lt cc", p=128)
            scrw = Buf(None, 'scrw')
            for hc in range(NHC):
                proj_mix(hc * 128, rb, None, hc)
                proj_mix(512 + hc * 128, kb, None, 4 + hc)
                proj_mix(1024 + hc * 128, vb, None, 8 + hc)
                c.barrier()
                cs_ = slice(hc * 128, (hc + 1) * 128)
                def prepass_g(par_, hc=hc, cs_=cs_):
                    g_ = DB[par_]
                    icl, icl1, tmpa, opr = g_['icl'], g_['pre'], g_['tmpa'], g_['opr']
                    P0, P3, P1, P2 = (PS[4 * par_ + j] for j in range(4))
                    for b in range(1 + par_, NBLK, 2):
                        t0 = b * BW
                        tsl = slice(t0, t0 + BW)
                        lsl = slice(t0 - CTX, t0 - CTX + BW)
                        c.mm(P0[:, 0:BW], a2b[0:64, cs_], alo[0:64, tsl], True, True, [a2b, alo], [P0])
                        c.mm(P3[:, 0:BW], a2b[64:128, cs_], alo[64:128, tsl], True, True, [a2b, alo], [P3])
                        c.mm(P2[:, 0:BW], g2b[:, cs_], sgl[:, tsl], True, True, [g2b, sgl], [P2])
                        yield
                        c.act(icl[:], P0[:, 0:BW], AF.Sigmoid, [P0, a0], [icl], bias=a0[:, 0, hc:hc + 1])
                        c.act(icl1[:], P3[:, 0:BW], AF.Sigmoid, [P3, a0], [icl1], bias=a0[:, 1, hc:hc + 1])
                        c.cp('act', G1[:, lsl], P2[:, 0:BW], [P2], [G1])
                        yield
                        c.tt('dve', tmpa[:], icl[:], icl1[:], ALU.add, [icl, icl1], [tmpa])
                        c.ts('dve', tmpa[:], tmpa[:], rw[:, 1, hc:hc + 1], oka2[:, hc:hc + 1], ALU.mult, ALU.add, [tmpa, rw, oka2], [tmpa])
                        yield
                        c.tt('dve', tmpa[:], tmpa[:], kb[:, tsl], ALU.mult, [tmpa, kb], [tmpa])
                        c.tt('dve', tmpa[:], tmpa[:], rb[:, tsl], ALU.mult, [tmpa, rb], [tmpa])
                        yield
                        c.ts('dve', R(opr[:]), tmpa[:], rw[:, 2, hc:hc + 1], None, ALU.mult, None, [tmpa, rw], [opr])
                        yield
                        c.mm(P1[:, 0:BW], R(blk_r[:]), R(opr[:]), True, True, [blk_r, opr], [P1])
                        yield
                        c.tt('dve', tmpa[:], P1[:, 0:BW], vb[:, tsl], ALU.mult, [P1, vb], [tmpa])
                        yield
                        c.stt(G2[:, lsl], tmpa[:], rw[:, 4, hc:hc + 1], P2[:, 0:BW], ALU.add, ALU.mult, [tmpa, rw, P2], [G2])
                        yield

                run_threads([prepass_g(0), prepass_g(1)])
                c.barrier()
                run_threads([dir_g(0, hc), dir_g(1, hc)])
                c.barrier()
                def rwkv_out_g(par_, hc=hc, cs_=cs_):
                    gst, yn, obf, otf = gst2[par_], yn2[par_], obf2[par_], otf2[par_]
                    bkA, bkB = PS[par_], PS[2 + par_]
                    pbk = psb(2 + par_)
                    for lt in range(par_, 0 if os.environ.get('K_NOOUT') else 16, 2):
                        for hh in range(2):
                            rows = slice(hh * 64, hh * 64 + 64)
                            c.op('dve', lambda e, hh=hh, rows=rows: e.bn_stats(out=gst[:, hh * 6:(hh + 1) * 6], in_=y_acc[:, lt, rows]), reads=[y_acc], writes=[gst])
                            c.op('dve', lambda e, hh=hh: e.bn_aggr(out=gst[:, 12 + hh * 2:14 + hh * 2], in_=gst[:, hh * 6:(hh + 1) * 6]), reads=[gst], writes=[gst])
                        yield
                        c.ts('dve', gst[:, 16:18], gst[:, 13:16:2], LNX_EPS, None, ALU.add, None, [gst], [gst])
                        yield
                        c.act(gst[:, 16:18], gst[:, 16:18], AF.Sqrt, [gst], [gst])
                        yield
                        c.op('dve', lambda e: e.reciprocal(out=gst[:, 18:20], in_=gst[:, 16:18]), reads=[gst], writes=[gst])
                        for hh in range(2):
                            rows = slice(hh * 64, hh * 64 + 64)
                            c.ts('dve', yn[:, rows], y_acc[:, lt, rows], gst[:, 12 + hh * 2:13 + hh * 2], gst[:, 18 + hh:19 + hh], ALU.subtract, ALU.mult,
                                 [y_acc, gst], [yn])
                        yield
                        c.tr(bkA[:, 0:128], yn[:], ident[:], [yn, ident], [bkA])
                        yield
                        lsl = slice(lt * 128, (lt + 1) * 128)
                        c.stt(otf[:], bkA[:, 0:128], rw[:, 3, hc:hc + 1], G1[:, lsl], ALU.mult, ALU.mult, [bkA, rw, G1], [otf])
                        c.tt('dve', obf[:], otf[:], G2[:, lsl], ALU.add, [otf, G2], [obf])
                        yield
                        c.tr(pbk[:, 0:128], obf[:], identb[:], [obf, identb], [bkB])
                        yield
                        c.cp('act', ob_hc[:, lt, :], pbk[:, 0:128], [bkB], [ob_hc])
                        yield

                run_threads([rwkv_out_g(0), rwkv_out_g(1)])
                for q4 in range(4):
                    c.dma(scr_ob_v[:, q4 * 4:(q4 + 1) * 4, cs_], ob_hc[:, q4 * 4:(q4 + 1) * 4, :], reads=[ob_hc], writes=[scrw])
            c.barrier()

        scrob_buf = scrw
        scrh_buf = Buf(None, 'scr_h_dram')
        esF = ExitStack()
        with esF:
            tT = c.sb(esF, [128, 8, SEQ], BF16, 'tT')
            cwT = c.sb(esF, [32, SEQ], F32, 'cwT')
            with ExitStack() as esY:
                yT = c.sb(esY, [128, 8, SEQ], BF16, 'yT')
                with ExitStack() as esM1:
                    woa = c.sb(esM1, [128, 4, D], BF16, 'woa')
                    wob = c.sb(esM1, [128, 4, D], BF16, 'wob')
                    c.dma(woa[:], w_o_a.rearrange("(cc p) n -> p cc n", p=128), writes=[woa], q='pool')
                    c.dma(wob[:], w_o_b.rearrange("(cc p) n -> p cc n", p=128), writes=[wob], q='pool')
                    uTb = c.sb(esM1, [128, 8, 512], BF16, 'uTb')
                    obTb = c.sb(esM1, [128, 4, 512], BF16, 'obTb')
                    obt = [c.sb(esM1, [128, 512], BF16, 'obt%d' % i) for i in range(2)]
                    wg_ = [c.sb(esM1, [128, 8, 128], BF16, 'wgm%d' % i) for i in range(4)]
                    sga2 = [c.sb(esM1, [128, 512], F32, 'sga%d' % i) for i in range(2)]
                    sgb2 = [c.sb(esM1, [128, 512], F32, 'sgb%d' % i) for i in range(2)]
                    ya2 = [c.sb(esM1, [128, 512], F32, 'ya%d' % i) for i in range(2)]
                    yb2 = [c.sb(esM1, [128, 512], F32, 'yb%d' % i) for i in range(2)]
                    ob_v = scr_ob.rearrange("(w r) cc -> r w cc", r=32)
                    k = 0
                    wn = 0
                    for tb in range(4):
                        c.dma(uTb[:], scr_u[:, :, tb * 512:(tb + 1) * 512], reads=[scru_buf], writes=[uTb])
                        for j in range(4):
                            i = tb * 4 + j
                            o_ = obt[i % 2]
                            c.dma(o_[0:64, :], ob_v[2 * i], reads=[scrob_buf], writes=[o_])
                            c.dma(o_[64:128, :], ob_v[2 * i + 1], reads=[scrob_buf], writes=[o_])
                            pb = psb(5 + i % 2)
                            for hc in range(4):
                                c.tr(pb[:, hc * 128:(hc + 1) * 128], o_[:, hc * 128:(hc + 1) * 128], identb[:], [o_, identb], [PS[5 + i % 2]],
                                     inc=(hc == 3))
                            c.cp('act', obTb[:, :, j * 128:(j + 1) * 128], pb[:, 0:512].rearrange("p (a b) -> p a b", a=4), [PS[5 + i % 2]], [obTb])
                        tsl = slice(tb * 512, (tb + 1) * 512)
                        for fc in range(8):
                            wa_ = wg_[wn % 4]
                            wb_ = wg_[(wn + 1) % 4]
                            wn += 2
                            ga0 = A_COLS + B_COLS + fc * 128
                            c.dma(wa_[:], w_in_c[cidx[ga0]], writes=[wa_], q='pool')
                            c.dma(wb_[:], w_in_c[cidx[ga0 + D]], writes=[wb_], q='pool')
                            fsl = slice(fc * 128, (fc + 1) * 128)
                            fp_ = fc % 2
                            Q0, Q1, Q2, Q3 = (PS[4 * fp_ + j_] for j_ in range(4))
                            sga, sgb, ya, yb = sga2[fp_], sgb2[fp_], ya2[fp_], yb2[fp_]
                            for kc in range(8):
                                c.mm(Q2[:, :], wa_[:, kc, :], uTb[:, kc, :], kc == 0, kc == 7, [wa_, uTb], [Q2])
                            for kc in range(8):
                                c.mm(Q3[:, :], wb_[:, kc, :], uTb[:, kc, :], kc == 0, kc == 7, [wb_, uTb], [Q3])
                            for cc in range(4):
                                c.mm(Q0[:, :], woa[:, cc, fsl], oaT[:, cc, tsl], cc == 0, cc == 3, [woa, oaT], [Q0])
                            for cc in range(4):
                                c.mm(Q1[:, :], wob[:, cc, fsl], obTb[:, cc, :], cc == 0, cc == 3, [wob, obTb], [Q1])
                            c.act(sga[:], Q2[:, :], AF.Sigmoid, [Q2], [sga])
                            c.act(sgb[:], Q3[:, :], AF.Sigmoid, [Q3], [sgb])
                            c.tt('dve', ya[:], Q0[:, :], sga[:], ALU.mult, [Q0, sga], [ya])
                            c.tt('dve', yb[:], Q1[:, :], sgb[:], ALU.mult, [Q1, sgb], [yb])
                            c.tt('dve', yT[:, fc, tsl], ya[:], yb[:], ALU.add, [ya, yb], [yT])
                    c.barrier()
                if 'd_yT' in T:
                    c.dma(T['d_yT'], yT[:], reads=[yT])
                with ExitStack() as esM2:
                    wout = c.sb(esM2, [128, 8, D], BF16, 'wout')
                    c.dma(wout[:, 0:4, :], w_out.rearrange("(cc p) n -> p cc n", p=128)[:, 0:4, :], writes=[wout], q='pool')
                    c.dma(wout[:, 4:8, :], w_out.rearrange("(cc p) n -> p cc n", p=128)[:, 4:8, :], writes=[wout], q='pool')
                    Bg1 = c.sb(esM2, [128, D], F32, 'Bg1')
                    make_Bg(esM2, Bg1, 16)
                    wr32 = c.sb(esM2, [128, 8, 36], F32, 'wr32')
                    rbb = c.sb(esM2, [128, 36], F32, 'rbb')
                    c.dma(wr32[:], wrt, writes=[wr32])
                    c.dma(rbb[:], rb_bc, writes=[rbb])
                    Xh = [c.sb(esM2, [128, D], F32, 'Xh%d' % i) for i in range(2)]
                    Hh = [c.sb(esM2, [128, D], F32, 'Hh%d' % i) for i in range(2)]
                    hn2 = [c.sb(esM2, [128, D], F32, 'hn%d' % i) for i in range(2)]
                    sqh2 = [c.sb(esM2, [128, D], BF16, 'sqh%d' % i) for i in range(2)]
                    t322 = [c.sb(esM2, [128, 8, 128], F32, 't32%d' % i) for i in range(2)]
                    st2 = [c.sb(esM2, [128, 64], F32, 'st%d' % i) for i in range(2)]
                    lg2 = [c.sb(esM2, [128, 36], F32, 'lg%d' % i) for i in range(2)]
                    tm32 = [c.sb(esM2, [128, 32], F32, 'tm3%d' % i) for i in range(2)]
                    cw2 = [c.sb(esM2, [128, 32], F32, 'cw%d' % i) for i in range(2)]

                    def tile_chain(t):
                        hn, sqh, t32, st, lg, tm3, cw = hn2[t], sqh2[t], t322[t], st2[t], lg2[t], tm32[t], cw2[t]
                        B = [PS[4 * t + j] for j in range(4)]
                        X_, H_ = Xh[t], Hh[t]
                        for i in range(t, 16, 2):
                            c.dma(X_[:], x[i * 128:(i + 1) * 128, :], writes=[X_])
                            for nh in range(2):
                                bank = B[nh]
                                for fc in range(8):
                                    c.mm(bank[:, :], yT[:, fc, i * 128:(i + 1) * 128], wout[:, fc, nh * 512:(nh + 1) * 512], fc == 0, fc == 7, [yT, wout], [bank])
                                hs = slice(nh * 512, (nh + 1) * 512)
                                c.tt('dve', H_[:, hs], bank[:, :], Bg1[:, hs], ALU.mult, [bank, Bg1], [H_])
                                yield
                            c.tt('pool', H_[:], H_[:], X_[:], ALU.add, [H_, X_], [H_])
                            yield
                            c.dma(scr_h[i * 128:(i + 1) * 128, :], H_[:], reads=[H_], writes=[scrh_buf])
                            c.act(sqh[:], H_[:], AF.Square, [H_], [sqh, st], accum_out=st[:, 0:1])
                            yield
                            c.ts('dve', st[:, 1:2], st[:, 0:1], 1.0 / D, EPS, ALU.mult, ALU.add, [st], [st])
                            c.act(st[:, 2:3], st[:, 1:2], AF.Sqrt, [st], [st])
                            yield
                            c.op('dve', lambda e: e.reciprocal(out=st[:, 3:4], in_=st[:, 2:3]), reads=[st], writes=[st])
                            c.ts('dve', hn[:], H_[:], st[:, 3:4], None, ALU.mult, None, [H_, st], [hn])
                            yield
                            for fc in range(8):
                                bank = B[2 + fc // 4]
                                c.tr(bank[:, (fc % 4) * 128:(fc % 4 + 1) * 128], hn[:, fc * 128:(fc + 1) * 128], ident[:], [hn, ident], [bank],
                                     inc=(fc % 4 == 3))
                            yield
                            for fc in range(8):
                                bank = B[2 + fc // 4]
                                i_ = bank[:, (fc % 4) * 128:(fc % 4 + 1) * 128]
                                c.ts('dve', t32[:, fc, :], i_, A2g[:, fc, 0:1], mT[:, 24 + fc, 0:1], ALU.mult, ALU.add, [bank, A2g, mT], [t32])
                                c.cp('act', tT[:, fc, i * 128:(i + 1) * 128], t32[:, fc, :], [t32], [tT])
                                if fc % 4 == 3:
                                    yield
                            for kc in range(8):
                                c.mm(B[0][:, 0:36], t32[:, kc, :], wr32[:, kc, :], kc == 0, kc == 7, [t32, wr32], [B[0]])
                            yield
                            c.tt('dve', lg[:], B[0][:, 0:36], rbb[:], ALU.add, [B[0], rbb], [lg])
                            c.op('dve', lambda e: e.tensor_reduce(out=st[:, 8:9], in_=lg[:, 0:4], axis=AX.X, op=ALU.max), reads=[lg], writes=[st])
                            c.ts('dve', st[:, 9:10], st[:, 8:9], -1.0, None, ALU.mult, None, [st], [st])
                            yield
                            c.act(st[:, 16:20], lg[:, 0:4], AF.Exp, [lg, st], [st], bias=st[:, 9:10], accum_out=st[:, 10:11])
                            yield
                            c.op('dve', lambda e: e.reciprocal(out=st[:, 11:12], in_=st[:, 10:11]), reads=[st], writes=[st])
                            c.ts('dve', st[:, 20:24], lg[:, 0:4], st[:, 8:9], None, ALU.is_ge, None, [lg, st], [st])
                            c.tt('dve', tm3[:].rearrange("p (g e) -> p g e", g=4), lg[:, 4:36].rearrange("p (g e) -> p g e", g=4),
                                 st[:, 20:24].unsqueeze(2).to_broadcast([128, 4, 8]), ALU.mult, [lg, st], [tm3])
                            yield
                            c.op('dve', lambda e: e.tensor_reduce(out=st[:, 24:32], in_=tm3[:].rearrange("p (g e) -> p e g", g=4), axis=AX.X, op=ALU.add),
                                 reads=[tm3], writes=[st])
                            c.op('dve', lambda e: e.max(out=st[:, 32:40], in_=st[:, 24:32]), reads=[st], writes=[st])
                            c.tt('dve', st[:, 40:41], st[:, 33:34], st[:, 32:33], ALU.subtract, [st], [st])
                            yield
                            c.act(st[:, 41:42], st[:, 40:41], AF.Exp, [st], [st])
                            yield
                            c.ts('dve', st[:, 42:43], st[:, 41:42], 1.0, None, ALU.add, None, [st], [st])
                            c.op('dve', lambda e: e.reciprocal(out=st[:, 43:44], in_=st[:, 42:43]), reads=[st], writes=[st])
                            c.tt('dve', st[:, 44:45], st[:, 43:44], st[:, 11:12], ALU.mult, [st], [st])
                            yield
                            c.tt('dve', st[:, 45:46], st[:, 44:45], st[:, 41:42], ALU.mult, [st], [st])
                            c.tt('dve', st[:, 46:47], st[:, 44:45], st[:, 45:46], ALU.subtract, [st], [st])
                            c.ts('dve', st[:, 48:56], st[:, 24:32], st[:, 32:33], st[:, 46:47], ALU.is_ge, ALU.mult, [st], [st])
                            yield
                            c.ts('dve', st[:, 56:64], st[:, 24:32], st[:, 33:34], st[:, 45:46], ALU.is_ge, ALU.mult, [st], [st])
                            c.tt('dve', st[:, 48:56], st[:, 48:56], st[:, 56:64], ALU.add, [st], [st])
                            c.tt('dve', cw[:].rearrange("p (g e) -> p g e", g=4), st[:, 20:24].unsqueeze(2).to_broadcast([128, 4, 8]),
                                 st[:, 48:56].unsqueeze(1).to_broadcast([128, 4, 8]), ALU.mult, [st], [cw])
                            yield
                            c.tr(B[1][0:32, 0:128], cw[:], ident[:], [cw, ident], [B[1]])
                            yield
                            c.cp('act', R(cwT[:, i * 128:(i + 1) * 128]), B[1][0:32, 0:128], [B[1]], [cwT])
                            yield

                    run_threads([tile_chain(0), tile_chain(1)])
                    c.barrier()
            if 'd_tT' in T:
                c.dma(T['d_tT'], tT[:], reads=[tT])
            if 'd_cwT' in T:
                c.dma(T['d_cwT'], cwT[:], reads=[cwT])

            with ExitStack() as esE:
                moe_acc = c.sb(esE, [128, 16, D], F32, 'moe_acc')
                macc = [[Buf(None, 'macc%d_%d' % (t_, n_)) for n_ in range(2)] for t_ in range(16)]
                evt = [c.sb(esE, [128, 512], F32, 'evt%d' % i) for i in range(3)]
                wgb = [c.sb(esE, [128, 8, 256], BF16, 'wgb%d' % i) for i in range(2)]
                wub = [c.sb(esE, [128, 8, 256], BF16, 'wub%d' % i) for i in range(2)]
                wdb = [c.sb(esE, [128, 2, D], BF16, 'wdb%d' % i) for i in range(2)]
                selt = [c.sb(esE, [32, 128], F32, 'selt%d' % i) for i in range(2)]
                sg_ = [c.sb(esE, [128, 512], F32, 'sg%d' % i) for i in range(2)]
                hu_ = [c.sb(esE, [128, 512], F32, 'hu%d' % i) for i in range(2)]
                hid = [c.sb(esE, [128, 2, 512], BF16, 'hid%d' % i) for i in range(2)]
                NEXP = int(os.environ.get('K_NEXP', '32'))
                def moe_gu(e_, tg, k):
                    wg, wu, wd = wgb[e_ % 2], wub[e_ % 2], wdb[e_ % 2]
                    se = selt[e_ % 2]
                    if tg == 0:
                        c.dma(wg[:], moe_wg[e_], writes=[wg], q='pool')
                        c.dma(wu[:], moe_wu[e_], writes=[wu], q='pool')
                        c.dma(wd[:], moe_wd[e_], writes=[wd], q='pool')
                        c.ts('dve', R(se[:]), onesf[0:32, :], ident[0:32, e_:e_ + 1], None, ALU.mult, None, [onesf, ident], [se])
                    tsl = slice(tg * 512, (tg + 1) * 512)
                    hd = hid[k % 2]
                    c.mm(PS[4][:, :], R(se[:]), R(cwT[:, tsl]), True, True, [se, cwT], [PS[4]])
                    for f2 in range(2):
                        fs = slice(f2 * 128, (f2 + 1) * 128)
                        for kc in range(8):
                            c.mm(PS[f2][:, :], wg[:, kc, fs], tT[:, kc, tsl], kc == 0, kc == 7, [wg, tT], [PS[f2]])
                        for kc in range(8):
                            c.mm(PS[2 + f2][:, :], wu[:, kc, fs], tT[:, kc, tsl], kc == 0, kc == 7, [wu, tT], [PS[2 + f2]])
                        c.act(sg_[f2][:], PS[f2][:, :], AF.Silu, [PS[f2]], [sg_[f2]])
                        c.tt('dve', hu_[f2][:], PS[2 + f2][:, :], sg_[f2][:], ALU.mult, [PS[2 + f2], sg_[f2]], [hu_[f2]])
                        c.tt('dve', hd[:, f2, :], PS[4][:, :], hu_[f2][:], ALU.mult, [PS[4], hu_[f2]], [hd])

                MOE_SPLIT = False

                def moe_dn(e_, tg, k):
                    wd = wdb[e_ % 2]
                    hd = hid[k % 2]
                    for tt_ in range(4):
                        tile = tg * 4 + tt_
                        for nh in range(2):
                            bank = PS[5 + (tt_ * 2 + nh) % 3]
                            for f2 in range(2):
                                c.mm(bank[:, :], hd[:, f2, tt_ * 128:(tt_ + 1) * 128], wd[:, f2, nh * 512:(nh + 1) * 512], f2 == 0, f2 == 1,
                                     [hd, wd], [bank])
                            hs = slice(nh * 512, (nh + 1) * 512)
                            ma = macc[tile][nh]
                            gi = tt_ * 2 + nh
                            if e_ == 0:
                                c.cp('act', moe_acc[:, tile, hs], bank[:, :], [bank], [ma])
                            elif gi % 2 == 0 or not MOE_SPLIT:
                                c.tt('dve', moe_acc[:, tile, hs], bank[:, :], moe_acc[:, tile, hs], ALU.add, [bank, ma], [ma])
                            else:
                                ev = evt[(gi // 2) % 3]
                                c.cp('act', ev[:], bank[:, :], [bank], [ev])
                                c.tt('pool', moe_acc[:, tile, hs], moe_acc[:, tile, hs], ev[:], ALU.add, [ma, ev], [ma])

                its = [(e_, tg) for e_ in range(NEXP) for tg in range(4)]
                for k, (e_, tg) in enumerate(its):
                    moe_gu(e_, tg, k)
                    if k > 0:
                        moe_dn(its[k - 1][0], its[k - 1][1], k - 1)
                moe_dn(its[-1][0], its[-1][1], len(its) - 1)
                Bg2 = c.sb(esE, [128, D], F32, 'Bg2')
                make_Bg(esE, Bg2, 40)
                gfin = c.sb(esE, [128, D], F32, 'gfin')
                c.dma(gfin[:], gfin_bc, writes=[gfin])
                Hf = [c.sb(esE, [128, D], F32, 'Hf%d' % i) for i in range(2)]
                sf = [c.sb(esE, [128, 4], F32, 'sf%d' % i) for i in range(2)]
                c.barrier()
                Hm = [Buf(wgb[i].t.bitcast(F32).rearrange("p a b -> p (a b)"), 'Hm%d' % i) for i in range(2)]
                sqf2 = [Buf(wub[i].t.rearrange("p a b -> p (a b)"), 'sqf%d' % i) for i in range(2)]

                def fin_g(par_):
                    H_, s_, Hm_, sq_ = Hf[par_], sf[par_], Hm[par_], sqf2[par_]
                    for i in range(par_, 16, 2):
                        c.dma(H_[:], scr_h[i * 128:(i + 1) * 128, :], reads=[scrh_buf], writes=[H_])
                        c.tt('dve', Hm_[:], moe_acc[:, i, :], Bg2[:], ALU.mult, [macc[i][0], macc[i][1], Bg2], [Hm_])
                        yield
                        c.tt('pool', H_[:], H_[:], Hm_[:], ALU.add, [H_, Hm_], [H_])
                        yield
                        c.act(sq_[:, 0:D], H_[:], AF.Square, [H_], [sq_, s_], accum_out=s_[:, 0:1])
                        yield
                        c.ts('dve', s_[:, 1:2], s_[:, 0:1], 1.0 / D, EPS, ALU.mult, ALU.add, [s_], [s_])
                        yield
                        c.act(s_[:, 2:3], s_[:, 1:2], AF.Sqrt, [s_], [s_])
                        yield
                        c.op('dve', lambda e, s_=s_: e.reciprocal(out=s_[:, 3:4], in_=s_[:, 2:3]), reads=[s_], writes=[s_])
                        c.stt(H_[:], H_[:], s_[:, 3:4], gfin[:], ALU.mult, ALU.mult, [H_, s_, gfin], [H_])
                        yield
                        c.dma(out[i * 128:(i + 1) * 128, :], H_[:], reads=[H_])
                        yield

                run_threads([fin_g(0), fin_g(1)])
                c.barrier()

        c.finish()
        print("ninstr", c.ninstr, {k_: v for k_, v in c.cnt.items()})
    return nc


def prep_inputs(inp):
    f = lambda a: np.ascontiguousarray(a, dtype=np.float32)
    fm = lambda v: f(np.asarray(v).reshape(-1, 128).T)
    shared = {
        "ada_w": f(inp["ada_w"][0]),
        "ada_bT": fm(inp["ada_b"][0]),
        "gmixT": fm(inp["norm_mix_g"][0]),
        "gffnT": fm(inp["norm_ffn_g"][0]),
        "gfin_bc": f(np.broadcast_to(inp["final_norm_g"][None, :], (128, D))),
        "w_in": f(inp["w_in"][0]),
        "w_in_c": f(np.stack([np.asarray(inp["w_in"][0])[:, c0:c0 + 128].reshape(8, 128, 128).transpose(1, 0, 2) for c0 in W_CHUNKS])),
        "convT": f(inp["gdn_conv"][0].T.reshape(12, 128, 5).transpose(1, 0, 2)),
        "alog_bc": f(np.broadcast_to(inp["gdn_a_log"][0].reshape(1, 1, 8), (128, 18, 8))),
        "dtb_bc": f(np.broadcast_to(inp["gdn_dt_bias"][0].reshape(1, 1, 8), (128, 18, 8))),
        "onormT": f(inp["gdn_onorm_g"][0].reshape(128, 1)),
        "muT": fm(inp["rwkv_mu"][0]),
        "w0T": f(inp["rwkv_w0"][0].reshape(2, 4, 128).transpose(2, 0, 1)),
        "a0T": f(inp["rwkv_a0"][0].reshape(2, 4, 128).transpose(2, 0, 1)),
        "w2m": f(inp["rwkv_w2"][0].reshape(128, 512)),
        "a2m": f(inp["rwkv_a2"][0].reshape(128, 512)),
        "g2m": f(inp["rwkv_g2"][0]),
        "w_o_a": f(inp["w_o_a"][0]),
        "w_o_b": f(inp["w_o_b"][0]),
        "w_out": f(inp["w_out"][0]),
        "wrt": f(np.concatenate([inp["router_grp"][0], inp["router_exp"][0]], axis=1).reshape(8, 128, 36).transpose(1, 0, 2)),
        "rb_bc": f(np.broadcast_to(np.concatenate([inp["router_grp_b"][0], inp["router_exp_b"][0]])[None, :], (128, 36))),
        "moe_wg": f(np.asarray(inp["moe_w_gate"][0]).reshape(32, 8, 128, 256).transpose(0, 2, 1, 3)),
        "moe_wu": f(np.asarray(inp["moe_w_up"][0]).reshape(32, 8, 128, 256).transpose(0, 2, 1, 3)),
        "moe_wd": f(np.asarray(inp["moe_w_down"][0]).reshape(32, 2, 128, D).transpose(0, 2, 1, 3)),
        "rwv": f(np.stack([fm(inp["rwkv_k_k"][0]), fm(inp["rwkv_k_a"][0]), fm(inp["rwkv_r_k"][0].reshape(-1)),
                           fm(inp["rwkv_lnx_g"][0]), fm(inp["rwkv_lnx_b"][0])], axis=1)),
    }
    maps = []
    for b in range(NCORES):
        m = dict(shared)
        m["x"] = f(inp["x"][b])
        m["ctx"] = f(inp["ctx"][b])
        m["cT"] = f(np.stack([fm(inp["c"][b]), fm(inp["c_ctx"])], axis=-1))
        maps.append(m)
    return maps


def kernel(**inputs):
    maps = prep_inputs(inputs)
    nc = build()
    res = run_bass_kernel_spmd(nc, maps, core_ids=list(range(NCORES)))
    return np.stack([np.asarray(r["out"]) for r in res.results], axis=0).astype(np.float32)
```
